# Optimizing a Trainium2 kernel written in Bass

```python
import jax, jax.numpy as jnp
from jax import lax
import numpy as np

D_MODEL = 2048
BATCH = 2
SEQ = 8192
DEPTH = 1

HEAD_DIM = 64
N_HEADS_SWA = 16
N_KV_SWA = 4
N_HEADS_FOX = 16
WINDOW = 128
BLOCK = 128
SWA_WIDTH = N_HEADS_SWA * HEAD_DIM
KV_WIDTH = N_KV_SWA * HEAD_DIM
FOX_WIDTH = N_HEADS_FOX * HEAD_DIM
MIX_WIDTH = SWA_WIDTH + FOX_WIDTH
IN_COLS = SWA_WIDTH + 2 * KV_WIDTH + 3 * FOX_WIDTH + N_HEADS_FOX
N_GROUPS = 4
EXPERTS_PER_GROUP = 8
N_EXPERTS = N_GROUPS * EXPERTS_PER_GROUP
TOP_K = 2
D_EXPERT = 512
MOE_BLOCK = 128
EPS = 1e-6

kernel_name = "hymba_swa_sink_fox_hier_moe_adaln"


def rmsnorm(x, g):
    xf = x.astype(jnp.float32)
    y = xf * lax.rsqrt(jnp.mean(xf * xf, axis=-1, keepdims=True) + EPS)
    return (y * g.astype(jnp.float32)).astype(x.dtype)


def alibi_slopes(n):
    return jnp.asarray(2.0 ** (-8.0 * np.arange(1, n + 1) / n), dtype=jnp.float32)


def sliding_window_gqa(q, k, v, sinks):
    B, S, Hq, d = q.shape
    Hkv = k.shape[2]
    G = Hq // Hkv
    nb = S // BLOCK
    qb = q.reshape(B, nb, BLOCK, Hkv, G, d)

    def with_prev(t):
        tb = t.reshape(B, nb, BLOCK, Hkv, d)
        prev = jnp.pad(tb, ((0, 0), (1, 0), (0, 0), (0, 0), (0, 0)))[:, :-1]
        return jnp.concatenate([prev, tb], axis=2)

    kb, vb = with_prev(k), with_prev(v)
    scores = jnp.einsum('bnqhgd,bnkhd->bhgnqk', qb, kb).astype(jnp.float32) * (d ** -0.5)
    qpos = jnp.arange(BLOCK)[:, None] + BLOCK
    kpos = jnp.arange(2 * BLOCK)[None, :]
    dist = qpos - kpos
    in_band = (dist >= 0) & (dist < WINDOW)
    has_prev = (jnp.arange(nb)[:, None, None] > 0) | (kpos >= BLOCK)[None]
    mask = in_band[None] & has_prev
    slopes = alibi_slopes(Hq).reshape(Hkv, G, 1, 1, 1)
    scores = scores - slopes * dist.astype(jnp.float32)
    scores = jnp.where(mask, scores, -jnp.inf)
    sink = sinks.astype(jnp.float32).reshape(Hkv, G, 1, 1, 1)
    m = jnp.maximum(jnp.max(scores, axis=-1, keepdims=True), sink)
    p = jnp.exp(scores - m)
    p = p / (jnp.sum(p, axis=-1, keepdims=True) + jnp.exp(sink - m))
    out = jnp.einsum('bhgnqk,bnkhd->bnqhgd', p.astype(v.dtype), vb)
    return out.reshape(B, S, Hq * d)


def forgetting_attention(q, k, v, log_f):
    B, S, H, d = q.shape
    nb = S // BLOCK
    cum = lax.cumsum(log_f, axis=1).transpose(0, 2, 1)
    outs = []
    for i in range(nb):
        q0 = i * BLOCK
        kend = q0 + BLOCK
        s = jnp.einsum('bqhd,bkhd->bhqk', q[:, q0:kend], k[:, :kend]).astype(jnp.float32) * (d ** -0.5)
        s = s + cum[:, :, q0:kend, None] - cum[:, :, None, :kend]
        causal = (q0 + jnp.arange(BLOCK))[:, None] >= jnp.arange(kend)[None, :]
        p = jax.nn.softmax(jnp.where(causal, s, -jnp.inf), axis=-1)
        outs.append(jnp.einsum('bhqk,bkhd->bqhd', p.astype(v.dtype), v[:, :kend]))
    return jnp.concatenate(outs, axis=1).reshape(B, S, H * d)


def routed_expert_ffn(t, expert_id, weight, w_gate, w_up, w_down):
    N, D = t.shape
    A = expert_id.shape[0]
    tok = jnp.arange(A, dtype=jnp.int32) // TOP_K
    order = jnp.argsort(expert_id)
    se, stok, sw = expert_id[order], tok[order], weight[order]
    counts = jnp.bincount(expert_id, length=N_EXPERTS)
    starts = jnp.cumsum(counts) - counts
    padded = (counts + MOE_BLOCK - 1) // MOE_BLOCK * MOE_BLOCK
    pend = jnp.cumsum(padded)
    pstart = pend - padded
    dest = pstart[se] + jnp.arange(A) - starts[se]
    n_blocks = -(-A // MOE_BLOCK) + N_EXPERTS
    P = n_blocks * MOE_BLOCK
    row_tok = jnp.zeros((P,), jnp.int32).at[dest].set(stok)
    row_w = jnp.zeros((P,), t.dtype).at[dest].set(sw.astype(t.dtype))
    block_e = jnp.minimum(jnp.searchsorted(pend, jnp.arange(n_blocks) * MOE_BLOCK, side='right'), N_EXPERTS - 1)
    xb = t[row_tok].reshape(n_blocks, MOE_BLOCK, D)

    def expert_block(args):
        xblk, e = args
        hid = jax.nn.silu(xblk @ w_gate[e]) * (xblk @ w_up[e])
        return hid @ w_down[e]

    yb = lax.map(expert_block, (xb, block_e)).reshape(P, D)
    return jnp.zeros((N, D), t.dtype).at[row_tok].add(yb * row_w[:, None])


def hierarchical_moe(h, w_group, b_group, w_expert, b_expert, w_gate, w_up, w_down):
    B, S, D = h.shape
    N = B * S
    t = h.reshape(N, D)
    g_logits = (t @ w_group).astype(jnp.float32) + b_group.astype(jnp.float32)
    g_prob = jax.nn.softmax(g_logits, axis=-1)
    g_val, g_sel = lax.top_k(g_prob, 1)
    e_logits = ((t @ w_expert).astype(jnp.float32) + b_expert.astype(jnp.float32)).reshape(N, N_GROUPS, EXPERTS_PER_GROUP)
    e_in = jnp.take_along_axis(e_logits, g_sel[:, :, None], axis=1)[:, 0]
    top_val, top_idx = lax.top_k(e_in, TOP_K)
    gate = jax.nn.softmax(top_val, axis=-1) * g_val
    expert_id = (g_sel * EXPERTS_PER_GROUP + top_idx).astype(jnp.int32)
    y = routed_expert_ffn(t, expert_id.reshape(-1), gate.reshape(-1), w_gate, w_up, w_down)
    return y.reshape(B, S, D)


def setup_inputs(seed: int = 0) -> dict:
    key = jax.random.key(seed)
    ks = jax.random.split(key, 20)
    f32 = jnp.float32
    L, D = DEPTH, D_MODEL

    def nrm(k, shape, s):
        return s * jax.random.normal(k, shape, f32)

    return {
        "x": nrm(ks[0], (BATCH, SEQ, D), 1.0),
        "c": nrm(ks[1], (BATCH, D), 1.0),
        "w_ada": nrm(ks[2], (L, D, 6 * D), 0.5 * D ** -0.5),
        "b_ada": nrm(ks[3], (L, 6 * D), 0.01),
        "norm_mix_g": 1.0 + nrm(ks[4], (L, D), 0.05),
        "w_in": nrm(ks[5], (L, D, IN_COLS), D ** -0.5),
        "b_forget": 4.0 + nrm(ks[6], (L, N_HEADS_FOX), 0.5),
        "sinks": nrm(ks[7], (L, N_HEADS_SWA), 1.0),
        "out_norm_swa_g": 1.0 + nrm(ks[8], (L, SWA_WIDTH), 0.05),
        "out_norm_fox_g": 1.0 + nrm(ks[9], (L, FOX_WIDTH), 0.05),
        "w_out": nrm(ks[10], (L, MIX_WIDTH, D), MIX_WIDTH ** -0.5),
        "norm_moe_g": 1.0 + nrm(ks[11], (L, D), 0.05),
        "w_group": nrm(ks[12], (L, D, N_GROUPS), D ** -0.5),
        "b_group": nrm(ks[13], (L, N_GROUPS), 0.01),
        "w_expert": nrm(ks[14], (L, D, N_EXPERTS), D ** -0.5),
        "b_expert": nrm(ks[15], (L, N_EXPERTS), 0.01),
        "w_gate": nrm(ks[16], (L, N_EXPERTS, D, D_EXPERT), D ** -0.5),
        "w_up": nrm(ks[17], (L, N_EXPERTS, D, D_EXPERT), D ** -0.5),
        "w_down": nrm(ks[18], (L, N_EXPERTS, D_EXPERT, D), D_EXPERT ** -0.5),
        "final_g": 1.0 + nrm(ks[19], (D,), 0.05),
    }


def reference(x, c, w_ada, b_ada, norm_mix_g, w_in, b_forget, sinks, out_norm_swa_g, out_norm_fox_g, w_out,
              norm_moe_g, w_group, b_group, w_expert, b_expert, w_gate, w_up, w_down, final_g):
    B, S, D = x.shape
    col_splits = list(np.cumsum([SWA_WIDTH, KV_WIDTH, KV_WIDTH, FOX_WIDTH, FOX_WIDTH, FOX_WIDTH]))
    for l in range(DEPTH):
        mod = jax.nn.silu(c) @ w_ada[l] + b_ada[l]
        sh_a, sc_a, g_a, sh_m, sc_m, g_m = [m[:, None, :] for m in jnp.split(mod, 6, axis=-1)]

        h = rmsnorm(x, norm_mix_g[l]) * (1.0 + sc_a) + sh_a
        proj = h @ w_in[l]
        q_a, k_a, v_a, q_b, k_b, v_b, f_b = jnp.split(proj, col_splits, axis=-1)
        o_a = sliding_window_gqa(q_a.reshape(B, S, N_HEADS_SWA, HEAD_DIM),
                                 k_a.reshape(B, S, N_KV_SWA, HEAD_DIM),
                                 v_a.reshape(B, S, N_KV_SWA, HEAD_DIM), sinks[l])
        log_f = jax.nn.log_sigmoid(f_b.astype(jnp.float32) + b_forget[l].astype(jnp.float32))
        o_b = forgetting_attention(q_b.reshape(B, S, N_HEADS_FOX, HEAD_DIM),
                                   k_b.reshape(B, S, N_HEADS_FOX, HEAD_DIM),
                                   v_b.reshape(B, S, N_HEADS_FOX, HEAD_DIM), log_f)
        mixed = jnp.concatenate([rmsnorm(o_a, out_norm_swa_g[l]), rmsnorm(o_b, out_norm_fox_g[l])], axis=-1)
        x = x + g_a * (mixed @ w_out[l])

        h2 = rmsnorm(x, norm_moe_g[l]) * (1.0 + sc_m) + sh_m
        x = x + g_m * hierarchical_moe(h2, w_group[l], b_group[l], w_expert[l], b_expert[l],
                                       w_gate[l], w_up[l], w_down[l])
    return rmsnorm(x, final_g)
```

```python
import bisect
import contextlib
import numpy as np
import concourse.bass as bass
import concourse.mybir as mybir
from concourse.bass_utils import run_bass_kernel_spmd

F32 = mybir.dt.float32
BF16 = mybir.dt.bfloat16
I32 = mybir.dt.int32
AF = mybir.ActivationFunctionType
ALU = mybir.AluOpType
AX = mybir.AxisListType

D = 2048
KC = 16
EPS = 1e-6
NE = 32
DE = 512
QA0, KA0, VA0, QB0, KB0, VB0, FB0 = 0, 1024, 1280, 1536, 2560, 3584, 4608


class Eng:
    def __init__(self, nc, e, sem, name, is_pe=False):
        self.nc, self.e, self.sem, self.name, self.is_pe = nc, e, sem, name, is_pe
        self.idx = 0
        self.marks = []
        self.mark_idx = []
        self.count = 0
        self.last = None
        self.waited = {}

    def issue(self, ins, is_dma=False):
        self.idx += 1
        self.last = ins
        self.last_is_dma = is_dma
        return ("E", self, self.idx)

    def mark_now(self):
        if self.marks and self.marks[-1][0] == self.idx:
            return
        self.count += 1
        self.last.then_inc(self.sem, 1)
        self.marks.append((self.idx, self.count))
        self.mark_idx.append(self.idx)

    def resolve(self, idx):
        p = bisect.bisect_left(self.mark_idx, idx)
        if p < len(self.marks):
            return self.marks[p][1]
        assert self.idx >= idx
        if self.last_is_dma:
            if self.name == "act":
                self.issue(self.e.memzero(self.mark_tile))
            else:
                self.issue(self.e.memset(self.mark_tile, 0.0))
        self.mark_now()
        return self.count

    def wait(self, toks):
        for t in toks:
            if t is None:
                continue
            if t[0] == "E":
                eng, idx = t[1], t[2]
                if eng is self and self.is_pe:
                    continue
                val = eng.resolve(idx)
                sem = eng.sem
                key = eng.name
            else:
                _, ds, val = t
                sem = ds.sem
                key = ds.name
            if self.waited.get(key, 0) >= val:
                continue
            self.e.wait_ge(sem, val)
            self.waited[key] = val


class DSem:
    def __init__(self, sem, name):
        self.sem, self.name, self.n = sem, name, 0


class Buf:
    __slots__ = ("w", "r")

    def __init__(self):
        self.w = None
        self.r = {}


class K:
    def __init__(self, nc, es):
        self.nc, self.es = nc, es
        self.dsems = []
        mk = lambda e, n, pe=False: Eng(nc, e, es.enter_context(nc.semaphore("sem_" + n)), n, pe)
        self.pe = mk(nc.tensor, "pe", True)
        self.act = mk(nc.scalar, "act")
        self.dve = mk(nc.vector, "dve")
        self.pool = mk(nc.gpsimd, "pool")
        self.sp = mk(nc.sync, "sp")
        self.engs = [self.pe, self.act, self.dve, self.pool, self.sp]
        self.nds = 0

    def dsem(self, es=None):
        es = es or self.es
        self.nds += 1
        name = "ds%d" % self.nds
        d = DSem(es.enter_context(self.nc.semaphore(name)), name)
        self.dsems.append(d)
        return d

    def _deps(self, eng, rd, wr):
        deps = []
        for b in rd:
            if b.w is not None:
                deps.append(b.w)
        for b in wr:
            if b.w is not None:
                deps.append(b.w)
            for k, t in b.r.items():
                if t[0] == "E" and t[1] is eng:
                    continue
                deps.append(t)
        return deps

    def _upd(self, tok, key, rd, wr):
        for b in rd:
            b.r[key] = tok
        for b in wr:
            b.w = tok
            b.r = {}

    def op(self, eng, method, *a, rd=(), wr=(), **kw):
        eng.wait(self._deps(eng, rd, wr))
        ins = getattr(eng.e, method)(*a, **kw)
        tok = eng.issue(ins)
        if (not eng.is_pe) or kw.get("stop") or method == "transpose":
            eng.mark_now()
        self._upd(tok, eng.name, rd, wr)
        return tok

    def dma(self, q, out, in_, ds, rd=(), wr=(), **kw):
        q.wait(self._deps(q, rd, wr))
        ins = q.e.dma_start(out=out, in_=in_, **kw)
        q.issue(ins, True)
        ds.n += 16
        ins.then_inc(ds.sem, 16)
        tok = ("D", ds, ds.n)
        self._upd(tok, ds.name, rd, wr)
        return tok

    def idma(self, out, out_off, in_, in_off, ds, bound, rd=(), wr=()):
        q = self.pool
        q.wait(self._deps(q, rd, wr))
        if not hasattr(self, "_bound_reg"):
            self._bound_reg = {}
        if bound not in self._bound_reg:
            self._bound_reg[bound] = q.e.to_reg(bound)
        ins = q.e.indirect_dma_start(out=out, out_offset=out_off, in_=in_, in_offset=in_off,
                                     bounds_check=self._bound_reg[bound], oob_is_err=False)
        q.issue(ins, True)
        ds.n += 16
        ins.then_inc(ds.sem, 16)
        tok = ("D", ds, ds.n)
        self._upd(tok, ds.name, rd, wr)
        return tok

    def barrier(self):
        toks = []
        for e in self.engs:
            if e.idx > 0 and e is not self.sp:
                toks.append(("E", e, e.idx))
        for d in self.dsems:
            if d.n > 0:
                toks.append(("D", d, d.n))
        for e in self.engs:
            e.wait([t for t in toks if not (t[0] == "E" and t[1] is e)])


def build(S, CAP, dbg=False):
    NB = S // 128
    NOWN = NB // 4
    NG = NB // 4
    TOWN = NOWN * 128
    NSLOT = NE * CAP
    NBLK = CAP // 128
    SQD = float(np.sqrt(D))

    nc = bass.Bass("TRN2", target_bir_lowering=False)

    def inp(name, shape, dt=F32):
        return nc.dram_tensor(name, shape, dt, kind="ExternalInput").ap()

    def scr(name, shape, dt):
        return nc.dram_tensor(name, shape, dt, kind="Internal").ap()

    x_seq = inp("x_seq", [S, D]); x_own = inp("x_own", [TOWN, D]); x_prev = inp("x_prev", [TOWN, D])
    cT = inp("cT", [128, KC]); w_ada = inp("w_ada", [D, 6 * D]); b_ada = inp("b_ada", [1, 6 * D])
    g_mix = inp("g_mix", [1, D]); w_in = inp("w_in", [D, 4624]); b_f = inp("b_f", [1, 16]); sinks = inp("sinks", [1, 16])
    g_out = inp("g_out", [1, D]); w_out = inp("w_out", [D, D]); g_moe = inp("g_moe", [1, D])
    w_r = inp("w_r", [D, 36]); b_r = inp("b_r", [1, 36])
    w_gate = inp("w_gate", [NE, D, DE]); w_up = inp("w_up", [NE, D, DE]); w_down = inp("w_down", [NE, DE, D])
    g_fin = inp("g_fin", [1, D])
    fmask = inp("fmask", [128, 512]); swaA = inp("swaA", [128, 3 * 4 * 512]); rsel = inp("rsel", [128, 4])
    cst = inp("cst", [128, 4 * 128]); ebase = inp("ebase", [128, NE])
    out = nc.dram_tensor("out", [TOWN, D], F32, kind="ExternalOutput").ap()

    mod_s = scr("mod_s", [1, 6 * D], F32)
    KT_s = scr("KT_s", [16 * 64, S], BF16)
    VB_s = scr("VB_s", [S, 1024], BF16)
    QT_s = scr("QT_s", [16 * 64, TOWN], BF16)
    QAUG_s = scr("QAUG_s", [NOWN * 16, 3, 128], BF16)
    X1_s = scr("X1_s", [TOWN, D], F32)
    if dbg:
        Xs_s = nc.dram_tensor("Xs_s", [NSLOT, D], BF16, kind="ExternalOutput").ap()
        Ys_s = nc.dram_tensor("Ys_s", [NSLOT, D], F32, kind="ExternalOutput").ap()
        d_pg = nc.dram_tensor("d_pg", [128, NOWN * 2], I32, kind="ExternalOutput").ap()
        d_w = nc.dram_tensor("d_w", [128, NOWN * 2], F32, kind="ExternalOutput").ap()
    else:
        Xs_s = scr("Xs_s", [NSLOT, D], BF16)
        Ys_s = scr("Ys_s", [NSLOT, D], F32)

    w_in_v = w_in.rearrange("(kc p) n -> p kc n", p=128)

    with contextlib.ExitStack() as es:
        k = K(nc, es)
        pe, act, dve, pool, sp = k.pe, k.act, k.dve, k.pool, k.sp
        mark_tile = es.enter_context(nc.sbuf_tensor("mark_tile", [128, 8], F32))
        pool.mark_tile = mark_tile[:, 0:4]
        act.mark_tile = mark_tile[:, 4:8]
        for e_ in k.engs:
            e_.last_is_dma = False

        def sb(name, shape, dt, st=None):
            return (st or es).enter_context(nc.sbuf_tensor(name, shape, dt))

        def ps(name, shape, dt, st):
            return st.enter_context(nc.psum_tensor(name, shape, dt))

        cst_f = sb("cst_f", [128, 512], F32)
        ident_b = sb("ident_b", [128, 128], BF16)
        ssq = sb("ssq", [128, NB + 6 * NOWN + 8], F32)
        junk = sb("junk", [128, D], BF16)
        B_cst, B_identb, B_ssq, B_junk = Buf(), Buf(), Buf(), Buf()
        ds0 = k.dsem()
        k.dma(sp, cst_f[:], cst, ds0, wr=[B_cst])
        k.op(dve, "tensor_copy", out=ident_b[:], in_=cst_f[:, 0:128], rd=[B_cst], wr=[B_identb])
        k.op(pool, "memset", ssq[:], 0.0, wr=[B_ssq])
        ident_f = cst_f[:, 0:128]
        U_incl = cst_f[:, 128:256]
        U_strict = cst_f[:, 256:384]
        ones_f = cst_f[:, 384:512]
        ssq_ctr = [0]

        def new_ssq():
            i = ssq_ctr[0]
            ssq_ctr[0] += 1
            return ssq[:, i:i + 1]

        rstd_all = sb("rstd_all", [128, NB + 6 * NOWN + 8], F32)

        def rms_scale(src_ap, B_src, width, eps_scaled):
            col = new_ssq()
            i = ssq_ctr[0] - 1
            Bc = Buf()
            Bc.w = B_ssq.w
            k.op(act, "activation", out=junk[:, 0:width], in_=src_ap, func=AF.Square, accum_out=col,
                 rd=[B_src], wr=[Bc, B_junk])
            r = rstd_all[:, i:i + 1]
            Br = Buf()
            k.op(act, "activation", out=r, in_=col, func=AF.Sqrt, bias=float(eps_scaled), scale=1.0, rd=[Bc], wr=[Br])
            k.op(dve, "reciprocal", out=r, in_=r, rd=[Br], wr=[Br])
            return r, Br

        cumN = sb("cumN", [128, NB, 16], F32)
        B_cumN = Buf()
        stA = contextlib.ExitStack()
        es.enter_context(stA)
        stB = stA
        cT_sb = sb("cT_sb", [128, KC], F32, stA)
        sil = sb("sil", [128, KC], BF16, stA)
        B_cT, B_sil = Buf(), Buf()
        k.dma(sp, cT_sb[:], cT, ds0, wr=[B_cT])
        k.op(act, "activation", out=sil[:], in_=cT_sb[:], func=AF.Silu, rd=[B_cT], wr=[B_sil])
        wa = [sb("wa%d" % i, [128, KC, 512], BF16, stA) for i in range(2)]
        B_wa = [Buf(), Buf()]
        ds_wa = [k.dsem(), k.dsem()]
        brow = [sb("brow%d" % i, [1, 512], F32, stA) for i in range(2)]
        B_brow = [Buf(), Buf()]
        ds_br = [k.dsem(), k.dsem()]
        mrow = [sb("mrow%d" % i, [1, 512], F32, stA) for i in range(2)]
        B_mrow = [Buf(), Buf()]
        ds_mr = [k.dsem(), k.dsem()]
        B_mod = [Buf() for _ in range(24)]
        w_ada_v = w_ada.rearrange("(kc p) n -> p kc n", p=128)
        ada_state = {"loaded": 0, "done": 0}

        def ada_load(blk):
            s = blk % 2
            k.dma(pool, wa[s][:], w_ada_v[:, :, blk * 512:(blk + 1) * 512], ds_wa[s], wr=[B_wa[s]])
            k.dma(sp, brow[s][:], b_ada[0:1, blk * 512:(blk + 1) * 512], ds_br[s], wr=[B_brow[s]])

        def ada_block(blk, mod_ps, B_modps):
            s = blk % 2
            for kc in range(KC):
                k.op(pe, "matmul", mod_ps[0:1, :], lhsT=sil[:, kc:kc + 1], rhs=wa[s][:, kc, :], start=(kc == 0),
                     stop=(kc == KC - 1), rd=[B_sil, B_wa[s]], wr=[B_modps])
            if (blk // 4) in (1, 4):
                k.op(dve, "scalar_tensor_tensor", out=mrow[s][:], in0=mod_ps[0:1, :], scalar=1.0, in1=brow[s][:],
                     op0=ALU.add, op1=ALU.add, rd=[B_modps, B_brow[s]], wr=[B_mrow[s]])
            else:
                k.op(dve, "tensor_tensor", out=mrow[s][:], in0=mod_ps[0:1, :], in1=brow[s][:], op=ALU.add,
                     rd=[B_modps, B_brow[s]], wr=[B_mrow[s]])
            k.dma(sp, mod_s[0:1, blk * 512:(blk + 1) * 512], mrow[s][:], ds_mr[s], rd=[B_mrow[s]], wr=[B_mod[blk]])

        def ada_step(mod_ps, B_modps):
            b = ada_state["done"]
            if b >= 24:
                return
            while ada_state["loaded"] < min(24, b + 2):
                ada_load(ada_state["loaded"])
                ada_state["loaded"] += 1
            ada_block(b, mod_ps, B_modps)
            ada_state["done"] += 1

        def bcast_load(dst, B_dst, chunk, ds):
            k.dma(sp, dst[:], mod_s[0:1, chunk * D:(chunk + 1) * D].partition_broadcast(128), ds,
                  rd=[B_mod[chunk * 4 + i] for i in range(4)], wr=[B_dst])

        def vec_bcast_load(dst, B_dst, src, ds, n=D):
            k.dma(sp, dst, src[0:1, 0:n].partition_broadcast(128), ds, wr=[B_dst])

        G1s = sb("G1s", [128, D], F32, stB)
        SHa = sb("SHa", [128, D], F32, stB)
        B_G1s, B_SHa = Buf(), Buf()
        mod_ps = ps("mod_ps", [128, 512], F32, stB)
        B_modps = Buf()
        for _ in range(8):
            ada_step(mod_ps, B_modps)
        B_tmpg = Buf()
        bcast_load(G1s, B_G1s, 1, ds0)
        bcast_load(SHa, B_SHa, 0, ds0)

        Wkb = sb("Wkb", [128, KC, 1024], BF16, stB)
        Wvb = sb("Wvb", [128, KC, 1024], BF16, stB)
        Wf = sb("Wf", [128, KC, 16], BF16, stB)
        B_Wkb, B_Wvb, B_Wf = Buf(), Buf(), Buf()
        ds_w = k.dsem()
        for kc in range(KC):
            k.dma(pool, Wkb[:, kc, :], w_in_v[:, kc, KB0:KB0 + 1024], ds_w, wr=[B_Wkb])
        for kc in range(KC):
            k.dma(pool, Wvb[:, kc, :], w_in_v[:, kc, VB0:VB0 + 1024], ds_w, wr=[B_Wvb])
        k.dma(pool, Wf[:], w_in_v[:, :, FB0:FB0 + 16], ds_w, wr=[B_Wf])
        bF = sb("bF", [128, 16], F32, stB)
        B_bF = Buf()
        vec_bcast_load(bF[:], B_bF, b_f, ds0, 16)

        NXS = 2
        xts = [sb("xt%d" % i, [128, D], F32, stB) for i in range(NXS)]
        B_xt = [Buf() for _ in range(NXS)]
        ds_xt = [k.dsem() for _ in range(NXS)]
        vec_bcast_load(xts[0][:], B_xt[0], g_mix, ds0)
        k.op(dve, "scalar_tensor_tensor", out=G1s[:], in0=G1s[:], scalar=SQD, in1=xts[0][:], op0=ALU.mult, op1=ALU.mult,
             rd=[B_xt[0]], wr=[B_G1s])
        hb = [sb("hb%d" % i, [128, D], BF16, stB) for i in range(2)]
        B_hb = [Buf(), Buf()]
        hTg = [sb("hTg%d" % i, [128, KC, 512], BF16, stB) for i in range(2)]
        B_hTg = [[Buf() for _ in range(4)] for _ in range(2)]
        hT_ps = [ps("hT_ps%d" % i, [128, D], BF16, stB) for i in range(1)]
        B_hTps = [Buf()]
        NMM = 4
        mm_ps = [ps("mm_ps%d" % i, [128, 512], F32, stB) for i in range(NMM)]
        B_mm = [Buf() for _ in range(NMM)]
        f_ps = ps("f_ps", [128, 512], F32, stB)
        B_fps = [Buf(), Buf()]
        kT_sb = [sb("kT_sb%d" % i, [128, 512], BF16, stB) for i in range(2)]
        B_kTsb = [Buf(), Buf()]
        ds_kT = [k.dsem(), k.dsem()]
        v_sb = [sb("v_sb%d" % i, [128, 1024], BF16, stB) for i in range(2)]
        B_vsb = [Buf(), Buf()]
        ds_v = [k.dsem(), k.dsem()]
        zf = sb("zf", [128, NB, 16], F32, stB)
        B_zf = Buf()
        cnt = {"xt": 0, "hb": 0, "mm": 0, "kT": 0, "v": 0, "f": 0, "hTps": 0}

        def make_h(src_rows, Bx_extra_rd=()):
            s = cnt["xt"] % NXS
            cnt["xt"] += 1
            k.dma(sp, xts[s][:], src_rows, ds_xt[s], wr=[B_xt[s]])
            r, Br = rms_scale(xts[s][:], B_xt[s], D, D * EPS)
            k.op(dve, "scalar_tensor_tensor", out=xts[s][:], in0=xts[s][:], scalar=r, in1=G1s[:], op0=ALU.mult,
                 op1=ALU.mult, rd=[Br, B_G1s], wr=[B_xt[s]])
            hs = cnt["hb"] % 2
            cnt["hb"] += 1
            k.op(dve, "tensor_tensor", out=hb[hs][:], in0=xts[s][:], in1=SHa[:], op=ALU.add,
                 rd=[B_xt[s], B_SHa], wr=[B_hb[hs]])
            return hb[hs], B_hb[hs]

        def transpose_to(h_t, B_h, dst_ap3, B_dst, eng=None):
            s = cnt["hTps"] % len(hT_ps)
            cnt["hTps"] += 1
            for kc in range(KC):
                k.op(pe, "transpose", out=hT_ps[s][:, kc * 128:(kc + 1) * 128], in_=h_t[:, kc * 128:(kc + 1) * 128],
                     identity=ident_b[:], rd=[B_h, B_identb], wr=[B_hTps[s]])
            e = eng or act
            if e is act:
                k.op(act, "copy", out=dst_ap3, in_=hT_ps[s][:].rearrange("p (k t) -> p k t", k=KC),
                     rd=[B_hTps[s]], wr=[B_dst])
            else:
                k.op(e, "tensor_copy", out=dst_ap3, in_=hT_ps[s][:].rearrange("p (k t) -> p k t", k=KC),
                     rd=[B_hTps[s]], wr=[B_dst])

        for i in range(4):
            h_t, B_h = make_h(x_seq[i * 128:(i + 1) * 128, :])
            transpose_to(h_t, B_h, hTg[0][:, :, i * 128:(i + 1) * 128], B_hTg[0][i])
        for g in range(NG):
            gs = g % 2
            hq = None
            for c in range(8):
                ms = cnt["mm"] % NMM
                cnt["mm"] += 1
                for kc in range(KC):
                    k.op(pe, "matmul", mm_ps[ms][:], lhsT=Wkb[:, kc, c * 128:(c + 1) * 128], rhs=hTg[gs][:, kc, :],
                         start=(kc == 0), stop=(kc == KC - 1), rd=[B_Wkb] + B_hTg[gs], wr=[B_mm[ms]])
                ks = cnt["kT"] % 2
                cnt["kT"] += 1
                k.op(act if c % 2 else dve, "copy" if c % 2 else "tensor_copy", out=kT_sb[ks][:], in_=mm_ps[ms][:],
                     rd=[B_mm[ms]], wr=[B_kTsb[ks]])
                k.dma(pool, KT_s[c * 128:(c + 1) * 128, g * 512:(g + 1) * 512], kT_sb[ks][:], ds_kT[ks], rd=[B_kTsb[ks]])
                if g + 1 < NG:
                    i_n = c // 2
                    t_n = 4 * (g + 1) + i_n
                    if c % 2 == 0:
                        hq = make_h(x_seq[t_n * 128:(t_n + 1) * 128, :])
                    else:
                        transpose_to(hq[0], hq[1], hTg[1 - gs][:, :, i_n * 128:(i_n + 1) * 128], B_hTg[1 - gs][i_n])
            for i in range(4):
                t = 4 * g + i
                vs = cnt["v"] % 2
                cnt["v"] += 1
                for n in range(2):
                    ms = cnt["mm"] % NMM
                    cnt["mm"] += 1
                    for kc in range(KC):
                        k.op(pe, "matmul", mm_ps[ms][:], lhsT=hTg[gs][:, kc, i * 128:(i + 1) * 128],
                             rhs=Wvb[:, kc, n * 512:(n + 1) * 512], start=(kc == 0), stop=(kc == KC - 1),
                             rd=[B_Wvb, B_hTg[gs][i]], wr=[B_mm[ms]])
                    k.op(act, "copy", out=v_sb[vs][:, n * 512:(n + 1) * 512], in_=mm_ps[ms][:], rd=[B_mm[ms]],
                         wr=[B_vsb[vs]])
                k.dma(pool, VB_s[t * 128:(t + 1) * 128, :], v_sb[vs][:], ds_v[vs], rd=[B_vsb[vs]])
                fs = cnt["f"] % 2
                cnt["f"] += 1
                for kc in range(KC):
                    k.op(pe, "matmul", f_ps[:, fs * 16:(fs + 1) * 16], lhsT=hTg[gs][:, kc, i * 128:(i + 1) * 128],
                         rhs=Wf[:, kc, :], start=(kc == 0), stop=(kc == KC - 1), rd=[B_Wf, B_hTg[gs][i]],
                         wr=[B_fps[fs]])
                k.op(dve, "tensor_tensor", out=zf[:, t, :], in0=f_ps[:, fs * 16:(fs + 1) * 16], in1=bF[:], op=ALU.add,
                     rd=[B_fps[fs], B_bF], wr=[B_zf])
            ada_step(mod_ps, B_modps)
        while ada_state["done"] < 24:
            ada_step(mod_ps, B_modps)

        NC16 = NB * 16
        zf2 = zf[:].rearrange("p t h -> p (t h)")
        k.op(act, "activation", out=zf2, in_=zf2, func=AF.Exp, scale=-1.0, rd=[B_zf], wr=[B_zf])
        k.op(act, "activation", out=zf2, in_=zf2, func=AF.Ln, bias=1.0, scale=1.0, rd=[B_zf], wr=[B_zf])
        cumN2 = cumN[:].rearrange("p t h -> p (t h)")
        class _V:
            def __init__(self, t):
                self.t = t
            def __getitem__(self, idx):
                return self.t[:, 0:NB * 16].rearrange("p (t h) -> p t h", h=16)[idx]
        pfx = [_V(xts[i]) for i in range(2)]
        B_pfx = [B_xt[0], B_xt[1]]
        for c0 in range(0, NC16, 512):
            c1 = min(NC16, c0 + 512)
            w = c1 - c0
            k.op(pe, "matmul", mm_ps[0][:, 0:w], lhsT=U_incl, rhs=zf2[:, c0:c1], start=True, stop=True,
                 rd=[B_zf, B_cst], wr=[B_mm[0]])
            k.op(pe, "matmul", mm_ps[1][:, 0:w], lhsT=ones_f, rhs=zf2[:, c0:c1], start=True, stop=True,
                 rd=[B_zf, B_cst], wr=[B_mm[1]])
            k.op(dve, "tensor_copy", out=cumN2[:, c0:c1], in_=mm_ps[0][:, 0:w], rd=[B_mm[0]], wr=[B_cumN])
            k.op(dve, "tensor_copy", out=pfx[0][:].rearrange("p t h -> p (t h)")[:, c0:c1], in_=mm_ps[1][:, 0:w],
                 rd=[B_mm[1]], wr=[B_pfx[0]])
        cur = 0
        sh = 1
        while sh < NB:
            nx = 1 - cur
            k.op(dve, "tensor_copy", out=pfx[nx][:, 0:sh, :], in_=pfx[cur][:, 0:sh, :], rd=[B_pfx[cur]], wr=[B_pfx[nx]])
            k.op(dve, "tensor_tensor", out=pfx[nx][:, sh:NB, :], in0=pfx[cur][:, sh:NB, :], in1=pfx[cur][:, 0:NB - sh, :],
                 op=ALU.add, rd=[B_pfx[cur]], wr=[B_pfx[nx]])
            cur = nx
            sh *= 2
        k.op(dve, "tensor_tensor", out=cumN[:, 1:NB, :], in0=cumN[:, 1:NB, :], in1=pfx[cur][:, 0:NB - 1, :], op=ALU.add,
             rd=[B_pfx[cur]], wr=[B_cumN])
        rsel_sb = sb("rsel_sb", [128, 4], F32, stB)
        B_rsel = Buf()
        k.dma(sp, rsel_sb[:], rsel, ds0, wr=[B_rsel])
        cq = sb("cq", [128, NOWN, 16], F32, stB)
        B_cq = Buf()
        cumN4 = cumN[:].rearrange("p (j u) h -> p j u h", u=4)
        k.op(dve, "tensor_scalar", out=cq[:], in0=cumN4[:, :, 0, :], scalar1=rsel_sb[:, 0:1], scalar2=-8.0, op0=ALU.mult,
             op1=ALU.mult, rd=[B_cumN, B_rsel], wr=[B_cq])
        cq8 = sb("cq8", [128, NOWN, 16], F32, stB)
        for u in range(1, 4):
            k.op(dve, "tensor_scalar", out=cq8[:], in0=cumN4[:, :, u, :], scalar1=rsel_sb[:, u:u + 1], scalar2=-8.0,
                 op0=ALU.mult, op1=ALU.mult, rd=[B_cumN, B_rsel], wr=[B_tmpg])
            k.op(dve, "tensor_tensor", out=cq[:], in0=cq[:], in1=cq8[:], op=ALU.add, rd=[B_tmpg], wr=[B_cq])
        NQ = NOWN * 16
        cqf = cq[:].rearrange("p j h -> p (j h)")
        c3 = [sb("c3_%d" % i, [128, NQ], BF16, stB) for i in range(3)]
        B_c3 = [Buf() for _ in range(3)]
        for i in range(3):
            k.op(dve, "tensor_copy", out=c3[i][:], in_=cqf, rd=[B_cq], wr=[B_c3[i]])
            if i < 2:
                k.op(dve, "tensor_tensor", out=cqf, in0=cqf, in1=c3[i][:], op=ALU.subtract, rd=[B_c3[i]], wr=[B_cq])
        qa_sb = sb("qa_sb", [128, 3, 128], BF16, stB)
        B_qasb = Buf()
        ds_qa = k.dsem()
        for c0 in range(0, NQ, 128):
            w = min(128, NQ - c0)
            for i in range(3):
                k.op(pe, "transpose", out=hT_ps[0][0:w, i * 128:(i + 1) * 128], in_=c3[i][:, c0:c0 + w],
                     identity=ident_b[:], rd=[B_c3[i], B_identb], wr=[B_hTps[0]])
            k.op(dve, "tensor_copy", out=qa_sb[0:w, :, :], in_=hT_ps[0][0:w, 0:384].rearrange("p (k t) -> p k t", k=3),
                 rd=[B_hTps[0]], wr=[B_qasb])
            k.dma(sp, QAUG_s[c0:c0 + w, :, :], qa_sb[0:w, :, :], ds_qa, rd=[B_qasb])
        k.barrier()
        stB.close()

        stOA = contextlib.ExitStack()
        es.enter_context(stOA)
        o_a = sb("o_a", [128, NOWN, 1024], BF16, stOA)
        B_oa = [Buf() for _ in range(NOWN)]
        esink = sb("esink", [128, 16], F32, stOA)
        B_esink = Buf()
        vec_bcast_load(esink[:], B_esink, sinks, ds0, 16)
        k.op(act, "activation", out=esink[:], in_=esink[:], func=AF.Exp, rd=[B_esink], wr=[B_esink])

        stC = contextlib.ExitStack()
        es.enter_context(stC)
        G1s = sb("G1s_c", [128, D], F32, stC)
        SHa = sb("SHa_c", [128, D], F32, stC)
        B_G1s, B_SHa = Buf(), Buf()
        bcast_load(G1s, B_G1s, 1, ds0)
        bcast_load(SHa, B_SHa, 0, ds0)
        Wq = sb("Wq", [128, KC, 2048], BF16, stC)
        Wkv = sb("Wkv", [128, KC, 512], BF16, stC)
        B_Wq, B_Wkv = Buf(), Buf()
        for kc in range(KC):
            k.dma(pool, Wq[:, kc, 0:1024], w_in_v[:, kc, QA0:QA0 + 1024], ds_w, wr=[B_Wq])
            k.dma(pool, Wq[:, kc, 1024:2048], w_in_v[:, kc, QB0:QB0 + 1024], ds_w, wr=[B_Wq])
        k.dma(pool, Wkv[:], w_in_v[:, :, KA0:KA0 + 512], ds_w, wr=[B_Wkv])
        swaA_sb = sb("swaA_sb", [128, 3 * 4 * 512], BF16, stC)
        B_swaA = Buf()
        for i in range(6):
            k.dma(pool, swaA_sb[:, i * 1024:(i + 1) * 1024], swaA[:, i * 1024:(i + 1) * 1024], ds_w, wr=[B_swaA])
        NXS = 2
        xts = [sb("xtc%d" % i, [128, D], F32, stC) for i in range(NXS)]
        B_xt = [Buf() for _ in range(NXS)]
        vec_bcast_load(xts[0][:], B_xt[0], g_mix, ds0)
        k.op(dve, "scalar_tensor_tensor", out=G1s[:], in0=G1s[:], scalar=SQD, in1=xts[0][:], op0=ALU.mult, op1=ALU.mult,
             rd=[B_xt[0]], wr=[B_G1s])
        hb = [sb("hbc%d" % i, [128, D], BF16, stC) for i in range(2)]
        B_hb = [Buf(), Buf()]
        hT2 = [sb("hT2_%d" % i, [128, KC, 128], BF16, stC) for i in range(2)]
        B_hT2 = [Buf(), Buf()]
        hT_ps = [ps("hT_psc%d" % i, [128, D], BF16, stC) for i in range(1)]
        B_hTps = [Buf()]
        mm_ps = [ps("mm_psc%d" % i, [128, 512], F32, stC) for i in range(2)]
        B_mm = [Buf(), Buf()]
        s_ps = ps("s_psc", [128, 512], F32, stC)
        B_sps = Buf()
        o_ps = ps("o_psc", [128, 512], F32, stC)
        B_ops = Buf()
        tr_ps = ps("tr_psc", [128, 512], F32, stC)
        B_trps = Buf()
        q_tok = sb("q_tok", [128, 2048], BF16, stC)
        B_qtok = Buf()
        kv_tok = [sb("kv_tok%d" % i, [128, 512], BF16, stC) for i in range(2)]
        B_kvtok = [Buf(), Buf()]
        qaT = sb("qaT", [64, 16, 128], BF16, stC)
        qbT = sb("qbT", [64, 16, 128], BF16, stC)
        kaT = sb("kaT", [64, 2, 4, 128], BF16, stC)
        va = sb("va", [128, 2, 4, 65], BF16, stC)
        B_qaT, B_qbT, B_kaT, B_va = Buf(), Buf(), Buf(), Buf()
        k.op(pool, "memset", va[:], 1.0, wr=[B_va])
        pT = [sb("pTc%d" % i, [128, 512], BF16, stC) for i in range(2)]
        B_pT = [Buf(), Buf()]
        oT_sb = sb("oT_sbc", [65, 512], F32, stC)
        B_oTsb = Buf()
        den = sb("denc", [128, 8], F32, stC)
        B_den = Buf()
        ds_qb = k.dsem()
        cnt = {"xt": 0, "hb": 0, "mm": 0, "hTps": 0, "pT": 0}
        QT_v = QT_s.rearrange("(h d) t -> d h t", d=64)

        for j in range(NOWN):
            for which, src in ((0, x_own), (1, x_prev)):
                h_t, B_h = make_h(src[j * 128:(j + 1) * 128, :])
                transpose_to(h_t, B_h, hT2[which][:], B_hT2[which])
            for n in range(4):
                ms = cnt["mm"] % 2
                cnt["mm"] += 1
                for kc in range(KC):
                    k.op(pe, "matmul", mm_ps[ms][:], lhsT=hT2[0][:, kc, :], rhs=Wq[:, kc, n * 512:(n + 1) * 512],
                         start=(kc == 0), stop=(kc == KC - 1), rd=[B_Wq, B_hT2[0]], wr=[B_mm[ms]])
                k.op(dve if n % 2 else act, "tensor_copy" if n % 2 else "copy", out=q_tok[:, n * 512:(n + 1) * 512],
                     in_=mm_ps[ms][:], rd=[B_mm[ms]], wr=[B_qtok])
            for which in range(2):
                ms = cnt["mm"] % 2
                cnt["mm"] += 1
                for kc in range(KC):
                    k.op(pe, "matmul", mm_ps[ms][:], lhsT=hT2[which][:, kc, :], rhs=Wkv[:, kc, :],
                         start=(kc == 0), stop=(kc == KC - 1), rd=[B_Wkv, B_hT2[which]], wr=[B_mm[ms]])
                k.op(dve, "tensor_copy", out=kv_tok[which][:], in_=mm_ps[ms][:], rd=[B_mm[ms]], wr=[B_kvtok[which]])
                kb = 1 - which
                k.op(dve, "tensor_copy", out=va[:, kb, :, 0:64],
                     in_=kv_tok[which][:, 256:512].rearrange("p (h d) -> p h d", h=4), rd=[B_kvtok[which]], wr=[B_va])
            for half, dstT, B_dst in ((0, qaT, B_qaT), (1, qbT, B_qbT)):
                for hh in range(2):
                    s = cnt["hTps"] % len(hT_ps)
                    cnt["hTps"] += 1
                    for i8 in range(8):
                        g_ = hh * 8 + i8
                        c0 = half * 1024 + g_ * 64
                        k.op(pe, "transpose", out=hT_ps[s][0:64, i8 * 128:(i8 + 1) * 128], in_=q_tok[:, c0:c0 + 64],
                             identity=ident_b[:], rd=[B_qtok, B_identb], wr=[B_hTps[s]])
                    k.op(act if hh else dve, "copy" if hh else "tensor_copy", out=dstT[:, hh * 8:(hh + 1) * 8, :],
                         in_=hT_ps[s][0:64, 0:1024].rearrange("p (h t) -> p h t", h=8), rd=[B_hTps[s]], wr=[B_dst])
            k.dma(sp, QT_v[:, :, j * 128:(j + 1) * 128], qbT[:], ds_qb, rd=[B_qbT])
            s = cnt["hTps"] % len(hT_ps)
            cnt["hTps"] += 1
            for which in range(2):
                kb = 1 - which
                for hk in range(4):
                    k.op(pe, "transpose", out=hT_ps[s][0:64, (kb * 4 + hk) * 128:(kb * 4 + hk + 1) * 128],
                         in_=kv_tok[which][:, hk * 64:(hk + 1) * 64], identity=ident_b[:],
                         rd=[B_kvtok[which], B_identb], wr=[B_hTps[s]])
            k.op(dve, "tensor_copy", out=kaT[:].rearrange("p a h t -> p (a h) t"),
                 in_=hT_ps[s][0:64, 0:1024].rearrange("p (h t) -> p h t", h=8), rd=[B_hTps[s]], wr=[B_kaT])
            for hk in range(4):
                for kb in range(2):
                    for i in range(4):
                        k.op(pe, "matmul", s_ps[:, i * 128:(i + 1) * 128], lhsT=kaT[:, kb, hk, :],
                             rhs=qaT[:, hk * 4 + i, :], start=True, stop=True, rd=[B_kaT, B_qaT], wr=[B_sps])
                    p_ = cnt["pT"] % 2
                    cnt["pT"] += 1
                    k.op(act, "activation", out=pT[p_][:], in_=s_ps[:], func=AF.Exp, scale=0.125, rd=[B_sps],
                         wr=[B_pT[p_]])
                    tab = (0 if j == 0 else 1) if kb == 0 else 2
                    a0 = (tab * 4 + hk) * 512
                    k.op(dve, "tensor_tensor", out=pT[p_][:], in0=pT[p_][:], in1=swaA_sb[:, a0:a0 + 512], op=ALU.mult,
                         rd=[B_swaA], wr=[B_pT[p_]])
                    k.op(pe, "matmul", o_ps[0:65, :], lhsT=va[:, kb, hk, :], rhs=pT[p_][:], start=(kb == 0),
                         stop=(kb == 1), rd=[B_va, B_pT[p_]], wr=[B_ops])
                k.op(act, "copy", out=oT_sb[:], in_=o_ps[0:65, :], rd=[B_ops], wr=[B_oTsb])
                for i in range(4):
                    k.op(pe, "transpose", out=tr_ps[:, i * 65:(i + 1) * 65], in_=oT_sb[:, i * 128:(i + 1) * 128],
                         identity=ident_f[0:65, 0:65], rd=[B_oTsb, B_cst], wr=[B_trps])
                tr3 = tr_ps[:, 0:260].rearrange("p (h c) -> p h c", h=4)
                k.op(dve, "tensor_tensor", out=den[:, 0:4], in0=tr3[:, :, 64], in1=esink[:, hk * 4:(hk + 1) * 4],
                     op=ALU.add, rd=[B_trps, B_esink], wr=[B_den])
                k.op(dve, "reciprocal", out=den[:, 4:8], in_=den[:, 0:4], rd=[B_den], wr=[B_den])
                for i in range(4):
                    g_ = hk * 4 + i
                    k.op(dve, "tensor_scalar", out=o_a[:, j, g_ * 64:(g_ + 1) * 64], in0=tr3[:, i, 0:64],
                         scalar1=den[:, 4 + i:5 + i], scalar2=None, op0=ALU.mult, rd=[B_trps, B_den], wr=[B_oa[j]])
        k.barrier()
        stC.close()

        stOB = contextlib.ExitStack()
        es.enter_context(stOB)
        o_b = sb("o_b", [128, NOWN, 1024], BF16, stOB)
        B_ob = [Buf() for _ in range(NOWN)]
        stF = contextlib.ExitStack()
        es.enter_context(stF)
        kTa = [sb("kTa%d" % i, [67, S], BF16, stF) for i in range(2)]
        vau = [sb("vau%d" % i, [128, NB, 65], BF16, stF) for i in range(2)]
        qTa = [sb("qTa%d" % i, [67, TOWN], BF16, stF) for i in range(2)]
        B_kTa, B_vau, B_qTa = [Buf(), Buf()], [Buf(), Buf()], [Buf(), Buf()]
        ds_hd = [k.dsem(), k.dsem()]
        for i in range(2):
            k.op(pool, "memset", kTa[i][64:67, :], 1.0, wr=[B_kTa[i]])
            k.op(pool, "memset", vau[i][:], 1.0, wr=[B_vau[i]])
        fm_sb = sb("fm_sb", [128, 512], BF16, stF)
        B_fm = Buf()
        k.dma(pool, fm_sb[:], fmask, ds_w, wr=[B_fm])
        NPT = 4
        pT = [sb("pTf%d" % i, [128, 1024], BF16, stF) for i in range(NPT)]
        B_pT = [Buf() for _ in range(NPT)]
        NSP = 3
        s_ps = [ps("s_psf%d" % i, [128, 1024], F32, stF) for i in range(NSP)]
        B_sps = [Buf() for _ in range(NSP)]
        o_ps = ps("o_psf", [128, 1024], F32, stF)
        B_ops = Buf()
        tr_ps = [s_ps[0][:, 0:512], s_ps[0][:, 512:1024]]
        B_trps = [B_sps[0], B_sps[0]]
        oT_sb = sb("oT_sbf", [65, 1024], F32, stF)
        B_oTsb = Buf()
        den = sb("denf", [128, 8], F32, stF)
        B_den = Buf()
        VB_v = VB_s.rearrange("(t p) c -> p t c", p=128)
        QAUG_v = QAUG_s.rearrange("(j h) k t -> h k j t", h=16)
        HB = (NOWN + 1) // 2
        halves = [(0, HB), (HB, NOWN)] if NOWN > 1 else [(0, 1)]
        cnt = {"pT": 0, "s": 0, "tr": 0}

        def load_head(h):
            s = h % 2
            k.dma(sp, kTa[s][0:64, :], KT_s[h * 64:(h + 1) * 64, :], ds_hd[s], wr=[B_kTa[s]])
            k.dma(sp, qTa[s][0:64, :], QT_s[h * 64:(h + 1) * 64, :], ds_hd[s], wr=[B_qTa[s]])
            k.dma(sp, qTa[s][64:67, :].rearrange("k (j t) -> k j t", t=128), QAUG_v[h], ds_hd[s], wr=[B_qTa[s]])
            for t0 in range(0, NB, 16):
                t1 = min(NB, t0 + 16)
                k.dma(sp, vau[s][:, t0:t1, 0:64], VB_v[:, t0:t1, h * 64:(h + 1) * 64], ds_hd[s], wr=[B_vau[s]])

        load_head(0)
        for h in range(16):
            hs = h % 2
            if h + 1 < 16:
                load_head(h + 1)
            for (j0, j1) in halves:
                nb = j1 - j0
                def stage1(kt):
                    g = kt // 4
                    u = kt % 4
                    ja = max(g, j0)
                    c_lo = (ja - j0) * 128
                    c_hi = nb * 128
                    ss = cnt["s"] % NSP
                    cnt["s"] += 1
                    for b0 in range(0, 1024, 512):
                        lo, hi = max(c_lo, b0), min(c_hi, b0 + 512)
                        if lo >= hi:
                            continue
                        k.op(pe, "matmul", s_ps[ss][:, lo:hi], lhsT=kTa[hs][:, kt * 128:(kt + 1) * 128],
                             rhs=qTa[hs][:, j0 * 128 + lo:j0 * 128 + hi], start=True, stop=True,
                             rd=[B_kTa[hs], B_qTa[hs]], wr=[B_sps[ss]])
                    p_ = cnt["pT"] % NPT
                    cnt["pT"] += 1
                    k.op(act, "activation", out=pT[p_][:, c_lo:c_hi], in_=s_ps[ss][:, c_lo:c_hi], func=AF.Exp,
                         bias=cumN[:, kt, h:h + 1], scale=0.125, rd=[B_sps[ss], B_cumN], wr=[B_pT[p_]])
                    if g >= j0:
                        k.op(dve, "scalar_tensor_tensor", out=pT[p_][:, c_lo:c_lo + 128], in0=pT[p_][:, c_lo:c_lo + 128],
                             scalar=1e30, in1=fm_sb[:, u * 128:(u + 1) * 128], op0=ALU.min, op1=ALU.mult,
                             rd=[B_fm], wr=[B_pT[p_]])
                    return p_

                def stage2(kt, p_):
                    g = kt // 4
                    u = kt % 4
                    ja = max(g, j0)
                    c_lo = (ja - j0) * 128
                    c_hi = nb * 128
                    started = set()

                    def st_flag(lo_):
                        bank = lo_ // 512
                        if kt == 0 and bank not in started:
                            started.add(bank)
                            return True
                        return False
                    if g >= j0:
                        k.op(pe, "matmul", o_ps[0:65, c_lo:c_lo + 128], lhsT=vau[hs][:, kt, :],
                             rhs=pT[p_][:, c_lo:c_lo + 128], start=st_flag(c_lo), stop=(u == 3), skip_group_check=True,
                             rd=[B_vau[hs], B_pT[p_]], wr=[B_ops])
                        r_lo = c_lo + 128
                    else:
                        r_lo = c_lo
                    for b0 in range(0, 1024, 512):
                        lo, hi = max(r_lo, b0), min(c_hi, b0 + 512)
                        if lo >= hi:
                            continue
                        k.op(pe, "matmul", o_ps[0:65, lo:hi], lhsT=vau[hs][:, kt, :], rhs=pT[p_][:, lo:hi],
                             start=st_flag(lo), stop=False, skip_group_check=True, rd=[B_vau[hs], B_pT[p_]], wr=[B_ops])

                nkt = 4 * j1
                pq = []
                for kt in range(nkt + 2):
                    if kt < nkt:
                        pq.append((kt, stage1(kt)))
                    if kt >= 2:
                        k0, p0 = pq.pop(0)
                        stage2(k0, p0)
                assert not pq
                for b0 in range(0, nb * 128, 512):
                    b1 = min(nb * 128, b0 + 512)
                    k.op(act, "copy", out=oT_sb[:, b0:b1], in_=o_ps[0:65, b0:b1], rd=[B_ops], wr=[B_oTsb])
                for q0 in range(0, nb, 4):
                    q1 = min(nb, q0 + 4)
                    ts_ = cnt["tr"] % 2
                    cnt["tr"] += 1
                    for i in range(q1 - q0):
                        k.op(pe, "transpose", out=tr_ps[ts_][:, i * 65:(i + 1) * 65],
                             in_=oT_sb[:, (q0 + i) * 128:(q0 + i + 1) * 128], identity=ident_f[0:65, 0:65],
                             rd=[B_oTsb, B_cst], wr=[B_trps[ts_]])
                    tr3 = tr_ps[ts_][:, 0:260].rearrange("p (h c) -> p h c", h=4)
                    k.op(dve, "reciprocal", out=den[:, 0:q1 - q0], in_=tr3[:, 0:q1 - q0, 64], rd=[B_trps[ts_]], wr=[B_den])
                    for i in range(q1 - q0):
                        jj = j0 + q0 + i
                        k.op(dve, "tensor_scalar", out=o_b[:, jj, h * 64:(h + 1) * 64], in0=tr3[:, i, 0:64],
                             scalar1=den[:, i:i + 1], scalar2=None, op0=ALU.mult, rd=[B_trps[ts_], B_den], wr=[B_ob[jj]])
        k.barrier()
        stF.close()

        stD = contextlib.ExitStack()
        es.enter_context(stD)
        Wout = sb("Wout", [128, KC, D], BF16, stD)
        B_Wout = Buf()
        w_out_v = w_out.rearrange("(kc p) n -> p kc n", p=128)
        for kc in range(KC):
            for hh in range(2):
                k.dma(pool, Wout[:, kc, hh * 1024:(hh + 1) * 1024], w_out_v[:, kc, hh * 1024:(hh + 1) * 1024], ds_w,
                      wr=[B_Wout])
        gout = sb("gout", [128, D], F32, stD)
        GA = sb("GA", [128, D], F32, stD)
        B_gout, B_GA = Buf(), Buf()
        vec_bcast_load(gout[:], B_gout, g_out, ds0)
        k.op(dve, "tensor_scalar", out=gout[:], in0=gout[:], scalar1=32.0, scalar2=None, op0=ALU.mult, wr=[B_gout])
        bcast_load(GA, B_GA, 2, ds0)
        xts = [sb("xtd%d" % i, [128, D], F32, stD) for i in range(2)]
        B_xt = [Buf(), Buf()]
        ds_xt = [k.dsem(), k.dsem()]
        x1t = [sb("x1t%d" % i, [128, D], F32, stD) for i in range(2)]
        B_x1t = [Buf(), Buf()]
        ds_x1 = [k.dsem(), k.dsem()]
        mixed = sb("mixed", [128, D], BF16, stD)
        B_mixed = Buf()
        mT = sb("mT", [128, KC, 128], BF16, stD)
        B_mT = Buf()
        hT_ps = [ps("hT_psd%d" % i, [128, D], BF16, stD) for i in range(2)]
        B_hTps = [Buf(), Buf()]
        mm_ps = [ps("mm_psd%d" % i, [128, 512], F32, stD) for i in range(2)]
        B_mm = [Buf(), Buf()]
        cnt = {"mm": 0, "hTps": 0}
        for j in range(NOWN):
            s = j % 2
            k.dma(sp, xts[s][:], x_own[j * 128:(j + 1) * 128, :], ds_xt[s], wr=[B_xt[s]])
            ra, Bra = rms_scale(o_a[:, j, :], B_oa[j], 1024, 1024 * EPS)
            rb, Brb = rms_scale(o_b[:, j, :], B_ob[j], 1024, 1024 * EPS)
            k.op(dve, "scalar_tensor_tensor", out=mixed[:, 0:1024], in0=o_a[:, j, :], scalar=ra, in1=gout[:, 0:1024],
                 op0=ALU.mult, op1=ALU.mult, rd=[B_oa[j], Bra, B_gout], wr=[B_mixed])
            k.op(dve, "scalar_tensor_tensor", out=mixed[:, 1024:2048], in0=o_b[:, j, :], scalar=rb, in1=gout[:, 1024:2048],
                 op0=ALU.mult, op1=ALU.mult, rd=[B_ob[j], Brb, B_gout], wr=[B_mixed])
            transpose_to(mixed, B_mixed, mT[:], B_mT)
            for n in range(4):
                ms = cnt["mm"] % 2
                cnt["mm"] += 1
                for kc in range(KC):
                    k.op(pe, "matmul", mm_ps[ms][:], lhsT=mT[:, kc, :], rhs=Wout[:, kc, n * 512:(n + 1) * 512],
                         start=(kc == 0), stop=(kc == KC - 1), rd=[B_Wout, B_mT], wr=[B_mm[ms]])
                sl = slice(n * 512, (n + 1) * 512)
                k.op(dve, "tensor_tensor", out=x1t[s][:, sl], in0=mm_ps[ms][:], in1=GA[:, sl], op=ALU.mult,
                     rd=[B_mm[ms], B_GA], wr=[B_x1t[s]])
                k.op(pool, "tensor_tensor", out=x1t[s][:, sl], in0=x1t[s][:, sl], in1=xts[s][:, sl], op=ALU.add,
                     rd=[B_xt[s]], wr=[B_x1t[s]])
            k.dma(sp, X1_s[j * 128:(j + 1) * 128, :], x1t[s][:], ds_x1[s], rd=[B_x1t[s]])
        k.barrier()
        stD.close()
        stOB.close()
        stOA.close()

        rt_w = sb("rt_w", [128, NOWN, 2], F32)
        rt_pg = sb("rt_pg", [128, NOWN, 2], I32)
        B_rtw, B_rtpg = Buf(), Buf()
        stR = contextlib.ExitStack()
        es.enter_context(stR)
        G2s = sb("G2s", [128, D], F32, stR)
        SHm = sb("SHm", [128, D], F32, stR)
        tmp2 = sb("tmp2", [128, D], F32, stR)
        B_G2s, B_SHm, B_tmp2 = Buf(), Buf(), Buf()
        bcast_load(G2s, B_G2s, 4, ds0)
        bcast_load(SHm, B_SHm, 3, ds0)
        vec_bcast_load(tmp2[:], B_tmp2, g_moe, ds0)
        k.op(dve, "scalar_tensor_tensor", out=G2s[:], in0=G2s[:], scalar=SQD, in1=tmp2[:], op0=ALU.mult, op1=ALU.mult,
             rd=[B_tmp2], wr=[B_G2s])
        Wr = sb("Wr", [128, KC, 36], F32, stR)
        B_Wr = Buf()
        k.dma(sp, Wr[:], w_r.rearrange("(kc p) n -> p kc n", p=128), ds0, wr=[B_Wr])
        bR = sb("bR", [128, 36], F32, stR)
        B_bR = Buf()
        vec_bcast_load(bR[:], B_bR, b_r, ds0, 36)
        eb_sb = sb("eb_sb", [128, NE], F32, stR)
        B_eb = Buf()
        k.dma(sp, eb_sb[:], ebase, ds0, wr=[B_eb])
        cnt_run = sb("cnt_run", [128, NE], F32, stR)
        B_cnt = Buf()
        k.op(dve, "memset", cnt_run[:], 0.0, wr=[B_cnt])
        zt = sb("zt", [128, D], BF16, stR)
        B_zt = Buf()
        k.op(pool, "memset", zt[:], 0.0, wr=[B_zt])
        ds_z = k.dsem()
        B_Xs = Buf()
        for s0 in range(0, NSLOT, 128):
            k.dma(sp, Xs_s[s0:s0 + 128, :], zt[:], ds_z, rd=[B_zt], wr=[B_Xs])
        x1t = [sb("x1r%d" % i, [128, D], F32, stR) for i in range(2)]
        B_x1t = [Buf(), Buf()]
        ds_x1 = [k.dsem(), k.dsem()]
        h2b = [sb("h2b%d" % i, [128, D], BF16, stR) for i in range(2)]
        B_h2b = [Buf(), Buf()]
        ds_sc = [k.dsem(), k.dsem()]
        h2T = sb("h2T", [128, KC, 128], F32, stR)
        B_h2T = Buf()
        trf_ps = [ps("trf_ps%d" % i, [128, 1024], F32, stR) for i in range(2)]
        B_trf = [Buf(), Buf()]
        lg_ps = ps("lg_ps", [128, 512], F32, stR)
        B_lgps = Buf()
        rk_ps = ps("rk_ps", [128, 512], F32, stR)
        B_rkps = Buf()
        R = sb("R", [128, 512], F32, stR)
        B_R = Buf()
        psc = [sb("psc%d" % i, [128, 2], I32, stR) for i in range(2)]
        B_psc = [Buf(), Buf()]
        BIGI = float(4 * NSLOT)

        def rop(method, **kw):
            return k.op(dve, method, rd=[B_R], wr=[B_R], **kw)

        for j in range(NOWN):
            s = j % 2
            k.dma(sp, x1t[s][:], X1_s[j * 128:(j + 1) * 128, :], ds_x1[s], wr=[B_x1t[s]])
            r2, Br2 = rms_scale(x1t[s][:], B_x1t[s], D, D * EPS)
            k.op(dve, "scalar_tensor_tensor", out=x1t[s][:], in0=x1t[s][:], scalar=r2, in1=G2s[:], op0=ALU.mult,
                 op1=ALU.mult, rd=[Br2, B_G2s], wr=[B_x1t[s]])
            k.op(dve, "tensor_tensor", out=x1t[s][:], in0=x1t[s][:], in1=SHm[:], op=ALU.add, rd=[B_SHm], wr=[B_x1t[s]])
            k.op(act, "copy", out=h2b[s][:], in_=x1t[s][:], rd=[B_x1t[s]], wr=[B_h2b[s]])
            for hh in range(2):
                for i8 in range(8):
                    kc = hh * 8 + i8
                    k.op(pe, "transpose", out=trf_ps[hh][:, i8 * 128:(i8 + 1) * 128], in_=x1t[s][:, kc * 128:(kc + 1) * 128],
                         identity=ident_f, rd=[B_x1t[s], B_cst], wr=[B_trf[hh]])
                k.op(act if hh else dve, "copy" if hh else "tensor_copy", out=h2T[:, hh * 8:(hh + 1) * 8, :],
                     in_=trf_ps[hh][:].rearrange("p (k t) -> p k t", k=8), rd=[B_trf[hh]], wr=[B_h2T])
            for kc in range(KC):
                k.op(pe, "matmul", lg_ps[:, 0:36], lhsT=h2T[:, kc, :], rhs=Wr[:, kc, :], start=(kc == 0),
                     stop=(kc == KC - 1), rd=[B_h2T, B_Wr], wr=[B_lgps])
            LG = R[:, 0:36]; GL = R[:, 0:4]; EL = R[:, 4:36]
            GMAX = R[:, 40:41]; NGMAX = R[:, 41:42]; GSUM = R[:, 42:43]; GVAL = R[:, 43:44]
            GOH = R[:, 44:48]; GPEN = R[:, 48:52]; GEXP = R[:, 52:56]
            EM = R[:, 64:96]; T1 = R[:, 96:97]; T2 = R[:, 97:98]; OH1 = R[:, 100:132]; OH2 = R[:, 132:164]
            E2 = R[:, 164:196]; AA = R[:, 196:228]; RK = R[:, 228:260]; RKB = R[:, 260:292]; TMP = R[:, 292:324]
            DD = R[:, 324:325]; ED = R[:, 325:326]; W1 = R[:, 326:327]; W2 = R[:, 327:328]
            P1 = R[:, 328:329]; P2 = R[:, 329:330]; R1 = R[:, 330:331]; R2 = R[:, 331:332]
            V1 = R[:, 332:333]; V2 = R[:, 333:334]; OF1 = R[:, 334:335]; OF2 = R[:, 335:336]
            PS1 = R[:, 336:337]; PS2 = R[:, 337:338]
            k.op(dve, "tensor_tensor", out=LG, in0=lg_ps[:, 0:36], in1=bR[:], op=ALU.add, rd=[B_lgps, B_bR, B_R], wr=[B_R])
            rop("reduce_max", out=GMAX, in_=GL, axis=AX.X)
            rop("tensor_scalar", out=GOH, in0=GL, scalar1=GMAX, scalar2=None, op0=ALU.is_equal)
            rop("tensor_scalar", out=NGMAX, in0=GMAX, scalar1=-1.0, scalar2=None, op0=ALU.mult)
            k.op(act, "activation", out=GEXP, in_=GL, func=AF.Exp, bias=NGMAX, scale=1.0, rd=[B_R], wr=[B_R])
            rop("reduce_sum", out=GSUM, in_=GEXP, axis=AX.X)
            rop("reciprocal", out=GVAL, in_=GSUM)
            rop("tensor_scalar", out=GPEN, in0=GOH, scalar1=-1.0, scalar2=1e30, op0=ALU.add, op1=ALU.mult)
            for gi in range(4):
                rop("tensor_scalar", out=EM[:, gi * 8:(gi + 1) * 8], in0=EL[:, gi * 8:(gi + 1) * 8],
                    scalar1=GPEN[:, gi:gi + 1], scalar2=None, op0=ALU.add)
            rop("reduce_max", out=T1, in_=EM, axis=AX.X)
            rop("tensor_scalar", out=OH1, in0=EM, scalar1=T1, scalar2=None, op0=ALU.is_equal)
            rop("scalar_tensor_tensor", out=E2, in0=OH1, scalar=-1e30, in1=EM, op0=ALU.mult, op1=ALU.add)
            rop("reduce_max", out=T2, in_=E2, axis=AX.X)
            rop("tensor_scalar", out=OH2, in0=E2, scalar1=T2, scalar2=None, op0=ALU.is_equal)
            rop("tensor_tensor", out=DD, in0=T2, in1=T1, op=ALU.subtract)
            k.op(act, "activation", out=ED, in_=DD, func=AF.Exp, rd=[B_R], wr=[B_R])
            rop("tensor_scalar", out=ED, in0=ED, scalar1=1.0, scalar2=None, op0=ALU.add)
            rop("reciprocal", out=ED, in_=ED)
            rop("tensor_tensor", out=W1, in0=GVAL, in1=ED, op=ALU.mult)
            rop("tensor_tensor", out=W2, in0=GVAL, in1=W1, op=ALU.subtract)
            rop("tensor_tensor", out=AA, in0=OH1, in1=OH2, op=ALU.add)
            k.op(pe, "matmul", rk_ps[:, 0:32], lhsT=U_strict, rhs=AA, start=True, stop=True, rd=[B_R, B_cst], wr=[B_rkps])
            k.op(pe, "matmul", rk_ps[:, 32:64], lhsT=ones_f, rhs=AA, start=True, stop=True, rd=[B_R, B_cst], wr=[B_rkps])
            k.op(dve, "tensor_tensor", out=RK, in0=rk_ps[:, 0:32], in1=cnt_run[:], op=ALU.add, rd=[B_rkps, B_cnt, B_R],
                 wr=[B_R])
            k.op(dve, "tensor_tensor", out=cnt_run[:], in0=rk_ps[:, 32:64], in1=cnt_run[:], op=ALU.add, rd=[B_rkps, B_cnt],
                 wr=[B_cnt])
            k.op(dve, "tensor_tensor", out=RKB, in0=RK, in1=eb_sb[:], op=ALU.add, rd=[B_R, B_eb], wr=[B_R])
            for (OH, P_, R_, V_, OF_, PS_, W_, col) in ((OH1, P1, R1, V1, OF1, PS1, W1, 0), (OH2, P2, R2, V2, OF2, PS2, W2, 1)):
                rop("tensor_tensor", out=TMP, in0=OH, in1=RKB, op=ALU.mult)
                rop("reduce_sum", out=P_, in_=TMP, axis=AX.X)
                rop("tensor_tensor", out=TMP, in0=OH, in1=RK, op=ALU.mult)
                rop("reduce_sum", out=R_, in_=TMP, axis=AX.X)
                rop("tensor_scalar", out=V_, in0=R_, scalar1=float(CAP), scalar2=None, op0=ALU.is_lt)
                rop("tensor_scalar", out=OF_, in0=V_, scalar1=-BIGI, scalar2=BIGI, op0=ALU.mult, op1=ALU.add)
                rop("tensor_tensor", out=PS_, in0=P_, in1=OF_, op=ALU.add)
                k.op(dve, "tensor_copy", out=psc[s][:, col:col + 1], in_=PS_, rd=[B_R], wr=[B_psc[s]])
                rop("tensor_tensor", out=P_, in0=P_, in1=V_, op=ALU.mult)
                k.op(dve, "tensor_copy", out=rt_pg[:, j, col:col + 1], in_=P_, rd=[B_R], wr=[B_rtpg])
                k.op(dve, "tensor_tensor", out=rt_w[:, j, col:col + 1], in0=W_, in1=V_, op=ALU.mult, rd=[B_R], wr=[B_rtw])
            for col in range(2):
                k.idma(Xs_s, bass.IndirectOffsetOnAxis(ap=psc[s][:, col:col + 1], axis=0), h2b[s][:, :], None, ds_sc[s],
                       NSLOT - 1, rd=[B_h2b[s], B_psc[s], B_Xs])
        k.barrier()
        stR.close()

        stE = contextlib.ExitStack()
        es.enter_context(stE)
        wg = [sb("wg%d" % i, [128, KC, DE], BF16, stE) for i in range(2)]
        wu = [sb("wu%d" % i, [128, KC, DE], BF16, stE) for i in range(2)]
        wd = [sb("wd%d" % i, [128, 4, D], BF16, stE) for i in range(2)]
        B_we = [Buf(), Buf()]
        ds_we = [k.dsem(), k.dsem()]
        NXS_E = 4
        xs = [sb("xs%d" % i, [128, D], BF16, stE) for i in range(NXS_E)]
        B_xs = [Buf() for _ in range(NXS_E)]
        ds_xs = [k.dsem() for _ in range(NXS_E)]
        xsT = [sb("xsT%d" % i, [128, KC, 128], BF16, stE) for i in range(2)]
        B_xsT = [Buf(), Buf()]
        sg = [sb("sg%d" % i, [128, DE], BF16, stE) for i in range(2)]
        hid = [sb("hid%d" % i, [128, DE], BF16, stE) for i in range(2)]
        hidT = [sb("hidT%d" % i, [128, 4, 128], BF16, stE) for i in range(2)]
        B_sg, B_hid, B_hidT = [Buf(), Buf()], [Buf(), Buf()], [Buf(), Buf()]
        y_sb = [sb("y_sb%d" % i, [128, D], F32, stE) for i in range(2)]
        B_ysb = [Buf(), Buf()]
        ds_y = [k.dsem(), k.dsem()]
        hT_ps = [ps("hT_pse%d" % i, [128, 1024], BF16, stE) for i in range(1)]
        B_hTps = [Buf()]
        g_ps = [ps("g_ps%d" % i, [128, 512], F32, stE) for i in range(2)]
        u_ps = [ps("u_ps%d" % i, [128, 512], F32, stE) for i in range(2)]
        B_gps, B_ups = [Buf(), Buf()], [Buf(), Buf()]
        ht_ps = ps("ht_ps", [128, 512], BF16, stE)
        B_htps = Buf()
        y_ps = [ps("y_ps%d" % i, [128, 512], F32, stE) for i in range(2)]
        B_yps = [Buf(), Buf()]
        cnt = {"hTps": 0, "y": 0}

        NSTG = 6
        stg = [sb("stg%d" % i, [128, 2048], F32, stE) for i in range(NSTG)]
        B_stg = [Buf() for _ in range(NSTG)]
        ds_stg = [k.dsem() for _ in range(NSTG)]
        cast_rr = [0]
        B_wch = [[Buf() for _ in range(12)] for _ in range(2)]

        def expert_chunks(e):
            s = e % 2
            wgv = w_gate[e].rearrange("(kc p) n -> p kc n", p=128)
            wuv = w_up[e].rearrange("(kc p) n -> p kc n", p=128)
            wdv = w_down[e].rearrange("(kc p) n -> p kc n", p=128)
            tasks = []
            for q in range(4):
                tasks.append((wgv[:, q * 4:(q + 1) * 4, :], wg[s][:, q * 4:(q + 1) * 4, :], "p (k n) -> p k n", 4))
            for q in range(4):
                tasks.append((wuv[:, q * 4:(q + 1) * 4, :], wu[s][:, q * 4:(q + 1) * 4, :], "p (k n) -> p k n", 4))
            for q in range(4):
                tasks.append((wdv[:, q, :], wd[s][:, q, :], None, 1))
            out_ = []
            for ci, (src, dst, rr, kk) in enumerate(tasks):
                def task(src=src, dst=dst, rr=rr, kk=kk, s=s, ci=ci):
                    i = cast_rr[0] % NSTG
                    c = cast_rr[0]
                    cast_rr[0] += 1
                    sview = stg[i][:].rearrange(rr, k=kk) if rr else stg[i][:]
                    k.dma(sp, sview, src, ds_stg[i], wr=[B_stg[i]])
                    eng = (dve, act)[c % 2]
                    k.op(eng, "copy" if eng is act else "tensor_copy", out=dst, in_=sview, rd=[B_stg[i]], wr=[B_wch[s][ci]])
                out_.append(task)
            return out_

        blocks = [(e, blk) for e in range(NE) for blk in range(NBLK)]
        NBK = len(blocks)

        def xs_load(i):
            e, blk = blocks[i]
            row0 = e * CAP + blk * 128
            sl = i % NXS_E
            k.dma(pool, xs[sl][:], Xs_s[row0:row0 + 128, :], ds_xs[sl], wr=[B_xs[sl]])

        def stA(i):
            sl = i % NXS_E
            tl = i % 2
            for hh in range(2):
                for i8 in range(8):
                    kc = hh * 8 + i8
                    k.op(pe, "transpose", out=hT_ps[0][:, i8 * 128:(i8 + 1) * 128], in_=xs[sl][:, kc * 128:(kc + 1) * 128],
                         identity=ident_b[:], rd=[B_xs[sl], B_identb], wr=[B_hTps[0]])
                k.op(dve if hh else act, "tensor_copy" if hh else "copy", out=xsT[tl][:, hh * 8:(hh + 1) * 8, :],
                     in_=hT_ps[0][:].rearrange("p (k t) -> p k t", k=8), rd=[B_hTps[0]], wr=[B_xsT[tl]])

        def stB(i):
            e, blk = blocks[i]
            es_ = e % 2
            sl = i % 2
            for kc in range(KC):
                k.op(pe, "matmul", g_ps[sl][:], lhsT=xsT[sl][:, kc, :], rhs=wg[es_][:, kc, :], start=(kc == 0),
                     stop=(kc == KC - 1), rd=[B_xsT[sl]] + B_wch[es_], wr=[B_gps[sl]])
            for kc in range(KC):
                k.op(pe, "matmul", u_ps[sl][:], lhsT=xsT[sl][:, kc, :], rhs=wu[es_][:, kc, :], start=(kc == 0),
                     stop=(kc == KC - 1), rd=[B_xsT[sl]] + B_wch[es_], wr=[B_ups[sl]])
            k.op(act, "activation", out=sg[sl][:], in_=g_ps[sl][:], func=AF.Silu, rd=[B_gps[sl]], wr=[B_sg[sl]])
            k.op(dve, "tensor_tensor", out=hid[sl][:], in0=sg[sl][:], in1=u_ps[sl][:], op=ALU.mult,
                 rd=[B_sg[sl], B_ups[sl]], wr=[B_hid[sl]])

        def stC(i):
            e, blk = blocks[i]
            es_ = e % 2
            sl = i % 2
            row0 = e * CAP + blk * 128
            for c in range(4):
                k.op(pe, "transpose", out=ht_ps[:, c * 128:(c + 1) * 128], in_=hid[sl][:, c * 128:(c + 1) * 128],
                     identity=ident_b[:], rd=[B_hid[sl], B_identb], wr=[B_htps])
            k.op(act, "copy", out=hidT[sl][:], in_=ht_ps[:].rearrange("p (k t) -> p k t", k=4), rd=[B_htps],
                 wr=[B_hidT[sl]])

        def stC2(i):
            e, blk = blocks[i]
            es_ = e % 2
            sl = i % 2
            row0 = e * CAP + blk * 128
            for n in range(4):
                yp = n % 2
                for c in range(4):
                    k.op(pe, "matmul", y_ps[yp][:], lhsT=hidT[sl][:, c, :], rhs=wd[es_][:, c, n * 512:(n + 1) * 512],
                         start=(c == 0), stop=(c == 3), rd=[B_hidT[sl]] + B_wch[es_], wr=[B_yps[yp]])
                k.op(act if n % 2 else dve, "copy" if n % 2 else "tensor_copy", out=y_sb[sl][:, n * 512:(n + 1) * 512],
                     in_=y_ps[yp][:], rd=[B_yps[yp]], wr=[B_ysb[sl]])
            k.dma(act, Ys_s[row0:row0 + 128, :], y_sb[sl][:], ds_y[sl], rd=[B_ysb[sl]])

        for e0 in (0, 1):
            for t_ in expert_chunks(e0):
                t_()
        pending = []
        xs_load(0)
        xs_load(1)
        for i in range(NBK + 2):
            if i + 2 < NBK:
                xs_load(i + 2)
            if i >= 2:
                stC(i - 2)
            if i < NBK:
                stA(i)
            if 1 <= i <= NBK:
                stB(i - 1)
            if i >= 2:
                stC2(i - 2)
                e_done, blk_done = blocks[i - 2]
                if blk_done == NBLK - 1 and e_done + 2 < NE:
                    pending += expert_chunks(e_done + 2)
            for _ in range(3):
                if pending:
                    pending.pop(0)()
        assert not pending
        k.barrier()
        stE.close()

        stG = contextlib.ExitStack()
        es.enter_context(stG)
        GM = sb("GM", [128, D], F32, stG)
        FG = sb("FG", [128, D], F32, stG)
        B_GM, B_FG = Buf(), Buf()
        bcast_load(GM, B_GM, 5, ds0)
        vec_bcast_load(FG[:], B_FG, g_fin, ds0)
        k.op(dve, "tensor_scalar", out=FG[:], in0=FG[:], scalar1=SQD, scalar2=None, op0=ALU.mult, wr=[B_FG])
        g1 = [sb("g1_%d" % i, [128, D], F32, stG) for i in range(2)]
        g2 = [sb("g2_%d" % i, [128, D], F32, stG) for i in range(2)]
        x1t = [sb("x1f%d" % i, [128, D], F32, stG) for i in range(2)]
        B_g1, B_g2, B_x1t = [Buf(), Buf()], [Buf(), Buf()], [Buf(), Buf()]
        ds_g = [k.dsem(), k.dsem()]
        ds_x1 = [k.dsem(), k.dsem()]
        ds_o = [k.dsem(), k.dsem()]
        for j in range(NOWN):
            s = j % 2
            k.dma(sp, x1t[s][:], X1_s[j * 128:(j + 1) * 128, :], ds_x1[s], wr=[B_x1t[s]])
            k.idma(g1[s][:, :], None, Ys_s, bass.IndirectOffsetOnAxis(ap=rt_pg[:, j, 0:1], axis=0), ds_g[s], NSLOT - 1,
                   rd=[B_rtpg], wr=[B_g1[s]])
            k.idma(g2[s][:, :], None, Ys_s, bass.IndirectOffsetOnAxis(ap=rt_pg[:, j, 1:2], axis=0), ds_g[s], NSLOT - 1,
                   rd=[B_rtpg], wr=[B_g2[s]])
            k.op(dve, "tensor_scalar", out=g1[s][:], in0=g1[s][:], scalar1=rt_w[:, j, 0:1], scalar2=None, op0=ALU.mult,
                 rd=[B_rtw], wr=[B_g1[s]])
            k.op(dve, "scalar_tensor_tensor", out=g1[s][:], in0=g2[s][:], scalar=rt_w[:, j, 1:2], in1=g1[s][:],
                 op0=ALU.mult, op1=ALU.add, rd=[B_g2[s], B_rtw], wr=[B_g1[s]])
            k.op(pool, "tensor_tensor", out=g1[s][:], in0=g1[s][:], in1=GM[:], op=ALU.mult, rd=[B_GM], wr=[B_g1[s]])
            k.op(dve, "tensor_tensor", out=x1t[s][:], in0=x1t[s][:], in1=g1[s][:], op=ALU.add, rd=[B_g1[s]], wr=[B_x1t[s]])
            rf, Brf = rms_scale(x1t[s][:], B_x1t[s], D, D * EPS)
            k.op(dve, "scalar_tensor_tensor", out=g2[s][:], in0=x1t[s][:], scalar=rf, in1=FG[:], op0=ALU.mult,
                 op1=ALU.mult, rd=[B_x1t[s], Brf, B_FG], wr=[B_g2[s]])
            k.dma(sp, out[j * 128:(j + 1) * 128, :], g2[s][:], ds_o[s], rd=[B_g2[s]], wr=[])
        if dbg:
            k.dma(sp, d_pg, rt_pg[:].rearrange("p j c -> p (j c)"), ds0, rd=[B_rtpg])
            k.dma(sp, d_w, rt_w[:].rearrange("p j c -> p (j c)"), ds0, rd=[B_rtw])
        k.barrier()
        stG.close()
    return nc


def _consts(r, CAP):
    p = np.arange(128)
    ident = np.eye(128, dtype=np.float32)
    U_incl = (p[:, None] <= p[None, :]).astype(np.float32)
    U_strict = (p[:, None] < p[None, :]).astype(np.float32)
    ones = np.ones((128, 128), np.float32)
    cst = np.concatenate([ident, U_incl, U_strict, ones], axis=1)
    tri = (p[:, None] <= p[None, :]).astype(np.float32)
    fm = [np.ones((128, 128), np.float32) if u < r else (tri if u == r else np.zeros((128, 128), np.float32)) for u in range(4)]
    fmask = np.concatenate(fm, axis=1)
    slopes = (2.0 ** (-8.0 * np.arange(1, 17) / 16)).astype(np.float64)
    kk, qq = p[:, None].astype(np.float64), p[None, :].astype(np.float64)
    A = np.zeros((128, 3, 4, 4, 128), np.float32)
    for g in range(16):
        prev = np.exp(-slopes[g] * (qq + 128 - kk)) * (kk > qq)
        cur = np.exp(-slopes[g] * (qq - kk)) * (kk <= qq)
        A[:, 0, g // 4, g % 4, :] = prev if r != 0 else 0.0
        A[:, 1, g // 4, g % 4, :] = prev
        A[:, 2, g // 4, g % 4, :] = cur
    rsel = np.zeros((128, 4), np.float32)
    rsel[:, r] = 1.0
    ebase = np.tile((np.arange(NE) * CAP).astype(np.float32)[None, :], (128, 1))
    return dict(cst=cst, fmask=fmask, swaA=A.reshape(128, -1), rsel=rsel, ebase=ebase)


_CACHE = {}


def run(inputs, S, CAP, dbg=False):
    f = lambda a: np.ascontiguousarray(np.asarray(a, dtype=np.float32))
    x = f(inputs["x"])[:, :S]
    B = x.shape[0]
    NB = S // 128
    NOWN = NB // 4
    key = (S, CAP, dbg)
    if key not in _CACHE:
        _CACHE[key] = build(S, CAP, dbg)
    nc = _CACHE[key]
    shared = dict(
        w_ada=f(inputs["w_ada"][0]), b_ada=f(inputs["b_ada"][0]).reshape(1, -1), g_mix=f(inputs["norm_mix_g"][0]).reshape(1, -1),
        w_in=f(inputs["w_in"][0]), b_f=f(inputs["b_forget"][0]).reshape(1, -1), sinks=f(inputs["sinks"][0]).reshape(1, -1),
        g_out=np.concatenate([f(inputs["out_norm_swa_g"][0]), f(inputs["out_norm_fox_g"][0])]).reshape(1, -1),
        w_out=f(inputs["w_out"][0]), g_moe=f(inputs["norm_moe_g"][0]).reshape(1, -1),
        w_r=np.ascontiguousarray(np.concatenate([f(inputs["w_group"][0]), f(inputs["w_expert"][0])], axis=1)),
        b_r=np.concatenate([f(inputs["b_group"][0]), f(inputs["b_expert"][0])]).reshape(1, -1),
        w_gate=f(inputs["w_gate"][0]), w_up=f(inputs["w_up"][0]), w_down=f(inputs["w_down"][0]),
        g_fin=f(inputs["final_g"]).reshape(1, -1),
    )
    in_maps = []
    for core in range(8):
        b, r = core // 4, core % 4
        xb = x[b].reshape(NB, 128, D)
        own = [4 * j + r for j in range(NOWN)]
        x_own = np.ascontiguousarray(xb[own].reshape(-1, D))
        xp = np.zeros((NOWN, 128, D), np.float32)
        for j, t in enumerate(own):
            if t > 0:
                xp[j] = xb[t - 1]
        m = dict(shared)
        m.update(x_seq=np.ascontiguousarray(x[b]), x_own=x_own, x_prev=xp.reshape(-1, D),
                 cT=np.ascontiguousarray(f(inputs["c"])[b].reshape(KC, 128).T))
        m.update(_consts(r, CAP))
        in_maps.append(m)
    res = run_bass_kernel_spmd(nc, in_maps, core_ids=list(range(8)))
    if dbg:
        _CACHE["dbg"] = res
    outp = np.zeros((B, NB, 128, D), np.float32)
    for core in range(8):
        b, r = core // 4, core % 4
        o = np.asarray(res.results[core]["out"]).reshape(NOWN, 128, D)
        for j in range(NOWN):
            outp[b, 4 * j + r] = o[j]
    return outp.reshape(B, S, D)


def kernel(**inputs):
    return run(inputs, 8192, 512)
```

```python
import bisect
import contextlib
import numpy as np
import concourse.bass as bass
import concourse.mybir as mybir
from concourse.bass_utils import run_bass_kernel_spmd

F32 = mybir.dt.float32
BF16 = mybir.dt.bfloat16
I32 = mybir.dt.int32
AF = mybir.ActivationFunctionType
ALU = mybir.AluOpType
AX = mybir.AxisListType

D = 2048
KC = 16
EPS = 1e-6
NE = 32
DE = 512
QA0, KA0, VA0, QB0, KB0, VB0, FB0 = 0, 1024, 1280, 1536, 2560, 3584, 4608


class Eng:
    def __init__(self, nc, e, sem, name, is_pe=False):
        self.nc, self.e, self.sem, self.name, self.is_pe = nc, e, sem, name, is_pe
        self.idx = 0
        self.marks = []
        self.mark_idx = []
        self.count = 0
        self.last = None
        self.waited = {}

    def issue(self, ins, is_dma=False):
        self.idx += 1
        self.last = ins
        self.last_is_dma = is_dma
        return ("E", self, self.idx)

    def mark_now(self):
        if self.marks and self.marks[-1][0] == self.idx:
            return
        self.count += 1
        self.last.then_inc(self.sem, 1)
        self.marks.append((self.idx, self.count))
        self.mark_idx.append(self.idx)

    def resolve(self, idx):
        p = bisect.bisect_left(self.mark_idx, idx)
        if p < len(self.marks):
            return self.marks[p][1]
        assert self.idx >= idx
        if self.last_is_dma:
            if self.name == "act":
                self.issue(self.e.memzero(self.mark_tile))
            else:
                self.issue(self.e.memset(self.mark_tile, 0.0))
        self.mark_now()
        return self.count

    def wait(self, toks):
        for t in toks:
            if t is None:
                continue
            if t[0] == "E":
                eng, idx = t[1], t[2]
                if eng is self and self.is_pe:
                    continue
                val = eng.resolve(idx)
                sem = eng.sem
                key = eng.name
            else:
                _, ds, val = t
                sem = ds.sem
                key = ds.name
            if self.waited.get(key, 0) >= val:
                continue
            self.e.wait_ge(sem, val)
            self.waited[key] = val


class DSem:
    def __init__(self, sem, name):
        self.sem, self.name, self.n = sem, name, 0


class Buf:
    __slots__ = ("w", "r")

    def __init__(self):
        self.w = None
        self.r = {}


class K:
    def __init__(self, nc, es):
        self.nc, self.es = nc, es
        self.dsems = []
        mk = lambda e, n, pe=False: Eng(nc, e, es.enter_context(nc.semaphore("sem_" + n)), n, pe)
        self.pe = mk(nc.tensor, "pe", True)
        self.act = mk(nc.scalar, "act")
        self.dve = mk(nc.vector, "dve")
        self.pool = mk(nc.gpsimd, "pool")
        self.sp = mk(nc.sync, "sp")
        self.engs = [self.pe, self.act, self.dve, self.pool, self.sp]
        self.nds = 0

    def dsem(self, es=None):
        es = es or self.es
        self.nds += 1
        name = "ds%d" % self.nds
        d = DSem(es.enter_context(self.nc.semaphore(name)), name)
        self.dsems.append(d)
        return d

    def _deps(self, eng, rd, wr):
        deps = []
        for b in rd:
            if b.w is not None:
                deps.append(b.w)
        for b in wr:
            if b.w is not None:
                deps.append(b.w)
            for k, t in b.r.items():
                if t[0] == "E" and t[1] is eng:
                    continue
                deps.append(t)
        return deps

    def _upd(self, tok, key, rd, wr):
        for b in rd:
            b.r[key] = tok
        for b in wr:
            b.w = tok
            b.r = {}

    def op(self, eng, method, *a, rd=(), wr=(), **kw):
        eng.wait(self._deps(eng, rd, wr))
        ins = getattr(eng.e, method)(*a, **kw)
        tok = eng.issue(ins)
        if (not eng.is_pe) or kw.get("stop") or method == "transpose":
            eng.mark_now()
        self._upd(tok, eng.name, rd, wr)
        return tok

    def dma(self, q, out, in_, ds, rd=(), wr=(), **kw):
        q.wait(self._deps(q, rd, wr))
        ins = q.e.dma_start(out=out, in_=in_, **kw)
        q.issue(ins, True)
        ds.n += 16
        ins.then_inc(ds.sem, 16)
        tok = ("D", ds, ds.n)
        self._upd(tok, ds.name, rd, wr)
        return tok

    def idma(self, out, out_off, in_, in_off, ds, bound, rd=(), wr=()):
        q = self.pool
        q.wait(self._deps(q, rd, wr))
        if not hasattr(self, "_bound_reg"):
            self._bound_reg = {}
        if bound not in self._bound_reg:
            self._bound_reg[bound] = q.e.to_reg(bound)
        ins = q.e.indirect_dma_start(out=out, out_offset=out_off, in_=in_, in_offset=in_off,
                                     bounds_check=self._bound_reg[bound], oob_is_err=False)
        q.issue(ins, True)
        ds.n += 16
        ins.then_inc(ds.sem, 16)
        tok = ("D", ds, ds.n)
        self._upd(tok, ds.name, rd, wr)
        return tok

    def barrier(self):
        toks = []
        for e in self.engs:
            if e.idx > 0 and e is not self.sp:
                toks.append(("E", e, e.idx))
        for d in self.dsems:
            if d.n > 0:
                toks.append(("D", d, d.n))
        for e in self.engs:
            e.wait([t for t in toks if not (t[0] == "E" and t[1] is e)])


def build(S, CAP, dbg=False):
    NB = S // 128
    NOWN = NB // 4
    NG = NB // 4
    TOWN = NOWN * 128
    NSLOT = NE * CAP
    NBLK = CAP // 128
    SQD = float(np.sqrt(D))

    nc = bass.Bass("TRN2", target_bir_lowering=False)

    def inp(name, shape, dt=F32):
        return nc.dram_tensor(name, shape, dt, kind="ExternalInput").ap()

    def scr(name, shape, dt):
        return nc.dram_tensor(name, shape, dt, kind="Internal").ap()

    x_seq = inp("x_seq", [S, D]); x_own = inp("x_own", [TOWN, D]); x_prev = inp("x_prev", [TOWN, D])
    cT = inp("cT", [128, KC]); w_ada = inp("w_ada", [D, 6 * D]); b_ada = inp("b_ada", [1, 6 * D])
    g_mix = inp("g_mix", [1, D]); w_in = inp("w_in", [D, 4624]); b_f = inp("b_f", [1, 16]); sinks = inp("sinks", [1, 16])
    g_out = inp("g_out", [1, D]); w_out = inp("w_out", [D, D]); g_moe = inp("g_moe", [1, D])
    w_r = inp("w_r", [D, 36]); b_r = inp("b_r", [1, 36])
    w_gate = inp("w_gate", [NE, D, DE]); w_up = inp("w_up", [NE, D, DE]); w_down = inp("w_down", [NE, DE, D])
    g_fin = inp("g_fin", [1, D])
    fmask = inp("fmask", [128, 512]); swaA = inp("swaA", [128, 3 * 4 * 512]); rsel = inp("rsel", [128, 4])
    cst = inp("cst", [128, 4 * 128]); ebase = inp("ebase", [128, NE])
    out = nc.dram_tensor("out", [TOWN, D], F32, kind="ExternalOutput").ap()

    mod_s = scr("mod_s", [1, 6 * D], F32)
    KT_s = scr("KT_s", [16 * 64, S], BF16)
    VB_s = scr("VB_s", [S, 1024], BF16)
    QT_s = scr("QT_s", [16 * 64, TOWN], BF16)
    QAUG_s = scr("QAUG_s", [NOWN * 16, 3, 128], BF16)
    X1_s = scr("X1_s", [TOWN, D], F32)
    if dbg:
        Xs_s = nc.dram_tensor("Xs_s", [NSLOT, D], BF16, kind="ExternalOutput").ap()
        Ys_s = nc.dram_tensor("Ys_s", [NSLOT, D], F32, kind="ExternalOutput").ap()
        d_pg = nc.dram_tensor("d_pg", [128, NOWN * 2], I32, kind="ExternalOutput").ap()
        d_w = nc.dram_tensor("d_w", [128, NOWN * 2], F32, kind="ExternalOutput").ap()
    else:
        Xs_s = scr("Xs_s", [NSLOT, D], BF16)
        Ys_s = scr("Ys_s", [NSLOT, D], F32)

    w_in_v = w_in.rearrange("(kc p) n -> p kc n", p=128)

    with contextlib.ExitStack() as es:
        k = K(nc, es)
        pe, act, dve, pool, sp = k.pe, k.act, k.dve, k.pool, k.sp
        mark_tile = es.enter_context(nc.sbuf_tensor("mark_tile", [128, 8], F32))
        pool.mark_tile = mark_tile[:, 0:4]
        act.mark_tile = mark_tile[:, 4:8]
        for e_ in k.engs:
            e_.last_is_dma = False

        def sb(name, shape, dt, st=None):
            return (st or es).enter_context(nc.sbuf_tensor(name, shape, dt))

        def ps(name, shape, dt, st):
            return st.enter_context(nc.psum_tensor(name, shape, dt))

        cst_f = sb("cst_f", [128, 512], F32)
        ident_b = sb("ident_b", [128, 128], BF16)
        ssq = sb("ssq", [128, NB + 6 * NOWN + 8], F32)
        junk = sb("junk", [128, D], BF16)
        B_cst, B_identb, B_ssq, B_junk = Buf(), Buf(), Buf(), Buf()
        ds0 = k.dsem()
        k.dma(sp, cst_f[:], cst, ds0, wr=[B_cst])
        k.op(dve, "tensor_copy", out=ident_b[:], in_=cst_f[:, 0:128], rd=[B_cst], wr=[B_identb])
        k.op(pool, "memset", ssq[:], 0.0, wr=[B_ssq])
        ident_f = cst_f[:, 0:128]
        U_incl = cst_f[:, 128:256]
        U_strict = cst_f[:, 256:384]
        ones_f = cst_f[:, 384:512]
        ssq_ctr = [0]

        def new_ssq():
            i = ssq_ctr[0]
            ssq_ctr[0] += 1
            return ssq[:, i:i + 1]

        rstd_all = sb("rstd_all", [128, NB + 6 * NOWN + 8], F32)

        def rms_scale(src_ap, B_src, width, eps_scaled):
            col = new_ssq()
            i = ssq_ctr[0] - 1
            Bc = Buf()
            Bc.w = B_ssq.w
            k.op(act, "activation", out=junk[:, 0:width], in_=src_ap, func=AF.Square, accum_out=col,
                 rd=[B_src], wr=[Bc, B_junk])
            r = rstd_all[:, i:i + 1]
            Br = Buf()
            k.op(act, "activation", out=r, in_=col, func=AF.Sqrt, bias=float(eps_scaled), scale=1.0, rd=[Bc], wr=[Br])
            k.op(dve, "reciprocal", out=r, in_=r, rd=[Br], wr=[Br])
            return r, Br

        cumN = sb("cumN", [128, NB, 16], F32)
        B_cumN = Buf()
        stA = contextlib.ExitStack()
        es.enter_context(stA)
        stB = stA
        cT_sb = sb("cT_sb", [128, KC], F32, stA)
        sil = sb("sil", [128, KC], BF16, stA)
        B_cT, B_sil = Buf(), Buf()
        k.dma(sp, cT_sb[:], cT, ds0, wr=[B_cT])
        k.op(act, "activation", out=sil[:], in_=cT_sb[:], func=AF.Silu, rd=[B_cT], wr=[B_sil])
        wa = [sb("wa%d" % i, [128, KC, 512], BF16, stA) for i in range(2)]
        B_wa = [Buf(), Buf()]
        ds_wa = [k.dsem(), k.dsem()]
        brow = [sb("brow%d" % i, [1, 512], F32, stA) for i in range(2)]
        B_brow = [Buf(), Buf()]
        ds_br = [k.dsem(), k.dsem()]
        mrow = [sb("mrow%d" % i, [1, 512], F32, stA) for i in range(2)]
        B_mrow = [Buf(), Buf()]
        ds_mr = [k.dsem(), k.dsem()]
        B_mod = [Buf() for _ in range(24)]
        w_ada_v = w_ada.rearrange("(kc p) n -> p kc n", p=128)
        ada_state = {"loaded": 0, "done": 0}

        def ada_load(blk):
            s = blk % 2
            k.dma(pool, wa[s][:], w_ada_v[:, :, blk * 512:(blk + 1) * 512], ds_wa[s], wr=[B_wa[s]])
            k.dma(sp, brow[s][:], b_ada[0:1, blk * 512:(blk + 1) * 512], ds_br[s], wr=[B_brow[s]])

        def ada_block(blk, mod_ps, B_modps):
            s = blk % 2
            for kc in range(KC):
                k.op(pe, "matmul", mod_ps[0:1, :], lhsT=sil[:, kc:kc + 1], rhs=wa[s][:, kc, :], start=(kc == 0),
                     stop=(kc == KC - 1), rd=[B_sil, B_wa[s]], wr=[B_modps])
            if (blk // 4) in (1, 4):
                k.op(dve, "scalar_tensor_tensor", out=mrow[s][:], in0=mod_ps[0:1, :], scalar=1.0, in1=brow[s][:],
                     op0=ALU.add, op1=ALU.add, rd=[B_modps, B_brow[s]], wr=[B_mrow[s]])
            else:
                k.op(dve, "tensor_tensor", out=mrow[s][:], in0=mod_ps[0:1, :], in1=brow[s][:], op=ALU.add,
                     rd=[B_modps, B_brow[s]], wr=[B_mrow[s]])
            k.dma(sp, mod_s[0:1, blk * 512:(blk + 1) * 512], mrow[s][:], ds_mr[s], rd=[B_mrow[s]], wr=[B_mod[blk]])

        def ada_step(mod_ps, B_modps):
            b = ada_state["done"]
            if b >= 24:
                return
            while ada_state["loaded"] < min(24, b + 2):
                ada_load(ada_state["loaded"])
                ada_state["loaded"] += 1
            ada_block(b, mod_ps, B_modps)
            ada_state["done"] += 1

        def bcast_load(dst, B_dst, chunk, ds):
            k.dma(sp, dst[:], mod_s[0:1, chunk * D:(chunk + 1) * D].partition_broadcast(128), ds,
                  rd=[B_mod[chunk * 4 + i] for i in range(4)], wr=[B_dst])

        def vec_bcast_load(dst, B_dst, src, ds, n=D):
            k.dma(sp, dst, src[0:1, 0:n].partition_broadcast(128), ds, wr=[B_dst])

        G1s = sb("G1s", [128, D], F32, stB)
        SHa = sb("SHa", [128, D], F32, stB)
        B_G1s, B_SHa = Buf(), Buf()
        mod_ps = ps("mod_ps", [128, 512], F32, stB)
        B_modps = Buf()
        for _ in range(8):
            ada_step(mod_ps, B_modps)
        B_tmpg = Buf()
        bcast_load(G1s, B_G1s, 1, ds0)
        bcast_load(SHa, B_SHa, 0, ds0)

        Wkb = sb("Wkb", [128, KC, 1024], BF16, stB)
        Wvb = sb("Wvb", [128, KC, 1024], BF16, stB)
        Wf = sb("Wf", [128, KC, 16], BF16, stB)
        B_Wkb, B_Wvb, B_Wf = Buf(), Buf(), Buf()
        ds_w = k.dsem()
        for kc in range(KC):
            k.dma(pool, Wkb[:, kc, :], w_in_v[:, kc, KB0:KB0 + 1024], ds_w, wr=[B_Wkb])
        for kc in range(KC):
            k.dma(pool, Wvb[:, kc, :], w_in_v[:, kc, VB0:VB0 + 1024], ds_w, wr=[B_Wvb])
        k.dma(pool, Wf[:], w_in_v[:, :, FB0:FB0 + 16], ds_w, wr=[B_Wf])
        bF = sb("bF", [128, 16], F32, stB)
        B_bF = Buf()
        vec_bcast_load(bF[:], B_bF, b_f, ds0, 16)

        NXS = 2
        xts = [sb("xt%d" % i, [128, D], F32, stB) for i in range(NXS)]
        B_xt = [Buf() for _ in range(NXS)]
        ds_xt = [k.dsem() for _ in range(NXS)]
        vec_bcast_load(xts[0][:], B_xt[0], g_mix, ds0)
        k.op(dve, "scalar_tensor_tensor", out=G1s[:], in0=G1s[:], scalar=SQD, in1=xts[0][:], op0=ALU.mult, op1=ALU.mult,
             rd=[B_xt[0]], wr=[B_G1s])
        hb = [sb("hb%d" % i, [128, D], BF16, stB) for i in range(2)]
        B_hb = [Buf(), Buf()]
        hTg = [sb("hTg%d" % i, [128, KC, 512], BF16, stB) for i in range(2)]
        B_hTg = [[Buf() for _ in range(4)] for _ in range(2)]
        hT_ps = [ps("hT_ps%d" % i, [128, D], BF16, stB) for i in range(1)]
        B_hTps = [Buf()]
        NMM = 4
        mm_ps = [ps("mm_ps%d" % i, [128, 512], F32, stB) for i in range(NMM)]
        B_mm = [Buf() for _ in range(NMM)]
        f_ps = ps("f_ps", [128, 512], F32, stB)
        B_fps = [Buf(), Buf()]
        kT_sb = [sb("kT_sb%d" % i, [128, 512], BF16, stB) for i in range(2)]
        B_kTsb = [Buf(), Buf()]
        ds_kT = [k.dsem(), k.dsem()]
        v_sb = [sb("v_sb%d" % i, [128, 1024], BF16, stB) for i in range(2)]
        B_vsb = [Buf(), Buf()]
        ds_v = [k.dsem(), k.dsem()]
        zf = sb("zf", [128, NB, 16], F32, stB)
        B_zf = Buf()
        cnt = {"xt": 0, "hb": 0, "mm": 0, "kT": 0, "v": 0, "f": 0, "hTps": 0}

        def make_h(src_rows, Bx_extra_rd=()):
            s = cnt["xt"] % NXS
            cnt["xt"] += 1
            k.dma(sp, xts[s][:], src_rows, ds_xt[s], wr=[B_xt[s]])
            r, Br = rms_scale(xts[s][:], B_xt[s], D, D * EPS)
            k.op(dve, "scalar_tensor_tensor", out=xts[s][:], in0=xts[s][:], scalar=r, in1=G1s[:], op0=ALU.mult,
                 op1=ALU.mult, rd=[Br, B_G1s], wr=[B_xt[s]])
            hs = cnt["hb"] % 2
            cnt["hb"] += 1
            k.op(dve, "tensor_tensor", out=hb[hs][:], in0=xts[s][:], in1=SHa[:], op=ALU.add,
                 rd=[B_xt[s], B_SHa], wr=[B_hb[hs]])
            return hb[hs], B_hb[hs]

        def transpose_to(h_t, B_h, dst_ap3, B_dst, eng=None):
            s = cnt["hTps"] % len(hT_ps)
            cnt["hTps"] += 1
            for kc in range(KC):
                k.op(pe, "transpose", out=hT_ps[s][:, kc * 128:(kc + 1) * 128], in_=h_t[:, kc * 128:(kc + 1) * 128],
                     identity=ident_b[:], rd=[B_h, B_identb], wr=[B_hTps[s]])
            e = eng or act
            if e is act:
                k.op(act, "copy", out=dst_ap3, in_=hT_ps[s][:].rearrange("p (k t) -> p k t", k=KC),
                     rd=[B_hTps[s]], wr=[B_dst])
            else:
                k.op(e, "tensor_copy", out=dst_ap3, in_=hT_ps[s][:].rearrange("p (k t) -> p k t", k=KC),
                     rd=[B_hTps[s]], wr=[B_dst])

        for i in range(4):
            h_t, B_h = make_h(x_seq[i * 128:(i + 1) * 128, :])
            transpose_to(h_t, B_h, hTg[0][:, :, i * 128:(i + 1) * 128], B_hTg[0][i])
        for g in range(NG):
            gs = g % 2
            hq = None
            for c in range(8):
                ms = cnt["mm"] % NMM
                cnt["mm"] += 1
                for kc in range(KC):
                    k.op(pe, "matmul", mm_ps[ms][:], lhsT=Wkb[:, kc, c * 128:(c + 1) * 128], rhs=hTg[gs][:, kc, :],
                         start=(kc == 0), stop=(kc == KC - 1), rd=[B_Wkb] + B_hTg[gs], wr=[B_mm[ms]])
                ks = cnt["kT"] % 2
                cnt["kT"] += 1
                k.op(act if c % 2 else dve, "copy" if c % 2 else "tensor_copy", out=kT_sb[ks][:], in_=mm_ps[ms][:],
                     rd=[B_mm[ms]], wr=[B_kTsb[ks]])
                k.dma(pool, KT_s[c * 128:(c + 1) * 128, g * 512:(g + 1) * 512], kT_sb[ks][:], ds_kT[ks], rd=[B_kTsb[ks]])
                if g + 1 < NG:
                    i_n = c // 2
                    t_n = 4 * (g + 1) + i_n
                    if c % 2 == 0:
                        hq = make_h(x_seq[t_n * 128:(t_n + 1) * 128, :])
                    else:
                        transpose_to(hq[0], hq[1], hTg[1 - gs][:, :, i_n * 128:(i_n + 1) * 128], B_hTg[1 - gs][i_n])
            for i in range(4):
                t = 4 * g + i
                vs = cnt["v"] % 2
                cnt["v"] += 1
                for n in range(2):
                    ms = cnt["mm"] % NMM
                    cnt["mm"] += 1
                    for kc in range(KC):
                        k.op(pe, "matmul", mm_ps[ms][:], lhsT=hTg[gs][:, kc, i * 128:(i + 1) * 128],
                             rhs=Wvb[:, kc, n * 512:(n + 1) * 512], start=(kc == 0), stop=(kc == KC - 1),
                             rd=[B_Wvb, B_hTg[gs][i]], wr=[B_mm[ms]])
                    k.op(act, "copy", out=v_sb[vs][:, n * 512:(n + 1) * 512], in_=mm_ps[ms][:], rd=[B_mm[ms]],
                         wr=[B_vsb[vs]])
                k.dma(pool, VB_s[t * 128:(t + 1) * 128, :], v_sb[vs][:], ds_v[vs], rd=[B_vsb[vs]])
                fs = cnt["f"] % 2
                cnt["f"] += 1
                for kc in range(KC):
                    k.op(pe, "matmul", f_ps[:, fs * 16:(fs + 1) * 16], lhsT=hTg[gs][:, kc, i * 128:(i + 1) * 128],
                         rhs=Wf[:, kc, :], start=(kc == 0), stop=(kc == KC - 1), rd=[B_Wf, B_hTg[gs][i]],
                         wr=[B_fps[fs]])
                k.op(dve, "tensor_tensor", out=zf[:, t, :], in0=f_ps[:, fs * 16:(fs + 1) * 16], in1=bF[:], op=ALU.add,
                     rd=[B_fps[fs], B_bF], wr=[B_zf])
            ada_step(mod_ps, B_modps)
        while ada_state["done"] < 24:
            ada_step(mod_ps, B_modps)

        NC16 = NB * 16
        zf2 = zf[:].rearrange("p t h -> p (t h)")
        k.op(act, "activation", out=zf2, in_=zf2, func=AF.Exp, scale=-1.0, rd=[B_zf], wr=[B_zf])
        k.op(act, "activation", out=zf2, in_=zf2, func=AF.Ln, bias=1.0, scale=1.0, rd=[B_zf], wr=[B_zf])
        cumN2 = cumN[:].rearrange("p t h -> p (t h)")
        class _V:
            def __init__(self, t):
                self.t = t
            def __getitem__(self, idx):
                return self.t[:, 0:NB * 16].rearrange("p (t h) -> p t h", h=16)[idx]
        pfx = [_V(xts[i]) for i in range(2)]
        B_pfx = [B_xt[0], B_xt[1]]
        for c0 in range(0, NC16, 512):
            c1 = min(NC16, c0 + 512)
            w = c1 - c0
            k.op(pe, "matmul", mm_ps[0][:, 0:w], lhsT=U_incl, rhs=zf2[:, c0:c1], start=True, stop=True,
                 rd=[B_zf, B_cst], wr=[B_mm[0]])
            k.op(pe, "matmul", mm_ps[1][:, 0:w], lhsT=ones_f, rhs=zf2[:, c0:c1], start=True, stop=True,
                 rd=[B_zf, B_cst], wr=[B_mm[1]])
            k.op(dve, "tensor_copy", out=cumN2[:, c0:c1], in_=mm_ps[0][:, 0:w], rd=[B_mm[0]], wr=[B_cumN])
            k.op(dve, "tensor_copy", out=pfx[0][:].rearrange("p t h -> p (t h)")[:, c0:c1], in_=mm_ps[1][:, 0:w],
                 rd=[B_mm[1]], wr=[B_pfx[0]])
        cur = 0
        sh = 1
        while sh < NB:
            nx = 1 - cur
            k.op(dve, "tensor_copy", out=pfx[nx][:, 0:sh, :], in_=pfx[cur][:, 0:sh, :], rd=[B_pfx[cur]], wr=[B_pfx[nx]])
            k.op(dve, "tensor_tensor", out=pfx[nx][:, sh:NB, :], in0=pfx[cur][:, sh:NB, :], in1=pfx[cur][:, 0:NB - sh, :],
                 op=ALU.add, rd=[B_pfx[cur]], wr=[B_pfx[nx]])
            cur = nx
            sh *= 2
        k.op(dve, "tensor_tensor", out=cumN[:, 1:NB, :], in0=cumN[:, 1:NB, :], in1=pfx[cur][:, 0:NB - 1, :], op=ALU.add,
             rd=[B_pfx[cur]], wr=[B_cumN])
        rsel_sb = sb("rsel_sb", [128, 4], F32, stB)
        B_rsel = Buf()
        k.dma(sp, rsel_sb[:], rsel, ds0, wr=[B_rsel])
        cq = sb("cq", [128, NOWN, 16], F32, stB)
        B_cq = Buf()
        cumN4 = cumN[:].rearrange("p (j u) h -> p j u h", u=4)
        k.op(dve, "tensor_scalar", out=cq[:], in0=cumN4[:, :, 0, :], scalar1=rsel_sb[:, 0:1], scalar2=-8.0, op0=ALU.mult,
             op1=ALU.mult, rd=[B_cumN, B_rsel], wr=[B_cq])
        cq8 = sb("cq8", [128, NOWN, 16], F32, stB)
        for u in range(1, 4):
            k.op(dve, "tensor_scalar", out=cq8[:], in0=cumN4[:, :, u, :], scalar1=rsel_sb[:, u:u + 1], scalar2=-8.0,
                 op0=ALU.mult, op1=ALU.mult, rd=[B_cumN, B_rsel], wr=[B_tmpg])
            k.op(dve, "tensor_tensor", out=cq[:], in0=cq[:], in1=cq8[:], op=ALU.add, rd=[B_tmpg], wr=[B_cq])
        NQ = NOWN * 16
        cqf = cq[:].rearrange("p j h -> p (j h)")
        c3 = [sb("c3_%d" % i, [128, NQ], BF16, stB) for i in range(3)]
        B_c3 = [Buf() for _ in range(3)]
        for i in range(3):
            k.op(dve, "tensor_copy", out=c3[i][:], in_=cqf, rd=[B_cq], wr=[B_c3[i]])
            if i < 2:
                k.op(dve, "tensor_tensor", out=cqf, in0=cqf, in1=c3[i][:], op=ALU.subtract, rd=[B_c3[i]], wr=[B_cq])
        qa_sb = sb("qa_sb", [128, 3, 128], BF16, stB)
        B_qasb = Buf()
        ds_qa = k.dsem()
        for c0 in range(0, NQ, 128):
            w = min(128, NQ - c0)
            for i in range(3):
                k.op(pe, "transpose", out=hT_ps[0][0:w, i * 128:(i + 1) * 128], in_=c3[i][:, c0:c0 + w],
                     identity=ident_b[:], rd=[B_c3[i], B_identb], wr=[B_hTps[0]])
            k.op(dve, "tensor_copy", out=qa_sb[0:w, :, :], in_=hT_ps[0][0:w, 0:384].rearrange("p (k t) -> p k t", k=3),
                 rd=[B_hTps[0]], wr=[B_qasb])
            k.dma(sp, QAUG_s[c0:c0 + w, :, :], qa_sb[0:w, :, :], ds_qa, rd=[B_qasb])
        k.barrier()
        stB.close()

        stOA = contextlib.ExitStack()
        es.enter_context(stOA)
        o_a = sb("o_a", [128, NOWN, 1024], BF16, stOA)
        B_oa = [Buf() for _ in range(NOWN)]
        esink = sb("esink", [128, 16], F32, stOA)
        B_esink = Buf()
        vec_bcast_load(esink[:], B_esink, sinks, ds0, 16)
        k.op(act, "activation", out=esink[:], in_=esink[:], func=AF.Exp, rd=[B_esink], wr=[B_esink])

        stC = contextlib.ExitStack()
        es.enter_context(stC)
        G1s = sb("G1s_c", [128, D], F32, stC)
        SHa = sb("SHa_c", [128, D], F32, stC)
        B_G1s, B_SHa = Buf(), Buf()
        bcast_load(G1s, B_G1s, 1, ds0)
        bcast_load(SHa, B_SHa, 0, ds0)
        Wq = sb("Wq", [128, KC, 2048], BF16, stC)
        Wkv = sb("Wkv", [128, KC, 512], BF16, stC)
        B_Wq, B_Wkv = Buf(), Buf()
        for kc in range(KC):
            k.dma(pool, Wq[:, kc, 0:1024], w_in_v[:, kc, QA0:QA0 + 1024], ds_w, wr=[B_Wq])
            k.dma(pool, Wq[:, kc, 1024:2048], w_in_v[:, kc, QB0:QB0 + 1024], ds_w, wr=[B_Wq])
        k.dma(pool, Wkv[:], w_in_v[:, :, KA0:KA0 + 512], ds_w, wr=[B_Wkv])
        swaA_sb = sb("swaA_sb", [128, 3 * 4 * 512], BF16, stC)
        B_swaA = Buf()
        for i in range(6):
            k.dma(pool, swaA_sb[:, i * 1024:(i + 1) * 1024], swaA[:, i * 1024:(i + 1) * 1024], ds_w, wr=[B_swaA])
        NXS = 2
        xts = [sb("xtc%d" % i, [128, D], F32, stC) for i in range(NXS)]
        B_xt = [Buf() for _ in range(NXS)]
        vec_bcast_load(xts[0][:], B_xt[0], g_mix, ds0)
        k.op(dve, "scalar_tensor_tensor", out=G1s[:], in0=G1s[:], scalar=SQD, in1=xts[0][:], op0=ALU.mult, op1=ALU.mult,
             rd=[B_xt[0]], wr=[B_G1s])
        hb = [sb("hbc%d" % i, [128, D], BF16, stC) for i in range(2)]
        B_hb = [Buf(), Buf()]
        hT2 = [sb("hT2_%d" % i, [128, KC, 128], BF16, stC) for i in range(2)]
        B_hT2 = [Buf(), Buf()]
        hT_ps = [ps("hT_psc%d" % i, [128, D], BF16, stC) for i in range(1)]
        B_hTps = [Buf()]
        mm_ps = [ps("mm_psc%d" % i, [128, 512], F32, stC) for i in range(2)]
        B_mm = [Buf(), Buf()]
        s_ps = ps("s_psc", [128, 512], F32, stC)
        B_sps = Buf()
        o_ps = ps("o_psc", [128, 512], F32, stC)
        B_ops = Buf()
        tr_ps = ps("tr_psc", [128, 512], F32, stC)
        B_trps = Buf()
        q_tok = sb("q_tok", [128, 2048], BF16, stC)
        B_qtok = Buf()
        kv_tok = [sb("kv_tok%d" % i, [128, 512], BF16, stC) for i in range(2)]
        B_kvtok = [Buf(), Buf()]
        qaT = sb("qaT", [64, 16, 128], BF16, stC)
        qbT = sb("qbT", [64, 16, 128], BF16, stC)
        kaT = sb("kaT", [64, 2, 4, 128], BF16, stC)
        va = sb("va", [128, 2, 4, 65], BF16, stC)
        B_qaT, B_qbT, B_kaT, B_va = Buf(), Buf(), Buf(), Buf()
        k.op(pool, "memset", va[:], 1.0, wr=[B_va])
        pT = [sb("pTc%d" % i, [128, 512], BF16, stC) for i in range(2)]
        B_pT = [Buf(), Buf()]
        oT_sb = sb("oT_sbc", [65, 512], F32, stC)
        B_oTsb = Buf()
        den = sb("denc", [128, 8], F32, stC)
        B_den = Buf()
        ds_qb = k.dsem()
        cnt = {"xt": 0, "hb": 0, "mm": 0, "hTps": 0, "pT": 0}
        QT_v = QT_s.rearrange("(h d) t -> d h t", d=64)

        for j in range(NOWN):
            for which, src in ((0, x_own), (1, x_prev)):
                h_t, B_h = make_h(src[j * 128:(j + 1) * 128, :])
                transpose_to(h_t, B_h, hT2[which][:], B_hT2[which])
            for n in range(4):
                ms = cnt["mm"] % 2
                cnt["mm"] += 1
                for kc in range(KC):
                    k.op(pe, "matmul", mm_ps[ms][:], lhsT=hT2[0][:, kc, :], rhs=Wq[:, kc, n * 512:(n + 1) * 512],
                         start=(kc == 0), stop=(kc == KC - 1), rd=[B_Wq, B_hT2[0]], wr=[B_mm[ms]])
                k.op(dve if n % 2 else act, "tensor_copy" if n % 2 else "copy", out=q_tok[:, n * 512:(n + 1) * 512],
                     in_=mm_ps[ms][:], rd=[B_mm[ms]], wr=[B_qtok])
            for which in range(2):
                ms = cnt["mm"] % 2
                cnt["mm"] += 1
                for kc in range(KC):
                    k.op(pe, "matmul", mm_ps[ms][:], lhsT=hT2[which][:, kc, :], rhs=Wkv[:, kc, :],
                         start=(kc == 0), stop=(kc == KC - 1), rd=[B_Wkv, B_hT2[which]], wr=[B_mm[ms]])
                k.op(dve, "tensor_copy", out=kv_tok[which][:], in_=mm_ps[ms][:], rd=[B_mm[ms]], wr=[B_kvtok[which]])
                kb = 1 - which
                k.op(dve, "tensor_copy", out=va[:, kb, :, 0:64],
                     in_=kv_tok[which][:, 256:512].rearrange("p (h d) -> p h d", h=4), rd=[B_kvtok[which]], wr=[B_va])
            for half, dstT, B_dst in ((0, qaT, B_qaT), (1, qbT, B_qbT)):
                for hh in range(2):
                    s = cnt["hTps"] % len(hT_ps)
                    cnt["hTps"] += 1
                    for i8 in range(8):
                        g_ = hh * 8 + i8
                        c0 = half * 1024 + g_ * 64
                        k.op(pe, "transpose", out=hT_ps[s][0:64, i8 * 128:(i8 + 1) * 128], in_=q_tok[:, c0:c0 + 64],
                             identity=ident_b[:], rd=[B_qtok, B_identb], wr=[B_hTps[s]])
                    k.op(act if hh else dve, "copy" if hh else "tensor_copy", out=dstT[:, hh * 8:(hh + 1) * 8, :],
                         in_=hT_ps[s][0:64, 0:1024].rearrange("p (h t) -> p h t", h=8), rd=[B_hTps[s]], wr=[B_dst])
            k.dma(sp, QT_v[:, :, j * 128:(j + 1) * 128], qbT[:], ds_qb, rd=[B_qbT])
            s = cnt["hTps"] % len(hT_ps)
            cnt["hTps"] += 1
            for which in range(2):
                kb = 1 - which
                for hk in range(4):
                    k.op(pe, "transpose", out=hT_ps[s][0:64, (kb * 4 + hk) * 128:(kb * 4 + hk + 1) * 128],
                         in_=kv_tok[which][:, hk * 64:(hk + 1) * 64], identity=ident_b[:],
                         rd=[B_kvtok[which], B_identb], wr=[B_hTps[s]])
            k.op(dve, "tensor_copy", out=kaT[:].rearrange("p a h t -> p (a h) t"),
                 in_=hT_ps[s][0:64, 0:1024].rearrange("p (h t) -> p h t", h=8), rd=[B_hTps[s]], wr=[B_kaT])
            for hk in range(4):
                for kb in range(2):
                    for i in range(4):
                        k.op(pe, "matmul", s_ps[:, i * 128:(i + 1) * 128], lhsT=kaT[:, kb, hk, :],
                             rhs=qaT[:, hk * 4 + i, :], start=True, stop=True, rd=[B_kaT, B_qaT], wr=[B_sps])
                    p_ = cnt["pT"] % 2
                    cnt["pT"] += 1
                    k.op(act, "activation", out=pT[p_][:], in_=s_ps[:], func=AF.Exp, scale=0.125, rd=[B_sps],
                         wr=[B_pT[p_]])
                    tab = (0 if j == 0 else 1) if kb == 0 else 2
                    a0 = (tab * 4 + hk) * 512
                    k.op(dve, "tensor_tensor", out=pT[p_][:], in0=pT[p_][:], in1=swaA_sb[:, a0:a0 + 512], op=ALU.mult,
                         rd=[B_swaA], wr=[B_pT[p_]])
                    k.op(pe, "matmul", o_ps[0:65, :], lhsT=va[:, kb, hk, :], rhs=pT[p_][:], start=(kb == 0),
                         stop=(kb == 1), rd=[B_va, B_pT[p_]], wr=[B_ops])
                k.op(act, "copy", out=oT_sb[:], in_=o_ps[0:65, :], rd=[B_ops], wr=[B_oTsb])
                for i in range(4):
                    k.op(pe, "transpose", out=tr_ps[:, i * 65:(i + 1) * 65], in_=oT_sb[:, i * 128:(i + 1) * 128],
                         identity=ident_f[0:65, 0:65], rd=[B_oTsb, B_cst], wr=[B_trps])
                tr3 = tr_ps[:, 0:260].rearrange("p (h c) -> p h c", h=4)
                k.op(dve, "tensor_tensor", out=den[:, 0:4], in0=tr3[:, :, 64], in1=esink[:, hk * 4:(hk + 1) * 4],
                     op=ALU.add, rd=[B_trps, B_esink], wr=[B_den])
                k.op(dve, "reciprocal", out=den[:, 4:8], in_=den[:, 0:4], rd=[B_den], wr=[B_den])
                for i in range(4):
                    g_ = hk * 4 + i
                    k.op(dve, "tensor_scalar", out=o_a[:, j, g_ * 64:(g_ + 1) * 64], in0=tr3[:, i, 0:64],
                         scalar1=den[:, 4 + i:5 + i], scalar2=None, op0=ALU.mult, rd=[B_trps, B_den], wr=[B_oa[j]])
        k.barrier()
        stC.close()

        stOB = contextlib.ExitStack()
        es.enter_context(stOB)
        o_b = sb("o_b", [128, NOWN, 1024], BF16, stOB)
        B_ob = [Buf() for _ in range(NOWN)]
        stF = contextlib.ExitStack()
        es.enter_context(stF)
        kTa = [sb("kTa%d" % i, [67, S], BF16, stF) for i in range(2)]
        vau = [sb("vau%d" % i, [128, NB, 65], BF16, stF) for i in range(2)]
        qTa = [sb("qTa%d" % i, [67, TOWN], BF16, stF) for i in range(2)]
        B_kTa, B_vau, B_qTa = [Buf(), Buf()], [Buf(), Buf()], [Buf(), Buf()]
        ds_hd = [k.dsem(), k.dsem()]
        for i in range(2):
            k.op(pool, "memset", kTa[i][64:67, :], 1.0, wr=[B_kTa[i]])
            k.op(pool, "memset", vau[i][:], 1.0, wr=[B_vau[i]])
        fm_sb = sb("fm_sb", [128, 512], BF16, stF)
        B_fm = Buf()
        k.dma(pool, fm_sb[:], fmask, ds_w, wr=[B_fm])
        NPT = 4
        pT = [sb("pTf%d" % i, [128, 1024], BF16, stF) for i in range(NPT)]
        B_pT = [Buf() for _ in range(NPT)]
        NSP = 3
        s_ps = [ps("s_psf%d" % i, [128, 1024], F32, stF) for i in range(NSP)]
        B_sps = [Buf() for _ in range(NSP)]
        o_ps = ps("o_psf", [128, 1024], F32, stF)
        B_ops = Buf()
        tr_ps = [s_ps[0][:, 0:512], s_ps[0][:, 512:1024]]
        B_trps = [B_sps[0], B_sps[0]]
        oT_sb = sb("oT_sbf", [65, 1024], F32, stF)
        B_oTsb = Buf()
        den = sb("denf", [128, 8], F32, stF)
        B_den = Buf()
        VB_v = VB_s.rearrange("(t p) c -> p t c", p=128)
        QAUG_v = QAUG_s.rearrange("(j h) k t -> h k j t", h=16)
        HB = (NOWN + 1) // 2
        halves = [(0, HB), (HB, NOWN)] if NOWN > 1 else [(0, 1)]
        cnt = {"pT": 0, "s": 0, "tr": 0}

        def load_head(h):
            s = h % 2
            k.dma(sp, kTa[s][0:64, :], KT_s[h * 64:(h + 1) * 64, :], ds_hd[s], wr=[B_kTa[s]])
            k.dma(sp, qTa[s][0:64, :], QT_s[h * 64:(h + 1) * 64, :], ds_hd[s], wr=[B_qTa[s]])
            k.dma(sp, qTa[s][64:67, :].rearrange("k (j t) -> k j t", t=128), QAUG_v[h], ds_hd[s], wr=[B_qTa[s]])
            for t0 in range(0, NB, 16):
                t1 = min(NB, t0 + 16)
                k.dma(sp, vau[s][:, t0:t1, 0:64], VB_v[:, t0:t1, h * 64:(h + 1) * 64], ds_hd[s], wr=[B_vau[s]])

        load_head(0)
        for h in range(16):
            hs = h % 2
            if h + 1 < 16:
                load_head(h + 1)
            for (j0, j1) in halves:
                nb = j1 - j0
                def stage1(kt):
                    g = kt // 4
                    u = kt % 4
                    ja = max(g, j0)
                    c_lo = (ja - j0) * 128
                    c_hi = nb * 128
                    ss = cnt["s"] % NSP
                    cnt["s"] += 1
                    for b0 in range(0, 1024, 512):
                        lo, hi = max(c_lo, b0), min(c_hi, b0 + 512)
                        if lo >= hi:
                            continue
                        k.op(pe, "matmul", s_ps[ss][:, lo:hi], lhsT=kTa[hs][:, kt * 128:(kt + 1) * 128],
                             rhs=qTa[hs][:, j0 * 128 + lo:j0 * 128 + hi], start=True, stop=True,
                             rd=[B_kTa[hs], B_qTa[hs]], wr=[B_sps[ss]])
                    p_ = cnt["pT"] % NPT
                    cnt["pT"] += 1
                    k.op(act, "activation", out=pT[p_][:, c_lo:c_hi], in_=s_ps[ss][:, c_lo:c_hi], func=AF.Exp,
                         bias=cumN[:, kt, h:h + 1], scale=0.125, rd=[B_sps[ss], B_cumN], wr=[B_pT[p_]])
                    if g >= j0:
                        k.op(dve, "scalar_tensor_tensor", out=pT[p_][:, c_lo:c_lo + 128], in0=pT[p_][:, c_lo:c_lo + 128],
                             scalar=1e30, in1=fm_sb[:, u * 128:(u + 1) * 128], op0=ALU.min, op1=ALU.mult,
                             rd=[B_fm], wr=[B_pT[p_]])
                    return p_

                def stage2(kt, p_):
                    g = kt // 4
                    u = kt % 4
                    ja = max(g, j0)
                    c_lo = (ja - j0) * 128
                    c_hi = nb * 128
                    started = set()

                    def st_flag(lo_):
                        bank = lo_ // 512
                        if kt == 0 and bank not in started:
                            started.add(bank)
                            return True
                        return False
                    if g >= j0:
                        k.op(pe, "matmul", o_ps[0:65, c_lo:c_lo + 128], lhsT=vau[hs][:, kt, :],
                             rhs=pT[p_][:, c_lo:c_lo + 128], start=st_flag(c_lo), stop=(u == 3), skip_group_check=True,
                             rd=[B_vau[hs], B_pT[p_]], wr=[B_ops])
                        r_lo = c_lo + 128
                    else:
                        r_lo = c_lo
                    for b0 in range(0, 1024, 512):
                        lo, hi = max(r_lo, b0), min(c_hi, b0 + 512)
                        if lo >= hi:
                            continue
                        k.op(pe, "matmul", o_ps[0:65, lo:hi], lhsT=vau[hs][:, kt, :], rhs=pT[p_][:, lo:hi],
                             start=st_flag(lo), stop=False, skip_group_check=True, rd=[B_vau[hs], B_pT[p_]], wr=[B_ops])

                nkt = 4 * j1
                pq = []
                for kt in range(nkt + 2):
                    if kt < nkt:
                        pq.append((kt, stage1(kt)))
                    if kt >= 2:
                        k0, p0 = pq.pop(0)
                        stage2(k0, p0)
                assert not pq
                for b0 in range(0, nb * 128, 512):
                    b1 = min(nb * 128, b0 + 512)
                    k.op(act, "copy", out=oT_sb[:, b0:b1], in_=o_ps[0:65, b0:b1], rd=[B_ops], wr=[B_oTsb])
                for q0 in range(0, nb, 4):
                    q1 = min(nb, q0 + 4)
                    ts_ = cnt["tr"] % 2
                    cnt["tr"] += 1
                    for i in range(q1 - q0):
                        k.op(pe, "transpose", out=tr_ps[ts_][:, i * 65:(i + 1) * 65],
                             in_=oT_sb[:, (q0 + i) * 128:(q0 + i + 1) * 128], identity=ident_f[0:65, 0:65],
                             rd=[B_oTsb, B_cst], wr=[B_trps[ts_]])
                    tr3 = tr_ps[ts_][:, 0:260].rearrange("p (h c) -> p h c", h=4)
                    k.op(dve, "reciprocal", out=den[:, 0:q1 - q0], in_=tr3[:, 0:q1 - q0, 64], rd=[B_trps[ts_]], wr=[B_den])
                    for i in range(q1 - q0):
                        jj = j0 + q0 + i
                        k.op(dve, "tensor_scalar", out=o_b[:, jj, h * 64:(h + 1) * 64], in0=tr3[:, i, 0:64],
                             scalar1=den[:, i:i + 1], scalar2=None, op0=ALU.mult, rd=[B_trps[ts_], B_den], wr=[B_ob[jj]])
        k.barrier()
        stF.close()

        stD = contextlib.ExitStack()
        es.enter_context(stD)
        Wout = sb("Wout", [128, KC, D], BF16, stD)
        B_Wout = Buf()
        w_out_v = w_out.rearrange("(kc p) n -> p kc n", p=128)
        for kc in range(KC):
            for hh in range(2):
                k.dma(pool, Wout[:, kc, hh * 1024:(hh + 1) * 1024], w_out_v[:, kc, hh * 1024:(hh + 1) * 1024], ds_w,
                      wr=[B_Wout])
        gout = sb("gout", [128, D], F32, stD)
        GA = sb("GA", [128, D], F32, stD)
        B_gout, B_GA = Buf(), Buf()
        vec_bcast_load(gout[:], B_gout, g_out, ds0)
        k.op(dve, "tensor_scalar", out=gout[:], in0=gout[:], scalar1=32.0, scalar2=None, op0=ALU.mult, wr=[B_gout])
        bcast_load(GA, B_GA, 2, ds0)
        xts = [sb("xtd%d" % i, [128, D], F32, stD) for i in range(2)]
        B_xt = [Buf(), Buf()]
        ds_xt = [k.dsem(), k.dsem()]
        x1t = [sb("x1t%d" % i, [128, D], F32, stD) for i in range(2)]
        B_x1t = [Buf(), Buf()]
        ds_x1 = [k.dsem(), k.dsem()]
        mixed = sb("mixed", [128, D], BF16, stD)
        B_mixed = Buf()
        mT = sb("mT", [128, KC, 128], BF16, stD)
        B_mT = Buf()
        hT_ps = [ps("hT_psd%d" % i, [128, D], BF16, stD) for i in range(2)]
        B_hTps = [Buf(), Buf()]
        mm_ps = [ps("mm_psd%d" % i, [128, 512], F32, stD) for i in range(2)]
        B_mm = [Buf(), Buf()]
        cnt = {"mm": 0, "hTps": 0}
        for j in range(NOWN):
            s = j % 2
            k.dma(sp, xts[s][:], x_own[j * 128:(j + 1) * 128, :], ds_xt[s], wr=[B_xt[s]])
            ra, Bra = rms_scale(o_a[:, j, :], B_oa[j], 1024, 1024 * EPS)
            rb, Brb = rms_scale(o_b[:, j, :], B_ob[j], 1024, 1024 * EPS)
            k.op(dve, "scalar_tensor_tensor", out=mixed[:, 0:1024], in0=o_a[:, j, :], scalar=ra, in1=gout[:, 0:1024],
                 op0=ALU.mult, op1=ALU.mult, rd=[B_oa[j], Bra, B_gout], wr=[B_mixed])
            k.op(dve, "scalar_tensor_tensor", out=mixed[:, 1024:2048], in0=o_b[:, j, :], scalar=rb, in1=gout[:, 1024:2048],
                 op0=ALU.mult, op1=ALU.mult, rd=[B_ob[j], Brb, B_gout], wr=[B_mixed])
            transpose_to(mixed, B_mixed, mT[:], B_mT)
            for n in range(4):
                ms = cnt["mm"] % 2
                cnt["mm"] += 1
                for kc in range(KC):
                    k.op(pe, "matmul", mm_ps[ms][:], lhsT=mT[:, kc, :], rhs=Wout[:, kc, n * 512:(n + 1) * 512],
                         start=(kc == 0), stop=(kc == KC - 1), rd=[B_Wout, B_mT], wr=[B_mm[ms]])
                sl = slice(n * 512, (n + 1) * 512)
                k.op(dve, "tensor_tensor", out=x1t[s][:, sl], in0=mm_ps[ms][:], in1=GA[:, sl], op=ALU.mult,
                     rd=[B_mm[ms], B_GA], wr=[B_x1t[s]])
                k.op(pool, "tensor_tensor", out=x1t[s][:, sl], in0=x1t[s][:, sl], in1=xts[s][:, sl], op=ALU.add,
                     rd=[B_xt[s]], wr=[B_x1t[s]])
            k.dma(sp, X1_s[j * 128:(j + 1) * 128, :], x1t[s][:], ds_x1[s], rd=[B_x1t[s]])
        k.barrier()
        stD.close()
        stOB.close()
        stOA.close()

        rt_w = sb("rt_w", [128, NOWN, 2], F32)
        rt_pg = sb("rt_pg", [128, NOWN, 2], I32)
        B_rtw, B_rtpg = Buf(), Buf()
        stR = contextlib.ExitStack()
        es.enter_context(stR)
        G2s = sb("G2s", [128, D], F32, stR)
        SHm = sb("SHm", [128, D], F32, stR)
        tmp2 = sb("tmp2", [128, D], F32, stR)
        B_G2s, B_SHm, B_tmp2 = Buf(), Buf(), Buf()
        bcast_load(G2s, B_G2s, 4, ds0)
        bcast_load(SHm, B_SHm, 3, ds0)
        vec_bcast_load(tmp2[:], B_tmp2, g_moe, ds0)
        k.op(dve, "scalar_tensor_tensor", out=G2s[:], in0=G2s[:], scalar=SQD, in1=tmp2[:], op0=ALU.mult, op1=ALU.mult,
             rd=[B_tmp2], wr=[B_G2s])
        Wr = sb("Wr", [128, KC, 36], F32, stR)
        B_Wr = Buf()
        k.dma(sp, Wr[:], w_r.rearrange("(kc p) n -> p kc n", p=128), ds0, wr=[B_Wr])
        bR = sb("bR", [128, 36], F32, stR)
        B_bR = Buf()
        vec_bcast_load(bR[:], B_bR, b_r, ds0, 36)
        eb_sb = sb("eb_sb", [128, NE], F32, stR)
        B_eb = Buf()
        k.dma(sp, eb_sb[:], ebase, ds0, wr=[B_eb])
        cnt_run = sb("cnt_run", [128, NE], F32, stR)
        B_cnt = Buf()
        k.op(dve, "memset", cnt_run[:], 0.0, wr=[B_cnt])
        zt = sb("zt", [128, D], BF16, stR)
        B_zt = Buf()
        k.op(pool, "memset", zt[:], 0.0, wr=[B_zt])
        ds_z = k.dsem()
        B_Xs = Buf()
        for s0 in range(0, NSLOT, 128):
            k.dma(sp, Xs_s[s0:s0 + 128, :], zt[:], ds_z, rd=[B_zt], wr=[B_Xs])
        x1t = [sb("x1r%d" % i, [128, D], F32, stR) for i in range(2)]
        B_x1t = [Buf(), Buf()]
        ds_x1 = [k.dsem(), k.dsem()]
        h2b = [sb("h2b%d" % i, [128, D], BF16, stR) for i in range(2)]
        B_h2b = [Buf(), Buf()]
        ds_sc = [k.dsem(), k.dsem()]
        h2T = sb("h2T", [128, KC, 128], F32, stR)
        B_h2T = Buf()
        trf_ps = [ps("trf_ps%d" % i, [128, 1024], F32, stR) for i in range(2)]
        B_trf = [Buf(), Buf()]
        lg_ps = ps("lg_ps", [128, 512], F32, stR)
        B_lgps = Buf()
        rk_ps = ps("rk_ps", [128, 512], F32, stR)
        B_rkps = Buf()
        R = sb("R", [128, 512], F32, stR)
        B_R = Buf()
        psc = [sb("psc%d" % i, [128, 2], I32, stR) for i in range(2)]
        B_psc = [Buf(), Buf()]
        BIGI = float(4 * NSLOT)

        def rop(method, **kw):
            return k.op(dve, method, rd=[B_R], wr=[B_R], **kw)

        for j in range(NOWN):
            s = j % 2
            k.dma(sp, x1t[s][:], X1_s[j * 128:(j + 1) * 128, :], ds_x1[s], wr=[B_x1t[s]])
            r2, Br2 = rms_scale(x1t[s][:], B_x1t[s], D, D * EPS)
            k.op(dve, "scalar_tensor_tensor", out=x1t[s][:], in0=x1t[s][:], scalar=r2, in1=G2s[:], op0=ALU.mult,
                 op1=ALU.mult, rd=[Br2, B_G2s], wr=[B_x1t[s]])
            k.op(dve, "tensor_tensor", out=x1t[s][:], in0=x1t[s][:], in1=SHm[:], op=ALU.add, rd=[B_SHm], wr=[B_x1t[s]])
            k.op(act, "copy", out=h2b[s][:], in_=x1t[s][:], rd=[B_x1t[s]], wr=[B_h2b[s]])
            for hh in range(2):
                for i8 in range(8):
                    kc = hh * 8 + i8
                    k.op(pe, "transpose", out=trf_ps[hh][:, i8 * 128:(i8 + 1) * 128], in_=x1t[s][:, kc * 128:(kc + 1) * 128],
                         identity=ident_f, rd=[B_x1t[s], B_cst], wr=[B_trf[hh]])
                k.op(act if hh else dve, "copy" if hh else "tensor_copy", out=h2T[:, hh * 8:(hh + 1) * 8, :],
                     in_=trf_ps[hh][:].rearrange("p (k t) -> p k t", k=8), rd=[B_trf[hh]], wr=[B_h2T])
            for kc in range(KC):
                k.op(pe, "matmul", lg_ps[:, 0:36], lhsT=h2T[:, kc, :], rhs=Wr[:, kc, :], start=(kc == 0),
                     stop=(kc == KC - 1), rd=[B_h2T, B_Wr], wr=[B_lgps])
            LG = R[:, 0:36]; GL = R[:, 0:4]; EL = R[:, 4:36]
            GMAX = R[:, 40:41]; NGMAX = R[:, 41:42]; GSUM = R[:, 42:43]; GVAL = R[:, 43:44]
            GOH = R[:, 44:48]; GPEN = R[:, 48:52]; GEXP = R[:, 52:56]
            EM = R[:, 64:96]; T1 = R[:, 96:97]; T2 = R[:, 97:98]; OH1 = R[:, 100:132]; OH2 = R[:, 132:164]
            E2 = R[:, 164:196]; AA = R[:, 196:228]; RK = R[:, 228:260]; RKB = R[:, 260:292]; TMP = R[:, 292:324]
            DD = R[:, 324:325]; ED = R[:, 325:326]; W1 = R[:, 326:327]; W2 = R[:, 327:328]
            P1 = R[:, 328:329]; P2 = R[:, 329:330]; R1 = R[:, 330:331]; R2 = R[:, 331:332]
            V1 = R[:, 332:333]; V2 = R[:, 333:334]; OF1 = R[:, 334:335]; OF2 = R[:, 335:336]
            PS1 = R[:, 336:337]; PS2 = R[:, 337:338]
            k.op(dve, "tensor_tensor", out=LG, in0=lg_ps[:, 0:36], in1=bR[:], op=ALU.add, rd=[B_lgps, B_bR, B_R], wr=[B_R])
            rop("reduce_max", out=GMAX, in_=GL, axis=AX.X)
            rop("tensor_scalar", out=GOH, in0=GL, scalar1=GMAX, scalar2=None, op0=ALU.is_equal)
            rop("tensor_scalar", out=NGMAX, in0=GMAX, scalar1=-1.0, scalar2=None, op0=ALU.mult)
            k.op(act, "activation", out=GEXP, in_=GL, func=AF.Exp, bias=NGMAX, scale=1.0, rd=[B_R], wr=[B_R])
            rop("reduce_sum", out=GSUM, in_=GEXP, axis=AX.X)
            rop("reciprocal", out=GVAL, in_=GSUM)
            rop("tensor_scalar", out=GPEN, in0=GOH, scalar1=-1.0, scalar2=1e30, op0=ALU.add, op1=ALU.mult)
            for gi in range(4):
                rop("tensor_scalar", out=EM[:, gi * 8:(gi + 1) * 8], in0=EL[:, gi * 8:(gi + 1) * 8],
                    scalar1=GPEN[:, gi:gi + 1], scalar2=None, op0=ALU.add)
            rop("reduce_max", out=T1, in_=EM, axis=AX.X)
            rop("tensor_scalar", out=OH1, in0=EM, scalar1=T1, scalar2=None, op0=ALU.is_equal)
            rop("scalar_tensor_tensor", out=E2, in0=OH1, scalar=-1e30, in1=EM, op0=ALU.mult, op1=ALU.add)
            rop("reduce_max", out=T2, in_=E2, axis=AX.X)
            rop("tensor_scalar", out=OH2, in0=E2, scalar1=T2, scalar2=None, op0=ALU.is_equal)
            rop("tensor_tensor", out=DD, in0=T2, in1=T1, op=ALU.subtract)
            k.op(act, "activation", out=ED, in_=DD, func=AF.Exp, rd=[B_R], wr=[B_R])
            rop("tensor_scalar", out=ED, in0=ED, scalar1=1.0, scalar2=None, op0=ALU.add)
            rop("reciprocal", out=ED, in_=ED)
            rop("tensor_tensor", out=W1, in0=GVAL, in1=ED, op=ALU.mult)
            rop("tensor_tensor", out=W2, in0=GVAL, in1=W1, op=ALU.subtract)
            rop("tensor_tensor", out=AA, in0=OH1, in1=OH2, op=ALU.add)
            k.op(pe, "matmul", rk_ps[:, 0:32], lhsT=U_strict, rhs=AA, start=True, stop=True, rd=[B_R, B_cst], wr=[B_rkps])
            k.op(pe, "matmul", rk_ps[:, 32:64], lhsT=ones_f, rhs=AA, start=True, stop=True, rd=[B_R, B_cst], wr=[B_rkps])
            k.op(dve, "tensor_tensor", out=RK, in0=rk_ps[:, 0:32], in1=cnt_run[:], op=ALU.add, rd=[B_rkps, B_cnt, B_R],
                 wr=[B_R])
            k.op(dve, "tensor_tensor", out=cnt_run[:], in0=rk_ps[:, 32:64], in1=cnt_run[:], op=ALU.add, rd=[B_rkps, B_cnt],
                 wr=[B_cnt])
            k.op(dve, "tensor_tensor", out=RKB, in0=RK, in1=eb_sb[:], op=ALU.add, rd=[B_R, B_eb], wr=[B_R])
            for (OH, P_, R_, V_, OF_, PS_, W_, col) in ((OH1, P1, R1, V1, OF1, PS1, W1, 0), (OH2, P2, R2, V2, OF2, PS2, W2, 1)):
                rop("tensor_tensor", out=TMP, in0=OH, in1=RKB, op=ALU.mult)
                rop("reduce_sum", out=P_, in_=TMP, axis=AX.X)
                rop("tensor_tensor", out=TMP, in0=OH, in1=RK, op=ALU.mult)
                rop("reduce_sum", out=R_, in_=TMP, axis=AX.X)
                rop("tensor_scalar", out=V_, in0=R_, scalar1=float(CAP), scalar2=None, op0=ALU.is_lt)
                rop("tensor_scalar", out=OF_, in0=V_, scalar1=-BIGI, scalar2=BIGI, op0=ALU.mult, op1=ALU.add)
                rop("tensor_tensor", out=PS_, in0=P_, in1=OF_, op=ALU.add)
                k.op(dve, "tensor_copy", out=psc[s][:, col:col + 1], in_=PS_, rd=[B_R], wr=[B_psc[s]])
                rop("tensor_tensor", out=P_, in0=P_, in1=V_, op=ALU.mult)
                k.op(dve, "tensor_copy", out=rt_pg[:, j, col:col + 1], in_=P_, rd=[B_R], wr=[B_rtpg])
                k.op(dve, "tensor_tensor", out=rt_w[:, j, col:col + 1], in0=W_, in1=V_, op=ALU.mult, rd=[B_R], wr=[B_rtw])
            for col in range(2):
                k.idma(Xs_s, bass.IndirectOffsetOnAxis(ap=psc[s][:, col:col + 1], axis=0), h2b[s][:, :], None, ds_sc[s],
                       NSLOT - 1, rd=[B_h2b[s], B_psc[s], B_Xs])
        k.barrier()
        stR.close()

        stE = contextlib.ExitStack()
        es.enter_context(stE)
        wg = [sb("wg%d" % i, [128, KC, DE], BF16, stE) for i in range(2)]
        wu = [sb("wu%d" % i, [128, KC, DE], BF16, stE) for i in range(2)]
        wd = [sb("wd%d" % i, [128, 4, D], BF16, stE) for i in range(2)]
        B_we = [Buf(), Buf()]
        ds_we = [k.dsem(), k.dsem()]
        NXS_E = 4
        xs = [sb("xs%d" % i, [128, D], BF16, stE) for i in range(NXS_E)]
        B_xs = [Buf() for _ in range(NXS_E)]
        ds_xs = [k.dsem() for _ in range(NXS_E)]
        xsT = [sb("xsT%d" % i, [128, KC, 128], BF16, stE) for i in range(2)]
        B_xsT = [Buf(), Buf()]
        sg = [sb("sg%d" % i, [128, DE], BF16, stE) for i in range(2)]
        hid = [sb("hid%d" % i, [128, DE], BF16, stE) for i in range(2)]
        hidT = [sb("hidT%d" % i, [128, 4, 128], BF16, stE) for i in range(2)]
        B_sg, B_hid, B_hidT = [Buf(), Buf()], [Buf(), Buf()], [Buf(), Buf()]
        y_sb = [sb("y_sb%d" % i, [128, D], F32, stE) for i in range(2)]
        B_ysb = [Buf(), Buf()]
        ds_y = [k.dsem(), k.dsem()]
        hT_ps = [ps("hT_pse%d" % i, [128, 1024], BF16, stE) for i in range(1)]
        B_hTps = [Buf()]
        g_ps = [ps("g_ps%d" % i, [128, 512], F32, stE) for i in range(2)]
        u_ps = [ps("u_ps%d" % i, [128, 512], F32, stE) for i in range(2)]
        B_gps, B_ups = [Buf(), Buf()], [Buf(), Buf()]
        ht_ps = ps("ht_ps", [128, 512], BF16, stE)
        B_htps = Buf()
        y_ps = [ps("y_ps%d" % i, [128, 512], F32, stE) for i in range(2)]
        B_yps = [Buf(), Buf()]
        cnt = {"hTps": 0, "y": 0}

        NSTG = 6
        stg = [sb("stg%d" % i, [128, 2048], F32, stE) for i in range(NSTG)]
        B_stg = [Buf() for _ in range(NSTG)]
        ds_stg = [k.dsem() for _ in range(NSTG)]
        cast_rr = [0]
        B_wch = [[Buf() for _ in range(12)] for _ in range(2)]

        def expert_chunks(e):
            s = e % 2
            wgv = w_gate[e].rearrange("(p kc) n -> p kc n", kc=KC)
            wuv = w_up[e].rearrange("(p kc) n -> p kc n", kc=KC)
            wdv = w_down[e].rearrange("(kc p) n -> p kc n", p=128)
            tasks = []
            for q in range(4):
                tasks.append((wgv[:, q * 4:(q + 1) * 4, :], wg[s][:, q * 4:(q + 1) * 4, :], "p (k n) -> p k n", 4))
            for q in range(4):
                tasks.append((wuv[:, q * 4:(q + 1) * 4, :], wu[s][:, q * 4:(q + 1) * 4, :], "p (k n) -> p k n", 4))
            for q in range(4):
                tasks.append((wdv[:, q, :], wd[s][:, q, :], None, 1))
            out_ = []
            for ci, (src, dst, rr, kk) in enumerate(tasks):
                def task(src=src, dst=dst, rr=rr, kk=kk, s=s, ci=ci):
                    i = cast_rr[0] % NSTG
                    c = cast_rr[0]
                    cast_rr[0] += 1
                    sview = stg[i][:].rearrange(rr, k=kk) if rr else stg[i][:]
                    k.dma(sp, sview, src, ds_stg[i], wr=[B_stg[i]])
                    eng = (dve, act)[c % 2]
                    k.op(eng, "copy" if eng is act else "tensor_copy", out=dst, in_=sview, rd=[B_stg[i]], wr=[B_wch[s][ci]])
                out_.append(task)
            return out_

        blocks = [(e, blk) for e in range(NE) for blk in range(NBLK)]
        NBK = len(blocks)

        def xs_load(i):
            e, blk = blocks[i]
            row0 = e * CAP + blk * 128
            sl = i % NXS_E
            k.dma(pool, xs[sl][:], Xs_s[row0:row0 + 128, :], ds_xs[sl], wr=[B_xs[sl]])

        def stA(i):
            sl = i % NXS_E
            tl = i % 2
            for hh in range(2):
                for i8 in range(8):
                    kc = hh * 8 + i8
                    k.op(pe, "transpose", out=hT_ps[0][:, i8 * 128:(i8 + 1) * 128], in_=xs[sl][:, kc:D:KC],
                         identity=ident_b[:], rd=[B_xs[sl], B_identb], wr=[B_hTps[0]])
                k.op(dve if hh else act, "tensor_copy" if hh else "copy", out=xsT[tl][:, hh * 8:(hh + 1) * 8, :],
                     in_=hT_ps[0][:].rearrange("p (k t) -> p k t", k=8), rd=[B_hTps[0]], wr=[B_xsT[tl]])

        def stB(i):
            e, blk = blocks[i]
            es_ = e % 2
            sl = i % 2
            for kc in range(KC):
                k.op(pe, "matmul", g_ps[sl][:], lhsT=xsT[sl][:, kc, :], rhs=wg[es_][:, kc, :], start=(kc == 0),
                     stop=(kc == KC - 1), rd=[B_xsT[sl]] + B_wch[es_], wr=[B_gps[sl]])
            for kc in range(KC):
                k.op(pe, "matmul", u_ps[sl][:], lhsT=xsT[sl][:, kc, :], rhs=wu[es_][:, kc, :], start=(kc == 0),
                     stop=(kc == KC - 1), rd=[B_xsT[sl]] + B_wch[es_], wr=[B_ups[sl]])
            k.op(act, "activation", out=sg[sl][:], in_=g_ps[sl][:], func=AF.Silu, rd=[B_gps[sl]], wr=[B_sg[sl]])
            k.op(dve, "tensor_tensor", out=hid[sl][:], in0=sg[sl][:], in1=u_ps[sl][:], op=ALU.mult,
                 rd=[B_sg[sl], B_ups[sl]], wr=[B_hid[sl]])

        def stC(i):
            e, blk = blocks[i]
            es_ = e % 2
            sl = i % 2
            row0 = e * CAP + blk * 128
            for c in range(4):
                k.op(pe, "transpose", out=ht_ps[:, c * 128:(c + 1) * 128], in_=hid[sl][:, c * 128:(c + 1) * 128],
                     identity=ident_b[:], rd=[B_hid[sl], B_identb], wr=[B_htps])
            k.op(act, "copy", out=hidT[sl][:], in_=ht_ps[:].rearrange("p (k t) -> p k t", k=4), rd=[B_htps],
                 wr=[B_hidT[sl]])

        def stC2(i):
            e, blk = blocks[i]
            es_ = e % 2
            sl = i % 2
            row0 = e * CAP + blk * 128
            for n in range(4):
                yp = n % 2
                for c in range(4):
                    k.op(pe, "matmul", y_ps[yp][:], lhsT=hidT[sl][:, c, :], rhs=wd[es_][:, c, n * 512:(n + 1) * 512],
                         start=(c == 0), stop=(c == 3), rd=[B_hidT[sl]] + B_wch[es_], wr=[B_yps[yp]])
                k.op(act if n % 2 else dve, "copy" if n % 2 else "tensor_copy", out=y_sb[sl][:, n * 512:(n + 1) * 512],
                     in_=y_ps[yp][:], rd=[B_yps[yp]], wr=[B_ysb[sl]])
            k.dma(act, Ys_s[row0:row0 + 128, :], y_sb[sl][:], ds_y[sl], rd=[B_ysb[sl]])

        for e0 in (0, 1):
            for t_ in expert_chunks(e0):
                t_()
        pending = []
        xs_load(0)
        xs_load(1)
        for i in range(NBK + 2):
            if i + 2 < NBK:
                xs_load(i + 2)
            if i >= 2:
                stC(i - 2)
            if i < NBK:
                stA(i)
            if 1 <= i <= NBK:
                stB(i - 1)
            if i >= 2:
                stC2(i - 2)
                e_done, blk_done = blocks[i - 2]
                if blk_done == NBLK - 1 and e_done + 2 < NE:
                    pending += expert_chunks(e_done + 2)
            for _ in range(3):
                if pending:
                    pending.pop(0)()
        assert not pending
        k.barrier()
        stE.close()

        stG = contextlib.ExitStack()
        es.enter_context(stG)
        GM = sb("GM", [128, D], F32, stG)
        FG = sb("FG", [128, D], F32, stG)
        B_GM, B_FG = Buf(), Buf()
        bcast_load(GM, B_GM, 5, ds0)
        vec_bcast_load(FG[:], B_FG, g_fin, ds0)
        k.op(dve, "tensor_scalar", out=FG[:], in0=FG[:], scalar1=SQD, scalar2=None, op0=ALU.mult, wr=[B_FG])
        g1 = [sb("g1_%d" % i, [128, D], F32, stG) for i in range(2)]
        g2 = [sb("g2_%d" % i, [128, D], F32, stG) for i in range(2)]
        x1t = [sb("x1f%d" % i, [128, D], F32, stG) for i in range(2)]
        B_g1, B_g2, B_x1t = [Buf(), Buf()], [Buf(), Buf()], [Buf(), Buf()]
        ds_g = [k.dsem(), k.dsem()]
        ds_x1 = [k.dsem(), k.dsem()]
        ds_o = [k.dsem(), k.dsem()]
        for j in range(NOWN):
            s = j % 2
            k.dma(sp, x1t[s][:], X1_s[j * 128:(j + 1) * 128, :], ds_x1[s], wr=[B_x1t[s]])
            k.idma(g1[s][:, :], None, Ys_s, bass.IndirectOffsetOnAxis(ap=rt_pg[:, j, 0:1], axis=0), ds_g[s], NSLOT - 1,
                   rd=[B_rtpg], wr=[B_g1[s]])
            k.idma(g2[s][:, :], None, Ys_s, bass.IndirectOffsetOnAxis(ap=rt_pg[:, j, 1:2], axis=0), ds_g[s], NSLOT - 1,
                   rd=[B_rtpg], wr=[B_g2[s]])
            k.op(dve, "tensor_scalar", out=g1[s][:], in0=g1[s][:], scalar1=rt_w[:, j, 0:1], scalar2=None, op0=ALU.mult,
                 rd=[B_rtw], wr=[B_g1[s]])
            k.op(dve, "scalar_tensor_tensor", out=g1[s][:], in0=g2[s][:], scalar=rt_w[:, j, 1:2], in1=g1[s][:],
                 op0=ALU.mult, op1=ALU.add, rd=[B_g2[s], B_rtw], wr=[B_g1[s]])
            k.op(pool, "tensor_tensor", out=g1[s][:], in0=g1[s][:], in1=GM[:], op=ALU.mult, rd=[B_GM], wr=[B_g1[s]])
            k.op(dve, "tensor_tensor", out=x1t[s][:], in0=x1t[s][:], in1=g1[s][:], op=ALU.add, rd=[B_g1[s]], wr=[B_x1t[s]])
            rf, Brf = rms_scale(x1t[s][:], B_x1t[s], D, D * EPS)
            k.op(dve, "scalar_tensor_tensor", out=g2[s][:], in0=x1t[s][:], scalar=rf, in1=FG[:], op0=ALU.mult,
                 op1=ALU.mult, rd=[B_x1t[s], Brf, B_FG], wr=[B_g2[s]])
            k.dma(sp, out[j * 128:(j + 1) * 128, :], g2[s][:], ds_o[s], rd=[B_g2[s]], wr=[])
        if dbg:
            k.dma(sp, d_pg, rt_pg[:].rearrange("p j c -> p (j c)"), ds0, rd=[B_rtpg])
            k.dma(sp, d_w, rt_w[:].rearrange("p j c -> p (j c)"), ds0, rd=[B_rtw])
        k.barrier()
        stG.close()
    return nc


def _consts(r, CAP):
    p = np.arange(128)
    ident = np.eye(128, dtype=np.float32)
    U_incl = (p[:, None] <= p[None, :]).astype(np.float32)
    U_strict = (p[:, None] < p[None, :]).astype(np.float32)
    ones = np.ones((128, 128), np.float32)
    cst = np.concatenate([ident, U_incl, U_strict, ones], axis=1)
    tri = (p[:, None] <= p[None, :]).astype(np.float32)
    fm = [np.ones((128, 128), np.float32) if u < r else (tri if u == r else np.zeros((128, 128), np.float32)) for u in range(4)]
    fmask = np.concatenate(fm, axis=1)
    slopes = (2.0 ** (-8.0 * np.arange(1, 17) / 16)).astype(np.float64)
    kk, qq = p[:, None].astype(np.float64), p[None, :].astype(np.float64)
    A = np.zeros((128, 3, 4, 4, 128), np.float32)
    for g in range(16):
        prev = np.exp(-slopes[g] * (qq + 128 - kk)) * (kk > qq)
        cur = np.exp(-slopes[g] * (qq - kk)) * (kk <= qq)
        A[:, 0, g // 4, g % 4, :] = prev if r != 0 else 0.0
        A[:, 1, g // 4, g % 4, :] = prev
        A[:, 2, g // 4, g % 4, :] = cur
    rsel = np.zeros((128, 4), np.float32)
    rsel[:, r] = 1.0
    ebase = np.tile((np.arange(NE) * CAP).astype(np.float32)[None, :], (128, 1))
    return dict(cst=cst, fmask=fmask, swaA=A.reshape(128, -1), rsel=rsel, ebase=ebase)


_CACHE = {}


def run(inputs, S, CAP, dbg=False):
    f = lambda a: np.ascontiguousarray(np.asarray(a, dtype=np.float32))
    x = f(inputs["x"])[:, :S]
    B = x.shape[0]
    NB = S // 128
    NOWN = NB // 4
    key = (S, CAP, dbg)
    if key not in _CACHE:
        _CACHE[key] = build(S, CAP, dbg)
    nc = _CACHE[key]
    shared = dict(
        w_ada=f(inputs["w_ada"][0]), b_ada=f(inputs["b_ada"][0]).reshape(1, -1), g_mix=f(inputs["norm_mix_g"][0]).reshape(1, -1),
        w_in=f(inputs["w_in"][0]), b_f=f(inputs["b_forget"][0]).reshape(1, -1), sinks=f(inputs["sinks"][0]).reshape(1, -1),
        g_out=np.concatenate([f(inputs["out_norm_swa_g"][0]), f(inputs["out_norm_fox_g"][0])]).reshape(1, -1),
        w_out=f(inputs["w_out"][0]), g_moe=f(inputs["norm_moe_g"][0]).reshape(1, -1),
        w_r=np.ascontiguousarray(np.concatenate([f(inputs["w_group"][0]), f(inputs["w_expert"][0])], axis=1)),
        b_r=np.concatenate([f(inputs["b_group"][0]), f(inputs["b_expert"][0])]).reshape(1, -1),
        w_gate=f(inputs["w_gate"][0]), w_up=f(inputs["w_up"][0]), w_down=f(inputs["w_down"][0]),
        g_fin=f(inputs["final_g"]).reshape(1, -1),
    )
    in_maps = []
    for core in range(8):
        b, r = core // 4, core % 4
        xb = x[b].reshape(NB, 128, D)
        own = [4 * j + r for j in range(NOWN)]
        x_own = np.ascontiguousarray(xb[own].reshape(-1, D))
        xp = np.zeros((NOWN, 128, D), np.float32)
        for j, t in enumerate(own):
            if t > 0:
                xp[j] = xb[t - 1]
        m = dict(shared)
        m.update(x_seq=np.ascontiguousarray(x[b]), x_own=x_own, x_prev=xp.reshape(-1, D),
                 cT=np.ascontiguousarray(f(inputs["c"])[b].reshape(KC, 128).T))
        m.update(_consts(r, CAP))
        in_maps.append(m)
    res = run_bass_kernel_spmd(nc, in_maps, core_ids=list(range(8)))
    if dbg:
        _CACHE["dbg"] = res
    outp = np.zeros((B, NB, 128, D), np.float32)
    for core in range(8):
        b, r = core // 4, core % 4
        o = np.asarray(res.results[core]["out"]).reshape(NOWN, 128, D)
        for j in range(NOWN):
            outp[b, 4 * j + r] = o[j]
    return outp.reshape(B, S, D)


def kernel(**inputs):
    return run(inputs, 8192, 512)
```

```python
import bisect
import contextlib
import numpy as np
import concourse.bass as bass
import concourse.mybir as mybir
from concourse.bass_utils import run_bass_kernel_spmd

F32 = mybir.dt.float32
BF16 = mybir.dt.bfloat16
I32 = mybir.dt.int32
AF = mybir.ActivationFunctionType
ALU = mybir.AluOpType
AX = mybir.AxisListType

D = 2048
KC = 16
EPS = 1e-6
NE = 32
DE = 512
QA0, KA0, VA0, QB0, KB0, VB0, FB0 = 0, 1024, 1280, 1536, 2560, 3584, 4608


class Eng:
    def __init__(self, nc, e, sem, name, is_pe=False):
        self.nc, self.e, self.sem, self.name, self.is_pe = nc, e, sem, name, is_pe
        self.idx = 0
        self.marks = []
        self.mark_idx = []
        self.count = 0
        self.last = None
        self.waited = {}

    def issue(self, ins, is_dma=False):
        self.idx += 1
        self.last = ins
        self.last_is_dma = is_dma
        return ("E", self, self.idx)

    def mark_now(self):
        if self.marks and self.marks[-1][0] == self.idx:
            return
        self.count += 1
        self.last.then_inc(self.sem, 1)
        self.marks.append((self.idx, self.count))
        self.mark_idx.append(self.idx)

    def resolve(self, idx):
        p = bisect.bisect_left(self.mark_idx, idx)
        if p < len(self.marks):
            return self.marks[p][1]
        assert self.idx >= idx
        if self.last_is_dma:
            if self.name == "act":
                self.issue(self.e.memzero(self.mark_tile))
            else:
                self.issue(self.e.memset(self.mark_tile, 0.0))
        self.mark_now()
        return self.count

    def wait(self, toks):
        for t in toks:
            if t is None:
                continue
            if t[0] == "E":
                eng, idx = t[1], t[2]
                if eng is self and self.is_pe:
                    continue
                val = eng.resolve(idx)
                sem = eng.sem
                key = eng.name
            else:
                _, ds, val = t
                sem = ds.sem
                key = ds.name
            if self.waited.get(key, 0) >= val:
                continue
            self.e.wait_ge(sem, val)
            self.waited[key] = val


class DSem:
    def __init__(self, sem, name):
        self.sem, self.name, self.n = sem, name, 0


class Buf:
    __slots__ = ("w", "r")

    def __init__(self):
        self.w = None
        self.r = {}


class K:
    def __init__(self, nc, es):
        self.nc, self.es = nc, es
        self.dsems = []
        mk = lambda e, n, pe=False: Eng(nc, e, es.enter_context(nc.semaphore("sem_" + n)), n, pe)
        self.pe = mk(nc.tensor, "pe", True)
        self.act = mk(nc.scalar, "act")
        self.dve = mk(nc.vector, "dve")
        self.pool = mk(nc.gpsimd, "pool")
        self.sp = mk(nc.sync, "sp")
        self.engs = [self.pe, self.act, self.dve, self.pool, self.sp]
        self.nds = 0

    def dsem(self, es=None):
        es = es or self.es
        self.nds += 1
        name = "ds%d" % self.nds
        d = DSem(es.enter_context(self.nc.semaphore(name)), name)
        self.dsems.append(d)
        return d

    def _deps(self, eng, rd, wr):
        deps = []
        for b in rd:
            if b.w is not None:
                deps.append(b.w)
        for b in wr:
            if b.w is not None:
                deps.append(b.w)
            for k, t in b.r.items():
                if t[0] == "E" and t[1] is eng:
                    continue
                deps.append(t)
        return deps

    def _upd(self, tok, key, rd, wr):
        for b in rd:
            b.r[key] = tok
        for b in wr:
            b.w = tok
            b.r = {}

    def op(self, eng, method, *a, rd=(), wr=(), **kw):
        eng.wait(self._deps(eng, rd, wr))
        ins = getattr(eng.e, method)(*a, **kw)
        tok = eng.issue(ins)
        if (not eng.is_pe) or kw.get("stop") or method == "transpose":
            eng.mark_now()
        self._upd(tok, eng.name, rd, wr)
        return tok

    def dma(self, q, out, in_, ds, rd=(), wr=(), **kw):
        q.wait(self._deps(q, rd, wr))
        ins = q.e.dma_start(out=out, in_=in_, **kw)
        q.issue(ins, True)
        ds.n += 16
        ins.then_inc(ds.sem, 16)
        tok = ("D", ds, ds.n)
        self._upd(tok, ds.name, rd, wr)
        return tok

    def idma(self, out, out_off, in_, in_off, ds, bound, rd=(), wr=()):
        q = self.pool
        q.wait(self._deps(q, rd, wr))
        if not hasattr(self, "_bound_reg"):
            self._bound_reg = {}
        if bound not in self._bound_reg:
            self._bound_reg[bound] = q.e.to_reg(bound)
        ins = q.e.indirect_dma_start(out=out, out_offset=out_off, in_=in_, in_offset=in_off,
                                     bounds_check=self._bound_reg[bound], oob_is_err=False)
        q.issue(ins, True)
        ds.n += 16
        ins.then_inc(ds.sem, 16)
        tok = ("D", ds, ds.n)
        self._upd(tok, ds.name, rd, wr)
        return tok

    def barrier(self):
        toks = []
        for e in self.engs:
            if e.idx > 0 and e is not self.sp:
                toks.append(("E", e, e.idx))
        for d in self.dsems:
            if d.n > 0:
                toks.append(("D", d, d.n))
        for e in self.engs:
            e.wait([t for t in toks if not (t[0] == "E" and t[1] is e)])


def build(S, CAP, dbg=False):
    NB = S // 128
    NOWN = NB // 4
    NG = NB // 4
    TOWN = NOWN * 128
    NSLOT = NE * CAP
    NBLK = CAP // 128
    SQD = float(np.sqrt(D))

    nc = bass.Bass("TRN2", target_bir_lowering=False)

    def inp(name, shape, dt=F32):
        return nc.dram_tensor(name, shape, dt, kind="ExternalInput").ap()

    def scr(name, shape, dt):
        return nc.dram_tensor(name, shape, dt, kind="Internal").ap()

    x_seq = inp("x_seq", [S, D]); x_own = inp("x_own", [TOWN, D]); x_prev = inp("x_prev", [TOWN, D])
    cT = inp("cT", [128, KC]); w_ada = inp("w_ada", [D, 6 * D]); b_ada = inp("b_ada", [1, 6 * D])
    g_mix = inp("g_mix", [1, D]); w_in = inp("w_in", [D, 4624]); b_f = inp("b_f", [1, 16]); sinks = inp("sinks", [1, 16])
    g_out = inp("g_out", [1, D]); w_out = inp("w_out", [D, D]); g_moe = inp("g_moe", [1, D])
    w_r = inp("w_r", [D, 36]); b_r = inp("b_r", [1, 36])
    w_gate = inp("w_gate", [NE, D, DE]); w_up = inp("w_up", [NE, D, DE]); w_down = inp("w_down", [NE, DE, D])
    g_fin = inp("g_fin", [1, D])
    fmask = inp("fmask", [128, 512]); swaA = inp("swaA", [128, 3 * 4 * 512]); rsel = inp("rsel", [128, 4])
    cst = inp("cst", [128, 4 * 128]); ebase = inp("ebase", [128, NE])
    out = nc.dram_tensor("out", [TOWN, D], F32, kind="ExternalOutput").ap()

    mod_s = scr("mod_s", [1, 6 * D], F32)
    KT_s = scr("KT_s", [16 * 64, S], BF16)
    VB_s = scr("VB_s", [S, 1024], BF16)
    QT_s = scr("QT_s", [16 * 64, TOWN], BF16)
    QAUG_s = scr("QAUG_s", [NOWN * 16, 3, 128], BF16)
    X1_s = scr("X1_s", [TOWN, D], F32)
    if dbg:
        Xs_s = nc.dram_tensor("Xs_s", [NSLOT, D], BF16, kind="ExternalOutput").ap()
        Ys_s = nc.dram_tensor("Ys_s", [NSLOT, D], F32, kind="ExternalOutput").ap()
        d_pg = nc.dram_tensor("d_pg", [128, NOWN * 2], I32, kind="ExternalOutput").ap()
        d_w = nc.dram_tensor("d_w", [128, NOWN * 2], F32, kind="ExternalOutput").ap()
    else:
        Xs_s = scr("Xs_s", [NSLOT, D], BF16)
        Ys_s = scr("Ys_s", [NSLOT, D], F32)

    w_in_v = w_in.rearrange("(kc p) n -> p kc n", p=128)

    with contextlib.ExitStack() as es:
        k = K(nc, es)
        pe, act, dve, pool, sp = k.pe, k.act, k.dve, k.pool, k.sp
        mark_tile = es.enter_context(nc.sbuf_tensor("mark_tile", [128, 8], F32))
        pool.mark_tile = mark_tile[:, 0:4]
        act.mark_tile = mark_tile[:, 4:8]
        for e_ in k.engs:
            e_.last_is_dma = False

        def sb(name, shape, dt, st=None):
            return (st or es).enter_context(nc.sbuf_tensor(name, shape, dt))

        def ps(name, shape, dt, st):
            return st.enter_context(nc.psum_tensor(name, shape, dt))

        cst_f = sb("cst_f", [128, 512], F32)
        ident_b = sb("ident_b", [128, 128], BF16)
        ssq = sb("ssq", [128, NB + 6 * NOWN + 8], F32)
        junk = sb("junk", [128, D], BF16)
        B_cst, B_identb, B_ssq, B_junk = Buf(), Buf(), Buf(), Buf()
        ds0 = k.dsem()
        k.dma(sp, cst_f[:], cst, ds0, wr=[B_cst])
        k.op(dve, "tensor_copy", out=ident_b[:], in_=cst_f[:, 0:128], rd=[B_cst], wr=[B_identb])
        k.op(pool, "memset", ssq[:], 0.0, wr=[B_ssq])
        ident_f = cst_f[:, 0:128]
        U_incl = cst_f[:, 128:256]
        U_strict = cst_f[:, 256:384]
        ones_f = cst_f[:, 384:512]
        ssq_ctr = [0]

        def new_ssq():
            i = ssq_ctr[0]
            ssq_ctr[0] += 1
            return ssq[:, i:i + 1]

        rstd_all = sb("rstd_all", [128, NB + 6 * NOWN + 8], F32)

        def rms_scale(src_ap, B_src, width, eps_scaled):
            col = new_ssq()
            i = ssq_ctr[0] - 1
            Bc = Buf()
            Bc.w = B_ssq.w
            k.op(act, "activation", out=junk[:, 0:width], in_=src_ap, func=AF.Square, accum_out=col,
                 rd=[B_src], wr=[Bc, B_junk])
            r = rstd_all[:, i:i + 1]
            Br = Buf()
            k.op(act, "activation", out=r, in_=col, func=AF.Sqrt, bias=float(eps_scaled), scale=1.0, rd=[Bc], wr=[Br])
            k.op(dve, "reciprocal", out=r, in_=r, rd=[Br], wr=[Br])
            return r, Br

        cumN = sb("cumN", [128, NB, 16], F32)
        B_cumN = Buf()
        stA = contextlib.ExitStack()
        es.enter_context(stA)
        stB = stA
        cT_sb = sb("cT_sb", [128, KC], F32, stA)
        sil = sb("sil", [128, KC], BF16, stA)
        B_cT, B_sil = Buf(), Buf()
        k.dma(sp, cT_sb[:], cT, ds0, wr=[B_cT])
        k.op(act, "activation", out=sil[:], in_=cT_sb[:], func=AF.Silu, rd=[B_cT], wr=[B_sil])
        wa = [sb("wa%d" % i, [128, KC, 512], BF16, stA) for i in range(2)]
        B_wa = [Buf(), Buf()]
        ds_wa = [k.dsem(), k.dsem()]
        brow = [sb("brow%d" % i, [1, 512], F32, stA) for i in range(2)]
        B_brow = [Buf(), Buf()]
        ds_br = [k.dsem(), k.dsem()]
        mrow = [sb("mrow%d" % i, [1, 512], F32, stA) for i in range(2)]
        B_mrow = [Buf(), Buf()]
        ds_mr = [k.dsem(), k.dsem()]
        B_mod = [Buf() for _ in range(24)]
        w_ada_v = w_ada.rearrange("(kc p) n -> p kc n", p=128)
        ada_state = {"loaded": 0, "done": 0}

        def ada_load(blk):
            s = blk % 2
            k.dma(pool, wa[s][:], w_ada_v[:, :, blk * 512:(blk + 1) * 512], ds_wa[s], wr=[B_wa[s]])
            k.dma(sp, brow[s][:], b_ada[0:1, blk * 512:(blk + 1) * 512], ds_br[s], wr=[B_brow[s]])

        def ada_block(blk, mod_ps, B_modps):
            s = blk % 2
            for kc in range(KC):
                k.op(pe, "matmul", mod_ps[0:1, :], lhsT=sil[:, kc:kc + 1], rhs=wa[s][:, kc, :], start=(kc == 0),
                     stop=(kc == KC - 1), rd=[B_sil, B_wa[s]], wr=[B_modps])
            if (blk // 4) in (1, 4):
                k.op(dve, "scalar_tensor_tensor", out=mrow[s][:], in0=mod_ps[0:1, :], scalar=1.0, in1=brow[s][:],
                     op0=ALU.add, op1=ALU.add, rd=[B_modps, B_brow[s]], wr=[B_mrow[s]])
            else:
                k.op(dve, "tensor_tensor", out=mrow[s][:], in0=mod_ps[0:1, :], in1=brow[s][:], op=ALU.add,
                     rd=[B_modps, B_brow[s]], wr=[B_mrow[s]])
            k.dma(sp, mod_s[0:1, blk * 512:(blk + 1) * 512], mrow[s][:], ds_mr[s], rd=[B_mrow[s]], wr=[B_mod[blk]])

        def ada_step(mod_ps, B_modps):
            b = ada_state["done"]
            if b >= 24:
                return
            while ada_state["loaded"] < min(24, b + 2):
                ada_load(ada_state["loaded"])
                ada_state["loaded"] += 1
            ada_block(b, mod_ps, B_modps)
            ada_state["done"] += 1

        def bcast_load(dst, B_dst, chunk, ds):
            k.dma(sp, dst[:], mod_s[0:1, chunk * D:(chunk + 1) * D].partition_broadcast(128), ds,
                  rd=[B_mod[chunk * 4 + i] for i in range(4)], wr=[B_dst])

        def vec_bcast_load(dst, B_dst, src, ds, n=D):
            k.dma(sp, dst, src[0:1, 0:n].partition_broadcast(128), ds, wr=[B_dst])

        G1s = sb("G1s", [128, D], F32, stB)
        SHa = sb("SHa", [128, D], F32, stB)
        B_G1s, B_SHa = Buf(), Buf()
        mod_ps = ps("mod_ps", [128, 512], F32, stB)
        B_modps = Buf()
        for _ in range(8):
            ada_step(mod_ps, B_modps)
        B_tmpg = Buf()
        bcast_load(G1s, B_G1s, 1, ds0)
        bcast_load(SHa, B_SHa, 0, ds0)

        Wkb = sb("Wkb", [128, KC, 1024], BF16, stB)
        Wvb = sb("Wvb", [128, KC, 1024], BF16, stB)
        Wf = sb("Wf", [128, KC, 16], BF16, stB)
        B_Wkb, B_Wvb, B_Wf = Buf(), Buf(), Buf()
        ds_w = k.dsem()
        for kc in range(KC):
            k.dma(pool, Wkb[:, kc, :], w_in_v[:, kc, KB0:KB0 + 1024], ds_w, wr=[B_Wkb])
        for kc in range(KC):
            k.dma(pool, Wvb[:, kc, :], w_in_v[:, kc, VB0:VB0 + 1024], ds_w, wr=[B_Wvb])
        k.dma(pool, Wf[:], w_in_v[:, :, FB0:FB0 + 16], ds_w, wr=[B_Wf])
        bF = sb("bF", [128, 16], F32, stB)
        B_bF = Buf()
        vec_bcast_load(bF[:], B_bF, b_f, ds0, 16)

        NXS = 2
        xts = [sb("xt%d" % i, [128, D], F32, stB) for i in range(NXS)]
        B_xt = [Buf() for _ in range(NXS)]
        ds_xt = [k.dsem() for _ in range(NXS)]
        vec_bcast_load(xts[0][:], B_xt[0], g_mix, ds0)
        k.op(dve, "scalar_tensor_tensor", out=G1s[:], in0=G1s[:], scalar=SQD, in1=xts[0][:], op0=ALU.mult, op1=ALU.mult,
             rd=[B_xt[0]], wr=[B_G1s])
        hb = [sb("hb%d" % i, [128, D], BF16, stB) for i in range(2)]
        B_hb = [Buf(), Buf()]
        hTg = [sb("hTg%d" % i, [128, KC, 512], BF16, stB) for i in range(2)]
        B_hTg = [[Buf() for _ in range(4)] for _ in range(2)]
        hT_ps = [ps("hT_ps%d" % i, [128, D], BF16, stB) for i in range(1)]
        B_hTps = [Buf()]
        NMM = 4
        mm_ps = [ps("mm_ps%d" % i, [128, 512], F32, stB) for i in range(NMM)]
        B_mm = [Buf() for _ in range(NMM)]
        f_ps = ps("f_ps", [128, 512], F32, stB)
        B_fps = [Buf(), Buf()]
        kT_sb = [sb("kT_sb%d" % i, [128, 512], BF16, stB) for i in range(2)]
        B_kTsb = [Buf(), Buf()]
        ds_kT = [k.dsem(), k.dsem()]
        v_sb = [sb("v_sb%d" % i, [128, 1024], BF16, stB) for i in range(2)]
        B_vsb = [Buf(), Buf()]
        ds_v = [k.dsem(), k.dsem()]
        zf = sb("zf", [128, NB, 16], F32, stB)
        B_zf = Buf()
        cnt = {"xt": 0, "hb": 0, "mm": 0, "kT": 0, "v": 0, "f": 0, "hTps": 0}

        def make_h(src_rows, Bx_extra_rd=()):
            s = cnt["xt"] % NXS
            cnt["xt"] += 1
            k.dma(sp, xts[s][:], src_rows, ds_xt[s], wr=[B_xt[s]])
            r, Br = rms_scale(xts[s][:], B_xt[s], D, D * EPS)
            k.op(dve, "scalar_tensor_tensor", out=xts[s][:], in0=xts[s][:], scalar=r, in1=G1s[:], op0=ALU.mult,
                 op1=ALU.mult, rd=[Br, B_G1s], wr=[B_xt[s]])
            hs = cnt["hb"] % 2
            cnt["hb"] += 1
            k.op(dve, "tensor_tensor", out=hb[hs][:], in0=xts[s][:], in1=SHa[:], op=ALU.add,
                 rd=[B_xt[s], B_SHa], wr=[B_hb[hs]])
            return hb[hs], B_hb[hs]

        def transpose_to(h_t, B_h, dst_ap3, B_dst, eng=None):
            s = cnt["hTps"] % len(hT_ps)
            cnt["hTps"] += 1
            for kc in range(KC):
                k.op(pe, "transpose", out=hT_ps[s][:, kc * 128:(kc + 1) * 128], in_=h_t[:, kc * 128:(kc + 1) * 128],
                     identity=ident_b[:], rd=[B_h, B_identb], wr=[B_hTps[s]])
            e = eng or act
            if e is act:
                k.op(act, "copy", out=dst_ap3, in_=hT_ps[s][:].rearrange("p (k t) -> p k t", k=KC),
                     rd=[B_hTps[s]], wr=[B_dst])
            else:
                k.op(e, "tensor_copy", out=dst_ap3, in_=hT_ps[s][:].rearrange("p (k t) -> p k t", k=KC),
                     rd=[B_hTps[s]], wr=[B_dst])

        for i in range(4):
            h_t, B_h = make_h(x_seq[i * 128:(i + 1) * 128, :])
            transpose_to(h_t, B_h, hTg[0][:, :, i * 128:(i + 1) * 128], B_hTg[0][i])
        for g in range(NG):
            gs = g % 2
            hq = None
            for c in range(8):
                ms = cnt["mm"] % NMM
                cnt["mm"] += 1
                for kc in range(KC):
                    k.op(pe, "matmul", mm_ps[ms][:], lhsT=Wkb[:, kc, c * 128:(c + 1) * 128], rhs=hTg[gs][:, kc, :],
                         start=(kc == 0), stop=(kc == KC - 1), rd=[B_Wkb] + B_hTg[gs], wr=[B_mm[ms]])
                ks = cnt["kT"] % 2
                cnt["kT"] += 1
                k.op(act if c % 2 else dve, "copy" if c % 2 else "tensor_copy", out=kT_sb[ks][:], in_=mm_ps[ms][:],
                     rd=[B_mm[ms]], wr=[B_kTsb[ks]])
                k.dma(pool, KT_s[c * 128:(c + 1) * 128, g * 512:(g + 1) * 512], kT_sb[ks][:], ds_kT[ks], rd=[B_kTsb[ks]])
                if g + 1 < NG:
                    i_n = c // 2
                    t_n = 4 * (g + 1) + i_n
                    if c % 2 == 0:
                        hq = make_h(x_seq[t_n * 128:(t_n + 1) * 128, :])
                    else:
                        transpose_to(hq[0], hq[1], hTg[1 - gs][:, :, i_n * 128:(i_n + 1) * 128], B_hTg[1 - gs][i_n])
            for i in range(4):
                t = 4 * g + i
                vs = cnt["v"] % 2
                cnt["v"] += 1
                for n in range(2):
                    ms = cnt["mm"] % NMM
                    cnt["mm"] += 1
                    for kc in range(KC):
                        k.op(pe, "matmul", mm_ps[ms][:], lhsT=hTg[gs][:, kc, i * 128:(i + 1) * 128],
                             rhs=Wvb[:, kc, n * 512:(n + 1) * 512], start=(kc == 0), stop=(kc == KC - 1),
                             rd=[B_Wvb, B_hTg[gs][i]], wr=[B_mm[ms]])
                    k.op(act, "copy", out=v_sb[vs][:, n * 512:(n + 1) * 512], in_=mm_ps[ms][:], rd=[B_mm[ms]],
                         wr=[B_vsb[vs]])
                k.dma(pool, VB_s[t * 128:(t + 1) * 128, :], v_sb[vs][:], ds_v[vs], rd=[B_vsb[vs]])
                fs = cnt["f"] % 2
                cnt["f"] += 1
                for kc in range(KC):
                    k.op(pe, "matmul", f_ps[:, fs * 16:(fs + 1) * 16], lhsT=hTg[gs][:, kc, i * 128:(i + 1) * 128],
                         rhs=Wf[:, kc, :], start=(kc == 0), stop=(kc == KC - 1), rd=[B_Wf, B_hTg[gs][i]],
                         wr=[B_fps[fs]])
                k.op(dve, "tensor_tensor", out=zf[:, t, :], in0=f_ps[:, fs * 16:(fs + 1) * 16], in1=bF[:], op=ALU.add,
                     rd=[B_fps[fs], B_bF], wr=[B_zf])
            ada_step(mod_ps, B_modps)
        while ada_state["done"] < 24:
            ada_step(mod_ps, B_modps)

        NC16 = NB * 16
        zf2 = zf[:].rearrange("p t h -> p (t h)")
        k.op(act, "activation", out=zf2, in_=zf2, func=AF.Exp, scale=-1.0, rd=[B_zf], wr=[B_zf])
        k.op(act, "activation", out=zf2, in_=zf2, func=AF.Ln, bias=1.0, scale=1.0, rd=[B_zf], wr=[B_zf])
        cumN2 = cumN[:].rearrange("p t h -> p (t h)")
        class _V:
            def __init__(self, t):
                self.t = t
            def __getitem__(self, idx):
                return self.t[:, 0:NB * 16].rearrange("p (t h) -> p t h", h=16)[idx]
        pfx = [_V(xts[i]) for i in range(2)]
        B_pfx = [B_xt[0], B_xt[1]]
        for c0 in range(0, NC16, 512):
            c1 = min(NC16, c0 + 512)
            w = c1 - c0
            k.op(pe, "matmul", mm_ps[0][:, 0:w], lhsT=U_incl, rhs=zf2[:, c0:c1], start=True, stop=True,
                 rd=[B_zf, B_cst], wr=[B_mm[0]])
            k.op(pe, "matmul", mm_ps[1][:, 0:w], lhsT=ones_f, rhs=zf2[:, c0:c1], start=True, stop=True,
                 rd=[B_zf, B_cst], wr=[B_mm[1]])
            k.op(dve, "tensor_copy", out=cumN2[:, c0:c1], in_=mm_ps[0][:, 0:w], rd=[B_mm[0]], wr=[B_cumN])
            k.op(dve, "tensor_copy", out=pfx[0][:].rearrange("p t h -> p (t h)")[:, c0:c1], in_=mm_ps[1][:, 0:w],
                 rd=[B_mm[1]], wr=[B_pfx[0]])
        cur = 0
        sh = 1
        while sh < NB:
            nx = 1 - cur
            k.op(dve, "tensor_copy", out=pfx[nx][:, 0:sh, :], in_=pfx[cur][:, 0:sh, :], rd=[B_pfx[cur]], wr=[B_pfx[nx]])
            k.op(dve, "tensor_tensor", out=pfx[nx][:, sh:NB, :], in0=pfx[cur][:, sh:NB, :], in1=pfx[cur][:, 0:NB - sh, :],
                 op=ALU.add, rd=[B_pfx[cur]], wr=[B_pfx[nx]])
            cur = nx
            sh *= 2
        k.op(dve, "tensor_tensor", out=cumN[:, 1:NB, :], in0=cumN[:, 1:NB, :], in1=pfx[cur][:, 0:NB - 1, :], op=ALU.add,
             rd=[B_pfx[cur]], wr=[B_cumN])
        rsel_sb = sb("rsel_sb", [128, 4], F32, stB)
        B_rsel = Buf()
        k.dma(sp, rsel_sb[:], rsel, ds0, wr=[B_rsel])
        cq = sb("cq", [128, NOWN, 16], F32, stB)
        B_cq = Buf()
        cumN4 = cumN[:].rearrange("p (j u) h -> p j u h", u=4)
        k.op(dve, "tensor_scalar", out=cq[:], in0=cumN4[:, :, 0, :], scalar1=rsel_sb[:, 0:1], scalar2=-8.0, op0=ALU.mult,
             op1=ALU.mult, rd=[B_cumN, B_rsel], wr=[B_cq])
        cq8 = sb("cq8", [128, NOWN, 16], F32, stB)
        for u in range(1, 4):
            k.op(dve, "tensor_scalar", out=cq8[:], in0=cumN4[:, :, u, :], scalar1=rsel_sb[:, u:u + 1], scalar2=-8.0,
                 op0=ALU.mult, op1=ALU.mult, rd=[B_cumN, B_rsel], wr=[B_tmpg])
            k.op(dve, "tensor_tensor", out=cq[:], in0=cq[:], in1=cq8[:], op=ALU.add, rd=[B_tmpg], wr=[B_cq])
        NQ = NOWN * 16
        cqf = cq[:].rearrange("p j h -> p (j h)")
        c3 = [sb("c3_%d" % i, [128, NQ], BF16, stB) for i in range(3)]
        B_c3 = [Buf() for _ in range(3)]
        for i in range(3):
            k.op(dve, "tensor_copy", out=c3[i][:], in_=cqf, rd=[B_cq], wr=[B_c3[i]])
            if i < 2:
                k.op(dve, "tensor_tensor", out=cqf, in0=cqf, in1=c3[i][:], op=ALU.subtract, rd=[B_c3[i]], wr=[B_cq])
        qa_sb = sb("qa_sb", [128, 3, 128], BF16, stB)
        B_qasb = Buf()
        ds_qa = k.dsem()
        for c0 in range(0, NQ, 128):
            w = min(128, NQ - c0)
            for i in range(3):
                k.op(pe, "transpose", out=hT_ps[0][0:w, i * 128:(i + 1) * 128], in_=c3[i][:, c0:c0 + w],
                     identity=ident_b[:], rd=[B_c3[i], B_identb], wr=[B_hTps[0]])
            k.op(dve, "tensor_copy", out=qa_sb[0:w, :, :], in_=hT_ps[0][0:w, 0:384].rearrange("p (k t) -> p k t", k=3),
                 rd=[B_hTps[0]], wr=[B_qasb])
            k.dma(sp, QAUG_s[c0:c0 + w, :, :], qa_sb[0:w, :, :], ds_qa, rd=[B_qasb])
        k.barrier()
        stB.close()

        stOA = contextlib.ExitStack()
        es.enter_context(stOA)
        o_a = sb("o_a", [128, NOWN, 1024], BF16, stOA)
        B_oa = [Buf() for _ in range(NOWN)]
        esink = sb("esink", [128, 16], F32, stOA)
        B_esink = Buf()
        vec_bcast_load(esink[:], B_esink, sinks, ds0, 16)
        k.op(act, "activation", out=esink[:], in_=esink[:], func=AF.Exp, rd=[B_esink], wr=[B_esink])

        stC = contextlib.ExitStack()
        es.enter_context(stC)
        G1s = sb("G1s_c", [128, D], F32, stC)
        SHa = sb("SHa_c", [128, D], F32, stC)
        B_G1s, B_SHa = Buf(), Buf()
        bcast_load(G1s, B_G1s, 1, ds0)
        bcast_load(SHa, B_SHa, 0, ds0)
        Wq = sb("Wq", [128, KC, 2048], BF16, stC)
        Wkv = sb("Wkv", [128, KC, 512], BF16, stC)
        B_Wq, B_Wkv = Buf(), Buf()
        for kc in range(KC):
            k.dma(pool, Wq[:, kc, 0:1024], w_in_v[:, kc, QA0:QA0 + 1024], ds_w, wr=[B_Wq])
            k.dma(pool, Wq[:, kc, 1024:2048], w_in_v[:, kc, QB0:QB0 + 1024], ds_w, wr=[B_Wq])
        k.dma(pool, Wkv[:], w_in_v[:, :, KA0:KA0 + 512], ds_w, wr=[B_Wkv])
        swaA_sb = sb("swaA_sb", [128, 3 * 4 * 512], BF16, stC)
        B_swaA = Buf()
        for i in range(6):
            k.dma(pool, swaA_sb[:, i * 1024:(i + 1) * 1024], swaA[:, i * 1024:(i + 1) * 1024], ds_w, wr=[B_swaA])
        NXS = 2
        xts = [sb("xtc%d" % i, [128, D], F32, stC) for i in range(NXS)]
        B_xt = [Buf() for _ in range(NXS)]
        vec_bcast_load(xts[0][:], B_xt[0], g_mix, ds0)
        k.op(dve, "scalar_tensor_tensor", out=G1s[:], in0=G1s[:], scalar=SQD, in1=xts[0][:], op0=ALU.mult, op1=ALU.mult,
             rd=[B_xt[0]], wr=[B_G1s])
        hb = [sb("hbc%d" % i, [128, D], BF16, stC) for i in range(2)]
        B_hb = [Buf(), Buf()]
        hT2 = [sb("hT2_%d" % i, [128, KC, 128], BF16, stC) for i in range(2)]
        B_hT2 = [Buf(), Buf()]
        hT_ps = [ps("hT_psc%d" % i, [128, D], BF16, stC) for i in range(1)]
        B_hTps = [Buf()]
        mm_ps = [ps("mm_psc%d" % i, [128, 512], F32, stC) for i in range(2)]
        B_mm = [Buf(), Buf()]
        s_ps = ps("s_psc", [128, 512], F32, stC)
        B_sps = Buf()
        o_ps = ps("o_psc", [128, 512], F32, stC)
        B_ops = Buf()
        tr_ps = ps("tr_psc", [128, 512], F32, stC)
        B_trps = Buf()
        q_tok = sb("q_tok", [128, 2048], BF16, stC)
        B_qtok = Buf()
        kv_tok = [sb("kv_tok%d" % i, [128, 512], BF16, stC) for i in range(2)]
        B_kvtok = [Buf(), Buf()]
        qaT = sb("qaT", [64, 16, 128], BF16, stC)
        qbT = sb("qbT", [64, 16, 128], BF16, stC)
        kaT = sb("kaT", [64, 2, 4, 128], BF16, stC)
        va = sb("va", [128, 2, 4, 65], BF16, stC)
        B_qaT, B_qbT, B_kaT, B_va = Buf(), Buf(), Buf(), Buf()
        k.op(pool, "memset", va[:], 1.0, wr=[B_va])
        pT = [sb("pTc%d" % i, [128, 512], BF16, stC) for i in range(2)]
        B_pT = [Buf(), Buf()]
        oT_sb2 = [sb("oT_sbc%d" % i, [65, 512], F32, stC) for i in range(2)]
        B_oTsb2 = [Buf(), Buf()]
        den = sb("denc", [128, 8], F32, stC)
        B_den = Buf()
        ds_qb = k.dsem()
        cnt = {"xt": 0, "hb": 0, "mm": 0, "hTps": 0, "pT": 0}
        QT_v = QT_s.rearrange("(h d) t -> d h t", d=64)

        for j in range(NOWN):
            for which, src in ((0, x_own), (1, x_prev)):
                h_t, B_h = make_h(src[j * 128:(j + 1) * 128, :])
                transpose_to(h_t, B_h, hT2[which][:], B_hT2[which])
            for n in range(4):
                ms = cnt["mm"] % 2
                cnt["mm"] += 1
                for kc in range(KC):
                    k.op(pe, "matmul", mm_ps[ms][:], lhsT=hT2[0][:, kc, :], rhs=Wq[:, kc, n * 512:(n + 1) * 512],
                         start=(kc == 0), stop=(kc == KC - 1), rd=[B_Wq, B_hT2[0]], wr=[B_mm[ms]])
                k.op(dve if n % 2 else act, "tensor_copy" if n % 2 else "copy", out=q_tok[:, n * 512:(n + 1) * 512],
                     in_=mm_ps[ms][:], rd=[B_mm[ms]], wr=[B_qtok])
            for which in range(2):
                ms = cnt["mm"] % 2
                cnt["mm"] += 1
                for kc in range(KC):
                    k.op(pe, "matmul", mm_ps[ms][:], lhsT=hT2[which][:, kc, :], rhs=Wkv[:, kc, :],
                         start=(kc == 0), stop=(kc == KC - 1), rd=[B_Wkv, B_hT2[which]], wr=[B_mm[ms]])
                k.op(dve, "tensor_copy", out=kv_tok[which][:], in_=mm_ps[ms][:], rd=[B_mm[ms]], wr=[B_kvtok[which]])
                kb = 1 - which
                k.op(dve, "tensor_copy", out=va[:, kb, :, 0:64],
                     in_=kv_tok[which][:, 256:512].rearrange("p (h d) -> p h d", h=4), rd=[B_kvtok[which]], wr=[B_va])
            for half, dstT, B_dst in ((0, qaT, B_qaT), (1, qbT, B_qbT)):
                for hh in range(2):
                    s = cnt["hTps"] % len(hT_ps)
                    cnt["hTps"] += 1
                    for i8 in range(8):
                        g_ = hh * 8 + i8
                        c0 = half * 1024 + g_ * 64
                        k.op(pe, "transpose", out=hT_ps[s][0:64, i8 * 128:(i8 + 1) * 128], in_=q_tok[:, c0:c0 + 64],
                             identity=ident_b[:], rd=[B_qtok, B_identb], wr=[B_hTps[s]])
                    k.op(act if hh else dve, "copy" if hh else "tensor_copy", out=dstT[:, hh * 8:(hh + 1) * 8, :],
                         in_=hT_ps[s][0:64, 0:1024].rearrange("p (h t) -> p h t", h=8), rd=[B_hTps[s]], wr=[B_dst])
            k.dma(sp, QT_v[:, :, j * 128:(j + 1) * 128], qbT[:], ds_qb, rd=[B_qbT])
            s = cnt["hTps"] % len(hT_ps)
            cnt["hTps"] += 1
            for which in range(2):
                kb = 1 - which
                for hk in range(4):
                    k.op(pe, "transpose", out=hT_ps[s][0:64, (kb * 4 + hk) * 128:(kb * 4 + hk + 1) * 128],
                         in_=kv_tok[which][:, hk * 64:(hk + 1) * 64], identity=ident_b[:],
                         rd=[B_kvtok[which], B_identb], wr=[B_hTps[s]])
            k.op(dve, "tensor_copy", out=kaT[:].rearrange("p a h t -> p (a h) t"),
                 in_=hT_ps[s][0:64, 0:1024].rearrange("p (h t) -> p h t", h=8), rd=[B_hTps[s]], wr=[B_kaT])
            units = [(hk_, kb_) for hk_ in range(4) for kb_ in range(2)]
            s_bufs = [(s_ps, B_sps), (mm_ps[1], B_mm[1])]
            o_bufs = [(o_ps, B_ops), (mm_ps[0], B_mm[0])]

            def swa_S(u):
                hk, kb = units[u]
                sb_, Bsb = s_bufs[u % 2]
                for i in range(4):
                    k.op(pe, "matmul", sb_[:, i * 128:(i + 1) * 128], lhsT=kaT[:, kb, hk, :],
                         rhs=qaT[:, hk * 4 + i, :], start=True, stop=True, rd=[B_kaT, B_qaT], wr=[Bsb])
                p_ = u % 2
                k.op(act, "activation", out=pT[p_][:], in_=sb_[:], func=AF.Exp, scale=0.125, rd=[Bsb], wr=[B_pT[p_]])
                tab = (0 if j == 0 else 1) if kb == 0 else 2
                a0 = (tab * 4 + hk) * 512
                k.op(dve, "tensor_tensor", out=pT[p_][:], in0=pT[p_][:], in1=swaA_sb[:, a0:a0 + 512], op=ALU.mult,
                     rd=[B_swaA], wr=[B_pT[p_]])

            def swa_PV(u):
                hk, kb = units[u]
                ob, Bob = o_bufs[hk % 2]
                k.op(pe, "matmul", ob[0:65, :], lhsT=va[:, kb, hk, :], rhs=pT[u % 2][:], start=(kb == 0),
                     stop=(kb == 1), rd=[B_va, B_pT[u % 2]], wr=[Bob])
                if kb == 1:
                    k.op(act, "copy", out=oT_sb2[hk % 2][:], in_=ob[0:65, :], rd=[Bob], wr=[B_oTsb2[hk % 2]])

            def swa_tail(hk):
                osb, Bosb = oT_sb2[hk % 2], B_oTsb2[hk % 2]
                for i in range(4):
                    k.op(pe, "transpose", out=tr_ps[:, i * 65:(i + 1) * 65], in_=osb[:, i * 128:(i + 1) * 128],
                         identity=ident_f[0:65, 0:65], rd=[Bosb, B_cst], wr=[B_trps])
                tr3 = tr_ps[:, 0:260].rearrange("p (h c) -> p h c", h=4)
                k.op(dve, "tensor_tensor", out=den[:, 0:4], in0=tr3[:, :, 64], in1=esink[:, hk * 4:(hk + 1) * 4],
                     op=ALU.add, rd=[B_trps, B_esink], wr=[B_den])
                k.op(dve, "reciprocal", out=den[:, 4:8], in_=den[:, 0:4], rd=[B_den], wr=[B_den])
                for i in range(4):
                    g_ = hk * 4 + i
                    k.op(dve, "tensor_scalar", out=o_a[:, j, g_ * 64:(g_ + 1) * 64], in0=tr3[:, i, 0:64],
                         scalar1=den[:, 4 + i:5 + i], scalar2=None, op0=ALU.mult, rd=[B_trps, B_den], wr=[B_oa[j]])

            swa_S(0)
            tail_q = []
            for u in range(8):
                if u + 1 < 8:
                    swa_S(u + 1)
                swa_PV(u)
                if tail_q:
                    swa_tail(tail_q.pop(0))
                if units[u][1] == 1:
                    tail_q.append(units[u][0])
            while tail_q:
                swa_tail(tail_q.pop(0))
        k.barrier()
        stC.close()

        stOB = contextlib.ExitStack()
        es.enter_context(stOB)
        o_b = sb("o_b", [128, NOWN, 1024], BF16, stOB)
        B_ob = [Buf() for _ in range(NOWN)]
        stF = contextlib.ExitStack()
        es.enter_context(stF)
        kTa = [sb("kTa%d" % i, [67, S], BF16, stF) for i in range(2)]
        vau = [sb("vau%d" % i, [128, NB, 65], BF16, stF) for i in range(2)]
        qTa = [sb("qTa%d" % i, [67, TOWN], BF16, stF) for i in range(2)]
        B_kTa, B_vau, B_qTa = [Buf(), Buf()], [Buf(), Buf()], [Buf(), Buf()]
        ds_hd = [k.dsem(), k.dsem()]
        for i in range(2):
            k.op(pool, "memset", kTa[i][64:67, :], 1.0, wr=[B_kTa[i]])
            k.op(pool, "memset", vau[i][:], 1.0, wr=[B_vau[i]])
        fm_sb = sb("fm_sb", [128, 512], BF16, stF)
        B_fm = Buf()
        k.dma(pool, fm_sb[:], fmask, ds_w, wr=[B_fm])
        NPT = 4
        pT = [sb("pTf%d" % i, [128, 1024], BF16, stF) for i in range(NPT)]
        B_pT = [Buf() for _ in range(NPT)]
        NSP = 3
        s_ps = [ps("s_psf%d" % i, [128, 1024], F32, stF) for i in range(NSP)]
        B_sps = [Buf() for _ in range(NSP)]
        o_ps = ps("o_psf", [128, 1024], F32, stF)
        B_ops = Buf()
        tr_ps = [s_ps[0][:, 0:512], s_ps[0][:, 512:1024]]
        B_trps = [B_sps[0], B_sps[0]]
        oT_sb = sb("oT_sbf", [65, 1024], F32, stF)
        B_oTsb = Buf()
        den = sb("denf", [128, 8], F32, stF)
        B_den = Buf()
        VB_v = VB_s.rearrange("(t p) c -> p t c", p=128)
        QAUG_v = QAUG_s.rearrange("(j h) k t -> h k j t", h=16)
        HB = (NOWN + 1) // 2
        halves = [(0, HB), (HB, NOWN)] if NOWN > 1 else [(0, 1)]
        cnt = {"pT": 0, "s": 0, "tr": 0}

        def load_head(h):
            s = h % 2
            k.dma(sp, kTa[s][0:64, :], KT_s[h * 64:(h + 1) * 64, :], ds_hd[s], wr=[B_kTa[s]])
            k.dma(sp, qTa[s][0:64, :], QT_s[h * 64:(h + 1) * 64, :], ds_hd[s], wr=[B_qTa[s]])
            k.dma(sp, qTa[s][64:67, :].rearrange("k (j t) -> k j t", t=128), QAUG_v[h], ds_hd[s], wr=[B_qTa[s]])
            for t0 in range(0, NB, 16):
                t1 = min(NB, t0 + 16)
                k.dma(sp, vau[s][:, t0:t1, 0:64], VB_v[:, t0:t1, h * 64:(h + 1) * 64], ds_hd[s], wr=[B_vau[s]])

        load_head(0)
        for h in range(16):
            hs = h % 2
            if h + 1 < 16:
                load_head(h + 1)
            for (j0, j1) in halves:
                nb = j1 - j0
                def stage1(kt):
                    g = kt // 4
                    u = kt % 4
                    ja = max(g, j0)
                    c_lo = (ja - j0) * 128
                    c_hi = nb * 128
                    ss = cnt["s"] % NSP
                    cnt["s"] += 1
                    for b0 in range(0, 1024, 512):
                        lo, hi = max(c_lo, b0), min(c_hi, b0 + 512)
                        if lo >= hi:
                            continue
                        k.op(pe, "matmul", s_ps[ss][:, lo:hi], lhsT=kTa[hs][:, kt * 128:(kt + 1) * 128],
                             rhs=qTa[hs][:, j0 * 128 + lo:j0 * 128 + hi], start=True, stop=True,
                             rd=[B_kTa[hs], B_qTa[hs]], wr=[B_sps[ss]])
                    p_ = cnt["pT"] % NPT
                    cnt["pT"] += 1
                    k.op(act, "activation", out=pT[p_][:, c_lo:c_hi], in_=s_ps[ss][:, c_lo:c_hi], func=AF.Exp,
                         bias=cumN[:, kt, h:h + 1], scale=0.125, rd=[B_sps[ss], B_cumN], wr=[B_pT[p_]])
                    if g >= j0:
                        k.op(dve, "scalar_tensor_tensor", out=pT[p_][:, c_lo:c_lo + 128], in0=pT[p_][:, c_lo:c_lo + 128],
                             scalar=1e30, in1=fm_sb[:, u * 128:(u + 1) * 128], op0=ALU.min, op1=ALU.mult,
                             rd=[B_fm], wr=[B_pT[p_]])
                    return p_

                def stage2(kt, p_):
                    g = kt // 4
                    u = kt % 4
                    ja = max(g, j0)
                    c_lo = (ja - j0) * 128
                    c_hi = nb * 128
                    started = set()

                    def st_flag(lo_):
                        bank = lo_ // 512
                        if kt == 0 and bank not in started:
                            started.add(bank)
                            return True
                        return False
                    if g >= j0:
                        k.op(pe, "matmul", o_ps[0:65, c_lo:c_lo + 128], lhsT=vau[hs][:, kt, :],
                             rhs=pT[p_][:, c_lo:c_lo + 128], start=st_flag(c_lo), stop=(u == 3), skip_group_check=True,
                             rd=[B_vau[hs], B_pT[p_]], wr=[B_ops])
                        r_lo = c_lo + 128
                    else:
                        r_lo = c_lo
                    for b0 in range(0, 1024, 512):
                        lo, hi = max(r_lo, b0), min(c_hi, b0 + 512)
                        if lo >= hi:
                            continue
                        k.op(pe, "matmul", o_ps[0:65, lo:hi], lhsT=vau[hs][:, kt, :], rhs=pT[p_][:, lo:hi],
                             start=st_flag(lo), stop=False, skip_group_check=True, rd=[B_vau[hs], B_pT[p_]], wr=[B_ops])

                nkt = 4 * j1
                pq = []
                for kt in range(nkt + 2):
                    if kt < nkt:
                        pq.append((kt, stage1(kt)))
                    if kt >= 2:
                        k0, p0 = pq.pop(0)
                        stage2(k0, p0)
                assert not pq
                for b0 in range(0, nb * 128, 512):
                    b1 = min(nb * 128, b0 + 512)
                    k.op(act, "copy", out=oT_sb[:, b0:b1], in_=o_ps[0:65, b0:b1], rd=[B_ops], wr=[B_oTsb])
                for q0 in range(0, nb, 4):
                    q1 = min(nb, q0 + 4)
                    ts_ = cnt["tr"] % 2
                    cnt["tr"] += 1
                    for i in range(q1 - q0):
                        k.op(pe, "transpose", out=tr_ps[ts_][:, i * 65:(i + 1) * 65],
                             in_=oT_sb[:, (q0 + i) * 128:(q0 + i + 1) * 128], identity=ident_f[0:65, 0:65],
                             rd=[B_oTsb, B_cst], wr=[B_trps[ts_]])
                    tr3 = tr_ps[ts_][:, 0:260].rearrange("p (h c) -> p h c", h=4)
                    k.op(dve, "reciprocal", out=den[:, 0:q1 - q0], in_=tr3[:, 0:q1 - q0, 64], rd=[B_trps[ts_]], wr=[B_den])
                    for i in range(q1 - q0):
                        jj = j0 + q0 + i
                        k.op(dve, "tensor_scalar", out=o_b[:, jj, h * 64:(h + 1) * 64], in0=tr3[:, i, 0:64],
                             scalar1=den[:, i:i + 1], scalar2=None, op0=ALU.mult, rd=[B_trps[ts_], B_den], wr=[B_ob[jj]])
        k.barrier()
        stF.close()

        stD = contextlib.ExitStack()
        es.enter_context(stD)
        Wout = sb("Wout", [128, KC, D], BF16, stD)
        B_Wout = Buf()
        w_out_v = w_out.rearrange("(kc p) n -> p kc n", p=128)
        for kc in range(KC):
            for hh in range(2):
                k.dma(pool, Wout[:, kc, hh * 1024:(hh + 1) * 1024], w_out_v[:, kc, hh * 1024:(hh + 1) * 1024], ds_w,
                      wr=[B_Wout])
        gout = sb("gout", [128, D], F32, stD)
        GA = sb("GA", [128, D], F32, stD)
        B_gout, B_GA = Buf(), Buf()
        vec_bcast_load(gout[:], B_gout, g_out, ds0)
        k.op(dve, "tensor_scalar", out=gout[:], in0=gout[:], scalar1=32.0, scalar2=None, op0=ALU.mult, wr=[B_gout])
        bcast_load(GA, B_GA, 2, ds0)
        xts = [sb("xtd%d" % i, [128, D], F32, stD) for i in range(2)]
        B_xt = [Buf(), Buf()]
        ds_xt = [k.dsem(), k.dsem()]
        x1t = [sb("x1t%d" % i, [128, D], F32, stD) for i in range(2)]
        B_x1t = [Buf(), Buf()]
        ds_x1 = [k.dsem(), k.dsem()]
        mixed = sb("mixed", [128, D], BF16, stD)
        B_mixed = Buf()
        mT = sb("mT", [128, KC, 128], BF16, stD)
        B_mT = Buf()
        hT_ps = [ps("hT_psd%d" % i, [128, D], BF16, stD) for i in range(2)]
        B_hTps = [Buf(), Buf()]
        mm_ps = [ps("mm_psd%d" % i, [128, 512], F32, stD) for i in range(2)]
        B_mm = [Buf(), Buf()]
        cnt = {"mm": 0, "hTps": 0}
        for j in range(NOWN):
            s = j % 2
            k.dma(sp, xts[s][:], x_own[j * 128:(j + 1) * 128, :], ds_xt[s], wr=[B_xt[s]])
            ra, Bra = rms_scale(o_a[:, j, :], B_oa[j], 1024, 1024 * EPS)
            rb, Brb = rms_scale(o_b[:, j, :], B_ob[j], 1024, 1024 * EPS)
            k.op(dve, "scalar_tensor_tensor", out=mixed[:, 0:1024], in0=o_a[:, j, :], scalar=ra, in1=gout[:, 0:1024],
                 op0=ALU.mult, op1=ALU.mult, rd=[B_oa[j], Bra, B_gout], wr=[B_mixed])
            k.op(dve, "scalar_tensor_tensor", out=mixed[:, 1024:2048], in0=o_b[:, j, :], scalar=rb, in1=gout[:, 1024:2048],
                 op0=ALU.mult, op1=ALU.mult, rd=[B_ob[j], Brb, B_gout], wr=[B_mixed])
            transpose_to(mixed, B_mixed, mT[:], B_mT)
            for n in range(4):
                ms = cnt["mm"] % 2
                cnt["mm"] += 1
                for kc in range(KC):
                    k.op(pe, "matmul", mm_ps[ms][:], lhsT=mT[:, kc, :], rhs=Wout[:, kc, n * 512:(n + 1) * 512],
                         start=(kc == 0), stop=(kc == KC - 1), rd=[B_Wout, B_mT], wr=[B_mm[ms]])
                sl = slice(n * 512, (n + 1) * 512)
                k.op(dve, "tensor_tensor", out=x1t[s][:, sl], in0=mm_ps[ms][:], in1=GA[:, sl], op=ALU.mult,
                     rd=[B_mm[ms], B_GA], wr=[B_x1t[s]])
                k.op(pool, "tensor_tensor", out=x1t[s][:, sl], in0=x1t[s][:, sl], in1=xts[s][:, sl], op=ALU.add,
                     rd=[B_xt[s]], wr=[B_x1t[s]])
            k.dma(sp, X1_s[j * 128:(j + 1) * 128, :], x1t[s][:], ds_x1[s], rd=[B_x1t[s]])
        k.barrier()
        stD.close()
        stOB.close()
        stOA.close()

        rt_w = sb("rt_w", [128, NOWN, 2], F32)
        rt_pg = sb("rt_pg", [128, NOWN, 2], I32)
        B_rtw, B_rtpg = Buf(), Buf()
        stR = contextlib.ExitStack()
        es.enter_context(stR)
        G2s = sb("G2s", [128, D], F32, stR)
        SHm = sb("SHm", [128, D], F32, stR)
        tmp2 = sb("tmp2", [128, D], F32, stR)
        B_G2s, B_SHm, B_tmp2 = Buf(), Buf(), Buf()
        bcast_load(G2s, B_G2s, 4, ds0)
        bcast_load(SHm, B_SHm, 3, ds0)
        vec_bcast_load(tmp2[:], B_tmp2, g_moe, ds0)
        k.op(dve, "scalar_tensor_tensor", out=G2s[:], in0=G2s[:], scalar=SQD, in1=tmp2[:], op0=ALU.mult, op1=ALU.mult,
             rd=[B_tmp2], wr=[B_G2s])
        Wr = sb("Wr", [128, KC, 36], F32, stR)
        B_Wr = Buf()
        k.dma(sp, Wr[:], w_r.rearrange("(kc p) n -> p kc n", p=128), ds0, wr=[B_Wr])
        bR = sb("bR", [128, 36], F32, stR)
        B_bR = Buf()
        vec_bcast_load(bR[:], B_bR, b_r, ds0, 36)
        eb_sb = sb("eb_sb", [128, NE], F32, stR)
        B_eb = Buf()
        k.dma(sp, eb_sb[:], ebase, ds0, wr=[B_eb])
        cnt_run = sb("cnt_run", [128, NE], F32, stR)
        B_cnt = Buf()
        k.op(dve, "memset", cnt_run[:], 0.0, wr=[B_cnt])
        zt = sb("zt", [128, D], BF16, stR)
        B_zt = Buf()
        k.op(pool, "memset", zt[:], 0.0, wr=[B_zt])
        ds_z = k.dsem()
        B_Xs = Buf()
        for s0 in range(0, NSLOT, 128):
            k.dma(sp, Xs_s[s0:s0 + 128, :], zt[:], ds_z, rd=[B_zt], wr=[B_Xs])
        x1t = [sb("x1r%d" % i, [128, D], F32, stR) for i in range(2)]
        B_x1t = [Buf(), Buf()]
        ds_x1 = [k.dsem(), k.dsem()]
        h2b = [sb("h2b%d" % i, [128, D], BF16, stR) for i in range(2)]
        B_h2b = [Buf(), Buf()]
        ds_sc = [k.dsem(), k.dsem()]
        h2T = sb("h2T", [128, KC, 128], F32, stR)
        B_h2T = Buf()
        trf_ps = [ps("trf_ps%d" % i, [128, 1024], F32, stR) for i in range(2)]
        B_trf = [Buf(), Buf()]
        lg_ps = ps("lg_ps", [128, 512], F32, stR)
        B_lgps = Buf()
        rk_ps = ps("rk_ps", [128, 512], F32, stR)
        B_rkps = Buf()
        R = sb("R", [128, 512], F32, stR)
        B_R = Buf()
        psc = [sb("psc%d" % i, [128, 2], I32, stR) for i in range(2)]
        B_psc = [Buf(), Buf()]
        BIGI = float(4 * NSLOT)

        def rop(method, **kw):
            return k.op(dve, method, rd=[B_R], wr=[B_R], **kw)

        for j in range(NOWN):
            s = j % 2
            k.dma(sp, x1t[s][:], X1_s[j * 128:(j + 1) * 128, :], ds_x1[s], wr=[B_x1t[s]])
            r2, Br2 = rms_scale(x1t[s][:], B_x1t[s], D, D * EPS)
            k.op(dve, "scalar_tensor_tensor", out=x1t[s][:], in0=x1t[s][:], scalar=r2, in1=G2s[:], op0=ALU.mult,
                 op1=ALU.mult, rd=[Br2, B_G2s], wr=[B_x1t[s]])
            k.op(dve, "tensor_tensor", out=x1t[s][:], in0=x1t[s][:], in1=SHm[:], op=ALU.add, rd=[B_SHm], wr=[B_x1t[s]])
            k.op(act, "copy", out=h2b[s][:], in_=x1t[s][:], rd=[B_x1t[s]], wr=[B_h2b[s]])
            for hh in range(2):
                for i8 in range(8):
                    kc = hh * 8 + i8
                    k.op(pe, "transpose", out=trf_ps[hh][:, i8 * 128:(i8 + 1) * 128], in_=x1t[s][:, kc * 128:(kc + 1) * 128],
                         identity=ident_f, rd=[B_x1t[s], B_cst], wr=[B_trf[hh]])
                k.op(act if hh else dve, "copy" if hh else "tensor_copy", out=h2T[:, hh * 8:(hh + 1) * 8, :],
                     in_=trf_ps[hh][:].rearrange("p (k t) -> p k t", k=8), rd=[B_trf[hh]], wr=[B_h2T])
            for kc in range(KC):
                k.op(pe, "matmul", lg_ps[:, 0:36], lhsT=h2T[:, kc, :], rhs=Wr[:, kc, :], start=(kc == 0),
                     stop=(kc == KC - 1), rd=[B_h2T, B_Wr], wr=[B_lgps])
            LG = R[:, 0:36]; GL = R[:, 0:4]; EL = R[:, 4:36]
            GMAX = R[:, 40:41]; NGMAX = R[:, 41:42]; GSUM = R[:, 42:43]; GVAL = R[:, 43:44]
            GOH = R[:, 44:48]; GPEN = R[:, 48:52]; GEXP = R[:, 52:56]
            EM = R[:, 64:96]; T1 = R[:, 96:97]; T2 = R[:, 97:98]; OH1 = R[:, 100:132]; OH2 = R[:, 132:164]
            E2 = R[:, 164:196]; AA = R[:, 196:228]; RK = R[:, 228:260]; RKB = R[:, 260:292]; TMP = R[:, 292:324]
            DD = R[:, 324:325]; ED = R[:, 325:326]; W1 = R[:, 326:327]; W2 = R[:, 327:328]
            P1 = R[:, 328:329]; P2 = R[:, 329:330]; R1 = R[:, 330:331]; R2 = R[:, 331:332]
            V1 = R[:, 332:333]; V2 = R[:, 333:334]; OF1 = R[:, 334:335]; OF2 = R[:, 335:336]
            PS1 = R[:, 336:337]; PS2 = R[:, 337:338]
            k.op(dve, "tensor_tensor", out=LG, in0=lg_ps[:, 0:36], in1=bR[:], op=ALU.add, rd=[B_lgps, B_bR, B_R], wr=[B_R])
            rop("reduce_max", out=GMAX, in_=GL, axis=AX.X)
            rop("tensor_scalar", out=GOH, in0=GL, scalar1=GMAX, scalar2=None, op0=ALU.is_equal)
            rop("tensor_scalar", out=NGMAX, in0=GMAX, scalar1=-1.0, scalar2=None, op0=ALU.mult)
            k.op(act, "activation", out=GEXP, in_=GL, func=AF.Exp, bias=NGMAX, scale=1.0, rd=[B_R], wr=[B_R])
            rop("reduce_sum", out=GSUM, in_=GEXP, axis=AX.X)
            rop("reciprocal", out=GVAL, in_=GSUM)
            rop("tensor_scalar", out=GPEN, in0=GOH, scalar1=-1.0, scalar2=1e30, op0=ALU.add, op1=ALU.mult)
            for gi in range(4):
                rop("tensor_scalar", out=EM[:, gi * 8:(gi + 1) * 8], in0=EL[:, gi * 8:(gi + 1) * 8],
                    scalar1=GPEN[:, gi:gi + 1], scalar2=None, op0=ALU.add)
            rop("reduce_max", out=T1, in_=EM, axis=AX.X)
            rop("tensor_scalar", out=OH1, in0=EM, scalar1=T1, scalar2=None, op0=ALU.is_equal)
            rop("scalar_tensor_tensor", out=E2, in0=OH1, scalar=-1e30, in1=EM, op0=ALU.mult, op1=ALU.add)
            rop("reduce_max", out=T2, in_=E2, axis=AX.X)
            rop("tensor_scalar", out=OH2, in0=E2, scalar1=T2, scalar2=None, op0=ALU.is_equal)
            rop("tensor_tensor", out=DD, in0=T2, in1=T1, op=ALU.subtract)
            k.op(act, "activation", out=ED, in_=DD, func=AF.Exp, rd=[B_R], wr=[B_R])
            rop("tensor_scalar", out=ED, in0=ED, scalar1=1.0, scalar2=None, op0=ALU.add)
            rop("reciprocal", out=ED, in_=ED)
            rop("tensor_tensor", out=W1, in0=GVAL, in1=ED, op=ALU.mult)
            rop("tensor_tensor", out=W2, in0=GVAL, in1=W1, op=ALU.subtract)
            rop("tensor_tensor", out=AA, in0=OH1, in1=OH2, op=ALU.add)
            k.op(pe, "matmul", rk_ps[:, 0:32], lhsT=U_strict, rhs=AA, start=True, stop=True, rd=[B_R, B_cst], wr=[B_rkps])
            k.op(pe, "matmul", rk_ps[:, 32:64], lhsT=ones_f, rhs=AA, start=True, stop=True, rd=[B_R, B_cst], wr=[B_rkps])
            k.op(dve, "tensor_tensor", out=RK, in0=rk_ps[:, 0:32], in1=cnt_run[:], op=ALU.add, rd=[B_rkps, B_cnt, B_R],
                 wr=[B_R])
            k.op(dve, "tensor_tensor", out=cnt_run[:], in0=rk_ps[:, 32:64], in1=cnt_run[:], op=ALU.add, rd=[B_rkps, B_cnt],
                 wr=[B_cnt])
            k.op(dve, "tensor_tensor", out=RKB, in0=RK, in1=eb_sb[:], op=ALU.add, rd=[B_R, B_eb], wr=[B_R])
            for (OH, P_, R_, V_, OF_, PS_, W_, col) in ((OH1, P1, R1, V1, OF1, PS1, W1, 0), (OH2, P2, R2, V2, OF2, PS2, W2, 1)):
                rop("tensor_tensor", out=TMP, in0=OH, in1=RKB, op=ALU.mult)
                rop("reduce_sum", out=P_, in_=TMP, axis=AX.X)
                rop("tensor_tensor", out=TMP, in0=OH, in1=RK, op=ALU.mult)
                rop("reduce_sum", out=R_, in_=TMP, axis=AX.X)
                rop("tensor_scalar", out=V_, in0=R_, scalar1=float(CAP), scalar2=None, op0=ALU.is_lt)
                rop("tensor_scalar", out=OF_, in0=V_, scalar1=-BIGI, scalar2=BIGI, op0=ALU.mult, op1=ALU.add)
                rop("tensor_tensor", out=PS_, in0=P_, in1=OF_, op=ALU.add)
                k.op(dve, "tensor_copy", out=psc[s][:, col:col + 1], in_=PS_, rd=[B_R], wr=[B_psc[s]])
                rop("tensor_tensor", out=P_, in0=P_, in1=V_, op=ALU.mult)
                k.op(dve, "tensor_copy", out=rt_pg[:, j, col:col + 1], in_=P_, rd=[B_R], wr=[B_rtpg])
                k.op(dve, "tensor_tensor", out=rt_w[:, j, col:col + 1], in0=W_, in1=V_, op=ALU.mult, rd=[B_R], wr=[B_rtw])
            for col in range(2):
                k.idma(Xs_s, bass.IndirectOffsetOnAxis(ap=psc[s][:, col:col + 1], axis=0), h2b[s][:, :], None, ds_sc[s],
                       NSLOT - 1, rd=[B_h2b[s], B_psc[s], B_Xs])
        k.barrier()
        stR.close()

        stE = contextlib.ExitStack()
        es.enter_context(stE)
        wg = [sb("wg%d" % i, [128, KC, DE], BF16, stE) for i in range(2)]
        wu = [sb("wu%d" % i, [128, KC, DE], BF16, stE) for i in range(2)]
        wd = [sb("wd%d" % i, [128, 4, D], BF16, stE) for i in range(2)]
        B_we = [Buf(), Buf()]
        ds_we = [k.dsem(), k.dsem()]
        NXS_E = 4
        xs = [sb("xs%d" % i, [128, D], BF16, stE) for i in range(NXS_E)]
        B_xs = [Buf() for _ in range(NXS_E)]
        ds_xs = [k.dsem() for _ in range(NXS_E)]
        xsT = [sb("xsT%d" % i, [128, KC, 128], BF16, stE) for i in range(2)]
        B_xsT = [Buf(), Buf()]
        sg = [sb("sg%d" % i, [128, DE], BF16, stE) for i in range(2)]
        hid = [sb("hid%d" % i, [128, DE], BF16, stE) for i in range(2)]
        hidT = [sb("hidT%d" % i, [128, 4, 128], BF16, stE) for i in range(2)]
        B_sg, B_hid, B_hidT = [Buf(), Buf()], [Buf(), Buf()], [Buf(), Buf()]
        y_sb = [sb("y_sb%d" % i, [128, D], F32, stE) for i in range(2)]
        B_ysb = [Buf(), Buf()]
        ds_y = [k.dsem(), k.dsem()]
        hT_ps = [ps("hT_pse%d" % i, [128, 1024], BF16, stE) for i in range(1)]
        B_hTps = [Buf()]
        g_ps = [ps("g_ps%d" % i, [128, 512], F32, stE) for i in range(2)]
        u_ps = [ps("u_ps%d" % i, [128, 512], F32, stE) for i in range(2)]
        B_gps, B_ups = [Buf(), Buf()], [Buf(), Buf()]
        ht_ps = ps("ht_ps", [128, 512], BF16, stE)
        B_htps = Buf()
        y_ps = [ps("y_ps%d" % i, [128, 512], F32, stE) for i in range(2)]
        B_yps = [Buf(), Buf()]
        cnt = {"hTps": 0, "y": 0}

        NSTG = 6
        stg = [sb("stg%d" % i, [128, 2048], F32, stE) for i in range(NSTG)]
        B_stg = [Buf() for _ in range(NSTG)]
        ds_stg = [k.dsem() for _ in range(NSTG)]
        cast_rr = [0]
        B_wch = [[Buf() for _ in range(12)] for _ in range(2)]

        def expert_chunks(e):
            s = e % 2
            wgv = w_gate[e].rearrange("(p kc) n -> p kc n", kc=KC)
            wuv = w_up[e].rearrange("(p kc) n -> p kc n", kc=KC)
            wdv = w_down[e].rearrange("(kc p) n -> p kc n", p=128)
            tasks = []
            for q in range(4):
                tasks.append((wgv[:, q * 4:(q + 1) * 4, :], wg[s][:, q * 4:(q + 1) * 4, :], "p (k n) -> p k n", 4))
            for q in range(4):
                tasks.append((wuv[:, q * 4:(q + 1) * 4, :], wu[s][:, q * 4:(q + 1) * 4, :], "p (k n) -> p k n", 4))
            for q in range(4):
                tasks.append((wdv[:, q, :], wd[s][:, q, :], None, 1))
            out_ = []
            for ci, (src, dst, rr, kk) in enumerate(tasks):
                def task(src=src, dst=dst, rr=rr, kk=kk, s=s, ci=ci):
                    i = cast_rr[0] % NSTG
                    c = cast_rr[0]
                    cast_rr[0] += 1
                    sview = stg[i][:].rearrange(rr, k=kk) if rr else stg[i][:]
                    k.dma(sp, sview, src, ds_stg[i], wr=[B_stg[i]])
                    eng = (dve, act)[c % 2]
                    k.op(eng, "copy" if eng is act else "tensor_copy", out=dst, in_=sview, rd=[B_stg[i]], wr=[B_wch[s][ci]])
                out_.append(task)
            return out_

        blocks = [(e, blk) for e in range(NE) for blk in range(NBLK)]
        NBK = len(blocks)

        def xs_load(i):
            e, blk = blocks[i]
            row0 = e * CAP + blk * 128
            sl = i % NXS_E
            k.dma(pool, xs[sl][:], Xs_s[row0:row0 + 128, :], ds_xs[sl], wr=[B_xs[sl]])

        def stA(i):
            sl = i % NXS_E
            tl = i % 2
            for hh in range(2):
                for i8 in range(8):
                    kc = hh * 8 + i8
                    k.op(pe, "transpose", out=hT_ps[0][:, i8 * 128:(i8 + 1) * 128], in_=xs[sl][:, kc:D:KC],
                         identity=ident_b[:], rd=[B_xs[sl], B_identb], wr=[B_hTps[0]])
                k.op(dve if hh else act, "tensor_copy" if hh else "copy", out=xsT[tl][:, hh * 8:(hh + 1) * 8, :],
                     in_=hT_ps[0][:].rearrange("p (k t) -> p k t", k=8), rd=[B_hTps[0]], wr=[B_xsT[tl]])

        def stB(i):
            e, blk = blocks[i]
            es_ = e % 2
            sl = i % 2
            for kc in range(KC):
                k.op(pe, "matmul", g_ps[sl][:], lhsT=xsT[sl][:, kc, :], rhs=wg[es_][:, kc, :], start=(kc == 0),
                     stop=(kc == KC - 1), rd=[B_xsT[sl]] + B_wch[es_], wr=[B_gps[sl]])
            for kc in range(KC):
                k.op(pe, "matmul", u_ps[sl][:], lhsT=xsT[sl][:, kc, :], rhs=wu[es_][:, kc, :], start=(kc == 0),
                     stop=(kc == KC - 1), rd=[B_xsT[sl]] + B_wch[es_], wr=[B_ups[sl]])
            k.op(act, "activation", out=sg[sl][:], in_=g_ps[sl][:], func=AF.Silu, rd=[B_gps[sl]], wr=[B_sg[sl]])
            k.op(dve, "tensor_tensor", out=hid[sl][:], in0=sg[sl][:], in1=u_ps[sl][:], op=ALU.mult,
                 rd=[B_sg[sl], B_ups[sl]], wr=[B_hid[sl]])

        def stC(i):
            e, blk = blocks[i]
            es_ = e % 2
            sl = i % 2
            row0 = e * CAP + blk * 128
            for c in range(4):
                k.op(pe, "transpose", out=ht_ps[:, c * 128:(c + 1) * 128], in_=hid[sl][:, c * 128:(c + 1) * 128],
                     identity=ident_b[:], rd=[B_hid[sl], B_identb], wr=[B_htps])
            k.op(act, "copy", out=hidT[sl][:], in_=ht_ps[:].rearrange("p (k t) -> p k t", k=4), rd=[B_htps],
                 wr=[B_hidT[sl]])

        def stC2(i):
            e, blk = blocks[i]
            es_ = e % 2
            sl = i % 2
            row0 = e * CAP + blk * 128
            for n in range(4):
                yp = n % 2
                for c in range(4):
                    k.op(pe, "matmul", y_ps[yp][:], lhsT=hidT[sl][:, c, :], rhs=wd[es_][:, c, n * 512:(n + 1) * 512],
                         start=(c == 0), stop=(c == 3), rd=[B_hidT[sl]] + B_wch[es_], wr=[B_yps[yp]])
                k.op(act if n % 2 else dve, "copy" if n % 2 else "tensor_copy", out=y_sb[sl][:, n * 512:(n + 1) * 512],
                     in_=y_ps[yp][:], rd=[B_yps[yp]], wr=[B_ysb[sl]])
            k.dma(act, Ys_s[row0:row0 + 128, :], y_sb[sl][:], ds_y[sl], rd=[B_ysb[sl]])

        for e0 in (0, 1):
            for t_ in expert_chunks(e0):
                t_()
        pending = []
        xs_load(0)
        xs_load(1)
        for i in range(NBK + 2):
            if i + 2 < NBK:
                xs_load(i + 2)
            if i >= 2:
                stC(i - 2)
            if i < NBK:
                stA(i)
            if 1 <= i <= NBK:
                stB(i - 1)
            if i >= 2:
                stC2(i - 2)
                e_done, blk_done = blocks[i - 2]
                if blk_done == NBLK - 1 and e_done + 2 < NE:
                    pending += expert_chunks(e_done + 2)
            for _ in range(3):
                if pending:
                    pending.pop(0)()
        assert not pending
        k.barrier()
        stE.close()

        stG = contextlib.ExitStack()
        es.enter_context(stG)
        GM = sb("GM", [128, D], F32, stG)
        FG = sb("FG", [128, D], F32, stG)
        B_GM, B_FG = Buf(), Buf()
        bcast_load(GM, B_GM, 5, ds0)
        vec_bcast_load(FG[:], B_FG, g_fin, ds0)
        k.op(dve, "tensor_scalar", out=FG[:], in0=FG[:], scalar1=SQD, scalar2=None, op0=ALU.mult, wr=[B_FG])
        g1 = [sb("g1_%d" % i, [128, D], F32, stG) for i in range(2)]
        g2 = [sb("g2_%d" % i, [128, D], F32, stG) for i in range(2)]
        x1t = [sb("x1f%d" % i, [128, D], F32, stG) for i in range(2)]
        B_g1, B_g2, B_x1t = [Buf(), Buf()], [Buf(), Buf()], [Buf(), Buf()]
        ds_g = [k.dsem(), k.dsem()]
        ds_x1 = [k.dsem(), k.dsem()]
        ds_o = [k.dsem(), k.dsem()]
        for j in range(NOWN):
            s = j % 2
            k.dma(sp, x1t[s][:], X1_s[j * 128:(j + 1) * 128, :], ds_x1[s], wr=[B_x1t[s]])
            k.idma(g1[s][:, :], None, Ys_s, bass.IndirectOffsetOnAxis(ap=rt_pg[:, j, 0:1], axis=0), ds_g[s], NSLOT - 1,
                   rd=[B_rtpg], wr=[B_g1[s]])
            k.idma(g2[s][:, :], None, Ys_s, bass.IndirectOffsetOnAxis(ap=rt_pg[:, j, 1:2], axis=0), ds_g[s], NSLOT - 1,
                   rd=[B_rtpg], wr=[B_g2[s]])
            k.op(dve, "tensor_scalar", out=g1[s][:], in0=g1[s][:], scalar1=rt_w[:, j, 0:1], scalar2=None, op0=ALU.mult,
                 rd=[B_rtw], wr=[B_g1[s]])
            k.op(dve, "scalar_tensor_tensor", out=g1[s][:], in0=g2[s][:], scalar=rt_w[:, j, 1:2], in1=g1[s][:],
                 op0=ALU.mult, op1=ALU.add, rd=[B_g2[s], B_rtw], wr=[B_g1[s]])
            k.op(pool, "tensor_tensor", out=g1[s][:], in0=g1[s][:], in1=GM[:], op=ALU.mult, rd=[B_GM], wr=[B_g1[s]])
            k.op(dve, "tensor_tensor", out=x1t[s][:], in0=x1t[s][:], in1=g1[s][:], op=ALU.add, rd=[B_g1[s]], wr=[B_x1t[s]])
            rf, Brf = rms_scale(x1t[s][:], B_x1t[s], D, D * EPS)
            k.op(dve, "scalar_tensor_tensor", out=g2[s][:], in0=x1t[s][:], scalar=rf, in1=FG[:], op0=ALU.mult,
                 op1=ALU.mult, rd=[B_x1t[s], Brf, B_FG], wr=[B_g2[s]])
            k.dma(sp, out[j * 128:(j + 1) * 128, :], g2[s][:], ds_o[s], rd=[B_g2[s]], wr=[])
        if dbg:
            k.dma(sp, d_pg, rt_pg[:].rearrange("p j c -> p (j c)"), ds0, rd=[B_rtpg])
            k.dma(sp, d_w, rt_w[:].rearrange("p j c -> p (j c)"), ds0, rd=[B_rtw])
        k.barrier()
        stG.close()
    return nc


def _consts(r, CAP):
    p = np.arange(128)
    ident = np.eye(128, dtype=np.float32)
    U_incl = (p[:, None] <= p[None, :]).astype(np.float32)
    U_strict = (p[:, None] < p[None, :]).astype(np.float32)
    ones = np.ones((128, 128), np.float32)
    cst = np.concatenate([ident, U_incl, U_strict, ones], axis=1)
    tri = (p[:, None] <= p[None, :]).astype(np.float32)
    fm = [np.ones((128, 128), np.float32) if u < r else (tri if u == r else np.zeros((128, 128), np.float32)) for u in range(4)]
    fmask = np.concatenate(fm, axis=1)
    slopes = (2.0 ** (-8.0 * np.arange(1, 17) / 16)).astype(np.float64)
    kk, qq = p[:, None].astype(np.float64), p[None, :].astype(np.float64)
    A = np.zeros((128, 3, 4, 4, 128), np.float32)
    for g in range(16):
        prev = np.exp(-slopes[g] * (qq + 128 - kk)) * (kk > qq)
        cur = np.exp(-slopes[g] * (qq - kk)) * (kk <= qq)
        A[:, 0, g // 4, g % 4, :] = prev if r != 0 else 0.0
        A[:, 1, g // 4, g % 4, :] = prev
        A[:, 2, g // 4, g % 4, :] = cur
    rsel = np.zeros((128, 4), np.float32)
    rsel[:, r] = 1.0
    ebase = np.tile((np.arange(NE) * CAP).astype(np.float32)[None, :], (128, 1))
    return dict(cst=cst, fmask=fmask, swaA=A.reshape(128, -1), rsel=rsel, ebase=ebase)


_CACHE = {}


def run(inputs, S, CAP, dbg=False):
    f = lambda a: np.ascontiguousarray(np.asarray(a, dtype=np.float32))
    x = f(inputs["x"])[:, :S]
    B = x.shape[0]
    NB = S // 128
    NOWN = NB // 4
    key = (S, CAP, dbg)
    if key not in _CACHE:
        _CACHE[key] = build(S, CAP, dbg)
    nc = _CACHE[key]
    shared = dict(
        w_ada=f(inputs["w_ada"][0]), b_ada=f(inputs["b_ada"][0]).reshape(1, -1), g_mix=f(inputs["norm_mix_g"][0]).reshape(1, -1),
        w_in=f(inputs["w_in"][0]), b_f=f(inputs["b_forget"][0]).reshape(1, -1), sinks=f(inputs["sinks"][0]).reshape(1, -1),
        g_out=np.concatenate([f(inputs["out_norm_swa_g"][0]), f(inputs["out_norm_fox_g"][0])]).reshape(1, -1),
        w_out=f(inputs["w_out"][0]), g_moe=f(inputs["norm_moe_g"][0]).reshape(1, -1),
        w_r=np.ascontiguousarray(np.concatenate([f(inputs["w_group"][0]), f(inputs["w_expert"][0])], axis=1)),
        b_r=np.concatenate([f(inputs["b_group"][0]), f(inputs["b_expert"][0])]).reshape(1, -1),
        w_gate=f(inputs["w_gate"][0]), w_up=f(inputs["w_up"][0]), w_down=f(inputs["w_down"][0]),
        g_fin=f(inputs["final_g"]).reshape(1, -1),
    )
    in_maps = []
    for core in range(8):
        b, r = core // 4, core % 4
        xb = x[b].reshape(NB, 128, D)
        own = [4 * j + r for j in range(NOWN)]
        x_own = np.ascontiguousarray(xb[own].reshape(-1, D))
        xp = np.zeros((NOWN, 128, D), np.float32)
        for j, t in enumerate(own):
            if t > 0:
                xp[j] = xb[t - 1]
        m = dict(shared)
        m.update(x_seq=np.ascontiguousarray(x[b]), x_own=x_own, x_prev=xp.reshape(-1, D),
                 cT=np.ascontiguousarray(f(inputs["c"])[b].reshape(KC, 128).T))
        m.update(_consts(r, CAP))
        in_maps.append(m)
    res = run_bass_kernel_spmd(nc, in_maps, core_ids=list(range(8)))
    if dbg:
        _CACHE["dbg"] = res
    outp = np.zeros((B, NB, 128, D), np.float32)
    for core in range(8):
        b, r = core // 4, core % 4
        o = np.asarray(res.results[core]["out"]).reshape(NOWN, 128, D)
        for j in range(NOWN):
            outp[b, 4 * j + r] = o[j]
    return outp.reshape(B, S, D)


def kernel(**inputs):
    return run(inputs, 8192, 512)
```

```python
import bisect
import contextlib
import numpy as np
import concourse.bass as bass
import concourse.mybir as mybir
from concourse.bass_utils import run_bass_kernel_spmd

F32 = mybir.dt.float32
BF16 = mybir.dt.bfloat16
I32 = mybir.dt.int32
AF = mybir.ActivationFunctionType
ALU = mybir.AluOpType
AX = mybir.AxisListType

D = 2048
KC = 16
EPS = 1e-6
NE = 32
DE = 512
QA0, KA0, VA0, QB0, KB0, VB0, FB0 = 0, 1024, 1280, 1536, 2560, 3584, 4608


class Eng:
    def __init__(self, nc, e, sem, name, is_pe=False):
        self.nc, self.e, self.sem, self.name, self.is_pe = nc, e, sem, name, is_pe
        self.idx = 0
        self.marks = []
        self.mark_idx = []
        self.count = 0
        self.last = None
        self.waited = {}

    def issue(self, ins, is_dma=False):
        self.idx += 1
        self.last = ins
        self.last_is_dma = is_dma
        return ("E", self, self.idx)

    def mark_now(self):
        if self.marks and self.marks[-1][0] == self.idx:
            return
        self.count += 1
        self.last.then_inc(self.sem, 1)
        self.marks.append((self.idx, self.count))
        self.mark_idx.append(self.idx)

    def resolve(self, idx):
        p = bisect.bisect_left(self.mark_idx, idx)
        if p < len(self.marks):
            return self.marks[p][1]
        assert self.idx >= idx
        if self.last_is_dma:
            if self.name == "act":
                self.issue(self.e.memzero(self.mark_tile))
            else:
                self.issue(self.e.memset(self.mark_tile, 0.0))
        self.mark_now()
        return self.count

    def wait(self, toks):
        for t in toks:
            if t is None:
                continue
            if t[0] == "E":
                eng, idx = t[1], t[2]
                if eng is self and self.is_pe:
                    continue
                val = eng.resolve(idx)
                sem = eng.sem
                key = eng.name
            else:
                _, ds, val = t
                sem = ds.sem
                key = ds.name
            if self.waited.get(key, 0) >= val:
                continue
            self.e.wait_ge(sem, val)
            self.waited[key] = val


class DSem:
    def __init__(self, sem, name):
        self.sem, self.name, self.n = sem, name, 0


class Buf:
    __slots__ = ("w", "r")

    def __init__(self):
        self.w = None
        self.r = {}


class K:
    def __init__(self, nc, es):
        self.nc, self.es = nc, es
        self.dsems = []
        mk = lambda e, n, pe=False: Eng(nc, e, es.enter_context(nc.semaphore("sem_" + n)), n, pe)
        self.pe = mk(nc.tensor, "pe", True)
        self.act = mk(nc.scalar, "act")
        self.dve = mk(nc.vector, "dve")
        self.pool = mk(nc.gpsimd, "pool")
        self.sp = mk(nc.sync, "sp")
        self.engs = [self.pe, self.act, self.dve, self.pool, self.sp]
        self.nds = 0

    def dsem(self, es=None):
        es = es or self.es
        self.nds += 1
        name = "ds%d" % self.nds
        d = DSem(es.enter_context(self.nc.semaphore(name)), name)
        self.dsems.append(d)
        return d

    def _deps(self, eng, rd, wr):
        deps = []
        for b in rd:
            if b.w is not None:
                deps.append(b.w)
        for b in wr:
            if b.w is not None:
                deps.append(b.w)
            for k, t in b.r.items():
                if t[0] == "E" and t[1] is eng:
                    continue
                deps.append(t)
        return deps

    def _upd(self, tok, key, rd, wr):
        for b in rd:
            b.r[key] = tok
        for b in wr:
            b.w = tok
            b.r = {}

    def op(self, eng, method, *a, rd=(), wr=(), **kw):
        eng.wait(self._deps(eng, rd, wr))
        ins = getattr(eng.e, method)(*a, **kw)
        tok = eng.issue(ins)
        if (not eng.is_pe) or kw.get("stop") or method == "transpose":
            eng.mark_now()
        self._upd(tok, eng.name, rd, wr)
        return tok

    def dma(self, q, out, in_, ds, rd=(), wr=(), **kw):
        q.wait(self._deps(q, rd, wr))
        ins = q.e.dma_start(out=out, in_=in_, **kw)
        q.issue(ins, True)
        ds.n += 16
        ins.then_inc(ds.sem, 16)
        tok = ("D", ds, ds.n)
        self._upd(tok, ds.name, rd, wr)
        return tok

    def idma(self, out, out_off, in_, in_off, ds, bound, rd=(), wr=()):
        q = self.pool
        q.wait(self._deps(q, rd, wr))
        if not hasattr(self, "_bound_reg"):
            self._bound_reg = {}
        if bound not in self._bound_reg:
            self._bound_reg[bound] = q.e.to_reg(bound)
        ins = q.e.indirect_dma_start(out=out, out_offset=out_off, in_=in_, in_offset=in_off,
                                     bounds_check=self._bound_reg[bound], oob_is_err=False)
        q.issue(ins, True)
        ds.n += 16
        ins.then_inc(ds.sem, 16)
        tok = ("D", ds, ds.n)
        self._upd(tok, ds.name, rd, wr)
        return tok

    def barrier(self):
        toks = []
        for e in self.engs:
            if e.idx > 0 and e is not self.sp:
                toks.append(("E", e, e.idx))
        for d in self.dsems:
            if d.n > 0:
                toks.append(("D", d, d.n))
        for e in self.engs:
            e.wait([t for t in toks if not (t[0] == "E" and t[1] is e)])


def build(S, CAP, dbg=False):
    NB = S // 128
    NOWN = NB // 4
    NG = NB // 4
    TOWN = NOWN * 128
    NSLOT = NE * CAP
    NBLK = CAP // 128
    SQD = float(np.sqrt(D))

    nc = bass.Bass("TRN2", target_bir_lowering=False)

    def inp(name, shape, dt=F32):
        return nc.dram_tensor(name, shape, dt, kind="ExternalInput").ap()

    def scr(name, shape, dt):
        return nc.dram_tensor(name, shape, dt, kind="Internal").ap()

    x_seq = inp("x_seq", [S, D]); x_own = inp("x_own", [TOWN, D]); x_prev = inp("x_prev", [TOWN, D])
    cT = inp("cT", [128, KC]); w_ada = inp("w_ada", [D, 6 * D]); b_ada = inp("b_ada", [1, 6 * D])
    g_mix = inp("g_mix", [1, D]); w_in = inp("w_in", [D, 4624]); b_f = inp("b_f", [1, 16]); sinks = inp("sinks", [1, 16])
    g_out = inp("g_out", [1, D]); w_out = inp("w_out", [D, D]); g_moe = inp("g_moe", [1, D])
    w_r = inp("w_r", [D, 36]); b_r = inp("b_r", [1, 36])
    w_gate = inp("w_gate", [NE, D, DE]); w_up = inp("w_up", [NE, D, DE]); w_down = inp("w_down", [NE, DE, D])
    g_fin = inp("g_fin", [1, D])
    fmask = inp("fmask", [128, 512]); swaA = inp("swaA", [128, 3 * 4 * 512]); rsel = inp("rsel", [128, 4])
    cst = inp("cst", [128, 4 * 128]); ebase = inp("ebase", [128, NE])
    out = nc.dram_tensor("out", [TOWN, D], F32, kind="ExternalOutput").ap()

    mod_s = scr("mod_s", [1, 6 * D], F32)
    KT_s = scr("KT_s", [16 * 64, S], BF16)
    VB_s = scr("VB_s", [S, 1024], BF16)
    QT_s = scr("QT_s", [16 * 64, TOWN], BF16)
    QAUG_s = scr("QAUG_s", [NOWN * 16, 3, 128], BF16)
    X1_s = scr("X1_s", [TOWN, D], F32)
    if dbg:
        Xs_s = nc.dram_tensor("Xs_s", [NSLOT, D], BF16, kind="ExternalOutput").ap()
        Ys_s = nc.dram_tensor("Ys_s", [NSLOT, D], F32, kind="ExternalOutput").ap()
        d_pg = nc.dram_tensor("d_pg", [128, NOWN * 2], I32, kind="ExternalOutput").ap()
        d_w = nc.dram_tensor("d_w", [128, NOWN * 2], F32, kind="ExternalOutput").ap()
    else:
        Xs_s = scr("Xs_s", [NSLOT, D], BF16)
        Ys_s = scr("Ys_s", [NSLOT, D], F32)

    w_in_v = w_in.rearrange("(kc p) n -> p kc n", p=128)

    with contextlib.ExitStack() as es:
        k = K(nc, es)
        pe, act, dve, pool, sp = k.pe, k.act, k.dve, k.pool, k.sp
        mark_tile = es.enter_context(nc.sbuf_tensor("mark_tile", [128, 8], F32))
        pool.mark_tile = mark_tile[:, 0:4]
        act.mark_tile = mark_tile[:, 4:8]
        for e_ in k.engs:
            e_.last_is_dma = False

        def sb(name, shape, dt, st=None):
            return (st or es).enter_context(nc.sbuf_tensor(name, shape, dt))

        def ps(name, shape, dt, st):
            return st.enter_context(nc.psum_tensor(name, shape, dt))

        cst_f = sb("cst_f", [128, 512], F32)
        ident_b = sb("ident_b", [128, 128], BF16)
        ssq = sb("ssq", [128, NB + 6 * NOWN + 8], F32)
        junk = sb("junk", [128, D], BF16)
        B_cst, B_identb, B_ssq, B_junk = Buf(), Buf(), Buf(), Buf()
        ds0 = k.dsem()
        k.dma(sp, cst_f[:], cst, ds0, wr=[B_cst])
        k.op(dve, "tensor_copy", out=ident_b[:], in_=cst_f[:, 0:128], rd=[B_cst], wr=[B_identb])
        k.op(pool, "memset", ssq[:], 0.0, wr=[B_ssq])
        ident_f = cst_f[:, 0:128]
        U_incl = cst_f[:, 128:256]
        U_strict = cst_f[:, 256:384]
        ones_f = cst_f[:, 384:512]
        ssq_ctr = [0]

        def new_ssq():
            i = ssq_ctr[0]
            ssq_ctr[0] += 1
            return ssq[:, i:i + 1]

        rstd_all = sb("rstd_all", [128, NB + 6 * NOWN + 8], F32)

        def rms_scale(src_ap, B_src, width, eps_scaled):
            col = new_ssq()
            i = ssq_ctr[0] - 1
            Bc = Buf()
            Bc.w = B_ssq.w
            k.op(act, "activation", out=junk[:, 0:width], in_=src_ap, func=AF.Square, accum_out=col,
                 rd=[B_src], wr=[Bc, B_junk])
            r = rstd_all[:, i:i + 1]
            Br = Buf()
            k.op(act, "activation", out=r, in_=col, func=AF.Sqrt, bias=float(eps_scaled), scale=1.0, rd=[Bc], wr=[Br])
            k.op(dve, "reciprocal", out=r, in_=r, rd=[Br], wr=[Br])
            return r, Br

        cumN = sb("cumN", [128, NB, 16], F32)
        B_cumN = Buf()
        stA = contextlib.ExitStack()
        es.enter_context(stA)
        stB = stA
        cT_sb = sb("cT_sb", [128, KC], F32, stA)
        sil = sb("sil", [128, KC], BF16, stA)
        B_cT, B_sil = Buf(), Buf()
        k.dma(sp, cT_sb[:], cT, ds0, wr=[B_cT])
        k.op(act, "activation", out=sil[:], in_=cT_sb[:], func=AF.Silu, rd=[B_cT], wr=[B_sil])
        wa = [sb("wa%d" % i, [128, KC, 512], BF16, stA) for i in range(2)]
        B_wa = [Buf(), Buf()]
        ds_wa = [k.dsem(), k.dsem()]
        brow = [sb("brow%d" % i, [1, 512], F32, stA) for i in range(2)]
        B_brow = [Buf(), Buf()]
        ds_br = [k.dsem(), k.dsem()]
        mrow = [sb("mrow%d" % i, [1, 512], F32, stA) for i in range(2)]
        B_mrow = [Buf(), Buf()]
        ds_mr = [k.dsem(), k.dsem()]
        B_mod = [Buf() for _ in range(24)]
        w_ada_v = w_ada.rearrange("(kc p) n -> p kc n", p=128)
        ada_state = {"loaded": 0, "done": 0}

        def ada_load(blk):
            s = blk % 2
            k.dma(pool, wa[s][:], w_ada_v[:, :, blk * 512:(blk + 1) * 512], ds_wa[s], wr=[B_wa[s]])
            k.dma(sp, brow[s][:], b_ada[0:1, blk * 512:(blk + 1) * 512], ds_br[s], wr=[B_brow[s]])

        def ada_block(blk, mod_ps, B_modps):
            s = blk % 2
            for kc in range(KC):
                k.op(pe, "matmul", mod_ps[0:1, :], lhsT=sil[:, kc:kc + 1], rhs=wa[s][:, kc, :], start=(kc == 0),
                     stop=(kc == KC - 1), rd=[B_sil, B_wa[s]], wr=[B_modps])
            if (blk // 4) in (1, 4):
                k.op(dve, "scalar_tensor_tensor", out=mrow[s][:], in0=mod_ps[0:1, :], scalar=1.0, in1=brow[s][:],
                     op0=ALU.add, op1=ALU.add, rd=[B_modps, B_brow[s]], wr=[B_mrow[s]])
            else:
                k.op(dve, "tensor_tensor", out=mrow[s][:], in0=mod_ps[0:1, :], in1=brow[s][:], op=ALU.add,
                     rd=[B_modps, B_brow[s]], wr=[B_mrow[s]])
            k.dma(sp, mod_s[0:1, blk * 512:(blk + 1) * 512], mrow[s][:], ds_mr[s], rd=[B_mrow[s]], wr=[B_mod[blk]])

        def ada_step(mod_ps, B_modps):
            b = ada_state["done"]
            if b >= 24:
                return
            while ada_state["loaded"] < min(24, b + 2):
                ada_load(ada_state["loaded"])
                ada_state["loaded"] += 1
            ada_block(b, mod_ps, B_modps)
            ada_state["done"] += 1

        def bcast_load(dst, B_dst, chunk, ds):
            k.dma(sp, dst[:], mod_s[0:1, chunk * D:(chunk + 1) * D].partition_broadcast(128), ds,
                  rd=[B_mod[chunk * 4 + i] for i in range(4)], wr=[B_dst])

        def vec_bcast_load(dst, B_dst, src, ds, n=D):
            k.dma(sp, dst, src[0:1, 0:n].partition_broadcast(128), ds, wr=[B_dst])

        G1s = sb("G1s", [128, D], F32, stB)
        SHa = sb("SHa", [128, D], F32, stB)
        B_G1s, B_SHa = Buf(), Buf()
        mod_ps = ps("mod_ps", [128, 512], F32, stB)
        B_modps = Buf()
        for _ in range(8):
            ada_step(mod_ps, B_modps)
        B_tmpg = Buf()
        bcast_load(G1s, B_G1s, 1, ds0)
        bcast_load(SHa, B_SHa, 0, ds0)

        Wkb = sb("Wkb", [128, KC, 1024], BF16, stB)
        Wvb = sb("Wvb", [128, KC, 1024], BF16, stB)
        Wf = sb("Wf", [128, KC, 16], BF16, stB)
        B_Wkb, B_Wvb, B_Wf = Buf(), Buf(), Buf()
        ds_w = k.dsem()
        for kc in range(KC):
            k.dma(pool, Wkb[:, kc, :], w_in_v[:, kc, KB0:KB0 + 1024], ds_w, wr=[B_Wkb])
        for kc in range(KC):
            k.dma(pool, Wvb[:, kc, :], w_in_v[:, kc, VB0:VB0 + 1024], ds_w, wr=[B_Wvb])
        k.dma(pool, Wf[:], w_in_v[:, :, FB0:FB0 + 16], ds_w, wr=[B_Wf])
        bF = sb("bF", [128, 16], F32, stB)
        B_bF = Buf()
        vec_bcast_load(bF[:], B_bF, b_f, ds0, 16)

        NXS = 2
        xts = [sb("xt%d" % i, [128, D], F32, stB) for i in range(NXS)]
        B_xt = [Buf() for _ in range(NXS)]
        ds_xt = [k.dsem() for _ in range(NXS)]
        vec_bcast_load(xts[0][:], B_xt[0], g_mix, ds0)
        k.op(dve, "scalar_tensor_tensor", out=G1s[:], in0=G1s[:], scalar=SQD, in1=xts[0][:], op0=ALU.mult, op1=ALU.mult,
             rd=[B_xt[0]], wr=[B_G1s])
        hb = [sb("hb%d" % i, [128, D], BF16, stB) for i in range(2)]
        B_hb = [Buf(), Buf()]
        hTg = [sb("hTg%d" % i, [128, KC, 512], BF16, stB) for i in range(2)]
        B_hTg = [[Buf() for _ in range(4)] for _ in range(2)]
        hT_ps = [ps("hT_ps%d" % i, [128, D], BF16, stB) for i in range(1)]
        B_hTps = [Buf()]
        NMM = 4
        mm_ps = [ps("mm_ps%d" % i, [128, 512], F32, stB) for i in range(NMM)]
        B_mm = [Buf() for _ in range(NMM)]
        f_ps = ps("f_ps", [128, 512], F32, stB)
        B_fps = [Buf(), Buf()]
        kT_sb = [sb("kT_sb%d" % i, [128, 512], BF16, stB) for i in range(2)]
        B_kTsb = [Buf(), Buf()]
        ds_kT = [k.dsem(), k.dsem()]
        v_sb = [sb("v_sb%d" % i, [128, 1024], BF16, stB) for i in range(2)]
        B_vsb = [Buf(), Buf()]
        ds_v = [k.dsem(), k.dsem()]
        zf = sb("zf", [128, NB, 16], F32, stB)
        B_zf = Buf()
        cnt = {"xt": 0, "hb": 0, "mm": 0, "kT": 0, "v": 0, "f": 0, "hTps": 0}

        def make_h(src_rows, Bx_extra_rd=()):
            s = cnt["xt"] % NXS
            cnt["xt"] += 1
            k.dma(sp, xts[s][:], src_rows, ds_xt[s], wr=[B_xt[s]])
            r, Br = rms_scale(xts[s][:], B_xt[s], D, D * EPS)
            k.op(dve, "scalar_tensor_tensor", out=xts[s][:], in0=xts[s][:], scalar=r, in1=G1s[:], op0=ALU.mult,
                 op1=ALU.mult, rd=[Br, B_G1s], wr=[B_xt[s]])
            hs = cnt["hb"] % 2
            cnt["hb"] += 1
            k.op(dve, "tensor_tensor", out=hb[hs][:], in0=xts[s][:], in1=SHa[:], op=ALU.add,
                 rd=[B_xt[s], B_SHa], wr=[B_hb[hs]])
            return hb[hs], B_hb[hs]

        def transpose_to(h_t, B_h, dst_ap3, B_dst, eng=None):
            s = cnt["hTps"] % len(hT_ps)
            cnt["hTps"] += 1
            for kc in range(KC):
                k.op(pe, "transpose", out=hT_ps[s][:, kc * 128:(kc + 1) * 128], in_=h_t[:, kc * 128:(kc + 1) * 128],
                     identity=ident_b[:], rd=[B_h, B_identb], wr=[B_hTps[s]])
            e = eng or act
            if e is act:
                k.op(act, "copy", out=dst_ap3, in_=hT_ps[s][:].rearrange("p (k t) -> p k t", k=KC),
                     rd=[B_hTps[s]], wr=[B_dst])
            else:
                k.op(e, "tensor_copy", out=dst_ap3, in_=hT_ps[s][:].rearrange("p (k t) -> p k t", k=KC),
                     rd=[B_hTps[s]], wr=[B_dst])

        for i in range(4):
            h_t, B_h = make_h(x_seq[i * 128:(i + 1) * 128, :])
            transpose_to(h_t, B_h, hTg[0][:, :, i * 128:(i + 1) * 128], B_hTg[0][i])
        for g in range(NG):
            gs = g % 2
            hq = None
            for c in range(8):
                ms = cnt["mm"] % NMM
                cnt["mm"] += 1
                for kc in range(KC):
                    k.op(pe, "matmul", mm_ps[ms][:], lhsT=Wkb[:, kc, c * 128:(c + 1) * 128], rhs=hTg[gs][:, kc, :],
                         start=(kc == 0), stop=(kc == KC - 1), rd=[B_Wkb] + B_hTg[gs], wr=[B_mm[ms]])
                ks = cnt["kT"] % 2
                cnt["kT"] += 1
                k.op(act if c % 2 else dve, "copy" if c % 2 else "tensor_copy", out=kT_sb[ks][:], in_=mm_ps[ms][:],
                     rd=[B_mm[ms]], wr=[B_kTsb[ks]])
                k.dma(pool, KT_s[c * 128:(c + 1) * 128, g * 512:(g + 1) * 512], kT_sb[ks][:], ds_kT[ks], rd=[B_kTsb[ks]])
                if g + 1 < NG:
                    i_n = c // 2
                    t_n = 4 * (g + 1) + i_n
                    if c % 2 == 0:
                        hq = make_h(x_seq[t_n * 128:(t_n + 1) * 128, :])
                    else:
                        transpose_to(hq[0], hq[1], hTg[1 - gs][:, :, i_n * 128:(i_n + 1) * 128], B_hTg[1 - gs][i_n])
            for i in range(4):
                t = 4 * g + i
                vs = cnt["v"] % 2
                cnt["v"] += 1
                for n in range(2):
                    ms = cnt["mm"] % NMM
                    cnt["mm"] += 1
                    for kc in range(KC):
                        k.op(pe, "matmul", mm_ps[ms][:], lhsT=hTg[gs][:, kc, i * 128:(i + 1) * 128],
                             rhs=Wvb[:, kc, n * 512:(n + 1) * 512], start=(kc == 0), stop=(kc == KC - 1),
                             rd=[B_Wvb, B_hTg[gs][i]], wr=[B_mm[ms]])
                    k.op(act, "copy", out=v_sb[vs][:, n * 512:(n + 1) * 512], in_=mm_ps[ms][:], rd=[B_mm[ms]],
                         wr=[B_vsb[vs]])
                k.dma(pool, VB_s[t * 128:(t + 1) * 128, :], v_sb[vs][:], ds_v[vs], rd=[B_vsb[vs]])
                fs = cnt["f"] % 2
                cnt["f"] += 1
                for kc in range(KC):
                    k.op(pe, "matmul", f_ps[:, fs * 16:(fs + 1) * 16], lhsT=hTg[gs][:, kc, i * 128:(i + 1) * 128],
                         rhs=Wf[:, kc, :], start=(kc == 0), stop=(kc == KC - 1), rd=[B_Wf, B_hTg[gs][i]],
                         wr=[B_fps[fs]])
                k.op(dve, "tensor_tensor", out=zf[:, t, :], in0=f_ps[:, fs * 16:(fs + 1) * 16], in1=bF[:], op=ALU.add,
                     rd=[B_fps[fs], B_bF], wr=[B_zf])
            ada_step(mod_ps, B_modps)
        while ada_state["done"] < 24:
            ada_step(mod_ps, B_modps)

        NC16 = NB * 16
        zf2 = zf[:].rearrange("p t h -> p (t h)")
        k.op(act, "activation", out=zf2, in_=zf2, func=AF.Exp, scale=-1.0, rd=[B_zf], wr=[B_zf])
        k.op(act, "activation", out=zf2, in_=zf2, func=AF.Ln, bias=1.0, scale=1.0, rd=[B_zf], wr=[B_zf])
        cumN2 = cumN[:].rearrange("p t h -> p (t h)")
        class _V:
            def __init__(self, t):
                self.t = t
            def __getitem__(self, idx):
                return self.t[:, 0:NB * 16].rearrange("p (t h) -> p t h", h=16)[idx]
        pfx = [_V(xts[i]) for i in range(2)]
        B_pfx = [B_xt[0], B_xt[1]]
        for c0 in range(0, NC16, 512):
            c1 = min(NC16, c0 + 512)
            w = c1 - c0
            k.op(pe, "matmul", mm_ps[0][:, 0:w], lhsT=U_incl, rhs=zf2[:, c0:c1], start=True, stop=True,
                 rd=[B_zf, B_cst], wr=[B_mm[0]])
            k.op(pe, "matmul", mm_ps[1][:, 0:w], lhsT=ones_f, rhs=zf2[:, c0:c1], start=True, stop=True,
                 rd=[B_zf, B_cst], wr=[B_mm[1]])
            k.op(dve, "tensor_copy", out=cumN2[:, c0:c1], in_=mm_ps[0][:, 0:w], rd=[B_mm[0]], wr=[B_cumN])
            k.op(dve, "tensor_copy", out=pfx[0][:].rearrange("p t h -> p (t h)")[:, c0:c1], in_=mm_ps[1][:, 0:w],
                 rd=[B_mm[1]], wr=[B_pfx[0]])
        cur = 0
        sh = 1
        while sh < NB:
            nx = 1 - cur
            k.op(dve, "tensor_copy", out=pfx[nx][:, 0:sh, :], in_=pfx[cur][:, 0:sh, :], rd=[B_pfx[cur]], wr=[B_pfx[nx]])
            k.op(dve, "tensor_tensor", out=pfx[nx][:, sh:NB, :], in0=pfx[cur][:, sh:NB, :], in1=pfx[cur][:, 0:NB - sh, :],
                 op=ALU.add, rd=[B_pfx[cur]], wr=[B_pfx[nx]])
            cur = nx
            sh *= 2
        k.op(dve, "tensor_tensor", out=cumN[:, 1:NB, :], in0=cumN[:, 1:NB, :], in1=pfx[cur][:, 0:NB - 1, :], op=ALU.add,
             rd=[B_pfx[cur]], wr=[B_cumN])
        rsel_sb = sb("rsel_sb", [128, 4], F32, stB)
        B_rsel = Buf()
        k.dma(sp, rsel_sb[:], rsel, ds0, wr=[B_rsel])
        cq = sb("cq", [128, NOWN, 16], F32, stB)
        B_cq = Buf()
        cumN4 = cumN[:].rearrange("p (j u) h -> p j u h", u=4)
        k.op(dve, "tensor_scalar", out=cq[:], in0=cumN4[:, :, 0, :], scalar1=rsel_sb[:, 0:1], scalar2=-8.0, op0=ALU.mult,
             op1=ALU.mult, rd=[B_cumN, B_rsel], wr=[B_cq])
        cq8 = sb("cq8", [128, NOWN, 16], F32, stB)
        for u in range(1, 4):
            k.op(dve, "tensor_scalar", out=cq8[:], in0=cumN4[:, :, u, :], scalar1=rsel_sb[:, u:u + 1], scalar2=-8.0,
                 op0=ALU.mult, op1=ALU.mult, rd=[B_cumN, B_rsel], wr=[B_tmpg])
            k.op(dve, "tensor_tensor", out=cq[:], in0=cq[:], in1=cq8[:], op=ALU.add, rd=[B_tmpg], wr=[B_cq])
        NQ = NOWN * 16
        cqf = cq[:].rearrange("p j h -> p (j h)")
        c3 = [sb("c3_%d" % i, [128, NQ], BF16, stB) for i in range(3)]
        B_c3 = [Buf() for _ in range(3)]
        for i in range(3):
            k.op(dve, "tensor_copy", out=c3[i][:], in_=cqf, rd=[B_cq], wr=[B_c3[i]])
            if i < 2:
                k.op(dve, "tensor_tensor", out=cqf, in0=cqf, in1=c3[i][:], op=ALU.subtract, rd=[B_c3[i]], wr=[B_cq])
        qa_sb = sb("qa_sb", [128, 3, 128], BF16, stB)
        B_qasb = Buf()
        ds_qa = k.dsem()
        for c0 in range(0, NQ, 128):
            w = min(128, NQ - c0)
            for i in range(3):
                k.op(pe, "transpose", out=hT_ps[0][0:w, i * 128:(i + 1) * 128], in_=c3[i][:, c0:c0 + w],
                     identity=ident_b[:], rd=[B_c3[i], B_identb], wr=[B_hTps[0]])
            k.op(dve, "tensor_copy", out=qa_sb[0:w, :, :], in_=hT_ps[0][0:w, 0:384].rearrange("p (k t) -> p k t", k=3),
                 rd=[B_hTps[0]], wr=[B_qasb])
            k.dma(sp, QAUG_s[c0:c0 + w, :, :], qa_sb[0:w, :, :], ds_qa, rd=[B_qasb])
        k.barrier()
        stB.close()

        stOA = contextlib.ExitStack()
        es.enter_context(stOA)
        o_a = sb("o_a", [128, NOWN, 1024], BF16, stOA)
        B_oa = [Buf() for _ in range(NOWN)]
        esink = sb("esink", [128, 16], F32, stOA)
        B_esink = Buf()
        vec_bcast_load(esink[:], B_esink, sinks, ds0, 16)
        k.op(act, "activation", out=esink[:], in_=esink[:], func=AF.Exp, rd=[B_esink], wr=[B_esink])

        stC = contextlib.ExitStack()
        es.enter_context(stC)
        G1s = sb("G1s_c", [128, D], F32, stC)
        SHa = sb("SHa_c", [128, D], F32, stC)
        B_G1s, B_SHa = Buf(), Buf()
        bcast_load(G1s, B_G1s, 1, ds0)
        bcast_load(SHa, B_SHa, 0, ds0)
        Wq = sb("Wq", [128, KC, 2048], BF16, stC)
        Wkv = sb("Wkv", [128, KC, 512], BF16, stC)
        B_Wq, B_Wkv = Buf(), Buf()
        for kc in range(KC):
            k.dma(pool, Wq[:, kc, 0:1024], w_in_v[:, kc, QA0:QA0 + 1024], ds_w, wr=[B_Wq])
            k.dma(pool, Wq[:, kc, 1024:2048], w_in_v[:, kc, QB0:QB0 + 1024], ds_w, wr=[B_Wq])
        k.dma(pool, Wkv[:], w_in_v[:, :, KA0:KA0 + 512], ds_w, wr=[B_Wkv])
        swaA_sb = sb("swaA_sb", [128, 3 * 4 * 512], BF16, stC)
        B_swaA = Buf()
        for i in range(6):
            k.dma(pool, swaA_sb[:, i * 1024:(i + 1) * 1024], swaA[:, i * 1024:(i + 1) * 1024], ds_w, wr=[B_swaA])
        NXS = 2
        xts = [sb("xtc%d" % i, [128, D], F32, stC) for i in range(NXS)]
        B_xt = [Buf() for _ in range(NXS)]
        vec_bcast_load(xts[0][:], B_xt[0], g_mix, ds0)
        k.op(dve, "scalar_tensor_tensor", out=G1s[:], in0=G1s[:], scalar=SQD, in1=xts[0][:], op0=ALU.mult, op1=ALU.mult,
             rd=[B_xt[0]], wr=[B_G1s])
        hb = [sb("hbc%d" % i, [128, D], BF16, stC) for i in range(2)]
        B_hb = [Buf(), Buf()]
        hT2 = [sb("hT2_%d" % i, [128, KC, 128], BF16, stC) for i in range(2)]
        B_hT2 = [Buf(), Buf()]
        hT_ps = [ps("hT_psc%d" % i, [128, D], BF16, stC) for i in range(1)]
        B_hTps = [Buf()]
        mm_ps = [ps("mm_psc%d" % i, [128, 512], F32, stC) for i in range(2)]
        B_mm = [Buf(), Buf()]
        s_ps = ps("s_psc", [128, 512], F32, stC)
        B_sps = Buf()
        o_ps = ps("o_psc", [128, 512], F32, stC)
        B_ops = Buf()
        tr_ps = ps("tr_psc", [128, 512], F32, stC)
        B_trps = Buf()
        q_tok = sb("q_tok", [128, 2048], BF16, stC)
        B_qtok = Buf()
        kv_tok = [sb("kv_tok%d" % i, [128, 512], BF16, stC) for i in range(2)]
        B_kvtok = [Buf(), Buf()]
        qaT = sb("qaT", [64, 16, 128], BF16, stC)
        qbT = sb("qbT", [64, 16, 128], BF16, stC)
        kaT = sb("kaT", [64, 2, 4, 128], BF16, stC)
        va = sb("va", [128, 2, 4, 65], BF16, stC)
        B_qaT, B_qbT, B_kaT, B_va = Buf(), Buf(), Buf(), Buf()
        k.op(pool, "memset", va[:], 1.0, wr=[B_va])
        pT = [sb("pTc%d" % i, [128, 512], BF16, stC) for i in range(2)]
        B_pT = [Buf(), Buf()]
        oT_sb2 = [sb("oT_sbc%d" % i, [65, 512], F32, stC) for i in range(2)]
        B_oTsb2 = [Buf(), Buf()]
        den = sb("denc", [128, 8], F32, stC)
        B_den = Buf()
        ds_qb = k.dsem()
        cnt = {"xt": 0, "hb": 0, "mm": 0, "hTps": 0, "pT": 0}
        QT_v = QT_s.rearrange("(h d) t -> d h t", d=64)

        for j in range(NOWN):
            for which, src in ((0, x_own), (1, x_prev)):
                h_t, B_h = make_h(src[j * 128:(j + 1) * 128, :])
                transpose_to(h_t, B_h, hT2[which][:], B_hT2[which])
            for n in range(4):
                ms = cnt["mm"] % 2
                cnt["mm"] += 1
                for kc in range(KC):
                    k.op(pe, "matmul", mm_ps[ms][:], lhsT=hT2[0][:, kc, :], rhs=Wq[:, kc, n * 512:(n + 1) * 512],
                         start=(kc == 0), stop=(kc == KC - 1), rd=[B_Wq, B_hT2[0]], wr=[B_mm[ms]])
                k.op(dve if n % 2 else act, "tensor_copy" if n % 2 else "copy", out=q_tok[:, n * 512:(n + 1) * 512],
                     in_=mm_ps[ms][:], rd=[B_mm[ms]], wr=[B_qtok])
            for which in range(2):
                ms = cnt["mm"] % 2
                cnt["mm"] += 1
                for kc in range(KC):
                    k.op(pe, "matmul", mm_ps[ms][:], lhsT=hT2[which][:, kc, :], rhs=Wkv[:, kc, :],
                         start=(kc == 0), stop=(kc == KC - 1), rd=[B_Wkv, B_hT2[which]], wr=[B_mm[ms]])
                k.op(dve, "tensor_copy", out=kv_tok[which][:], in_=mm_ps[ms][:], rd=[B_mm[ms]], wr=[B_kvtok[which]])
                kb = 1 - which
                k.op(dve, "tensor_copy", out=va[:, kb, :, 0:64],
                     in_=kv_tok[which][:, 256:512].rearrange("p (h d) -> p h d", h=4), rd=[B_kvtok[which]], wr=[B_va])
            for half, dstT, B_dst in ((0, qaT, B_qaT), (1, qbT, B_qbT)):
                for hh in range(2):
                    s = cnt["hTps"] % len(hT_ps)
                    cnt["hTps"] += 1
                    for i8 in range(8):
                        g_ = hh * 8 + i8
                        c0 = half * 1024 + g_ * 64
                        k.op(pe, "transpose", out=hT_ps[s][0:64, i8 * 128:(i8 + 1) * 128], in_=q_tok[:, c0:c0 + 64],
                             identity=ident_b[:], rd=[B_qtok, B_identb], wr=[B_hTps[s]])
                    k.op(act if hh else dve, "copy" if hh else "tensor_copy", out=dstT[:, hh * 8:(hh + 1) * 8, :],
                         in_=hT_ps[s][0:64, 0:1024].rearrange("p (h t) -> p h t", h=8), rd=[B_hTps[s]], wr=[B_dst])
            k.dma(sp, QT_v[:, :, j * 128:(j + 1) * 128], qbT[:], ds_qb, rd=[B_qbT])
            s = cnt["hTps"] % len(hT_ps)
            cnt["hTps"] += 1
            for which in range(2):
                kb = 1 - which
                for hk in range(4):
                    k.op(pe, "transpose", out=hT_ps[s][0:64, (kb * 4 + hk) * 128:(kb * 4 + hk + 1) * 128],
                         in_=kv_tok[which][:, hk * 64:(hk + 1) * 64], identity=ident_b[:],
                         rd=[B_kvtok[which], B_identb], wr=[B_hTps[s]])
            k.op(dve, "tensor_copy", out=kaT[:].rearrange("p a h t -> p (a h) t"),
                 in_=hT_ps[s][0:64, 0:1024].rearrange("p (h t) -> p h t", h=8), rd=[B_hTps[s]], wr=[B_kaT])
            units = [(hk_, kb_) for hk_ in range(4) for kb_ in range(2)]
            s_bufs = [(s_ps, B_sps), (mm_ps[1], B_mm[1])]
            o_bufs = [(o_ps, B_ops), (mm_ps[0], B_mm[0])]

            def swa_S(u):
                hk, kb = units[u]
                sb_, Bsb = s_bufs[u % 2]
                for i in range(4):
                    k.op(pe, "matmul", sb_[:, i * 128:(i + 1) * 128], lhsT=kaT[:, kb, hk, :],
                         rhs=qaT[:, hk * 4 + i, :], start=True, stop=True, rd=[B_kaT, B_qaT], wr=[Bsb])
                p_ = u % 2
                k.op(act, "activation", out=pT[p_][:], in_=sb_[:], func=AF.Exp, scale=0.125, rd=[Bsb], wr=[B_pT[p_]])
                tab = (0 if j == 0 else 1) if kb == 0 else 2
                a0 = (tab * 4 + hk) * 512
                k.op(dve, "tensor_tensor", out=pT[p_][:], in0=pT[p_][:], in1=swaA_sb[:, a0:a0 + 512], op=ALU.mult,
                     rd=[B_swaA], wr=[B_pT[p_]])

            def swa_PV(u):
                hk, kb = units[u]
                ob, Bob = o_bufs[hk % 2]
                k.op(pe, "matmul", ob[0:65, :], lhsT=va[:, kb, hk, :], rhs=pT[u % 2][:], start=(kb == 0),
                     stop=(kb == 1), rd=[B_va, B_pT[u % 2]], wr=[Bob])
                if kb == 1:
                    k.op(act, "copy", out=oT_sb2[hk % 2][:], in_=ob[0:65, :], rd=[Bob], wr=[B_oTsb2[hk % 2]])

            def swa_tail(hk):
                osb, Bosb = oT_sb2[hk % 2], B_oTsb2[hk % 2]
                for i in range(4):
                    k.op(pe, "transpose", out=tr_ps[:, i * 65:(i + 1) * 65], in_=osb[:, i * 128:(i + 1) * 128],
                         identity=ident_f[0:65, 0:65], rd=[Bosb, B_cst], wr=[B_trps])
                tr3 = tr_ps[:, 0:260].rearrange("p (h c) -> p h c", h=4)
                k.op(dve, "tensor_tensor", out=den[:, 0:4], in0=tr3[:, :, 64], in1=esink[:, hk * 4:(hk + 1) * 4],
                     op=ALU.add, rd=[B_trps, B_esink], wr=[B_den])
                k.op(dve, "reciprocal", out=den[:, 4:8], in_=den[:, 0:4], rd=[B_den], wr=[B_den])
                for i in range(4):
                    g_ = hk * 4 + i
                    k.op(dve, "tensor_scalar", out=o_a[:, j, g_ * 64:(g_ + 1) * 64], in0=tr3[:, i, 0:64],
                         scalar1=den[:, 4 + i:5 + i], scalar2=None, op0=ALU.mult, rd=[B_trps, B_den], wr=[B_oa[j]])

            swa_S(0)
            tail_q = []
            for u in range(8):
                if u + 1 < 8:
                    swa_S(u + 1)
                swa_PV(u)
                if tail_q:
                    swa_tail(tail_q.pop(0))
                if units[u][1] == 1:
                    tail_q.append(units[u][0])
            while tail_q:
                swa_tail(tail_q.pop(0))
        k.barrier()
        stC.close()

        stOB = contextlib.ExitStack()
        es.enter_context(stOB)
        o_b = sb("o_b", [128, NOWN, 1024], BF16, stOB)
        B_ob = [Buf() for _ in range(NOWN)]
        stW = contextlib.ExitStack()
        es.enter_context(stW)
        Wout = sb("Wout", [128, KC, D], BF16, stW)
        B_Wout = Buf()
        w_out_v = w_out.rearrange("(kc p) n -> p kc n", p=128)
        for kc in range(KC):
            for hh in range(2):
                k.dma(pool, Wout[:, kc, hh * 1024:(hh + 1) * 1024], w_out_v[:, kc, hh * 1024:(hh + 1) * 1024], ds_w,
                      wr=[B_Wout])
        stF = contextlib.ExitStack()
        es.enter_context(stF)
        kTa = [sb("kTa%d" % i, [67, S], BF16, stF) for i in range(2)]
        vau = [sb("vau%d" % i, [128, NB, 65], BF16, stF) for i in range(2)]
        qTa = [sb("qTa%d" % i, [67, TOWN], BF16, stF) for i in range(2)]
        B_kTa, B_vau, B_qTa = [Buf(), Buf()], [Buf(), Buf()], [Buf(), Buf()]
        ds_hd = [k.dsem(), k.dsem()]
        for i in range(2):
            k.op(pool, "memset", kTa[i][64:67, :], 1.0, wr=[B_kTa[i]])
            k.op(pool, "memset", vau[i][:], 1.0, wr=[B_vau[i]])
        fm_sb = sb("fm_sb", [128, 512], BF16, stF)
        B_fm = Buf()
        k.dma(pool, fm_sb[:], fmask, ds_w, wr=[B_fm])
        NPT = 3
        pT = [sb("pTf%d" % i, [128, 1024], BF16, stF) for i in range(NPT)]
        B_pT = [Buf() for _ in range(NPT)]
        NSP = 3
        s_ps = [ps("s_psf%d" % i, [128, 1024], F32, stF) for i in range(NSP)]
        B_sps = [Buf() for _ in range(NSP)]
        o_ps = ps("o_psf", [128, 1024], F32, stF)
        B_ops = Buf()
        tr_ps = [s_ps[0][:, 0:512], s_ps[0][:, 512:1024]]
        B_trps = [B_sps[0], B_sps[0]]
        oT_sb = sb("oT_sbf", [65, 1024], F32, stF)
        B_oTsb = Buf()
        den = sb("denf", [128, 8], F32, stF)
        B_den = Buf()
        VB_v = VB_s.rearrange("(t p) c -> p t c", p=128)
        QAUG_v = QAUG_s.rearrange("(j h) k t -> h k j t", h=16)
        HB = (NOWN + 1) // 2
        halves = [(0, HB), (HB, NOWN)] if NOWN > 1 else [(0, 1)]
        cnt = {"pT": 0, "s": 0, "tr": 0}

        def load_head(h):
            s = h % 2
            k.dma(sp, kTa[s][0:64, :], KT_s[h * 64:(h + 1) * 64, :], ds_hd[s], wr=[B_kTa[s]])
            k.dma(sp, qTa[s][0:64, :], QT_s[h * 64:(h + 1) * 64, :], ds_hd[s], wr=[B_qTa[s]])
            k.dma(sp, qTa[s][64:67, :].rearrange("k (j t) -> k j t", t=128), QAUG_v[h], ds_hd[s], wr=[B_qTa[s]])
            for t0 in range(0, NB, 16):
                t1 = min(NB, t0 + 16)
                k.dma(sp, vau[s][:, t0:t1, 0:64], VB_v[:, t0:t1, h * 64:(h + 1) * 64], ds_hd[s], wr=[B_vau[s]])

        load_head(0)
        for h in range(16):
            hs = h % 2
            if h + 1 < 16:
                load_head(h + 1)
            for (j0, j1) in halves:
                nb = j1 - j0
                def stage1(kt):
                    g = kt // 4
                    u = kt % 4
                    ja = max(g, j0)
                    c_lo = (ja - j0) * 128
                    c_hi = nb * 128
                    ss = cnt["s"] % NSP
                    cnt["s"] += 1
                    for b0 in range(0, 1024, 512):
                        lo, hi = max(c_lo, b0), min(c_hi, b0 + 512)
                        if lo >= hi:
                            continue
                        k.op(pe, "matmul", s_ps[ss][:, lo:hi], lhsT=kTa[hs][:, kt * 128:(kt + 1) * 128],
                             rhs=qTa[hs][:, j0 * 128 + lo:j0 * 128 + hi], start=True, stop=True,
                             rd=[B_kTa[hs], B_qTa[hs]], wr=[B_sps[ss]])
                    p_ = cnt["pT"] % NPT
                    cnt["pT"] += 1
                    k.op(act, "activation", out=pT[p_][:, c_lo:c_hi], in_=s_ps[ss][:, c_lo:c_hi], func=AF.Exp,
                         bias=cumN[:, kt, h:h + 1], scale=0.125, rd=[B_sps[ss], B_cumN], wr=[B_pT[p_]])
                    if g >= j0:
                        k.op(dve, "scalar_tensor_tensor", out=pT[p_][:, c_lo:c_lo + 128], in0=pT[p_][:, c_lo:c_lo + 128],
                             scalar=1e30, in1=fm_sb[:, u * 128:(u + 1) * 128], op0=ALU.min, op1=ALU.mult,
                             rd=[B_fm], wr=[B_pT[p_]])
                    return p_

                def stage2(kt, p_):
                    g = kt // 4
                    u = kt % 4
                    ja = max(g, j0)
                    c_lo = (ja - j0) * 128
                    c_hi = nb * 128
                    started = set()

                    def st_flag(lo_):
                        bank = lo_ // 512
                        if kt == 0 and bank not in started:
                            started.add(bank)
                            return True
                        return False
                    if g >= j0:
                        k.op(pe, "matmul", o_ps[0:65, c_lo:c_lo + 128], lhsT=vau[hs][:, kt, :],
                             rhs=pT[p_][:, c_lo:c_lo + 128], start=st_flag(c_lo), stop=(u == 3), skip_group_check=True,
                             rd=[B_vau[hs], B_pT[p_]], wr=[B_ops])
                        r_lo = c_lo + 128
                    else:
                        r_lo = c_lo
                    for b0 in range(0, 1024, 512):
                        lo, hi = max(r_lo, b0), min(c_hi, b0 + 512)
                        if lo >= hi:
                            continue
                        k.op(pe, "matmul", o_ps[0:65, lo:hi], lhsT=vau[hs][:, kt, :], rhs=pT[p_][:, lo:hi],
                             start=st_flag(lo), stop=False, skip_group_check=True, rd=[B_vau[hs], B_pT[p_]], wr=[B_ops])

                nkt = 4 * j1
                pq = []
                for kt in range(nkt + 2):
                    if kt < nkt:
                        pq.append((kt, stage1(kt)))
                    if kt >= 2:
                        k0, p0 = pq.pop(0)
                        stage2(k0, p0)
                assert not pq
                for b0 in range(0, nb * 128, 512):
                    b1 = min(nb * 128, b0 + 512)
                    k.op(act, "copy", out=oT_sb[:, b0:b1], in_=o_ps[0:65, b0:b1], rd=[B_ops], wr=[B_oTsb])
                for q0 in range(0, nb, 4):
                    q1 = min(nb, q0 + 4)
                    ts_ = cnt["tr"] % 2
                    cnt["tr"] += 1
                    for i in range(q1 - q0):
                        k.op(pe, "transpose", out=tr_ps[ts_][:, i * 65:(i + 1) * 65],
                             in_=oT_sb[:, (q0 + i) * 128:(q0 + i + 1) * 128], identity=ident_f[0:65, 0:65],
                             rd=[B_oTsb, B_cst], wr=[B_trps[ts_]])
                    tr3 = tr_ps[ts_][:, 0:260].rearrange("p (h c) -> p h c", h=4)
                    k.op(dve, "reciprocal", out=den[:, 0:q1 - q0], in_=tr3[:, 0:q1 - q0, 64], rd=[B_trps[ts_]], wr=[B_den])
                    for i in range(q1 - q0):
                        jj = j0 + q0 + i
                        k.op(dve, "tensor_scalar", out=o_b[:, jj, h * 64:(h + 1) * 64], in0=tr3[:, i, 0:64],
                             scalar1=den[:, i:i + 1], scalar2=None, op0=ALU.mult, rd=[B_trps[ts_], B_den], wr=[B_ob[jj]])
        k.barrier()
        stF.close()

        stD = contextlib.ExitStack()
        es.enter_context(stD)
        gout = sb("gout", [128, D], F32, stD)
        GA = sb("GA", [128, D], F32, stD)
        B_gout, B_GA = Buf(), Buf()
        vec_bcast_load(gout[:], B_gout, g_out, ds0)
        k.op(dve, "tensor_scalar", out=gout[:], in0=gout[:], scalar1=32.0, scalar2=None, op0=ALU.mult, wr=[B_gout])
        bcast_load(GA, B_GA, 2, ds0)
        xts = [sb("xtd%d" % i, [128, D], F32, stD) for i in range(2)]
        B_xt = [Buf(), Buf()]
        ds_xt = [k.dsem(), k.dsem()]
        x1t = [sb("x1t%d" % i, [128, D], F32, stD) for i in range(2)]
        B_x1t = [Buf(), Buf()]
        ds_x1 = [k.dsem(), k.dsem()]
        mixed = [sb("mixed%d" % i, [128, D], BF16, stD) for i in range(2)]
        B_mixed = [Buf(), Buf()]
        mT = [sb("mT%d" % i, [128, KC, 128], BF16, stD) for i in range(2)]
        B_mT = [Buf(), Buf()]
        hT_ps = [ps("hT_psd%d" % i, [128, D], BF16, stD) for i in range(2)]
        B_hTps = [Buf(), Buf()]
        mm_ps = [ps("mm_psd%d" % i, [128, 512], F32, stD) for i in range(4)]
        B_mm = [Buf() for _ in range(4)]
        cnt = {"mm": 0, "hTps": 0}

        def d1_prep(j):
            s_ = j % 2
            k.dma(sp, xts[s_][:], x_own[j * 128:(j + 1) * 128, :], ds_xt[s_], wr=[B_xt[s_]])
            ra, Bra = rms_scale(o_a[:, j, :], B_oa[j], 1024, 1024 * EPS)
            rb, Brb = rms_scale(o_b[:, j, :], B_ob[j], 1024, 1024 * EPS)
            k.op(dve, "scalar_tensor_tensor", out=mixed[s_][:, 0:1024], in0=o_a[:, j, :], scalar=ra, in1=gout[:, 0:1024],
                 op0=ALU.mult, op1=ALU.mult, rd=[B_oa[j], Bra, B_gout], wr=[B_mixed[s_]])
            k.op(dve, "scalar_tensor_tensor", out=mixed[s_][:, 1024:2048], in0=o_b[:, j, :], scalar=rb,
                 in1=gout[:, 1024:2048], op0=ALU.mult, op1=ALU.mult, rd=[B_ob[j], Brb, B_gout], wr=[B_mixed[s_]])

        def d1_tr(j):
            s_ = j % 2
            transpose_to(mixed[s_], B_mixed[s_], mT[s_][:], B_mT[s_])

        d1_prep(0)
        d1_tr(0)
        for j in range(NOWN):
            s = j % 2
            for n in range(4):
                ms = cnt["mm"] % 4
                cnt["mm"] += 1
                for kc in range(KC):
                    k.op(pe, "matmul", mm_ps[ms][:], lhsT=mT[s][:, kc, :], rhs=Wout[:, kc, n * 512:(n + 1) * 512],
                         start=(kc == 0), stop=(kc == KC - 1), rd=[B_Wout, B_mT[s]], wr=[B_mm[ms]])
                sl = slice(n * 512, (n + 1) * 512)
                k.op(dve, "tensor_tensor", out=x1t[s][:, sl], in0=mm_ps[ms][:], in1=GA[:, sl], op=ALU.mult,
                     rd=[B_mm[ms], B_GA], wr=[B_x1t[s]])
                k.op(pool, "tensor_tensor", out=x1t[s][:, sl], in0=x1t[s][:, sl], in1=xts[s][:, sl], op=ALU.add,
                     rd=[B_xt[s]], wr=[B_x1t[s]])
                if j + 1 < NOWN:
                    if n == 0:
                        d1_prep(j + 1)
                    if n == 2:
                        d1_tr(j + 1)
            k.dma(sp, X1_s[j * 128:(j + 1) * 128, :], x1t[s][:], ds_x1[s], rd=[B_x1t[s]])
        k.barrier()
        stD.close()
        stW.close()
        stOB.close()
        stOA.close()

        rt_w = sb("rt_w", [128, NOWN, 2], F32)
        rt_pg = sb("rt_pg", [128, NOWN, 2], I32)
        B_rtw, B_rtpg = Buf(), Buf()
        stR = contextlib.ExitStack()
        es.enter_context(stR)
        G2s = sb("G2s", [128, D], F32, stR)
        SHm = sb("SHm", [128, D], F32, stR)
        tmp2 = sb("tmp2", [128, D], F32, stR)
        B_G2s, B_SHm, B_tmp2 = Buf(), Buf(), Buf()
        bcast_load(G2s, B_G2s, 4, ds0)
        bcast_load(SHm, B_SHm, 3, ds0)
        vec_bcast_load(tmp2[:], B_tmp2, g_moe, ds0)
        k.op(dve, "scalar_tensor_tensor", out=G2s[:], in0=G2s[:], scalar=SQD, in1=tmp2[:], op0=ALU.mult, op1=ALU.mult,
             rd=[B_tmp2], wr=[B_G2s])
        Wr = sb("Wr", [128, KC, 36], F32, stR)
        B_Wr = Buf()
        k.dma(sp, Wr[:], w_r.rearrange("(kc p) n -> p kc n", p=128), ds0, wr=[B_Wr])
        bR = sb("bR", [128, 36], F32, stR)
        B_bR = Buf()
        vec_bcast_load(bR[:], B_bR, b_r, ds0, 36)
        eb_sb = sb("eb_sb", [128, NE], F32, stR)
        B_eb = Buf()
        k.dma(sp, eb_sb[:], ebase, ds0, wr=[B_eb])
        cnt_run = sb("cnt_run", [128, NE], F32, stR)
        B_cnt = Buf()
        k.op(dve, "memset", cnt_run[:], 0.0, wr=[B_cnt])
        zt = sb("zt", [128, D], BF16, stR)
        B_zt = Buf()
        k.op(pool, "memset", zt[:], 0.0, wr=[B_zt])
        ds_z = k.dsem()
        B_Xs = Buf()
        for s0 in range(0, NSLOT, 128):
            k.dma(sp, Xs_s[s0:s0 + 128, :], zt[:], ds_z, rd=[B_zt], wr=[B_Xs])
        x1t = [sb("x1r%d" % i, [128, D], F32, stR) for i in range(2)]
        B_x1t = [Buf(), Buf()]
        ds_x1 = [k.dsem(), k.dsem()]
        h2b = [sb("h2b%d" % i, [128, D], BF16, stR) for i in range(2)]
        B_h2b = [Buf(), Buf()]
        ds_sc = [k.dsem(), k.dsem()]
        h2T = sb("h2T", [128, KC, 128], F32, stR)
        B_h2T = Buf()
        trf_ps = [ps("trf_ps%d" % i, [128, 1024], F32, stR) for i in range(2)]
        B_trf = [Buf(), Buf()]
        lg_ps = ps("lg_ps", [128, 512], F32, stR)
        B_lgps = Buf()
        rk_ps = ps("rk_ps", [128, 512], F32, stR)
        B_rkps = Buf()
        R = sb("R", [128, 512], F32, stR)
        B_R = Buf()
        psc = [sb("psc%d" % i, [128, 2], I32, stR) for i in range(2)]
        B_psc = [Buf(), Buf()]
        BIGI = float(4 * NSLOT)

        def rop(method, **kw):
            return k.op(dve, method, rd=[B_R], wr=[B_R], **kw)

        for j in range(NOWN):
            s = j % 2
            k.dma(sp, x1t[s][:], X1_s[j * 128:(j + 1) * 128, :], ds_x1[s], wr=[B_x1t[s]])
            r2, Br2 = rms_scale(x1t[s][:], B_x1t[s], D, D * EPS)
            k.op(dve, "scalar_tensor_tensor", out=x1t[s][:], in0=x1t[s][:], scalar=r2, in1=G2s[:], op0=ALU.mult,
                 op1=ALU.mult, rd=[Br2, B_G2s], wr=[B_x1t[s]])
            k.op(dve, "tensor_tensor", out=x1t[s][:], in0=x1t[s][:], in1=SHm[:], op=ALU.add, rd=[B_SHm], wr=[B_x1t[s]])
            k.op(act, "copy", out=h2b[s][:], in_=x1t[s][:], rd=[B_x1t[s]], wr=[B_h2b[s]])
            for hh in range(2):
                for i8 in range(8):
                    kc = hh * 8 + i8
                    k.op(pe, "transpose", out=trf_ps[hh][:, i8 * 128:(i8 + 1) * 128], in_=x1t[s][:, kc * 128:(kc + 1) * 128],
                         identity=ident_f, rd=[B_x1t[s], B_cst], wr=[B_trf[hh]])
                k.op(act if hh else dve, "copy" if hh else "tensor_copy", out=h2T[:, hh * 8:(hh + 1) * 8, :],
                     in_=trf_ps[hh][:].rearrange("p (k t) -> p k t", k=8), rd=[B_trf[hh]], wr=[B_h2T])
            for kc in range(KC):
                k.op(pe, "matmul", lg_ps[:, 0:36], lhsT=h2T[:, kc, :], rhs=Wr[:, kc, :], start=(kc == 0),
                     stop=(kc == KC - 1), rd=[B_h2T, B_Wr], wr=[B_lgps])
            LG = R[:, 0:36]; GL = R[:, 0:4]; EL = R[:, 4:36]
            GMAX = R[:, 40:41]; NGMAX = R[:, 41:42]; GSUM = R[:, 42:43]; GVAL = R[:, 43:44]
            GOH = R[:, 44:48]; GPEN = R[:, 48:52]; GEXP = R[:, 52:56]
            EM = R[:, 64:96]; T1 = R[:, 96:97]; T2 = R[:, 97:98]; OH1 = R[:, 100:132]; OH2 = R[:, 132:164]
            E2 = R[:, 164:196]; AA = R[:, 196:228]; RK = R[:, 228:260]; RKB = R[:, 260:292]; TMP = R[:, 292:324]
            DD = R[:, 324:325]; ED = R[:, 325:326]; W1 = R[:, 326:327]; W2 = R[:, 327:328]
            P1 = R[:, 328:329]; P2 = R[:, 329:330]; R1 = R[:, 330:331]; R2 = R[:, 331:332]
            V1 = R[:, 332:333]; V2 = R[:, 333:334]; OF1 = R[:, 334:335]; OF2 = R[:, 335:336]
            PS1 = R[:, 336:337]; PS2 = R[:, 337:338]
            k.op(dve, "tensor_tensor", out=LG, in0=lg_ps[:, 0:36], in1=bR[:], op=ALU.add, rd=[B_lgps, B_bR, B_R], wr=[B_R])
            rop("reduce_max", out=GMAX, in_=GL, axis=AX.X)
            rop("tensor_scalar", out=GOH, in0=GL, scalar1=GMAX, scalar2=None, op0=ALU.is_equal)
            rop("tensor_scalar", out=NGMAX, in0=GMAX, scalar1=-1.0, scalar2=None, op0=ALU.mult)
            k.op(act, "activation", out=GEXP, in_=GL, func=AF.Exp, bias=NGMAX, scale=1.0, rd=[B_R], wr=[B_R])
            rop("reduce_sum", out=GSUM, in_=GEXP, axis=AX.X)
            rop("reciprocal", out=GVAL, in_=GSUM)
            rop("tensor_scalar", out=GPEN, in0=GOH, scalar1=-1.0, scalar2=1e30, op0=ALU.add, op1=ALU.mult)
            for gi in range(4):
                rop("tensor_scalar", out=EM[:, gi * 8:(gi + 1) * 8], in0=EL[:, gi * 8:(gi + 1) * 8],
                    scalar1=GPEN[:, gi:gi + 1], scalar2=None, op0=ALU.add)
            rop("reduce_max", out=T1, in_=EM, axis=AX.X)
            rop("tensor_scalar", out=OH1, in0=EM, scalar1=T1, scalar2=None, op0=ALU.is_equal)
            rop("scalar_tensor_tensor", out=E2, in0=OH1, scalar=-1e30, in1=EM, op0=ALU.mult, op1=ALU.add)
            rop("reduce_max", out=T2, in_=E2, axis=AX.X)
            rop("tensor_scalar", out=OH2, in0=E2, scalar1=T2, scalar2=None, op0=ALU.is_equal)
            rop("tensor_tensor", out=DD, in0=T2, in1=T1, op=ALU.subtract)
            k.op(act, "activation", out=ED, in_=DD, func=AF.Exp, rd=[B_R], wr=[B_R])
            rop("tensor_scalar", out=ED, in0=ED, scalar1=1.0, scalar2=None, op0=ALU.add)
            rop("reciprocal", out=ED, in_=ED)
            rop("tensor_tensor", out=W1, in0=GVAL, in1=ED, op=ALU.mult)
            rop("tensor_tensor", out=W2, in0=GVAL, in1=W1, op=ALU.subtract)
            rop("tensor_tensor", out=AA, in0=OH1, in1=OH2, op=ALU.add)
            k.op(pe, "matmul", rk_ps[:, 0:32], lhsT=U_strict, rhs=AA, start=True, stop=True, rd=[B_R, B_cst], wr=[B_rkps])
            k.op(pe, "matmul", rk_ps[:, 32:64], lhsT=ones_f, rhs=AA, start=True, stop=True, rd=[B_R, B_cst], wr=[B_rkps])
            k.op(dve, "tensor_tensor", out=RK, in0=rk_ps[:, 0:32], in1=cnt_run[:], op=ALU.add, rd=[B_rkps, B_cnt, B_R],
                 wr=[B_R])
            k.op(dve, "tensor_tensor", out=cnt_run[:], in0=rk_ps[:, 32:64], in1=cnt_run[:], op=ALU.add, rd=[B_rkps, B_cnt],
                 wr=[B_cnt])
            k.op(dve, "tensor_tensor", out=RKB, in0=RK, in1=eb_sb[:], op=ALU.add, rd=[B_R, B_eb], wr=[B_R])
            for (OH, P_, R_, V_, OF_, PS_, W_, col) in ((OH1, P1, R1, V1, OF1, PS1, W1, 0), (OH2, P2, R2, V2, OF2, PS2, W2, 1)):
                rop("tensor_tensor", out=TMP, in0=OH, in1=RKB, op=ALU.mult)
                rop("reduce_sum", out=P_, in_=TMP, axis=AX.X)
                rop("tensor_tensor", out=TMP, in0=OH, in1=RK, op=ALU.mult)
                rop("reduce_sum", out=R_, in_=TMP, axis=AX.X)
                rop("tensor_scalar", out=V_, in0=R_, scalar1=float(CAP), scalar2=None, op0=ALU.is_lt)
                rop("tensor_scalar", out=OF_, in0=V_, scalar1=-BIGI, scalar2=BIGI, op0=ALU.mult, op1=ALU.add)
                rop("tensor_tensor", out=PS_, in0=P_, in1=OF_, op=ALU.add)
                k.op(dve, "tensor_copy", out=psc[s][:, col:col + 1], in_=PS_, rd=[B_R], wr=[B_psc[s]])
                rop("tensor_tensor", out=P_, in0=P_, in1=V_, op=ALU.mult)
                k.op(dve, "tensor_copy", out=rt_pg[:, j, col:col + 1], in_=P_, rd=[B_R], wr=[B_rtpg])
                k.op(dve, "tensor_tensor", out=rt_w[:, j, col:col + 1], in0=W_, in1=V_, op=ALU.mult, rd=[B_R], wr=[B_rtw])
            for col in range(2):
                k.idma(Xs_s, bass.IndirectOffsetOnAxis(ap=psc[s][:, col:col + 1], axis=0), h2b[s][:, :], None, ds_sc[s],
                       NSLOT - 1, rd=[B_h2b[s], B_psc[s], B_Xs])
        k.barrier()
        stR.close()

        stE = contextlib.ExitStack()
        es.enter_context(stE)
        wg = [sb("wg%d" % i, [128, KC, DE], BF16, stE) for i in range(2)]
        wu = [sb("wu%d" % i, [128, KC, DE], BF16, stE) for i in range(2)]
        wd = [sb("wd%d" % i, [128, 4, D], BF16, stE) for i in range(2)]
        B_we = [Buf(), Buf()]
        ds_we = [k.dsem(), k.dsem()]
        NXS_E = 4
        xs = [sb("xs%d" % i, [128, D], BF16, stE) for i in range(NXS_E)]
        B_xs = [Buf() for _ in range(NXS_E)]
        ds_xs = [k.dsem() for _ in range(NXS_E)]
        xsT = [sb("xsT%d" % i, [128, KC, 128], BF16, stE) for i in range(2)]
        B_xsT = [Buf(), Buf()]
        sg = [sb("sg%d" % i, [128, DE], BF16, stE) for i in range(2)]
        hid = [sb("hid%d" % i, [128, DE], BF16, stE) for i in range(2)]
        hidT = [sb("hidT%d" % i, [128, 4, 128], BF16, stE) for i in range(2)]
        B_sg, B_hid, B_hidT = [Buf(), Buf()], [Buf(), Buf()], [Buf(), Buf()]
        y_sb = [sb("y_sb%d" % i, [128, D], F32, stE) for i in range(2)]
        B_ysb = [Buf(), Buf()]
        ds_y = [k.dsem(), k.dsem()]
        hT_ps = [ps("hT_pse%d" % i, [128, 1024], BF16, stE) for i in range(1)]
        B_hTps = [Buf()]
        g_ps = [ps("g_ps%d" % i, [128, 512], F32, stE) for i in range(2)]
        u_ps = [ps("u_ps%d" % i, [128, 512], F32, stE) for i in range(2)]
        B_gps, B_ups = [Buf(), Buf()], [Buf(), Buf()]
        ht_ps = ps("ht_ps", [128, 512], BF16, stE)
        B_htps = Buf()
        y_ps = [ps("y_ps%d" % i, [128, 512], F32, stE) for i in range(2)]
        B_yps = [Buf(), Buf()]
        cnt = {"hTps": 0, "y": 0}

        NSTG = 6
        stg = [sb("stg%d" % i, [128, 2048], F32, stE) for i in range(NSTG)]
        B_stg = [Buf() for _ in range(NSTG)]
        ds_stg = [k.dsem() for _ in range(NSTG)]
        cast_rr = [0]
        B_wch = [[Buf() for _ in range(12)] for _ in range(2)]

        def expert_chunks(e):
            s = e % 2
            wgv = w_gate[e].rearrange("(p kc) n -> p kc n", kc=KC)
            wuv = w_up[e].rearrange("(p kc) n -> p kc n", kc=KC)
            wdv = w_down[e].rearrange("(kc p) n -> p kc n", p=128)
            tasks = []
            for q in range(4):
                tasks.append((wgv[:, q * 4:(q + 1) * 4, :], wg[s][:, q * 4:(q + 1) * 4, :], "p (k n) -> p k n", 4))
            for q in range(4):
                tasks.append((wuv[:, q * 4:(q + 1) * 4, :], wu[s][:, q * 4:(q + 1) * 4, :], "p (k n) -> p k n", 4))
            for q in range(4):
                tasks.append((wdv[:, q, :], wd[s][:, q, :], None, 1))
            out_ = []
            for ci, (src, dst, rr, kk) in enumerate(tasks):
                def task(src=src, dst=dst, rr=rr, kk=kk, s=s, ci=ci):
                    i = cast_rr[0] % NSTG
                    c = cast_rr[0]
                    cast_rr[0] += 1
                    sview = stg[i][:].rearrange(rr, k=kk) if rr else stg[i][:]
                    k.dma(sp, sview, src, ds_stg[i], wr=[B_stg[i]])
                    eng = (dve, act)[c % 2]
                    k.op(eng, "copy" if eng is act else "tensor_copy", out=dst, in_=sview, rd=[B_stg[i]], wr=[B_wch[s][ci]])
                out_.append(task)
            return out_

        blocks = [(e, blk) for e in range(NE) for blk in range(NBLK)]
        NBK = len(blocks)

        def xs_load(i):
            e, blk = blocks[i]
            row0 = e * CAP + blk * 128
            sl = i % NXS_E
            k.dma(pool, xs[sl][:], Xs_s[row0:row0 + 128, :], ds_xs[sl], wr=[B_xs[sl]])

        def stA(i):
            sl = i % NXS_E
            tl = i % 2
            for hh in range(2):
                for i8 in range(8):
                    kc = hh * 8 + i8
                    k.op(pe, "transpose", out=hT_ps[0][:, i8 * 128:(i8 + 1) * 128], in_=xs[sl][:, kc:D:KC],
                         identity=ident_b[:], rd=[B_xs[sl], B_identb], wr=[B_hTps[0]])
                k.op(dve if hh else act, "tensor_copy" if hh else "copy", out=xsT[tl][:, hh * 8:(hh + 1) * 8, :],
                     in_=hT_ps[0][:].rearrange("p (k t) -> p k t", k=8), rd=[B_hTps[0]], wr=[B_xsT[tl]])

        def stB(i):
            e, blk = blocks[i]
            es_ = e % 2
            sl = i % 2
            for kc in range(KC):
                k.op(pe, "matmul", g_ps[sl][:], lhsT=xsT[sl][:, kc, :], rhs=wg[es_][:, kc, :], start=(kc == 0),
                     stop=(kc == KC - 1), rd=[B_xsT[sl]] + B_wch[es_], wr=[B_gps[sl]])
            for kc in range(KC):
                k.op(pe, "matmul", u_ps[sl][:], lhsT=xsT[sl][:, kc, :], rhs=wu[es_][:, kc, :], start=(kc == 0),
                     stop=(kc == KC - 1), rd=[B_xsT[sl]] + B_wch[es_], wr=[B_ups[sl]])
            k.op(act, "activation", out=sg[sl][:], in_=g_ps[sl][:], func=AF.Silu, rd=[B_gps[sl]], wr=[B_sg[sl]])
            k.op(dve, "tensor_tensor", out=hid[sl][:], in0=sg[sl][:], in1=u_ps[sl][:], op=ALU.mult,
                 rd=[B_sg[sl], B_ups[sl]], wr=[B_hid[sl]])

        def stC(i):
            e, blk = blocks[i]
            es_ = e % 2
            sl = i % 2
            row0 = e * CAP + blk * 128
            for c in range(4):
                k.op(pe, "transpose", out=ht_ps[:, c * 128:(c + 1) * 128], in_=hid[sl][:, c * 128:(c + 1) * 128],
                     identity=ident_b[:], rd=[B_hid[sl], B_identb], wr=[B_htps])
            k.op(act, "copy", out=hidT[sl][:], in_=ht_ps[:].rearrange("p (k t) -> p k t", k=4), rd=[B_htps],
                 wr=[B_hidT[sl]])

        def stC2(i):
            e, blk = blocks[i]
            es_ = e % 2
            sl = i % 2
            row0 = e * CAP + blk * 128
            for n in range(4):
                yp = n % 2
                for c in range(4):
                    k.op(pe, "matmul", y_ps[yp][:], lhsT=hidT[sl][:, c, :], rhs=wd[es_][:, c, n * 512:(n + 1) * 512],
                         start=(c == 0), stop=(c == 3), rd=[B_hidT[sl]] + B_wch[es_], wr=[B_yps[yp]])
                k.op(act if n % 2 else dve, "copy" if n % 2 else "tensor_copy", out=y_sb[sl][:, n * 512:(n + 1) * 512],
                     in_=y_ps[yp][:], rd=[B_yps[yp]], wr=[B_ysb[sl]])
            k.dma(act, Ys_s[row0:row0 + 128, :], y_sb[sl][:], ds_y[sl], rd=[B_ysb[sl]])

        for e0 in (0, 1):
            for t_ in expert_chunks(e0):
                t_()
        pending = []
        xs_load(0)
        xs_load(1)
        for i in range(NBK + 2):
            if i + 2 < NBK:
                xs_load(i + 2)
            if i >= 2:
                stC(i - 2)
            if i < NBK:
                stA(i)
            if 1 <= i <= NBK:
                stB(i - 1)
            if i >= 2:
                stC2(i - 2)
                e_done, blk_done = blocks[i - 2]
                if blk_done == NBLK - 1 and e_done + 2 < NE:
                    pending += expert_chunks(e_done + 2)
            for _ in range(3):
                if pending:
                    pending.pop(0)()
        assert not pending
        k.barrier()
        stE.close()

        stG = contextlib.ExitStack()
        es.enter_context(stG)
        GM = sb("GM", [128, D], F32, stG)
        FG = sb("FG", [128, D], F32, stG)
        B_GM, B_FG = Buf(), Buf()
        bcast_load(GM, B_GM, 5, ds0)
        vec_bcast_load(FG[:], B_FG, g_fin, ds0)
        k.op(dve, "tensor_scalar", out=FG[:], in0=FG[:], scalar1=SQD, scalar2=None, op0=ALU.mult, wr=[B_FG])
        NF = 3
        g1 = [sb("g1_%d" % i, [128, D], F32, stG) for i in range(NF)]
        g2 = [sb("g2_%d" % i, [128, D], F32, stG) for i in range(NF)]
        x1t = [sb("x1f%d" % i, [128, D], F32, stG) for i in range(NF)]
        B_g1, B_g2, B_x1t = [Buf() for _ in range(NF)], [Buf() for _ in range(NF)], [Buf() for _ in range(NF)]
        ds_g = [k.dsem() for _ in range(NF)]
        ds_x1 = [k.dsem() for _ in range(NF)]
        ds_o = [k.dsem() for _ in range(NF)]

        def f_loads(j):
            s = j % NF
            k.dma(sp, x1t[s][:], X1_s[j * 128:(j + 1) * 128, :], ds_x1[s], wr=[B_x1t[s]])
            k.idma(g1[s][:, :], None, Ys_s, bass.IndirectOffsetOnAxis(ap=rt_pg[:, j, 0:1], axis=0), ds_g[s], NSLOT - 1,
                   rd=[B_rtpg], wr=[B_g1[s]])
            k.idma(g2[s][:, :], None, Ys_s, bass.IndirectOffsetOnAxis(ap=rt_pg[:, j, 1:2], axis=0), ds_g[s], NSLOT - 1,
                   rd=[B_rtpg], wr=[B_g2[s]])

        f_loads(0)
        if NOWN > 1:
            f_loads(1)
        for j in range(NOWN):
            s = j % NF
            if j + 2 < NOWN:
                f_loads(j + 2)
            k.op(dve, "tensor_scalar", out=g1[s][:], in0=g1[s][:], scalar1=rt_w[:, j, 0:1], scalar2=None, op0=ALU.mult,
                 rd=[B_rtw], wr=[B_g1[s]])
            k.op(dve, "scalar_tensor_tensor", out=g1[s][:], in0=g2[s][:], scalar=rt_w[:, j, 1:2], in1=g1[s][:],
                 op0=ALU.mult, op1=ALU.add, rd=[B_g2[s], B_rtw], wr=[B_g1[s]])
            k.op(pool, "tensor_tensor", out=g1[s][:], in0=g1[s][:], in1=GM[:], op=ALU.mult, rd=[B_GM], wr=[B_g1[s]])
            k.op(dve, "tensor_tensor", out=x1t[s][:], in0=x1t[s][:], in1=g1[s][:], op=ALU.add, rd=[B_g1[s]], wr=[B_x1t[s]])
            rf, Brf = rms_scale(x1t[s][:], B_x1t[s], D, D * EPS)
            k.op(dve, "scalar_tensor_tensor", out=g2[s][:], in0=x1t[s][:], scalar=rf, in1=FG[:], op0=ALU.mult,
                 op1=ALU.mult, rd=[B_x1t[s], Brf, B_FG], wr=[B_g2[s]])
            k.dma(sp, out[j * 128:(j + 1) * 128, :], g2[s][:], ds_o[s], rd=[B_g2[s]], wr=[])
        if dbg:
            k.dma(sp, d_pg, rt_pg[:].rearrange("p j c -> p (j c)"), ds0, rd=[B_rtpg])
            k.dma(sp, d_w, rt_w[:].rearrange("p j c -> p (j c)"), ds0, rd=[B_rtw])
        k.barrier()
        stG.close()
    return nc


def _consts(r, CAP):
    p = np.arange(128)
    ident = np.eye(128, dtype=np.float32)
    U_incl = (p[:, None] <= p[None, :]).astype(np.float32)
    U_strict = (p[:, None] < p[None, :]).astype(np.float32)
    ones = np.ones((128, 128), np.float32)
    cst = np.concatenate([ident, U_incl, U_strict, ones], axis=1)
    tri = (p[:, None] <= p[None, :]).astype(np.float32)
    fm = [np.ones((128, 128), np.float32) if u < r else (tri if u == r else np.zeros((128, 128), np.float32)) for u in range(4)]
    fmask = np.concatenate(fm, axis=1)
    slopes = (2.0 ** (-8.0 * np.arange(1, 17) / 16)).astype(np.float64)
    kk, qq = p[:, None].astype(np.float64), p[None, :].astype(np.float64)
    A = np.zeros((128, 3, 4, 4, 128), np.float32)
    for g in range(16):
        prev = np.exp(-slopes[g] * (qq + 128 - kk)) * (kk > qq)
        cur = np.exp(-slopes[g] * (qq - kk)) * (kk <= qq)
        A[:, 0, g // 4, g % 4, :] = prev if r != 0 else 0.0
        A[:, 1, g // 4, g % 4, :] = prev
        A[:, 2, g // 4, g % 4, :] = cur
    rsel = np.zeros((128, 4), np.float32)
    rsel[:, r] = 1.0
    ebase = np.tile((np.arange(NE) * CAP).astype(np.float32)[None, :], (128, 1))
    return dict(cst=cst, fmask=fmask, swaA=A.reshape(128, -1), rsel=rsel, ebase=ebase)


_CACHE = {}


def run(inputs, S, CAP, dbg=False):
    f = lambda a: np.ascontiguousarray(np.asarray(a, dtype=np.float32))
    x = f(inputs["x"])[:, :S]
    B = x.shape[0]
    NB = S // 128
    NOWN = NB // 4
    key = (S, CAP, dbg)
    if key not in _CACHE:
        _CACHE[key] = build(S, CAP, dbg)
    nc = _CACHE[key]
    shared = dict(
        w_ada=f(inputs["w_ada"][0]), b_ada=f(inputs["b_ada"][0]).reshape(1, -1), g_mix=f(inputs["norm_mix_g"][0]).reshape(1, -1),
        w_in=f(inputs["w_in"][0]), b_f=f(inputs["b_forget"][0]).reshape(1, -1), sinks=f(inputs["sinks"][0]).reshape(1, -1),
        g_out=np.concatenate([f(inputs["out_norm_swa_g"][0]), f(inputs["out_norm_fox_g"][0])]).reshape(1, -1),
        w_out=f(inputs["w_out"][0]), g_moe=f(inputs["norm_moe_g"][0]).reshape(1, -1),
        w_r=np.ascontiguousarray(np.concatenate([f(inputs["w_group"][0]), f(inputs["w_expert"][0])], axis=1)),
        b_r=np.concatenate([f(inputs["b_group"][0]), f(inputs["b_expert"][0])]).reshape(1, -1),
        w_gate=f(inputs["w_gate"][0]), w_up=f(inputs["w_up"][0]), w_down=f(inputs["w_down"][0]),
        g_fin=f(inputs["final_g"]).reshape(1, -1),
    )
    in_maps = []
    for core in range(8):
        b, r = core // 4, core % 4
        xb = x[b].reshape(NB, 128, D)
        own = [4 * j + r for j in range(NOWN)]
        x_own = np.ascontiguousarray(xb[own].reshape(-1, D))
        xp = np.zeros((NOWN, 128, D), np.float32)
        for j, t in enumerate(own):
            if t > 0:
                xp[j] = xb[t - 1]
        m = dict(shared)
        m.update(x_seq=np.ascontiguousarray(x[b]), x_own=x_own, x_prev=xp.reshape(-1, D),
                 cT=np.ascontiguousarray(f(inputs["c"])[b].reshape(KC, 128).T))
        m.update(_consts(r, CAP))
        in_maps.append(m)
    res = run_bass_kernel_spmd(nc, in_maps, core_ids=list(range(8)))
    if dbg:
        _CACHE["dbg"] = res
    outp = np.zeros((B, NB, 128, D), np.float32)
    for core in range(8):
        b, r = core // 4, core % 4
        o = np.asarray(res.results[core]["out"]).reshape(NOWN, 128, D)
        for j in range(NOWN):
            outp[b, 4 * j + r] = o[j]
    return outp.reshape(B, S, D)


def kernel(**inputs):
    return run(inputs, 8192, 512)
```

```python
import bisect
import contextlib
import numpy as np
import concourse.bass as bass
import concourse.mybir as mybir
from concourse.bass_utils import run_bass_kernel_spmd

F32 = mybir.dt.float32
BF16 = mybir.dt.bfloat16
I32 = mybir.dt.int32
AF = mybir.ActivationFunctionType
ALU = mybir.AluOpType
AX = mybir.AxisListType

D = 2048
KC = 16
EPS = 1e-6
NE = 32
DE = 512
QA0, KA0, VA0, QB0, KB0, VB0, FB0 = 0, 1024, 1280, 1536, 2560, 3584, 4608


class Eng:
    def __init__(self, nc, e, sem, name, is_pe=False):
        self.nc, self.e, self.sem, self.name, self.is_pe = nc, e, sem, name, is_pe
        self.idx = 0
        self.marks = []
        self.mark_idx = []
        self.count = 0
        self.last = None
        self.waited = {}

    def issue(self, ins, is_dma=False):
        self.idx += 1
        self.last = ins
        self.last_is_dma = is_dma
        return ("E", self, self.idx)

    def mark_now(self):
        if self.marks and self.marks[-1][0] == self.idx:
            return
        self.count += 1
        self.last.then_inc(self.sem, 1)
        self.marks.append((self.idx, self.count))
        self.mark_idx.append(self.idx)

    def resolve(self, idx):
        p = bisect.bisect_left(self.mark_idx, idx)
        if p < len(self.marks):
            return self.marks[p][1]
        assert self.idx >= idx
        if self.last_is_dma:
            if self.name == "act":
                self.issue(self.e.memzero(self.mark_tile))
            else:
                self.issue(self.e.memset(self.mark_tile, 0.0))
        self.mark_now()
        return self.count

    def wait(self, toks):
        for t in toks:
            if t is None:
                continue
            if t[0] == "E":
                eng, idx = t[1], t[2]
                if eng is self and self.is_pe:
                    continue
                val = eng.resolve(idx)
                sem = eng.sem
                key = eng.name
            else:
                _, ds, val = t
                sem = ds.sem
                key = ds.name
            if self.waited.get(key, 0) >= val:
                continue
            self.e.wait_ge(sem, val)
            self.waited[key] = val


class DSem:
    def __init__(self, sem, name):
        self.sem, self.name, self.n = sem, name, 0


class Buf:
    __slots__ = ("w", "r")

    def __init__(self):
        self.w = None
        self.r = {}


class K:
    def __init__(self, nc, es):
        self.nc, self.es = nc, es
        self.dsems = []
        mk = lambda e, n, pe=False: Eng(nc, e, es.enter_context(nc.semaphore("sem_" + n)), n, pe)
        self.pe = mk(nc.tensor, "pe", True)
        self.act = mk(nc.scalar, "act")
        self.dve = mk(nc.vector, "dve")
        self.pool = mk(nc.gpsimd, "pool")
        self.sp = mk(nc.sync, "sp")
        self.engs = [self.pe, self.act, self.dve, self.pool, self.sp]
        self.nds = 0

    def dsem(self, es=None):
        es = es or self.es
        self.nds += 1
        name = "ds%d" % self.nds
        d = DSem(es.enter_context(self.nc.semaphore(name)), name)
        self.dsems.append(d)
        return d

    def _deps(self, eng, rd, wr):
        deps = []
        for b in rd:
            if b.w is not None:
                deps.append(b.w)
        for b in wr:
            if b.w is not None:
                deps.append(b.w)
            for k, t in b.r.items():
                if t[0] == "E" and t[1] is eng:
                    continue
                deps.append(t)
        return deps

    def _upd(self, tok, key, rd, wr):
        for b in rd:
            b.r[key] = tok
        for b in wr:
            b.w = tok
            b.r = {}

    def op(self, eng, method, *a, rd=(), wr=(), **kw):
        eng.wait(self._deps(eng, rd, wr))
        ins = getattr(eng.e, method)(*a, **kw)
        tok = eng.issue(ins)
        if (not eng.is_pe) or kw.get("stop") or method == "transpose":
            eng.mark_now()
        self._upd(tok, eng.name, rd, wr)
        return tok

    def dma(self, q, out, in_, ds, rd=(), wr=(), **kw):
        if ds is None:
            if not hasattr(self, "_ds_by_buf"):
                self._ds_by_buf = {}
            if id(wr[0]) not in self._ds_by_buf:
                self._ds_by_buf[id(wr[0])] = self.dsem()
            ds = self._ds_by_buf[id(wr[0])]
        q.wait(self._deps(q, rd, wr))
        ins = q.e.dma_start(out=out, in_=in_, **kw)
        q.issue(ins, True)
        ds.n += 16
        ins.then_inc(ds.sem, 16)
        tok = ("D", ds, ds.n)
        self._upd(tok, ds.name, rd, wr)
        return tok

    def idma(self, out, out_off, in_, in_off, ds, bound, rd=(), wr=()):
        q = self.pool
        q.wait(self._deps(q, rd, wr))
        if not hasattr(self, "_bound_reg"):
            self._bound_reg = {}
        if bound not in self._bound_reg:
            self._bound_reg[bound] = q.e.to_reg(bound)
        ins = q.e.indirect_dma_start(out=out, out_offset=out_off, in_=in_, in_offset=in_off,
                                     bounds_check=self._bound_reg[bound], oob_is_err=False)
        q.issue(ins, True)
        ds.n += 16
        ins.then_inc(ds.sem, 16)
        tok = ("D", ds, ds.n)
        self._upd(tok, ds.name, rd, wr)
        return tok

    def barrier(self):
        toks = []
        for e in self.engs:
            if e.idx > 0 and e is not self.sp:
                toks.append(("E", e, e.idx))
        for d in self.dsems:
            if d.n > 0:
                toks.append(("D", d, d.n))
        for e in self.engs:
            e.wait([t for t in toks if not (t[0] == "E" and t[1] is e)])


def build(S, CAP, dbg=False):
    NB = S // 128
    NOWN = NB // 4
    NG = NB // 4
    TOWN = NOWN * 128
    NSLOT = NE * CAP
    NBLK = CAP // 128
    SQD = float(np.sqrt(D))

    nc = bass.Bass("TRN2", target_bir_lowering=False)

    def inp(name, shape, dt=F32):
        return nc.dram_tensor(name, shape, dt, kind="ExternalInput").ap()

    def scr(name, shape, dt):
        return nc.dram_tensor(name, shape, dt, kind="Internal").ap()

    x_seq = inp("x_seq", [S, D]); x_own = inp("x_own", [TOWN, D]); x_prev = inp("x_prev", [TOWN, D])
    cT = inp("cT", [128, KC]); w_ada = inp("w_ada", [D, 6 * D]); b_ada = inp("b_ada", [1, 6 * D])
    g_mix = inp("g_mix", [1, D]); w_in = inp("w_in", [D, 4624]); b_f = inp("b_f", [1, 16]); sinks = inp("sinks", [1, 16])
    g_out = inp("g_out", [1, D]); w_out = inp("w_out", [D, D]); g_moe = inp("g_moe", [1, D])
    w_r = inp("w_r", [D, 36]); b_r = inp("b_r", [1, 36])
    w_gate = inp("w_gate", [NE, D, DE]); w_up = inp("w_up", [NE, D, DE]); w_down = inp("w_down", [NE, DE, D])
    g_fin = inp("g_fin", [1, D])
    fmask = inp("fmask", [128, 512]); swaA = inp("swaA", [128, 3 * 4 * 512]); rsel = inp("rsel", [128, 4])
    cst = inp("cst", [128, 4 * 128]); ebase = inp("ebase", [128, NE])
    out = nc.dram_tensor("out", [TOWN, D], F32, kind="ExternalOutput").ap()

    mod_s = scr("mod_s", [1, 6 * D], F32)
    KT_s = scr("KT_s", [16 * 64, S], BF16)
    VB_s = scr("VB_s", [S, 1024], BF16)
    QT_s = scr("QT_s", [16 * 64, TOWN], BF16)
    QAUG_s = scr("QAUG_s", [NOWN * 16, 3, 128], BF16)
    X1_s = scr("X1_s", [TOWN, D], F32)
    if dbg:
        Xs_s = nc.dram_tensor("Xs_s", [NSLOT, D], BF16, kind="ExternalOutput").ap()
        Ys_s = nc.dram_tensor("Ys_s", [NSLOT, D], F32, kind="ExternalOutput").ap()
        d_pg = nc.dram_tensor("d_pg", [128, NOWN * 2], I32, kind="ExternalOutput").ap()
        d_w = nc.dram_tensor("d_w", [128, NOWN * 2], F32, kind="ExternalOutput").ap()
    else:
        Xs_s = scr("Xs_s", [NSLOT, D], BF16)
        Ys_s = scr("Ys_s", [NSLOT, D], F32)

    w_in_v = w_in.rearrange("(kc p) n -> p kc n", p=128)

    with contextlib.ExitStack() as es:
        k = K(nc, es)
        pe, act, dve, pool, sp = k.pe, k.act, k.dve, k.pool, k.sp
        mark_tile = es.enter_context(nc.sbuf_tensor("mark_tile", [128, 8], F32))
        pool.mark_tile = mark_tile[:, 0:4]
        act.mark_tile = mark_tile[:, 4:8]
        for e_ in k.engs:
            e_.last_is_dma = False

        def sb(name, shape, dt, st=None):
            return (st or es).enter_context(nc.sbuf_tensor(name, shape, dt))

        def ps(name, shape, dt, st):
            return st.enter_context(nc.psum_tensor(name, shape, dt))

        cst_f = sb("cst_f", [128, 512], F32)
        ident_b = sb("ident_b", [128, 128], BF16)
        ssq = sb("ssq", [128, NB + 6 * NOWN + 8], F32)
        junk = sb("junk", [128, D], BF16)
        B_cst, B_identb, B_ssq, B_junk = Buf(), Buf(), Buf(), Buf()
        k.dma(sp, cst_f[:], cst, k.dsem(), wr=[B_cst])
        k.op(dve, "tensor_copy", out=ident_b[:], in_=cst_f[:, 0:128], rd=[B_cst], wr=[B_identb])
        k.op(pool, "memset", ssq[:], 0.0, wr=[B_ssq])
        ident_f = cst_f[:, 0:128]
        U_incl = cst_f[:, 128:256]
        U_strict = cst_f[:, 256:384]
        ones_f = cst_f[:, 384:512]
        ssq_ctr = [0]

        def new_ssq():
            i = ssq_ctr[0]
            ssq_ctr[0] += 1
            return ssq[:, i:i + 1]

        rstd_all = sb("rstd_all", [128, NB + 6 * NOWN + 8], F32)

        def rms_scale(src_ap, B_src, width, eps_scaled):
            col = new_ssq()
            i = ssq_ctr[0] - 1
            Bc = Buf()
            Bc.w = B_ssq.w
            k.op(act, "activation", out=junk[:, 0:width], in_=src_ap, func=AF.Square, accum_out=col,
                 rd=[B_src], wr=[Bc, B_junk])
            r = rstd_all[:, i:i + 1]
            Br = Buf()
            k.op(act, "activation", out=r, in_=col, func=AF.Sqrt, bias=float(eps_scaled), scale=1.0, rd=[Bc], wr=[Br])
            k.op(dve, "reciprocal", out=r, in_=r, rd=[Br], wr=[Br])
            return r, Br

        cumN = sb("cumN", [128, NB, 16], F32)
        B_cumN = Buf()
        stA = contextlib.ExitStack()
        es.enter_context(stA)
        stB = stA
        cT_sb = sb("cT_sb", [128, KC], F32, stA)
        sil = sb("sil", [128, KC], BF16, stA)
        B_cT, B_sil = Buf(), Buf()
        k.dma(sp, cT_sb[:], cT, k.dsem(), wr=[B_cT])
        k.op(act, "activation", out=sil[:], in_=cT_sb[:], func=AF.Silu, rd=[B_cT], wr=[B_sil])
        wa = [sb("wa%d" % i, [128, KC, 512], BF16, stA) for i in range(2)]
        B_wa = [Buf(), Buf()]
        ds_wa = [k.dsem(), k.dsem()]
        brow = [sb("brow%d" % i, [1, 512], F32, stA) for i in range(2)]
        B_brow = [Buf(), Buf()]
        ds_br = [k.dsem(), k.dsem()]
        mrow = [sb("mrow%d" % i, [1, 512], F32, stA) for i in range(2)]
        B_mrow = [Buf(), Buf()]
        ds_mr = [k.dsem(), k.dsem()]
        B_mod = [Buf() for _ in range(24)]
        w_ada_v = w_ada.rearrange("(kc p) n -> p kc n", p=128)
        ada_state = {"loaded": 0, "done": 0}

        def ada_load(blk):
            s = blk % 2
            k.dma(pool, wa[s][:], w_ada_v[:, :, blk * 512:(blk + 1) * 512], ds_wa[s], wr=[B_wa[s]])
            k.dma(sp, brow[s][:], b_ada[0:1, blk * 512:(blk + 1) * 512], ds_br[s], wr=[B_brow[s]])

        def ada_block(blk, mod_ps, B_modps):
            s = blk % 2
            for kc in range(KC):
                k.op(pe, "matmul", mod_ps[0:1, :], lhsT=sil[:, kc:kc + 1], rhs=wa[s][:, kc, :], start=(kc == 0),
                     stop=(kc == KC - 1), rd=[B_sil, B_wa[s]], wr=[B_modps])
            if (blk // 4) in (1, 4):
                k.op(dve, "scalar_tensor_tensor", out=mrow[s][:], in0=mod_ps[0:1, :], scalar=1.0, in1=brow[s][:],
                     op0=ALU.add, op1=ALU.add, rd=[B_modps, B_brow[s]], wr=[B_mrow[s]])
            else:
                k.op(dve, "tensor_tensor", out=mrow[s][:], in0=mod_ps[0:1, :], in1=brow[s][:], op=ALU.add,
                     rd=[B_modps, B_brow[s]], wr=[B_mrow[s]])
            k.dma(sp, mod_s[0:1, blk * 512:(blk + 1) * 512], mrow[s][:], ds_mr[s], rd=[B_mrow[s]], wr=[B_mod[blk]])

        def ada_step(mod_ps, B_modps):
            b = ada_state["done"]
            if b >= 24:
                return
            while ada_state["loaded"] < min(24, b + 2):
                ada_load(ada_state["loaded"])
                ada_state["loaded"] += 1
            ada_block(b, mod_ps, B_modps)
            ada_state["done"] += 1

        def bcast_load(dst, B_dst, chunk, ds):
            k.dma(sp, dst[:], mod_s[0:1, chunk * D:(chunk + 1) * D].partition_broadcast(128), ds,
                  rd=[B_mod[chunk * 4 + i] for i in range(4)], wr=[B_dst])

        def vec_bcast_load(dst, B_dst, src, ds, n=D):
            k.dma(sp, dst, src[0:1, 0:n].partition_broadcast(128), ds, wr=[B_dst])

        G1s = sb("G1s", [128, D], F32, stB)
        SHa = sb("SHa", [128, D], F32, stB)
        B_G1s, B_SHa = Buf(), Buf()
        mod_ps = ps("mod_ps", [128, 512], F32, stB)
        B_modps = Buf()
        for _ in range(8):
            ada_step(mod_ps, B_modps)
        B_tmpg = Buf()
        bcast_load(G1s, B_G1s, 1, k.dsem())
        bcast_load(SHa, B_SHa, 0, k.dsem())

        Wkb = sb("Wkb", [128, KC, 1024], BF16, stB)
        Wvb = sb("Wvb", [128, KC, 1024], BF16, stB)
        Wf = sb("Wf", [128, KC, 16], BF16, stB)
        B_Wkb, B_Wvb, B_Wf = Buf(), Buf(), Buf()
        for kc in range(KC):
            k.dma(pool, Wkb[:, kc, :], w_in_v[:, kc, KB0:KB0 + 1024], None, wr=[B_Wkb])
        for kc in range(KC):
            k.dma(pool, Wvb[:, kc, :], w_in_v[:, kc, VB0:VB0 + 1024], None, wr=[B_Wvb])
        k.dma(pool, Wf[:], w_in_v[:, :, FB0:FB0 + 16], None, wr=[B_Wf])
        bF = sb("bF", [128, 16], F32, stB)
        B_bF = Buf()
        vec_bcast_load(bF[:], B_bF, b_f, k.dsem(), 16)

        NXS = 2
        xts = [sb("xt%d" % i, [128, D], F32, stB) for i in range(NXS)]
        B_xt = [Buf() for _ in range(NXS)]
        ds_xt = [k.dsem() for _ in range(NXS)]
        vec_bcast_load(xts[0][:], B_xt[0], g_mix, k.dsem())
        k.op(dve, "scalar_tensor_tensor", out=G1s[:], in0=G1s[:], scalar=SQD, in1=xts[0][:], op0=ALU.mult, op1=ALU.mult,
             rd=[B_xt[0]], wr=[B_G1s])
        hb = [sb("hb%d" % i, [128, D], BF16, stB) for i in range(2)]
        B_hb = [Buf(), Buf()]
        hTg = [sb("hTg%d" % i, [128, KC, 512], BF16, stB) for i in range(2)]
        B_hTg = [[Buf() for _ in range(4)] for _ in range(2)]
        hT_ps = [ps("hT_ps%d" % i, [128, D], BF16, stB) for i in range(1)]
        B_hTps = [Buf()]
        NMM = 4
        mm_ps = [ps("mm_ps%d" % i, [128, 512], F32, stB) for i in range(NMM)]
        B_mm = [Buf() for _ in range(NMM)]
        f_ps = ps("f_ps", [128, 512], F32, stB)
        B_fps = [Buf(), Buf()]
        kT_sb = [sb("kT_sb%d" % i, [128, 512], BF16, stB) for i in range(2)]
        B_kTsb = [Buf(), Buf()]
        ds_kT = [k.dsem(), k.dsem()]
        v_sb = [sb("v_sb%d" % i, [128, 1024], BF16, stB) for i in range(2)]
        B_vsb = [Buf(), Buf()]
        ds_v = [k.dsem(), k.dsem()]
        zf = sb("zf", [128, NB, 16], F32, stB)
        B_zf = Buf()
        cnt = {"xt": 0, "hb": 0, "mm": 0, "kT": 0, "v": 0, "f": 0, "hTps": 0}

        def make_h(src_rows, Bx_extra_rd=()):
            s = cnt["xt"] % NXS
            cnt["xt"] += 1
            k.dma(sp, xts[s][:], src_rows, ds_xt[s], wr=[B_xt[s]])
            r, Br = rms_scale(xts[s][:], B_xt[s], D, D * EPS)
            k.op(dve, "scalar_tensor_tensor", out=xts[s][:], in0=xts[s][:], scalar=r, in1=G1s[:], op0=ALU.mult,
                 op1=ALU.mult, rd=[Br, B_G1s], wr=[B_xt[s]])
            hs = cnt["hb"] % 2
            cnt["hb"] += 1
            k.op(dve, "tensor_tensor", out=hb[hs][:], in0=xts[s][:], in1=SHa[:], op=ALU.add,
                 rd=[B_xt[s], B_SHa], wr=[B_hb[hs]])
            return hb[hs], B_hb[hs]

        def transpose_to(h_t, B_h, dst_ap3, B_dst, eng=None):
            s = cnt["hTps"] % len(hT_ps)
            cnt["hTps"] += 1
            for kc in range(KC):
                k.op(pe, "transpose", out=hT_ps[s][:, kc * 128:(kc + 1) * 128], in_=h_t[:, kc * 128:(kc + 1) * 128],
                     identity=ident_b[:], rd=[B_h, B_identb], wr=[B_hTps[s]])
            e = eng or act
            if e is act:
                k.op(act, "copy", out=dst_ap3, in_=hT_ps[s][:].rearrange("p (k t) -> p k t", k=KC),
                     rd=[B_hTps[s]], wr=[B_dst])
            else:
                k.op(e, "tensor_copy", out=dst_ap3, in_=hT_ps[s][:].rearrange("p (k t) -> p k t", k=KC),
                     rd=[B_hTps[s]], wr=[B_dst])

        for i in range(4):
            h_t, B_h = make_h(x_seq[i * 128:(i + 1) * 128, :])
            transpose_to(h_t, B_h, hTg[0][:, :, i * 128:(i + 1) * 128], B_hTg[0][i])
        for g in range(NG):
            gs = g % 2
            hq = None
            for c in range(8):
                ms = cnt["mm"] % NMM
                cnt["mm"] += 1
                for kc in range(KC):
                    k.op(pe, "matmul", mm_ps[ms][:], lhsT=Wkb[:, kc, c * 128:(c + 1) * 128], rhs=hTg[gs][:, kc, :],
                         start=(kc == 0), stop=(kc == KC - 1), rd=[B_Wkb] + B_hTg[gs], wr=[B_mm[ms]])
                ks = cnt["kT"] % 2
                cnt["kT"] += 1
                k.op(act if c % 2 else dve, "copy" if c % 2 else "tensor_copy", out=kT_sb[ks][:], in_=mm_ps[ms][:],
                     rd=[B_mm[ms]], wr=[B_kTsb[ks]])
                k.dma(pool, KT_s[c * 128:(c + 1) * 128, g * 512:(g + 1) * 512], kT_sb[ks][:], ds_kT[ks], rd=[B_kTsb[ks]])
                if g + 1 < NG:
                    i_n = c // 2
                    t_n = 4 * (g + 1) + i_n
                    if c % 2 == 0:
                        hq = make_h(x_seq[t_n * 128:(t_n + 1) * 128, :])
                    else:
                        transpose_to(hq[0], hq[1], hTg[1 - gs][:, :, i_n * 128:(i_n + 1) * 128], B_hTg[1 - gs][i_n])
            for i in range(4):
                t = 4 * g + i
                vs = cnt["v"] % 2
                cnt["v"] += 1
                for n in range(2):
                    ms = cnt["mm"] % NMM
                    cnt["mm"] += 1
                    for kc in range(KC):
                        k.op(pe, "matmul", mm_ps[ms][:], lhsT=hTg[gs][:, kc, i * 128:(i + 1) * 128],
                             rhs=Wvb[:, kc, n * 512:(n + 1) * 512], start=(kc == 0), stop=(kc == KC - 1),
                             rd=[B_Wvb, B_hTg[gs][i]], wr=[B_mm[ms]])
                    k.op(act, "copy", out=v_sb[vs][:, n * 512:(n + 1) * 512], in_=mm_ps[ms][:], rd=[B_mm[ms]],
                         wr=[B_vsb[vs]])
                k.dma(pool, VB_s[t * 128:(t + 1) * 128, :], v_sb[vs][:], ds_v[vs], rd=[B_vsb[vs]])
                fs = cnt["f"] % 2
                cnt["f"] += 1
                for kc in range(KC):
                    k.op(pe, "matmul", f_ps[:, fs * 16:(fs + 1) * 16], lhsT=hTg[gs][:, kc, i * 128:(i + 1) * 128],
                         rhs=Wf[:, kc, :], start=(kc == 0), stop=(kc == KC - 1), rd=[B_Wf, B_hTg[gs][i]],
                         wr=[B_fps[fs]])
                k.op(dve, "tensor_tensor", out=zf[:, t, :], in0=f_ps[:, fs * 16:(fs + 1) * 16], in1=bF[:], op=ALU.add,
                     rd=[B_fps[fs], B_bF], wr=[B_zf])
            ada_step(mod_ps, B_modps)
        while ada_state["done"] < 24:
            ada_step(mod_ps, B_modps)

        NC16 = NB * 16
        zf2 = zf[:].rearrange("p t h -> p (t h)")
        k.op(act, "activation", out=zf2, in_=zf2, func=AF.Exp, scale=-1.0, rd=[B_zf], wr=[B_zf])
        k.op(act, "activation", out=zf2, in_=zf2, func=AF.Ln, bias=1.0, scale=1.0, rd=[B_zf], wr=[B_zf])
        cumN2 = cumN[:].rearrange("p t h -> p (t h)")
        class _V:
            def __init__(self, t):
                self.t = t
            def __getitem__(self, idx):
                return self.t[:, 0:NB * 16].rearrange("p (t h) -> p t h", h=16)[idx]
        pfx = [_V(xts[i]) for i in range(2)]
        B_pfx = [B_xt[0], B_xt[1]]
        for c0 in range(0, NC16, 512):
            c1 = min(NC16, c0 + 512)
            w = c1 - c0
            k.op(pe, "matmul", mm_ps[0][:, 0:w], lhsT=U_incl, rhs=zf2[:, c0:c1], start=True, stop=True,
                 rd=[B_zf, B_cst], wr=[B_mm[0]])
            k.op(pe, "matmul", mm_ps[1][:, 0:w], lhsT=ones_f, rhs=zf2[:, c0:c1], start=True, stop=True,
                 rd=[B_zf, B_cst], wr=[B_mm[1]])
            k.op(dve, "tensor_copy", out=cumN2[:, c0:c1], in_=mm_ps[0][:, 0:w], rd=[B_mm[0]], wr=[B_cumN])
            k.op(dve, "tensor_copy", out=pfx[0][:].rearrange("p t h -> p (t h)")[:, c0:c1], in_=mm_ps[1][:, 0:w],
                 rd=[B_mm[1]], wr=[B_pfx[0]])
        cur = 0
        sh = 1
        while sh < NB:
            nx = 1 - cur
            k.op(dve, "tensor_copy", out=pfx[nx][:, 0:sh, :], in_=pfx[cur][:, 0:sh, :], rd=[B_pfx[cur]], wr=[B_pfx[nx]])
            k.op(dve, "tensor_tensor", out=pfx[nx][:, sh:NB, :], in0=pfx[cur][:, sh:NB, :], in1=pfx[cur][:, 0:NB - sh, :],
                 op=ALU.add, rd=[B_pfx[cur]], wr=[B_pfx[nx]])
            cur = nx
            sh *= 2
        k.op(dve, "tensor_tensor", out=cumN[:, 1:NB, :], in0=cumN[:, 1:NB, :], in1=pfx[cur][:, 0:NB - 1, :], op=ALU.add,
             rd=[B_pfx[cur]], wr=[B_cumN])
        rsel_sb = sb("rsel_sb", [128, 4], F32, stB)
        B_rsel = Buf()
        k.dma(sp, rsel_sb[:], rsel, k.dsem(), wr=[B_rsel])
        cq = sb("cq", [128, NOWN, 16], F32, stB)
        B_cq = Buf()
        cumN4 = cumN[:].rearrange("p (j u) h -> p j u h", u=4)
        k.op(dve, "tensor_scalar", out=cq[:], in0=cumN4[:, :, 0, :], scalar1=rsel_sb[:, 0:1], scalar2=-8.0, op0=ALU.mult,
             op1=ALU.mult, rd=[B_cumN, B_rsel], wr=[B_cq])
        cq8 = sb("cq8", [128, NOWN, 16], F32, stB)
        for u in range(1, 4):
            k.op(dve, "tensor_scalar", out=cq8[:], in0=cumN4[:, :, u, :], scalar1=rsel_sb[:, u:u + 1], scalar2=-8.0,
                 op0=ALU.mult, op1=ALU.mult, rd=[B_cumN, B_rsel], wr=[B_tmpg])
            k.op(dve, "tensor_tensor", out=cq[:], in0=cq[:], in1=cq8[:], op=ALU.add, rd=[B_tmpg], wr=[B_cq])
        NQ = NOWN * 16
        cqf = cq[:].rearrange("p j h -> p (j h)")
        c3 = [sb("c3_%d" % i, [128, NQ], BF16, stB) for i in range(3)]
        B_c3 = [Buf() for _ in range(3)]
        for i in range(3):
            k.op(dve, "tensor_copy", out=c3[i][:], in_=cqf, rd=[B_cq], wr=[B_c3[i]])
            if i < 2:
                k.op(dve, "tensor_tensor", out=cqf, in0=cqf, in1=c3[i][:], op=ALU.subtract, rd=[B_c3[i]], wr=[B_cq])
        qa_sb = sb("qa_sb", [128, 3, 128], BF16, stB)
        B_qasb = Buf()
        ds_qa = k.dsem()
        for c0 in range(0, NQ, 128):
            w = min(128, NQ - c0)
            for i in range(3):
                k.op(pe, "transpose", out=hT_ps[0][0:w, i * 128:(i + 1) * 128], in_=c3[i][:, c0:c0 + w],
                     identity=ident_b[:], rd=[B_c3[i], B_identb], wr=[B_hTps[0]])
            k.op(dve, "tensor_copy", out=qa_sb[0:w, :, :], in_=hT_ps[0][0:w, 0:384].rearrange("p (k t) -> p k t", k=3),
                 rd=[B_hTps[0]], wr=[B_qasb])
            k.dma(sp, QAUG_s[c0:c0 + w, :, :], qa_sb[0:w, :, :], ds_qa, rd=[B_qasb])
        k.barrier()
        stB.close()

        stOA = contextlib.ExitStack()
        es.enter_context(stOA)
        o_a = sb("o_a", [128, NOWN, 1024], BF16, stOA)
        B_oa = [Buf() for _ in range(NOWN)]
        esink = sb("esink", [128, 16], F32, stOA)
        B_esink = Buf()
        vec_bcast_load(esink[:], B_esink, sinks, k.dsem(), 16)
        k.op(act, "activation", out=esink[:], in_=esink[:], func=AF.Exp, rd=[B_esink], wr=[B_esink])

        stC = contextlib.ExitStack()
        es.enter_context(stC)
        G1s = sb("G1s_c", [128, D], F32, stC)
        SHa = sb("SHa_c", [128, D], F32, stC)
        B_G1s, B_SHa = Buf(), Buf()
        bcast_load(G1s, B_G1s, 1, k.dsem())
        bcast_load(SHa, B_SHa, 0, k.dsem())
        Wq = sb("Wq", [128, KC, 2048], BF16, stC)
        Wkv = sb("Wkv", [128, KC, 512], BF16, stC)
        B_Wq, B_Wkv = Buf(), Buf()
        for kc in range(KC):
            k.dma(pool, Wq[:, kc, 0:1024], w_in_v[:, kc, QA0:QA0 + 1024], None, wr=[B_Wq])
            k.dma(pool, Wq[:, kc, 1024:2048], w_in_v[:, kc, QB0:QB0 + 1024], None, wr=[B_Wq])
        k.dma(pool, Wkv[:], w_in_v[:, :, KA0:KA0 + 512], None, wr=[B_Wkv])
        swaA_sb = sb("swaA_sb", [128, 3 * 4 * 512], BF16, stC)
        B_swaA = Buf()
        for i in range(6):
            k.dma(pool, swaA_sb[:, i * 1024:(i + 1) * 1024], swaA[:, i * 1024:(i + 1) * 1024], None, wr=[B_swaA])
        NXS = 2
        xts = [sb("xtc%d" % i, [128, D], F32, stC) for i in range(NXS)]
        B_xt = [Buf() for _ in range(NXS)]
        vec_bcast_load(xts[0][:], B_xt[0], g_mix, k.dsem())
        k.op(dve, "scalar_tensor_tensor", out=G1s[:], in0=G1s[:], scalar=SQD, in1=xts[0][:], op0=ALU.mult, op1=ALU.mult,
             rd=[B_xt[0]], wr=[B_G1s])
        hb = [sb("hbc%d" % i, [128, D], BF16, stC) for i in range(2)]
        B_hb = [Buf(), Buf()]
        hT2 = [sb("hT2_%d" % i, [128, KC, 128], BF16, stC) for i in range(2)]
        B_hT2 = [Buf(), Buf()]
        hT_ps = [ps("hT_psc%d" % i, [128, D], BF16, stC) for i in range(1)]
        B_hTps = [Buf()]
        mm_ps = [ps("mm_psc%d" % i, [128, 512], F32, stC) for i in range(2)]
        B_mm = [Buf(), Buf()]
        s_ps = ps("s_psc", [128, 512], F32, stC)
        B_sps = Buf()
        o_ps = ps("o_psc", [128, 512], F32, stC)
        B_ops = Buf()
        tr_ps = ps("tr_psc", [128, 512], F32, stC)
        B_trps = Buf()
        q_tok = sb("q_tok", [128, 2048], BF16, stC)
        B_qtok = Buf()
        kv_tok = [sb("kv_tok%d" % i, [128, 512], BF16, stC) for i in range(2)]
        B_kvtok = [Buf(), Buf()]
        qaT = sb("qaT", [64, 16, 128], BF16, stC)
        qbT = sb("qbT", [64, 16, 128], BF16, stC)
        kaT = sb("kaT", [64, 2, 4, 128], BF16, stC)
        va = sb("va", [128, 2, 4, 65], BF16, stC)
        B_qaT, B_qbT, B_kaT, B_va = Buf(), Buf(), Buf(), Buf()
        k.op(pool, "memset", va[:], 1.0, wr=[B_va])
        pT = [sb("pTc%d" % i, [128, 512], BF16, stC) for i in range(2)]
        B_pT = [Buf(), Buf()]
        oT_sb2 = [sb("oT_sbc%d" % i, [65, 512], F32, stC) for i in range(2)]
        B_oTsb2 = [Buf(), Buf()]
        den = sb("denc", [128, 8], F32, stC)
        B_den = Buf()
        ds_qb = k.dsem()
        cnt = {"xt": 0, "hb": 0, "mm": 0, "hTps": 0, "pT": 0}
        QT_v = QT_s.rearrange("(h d) t -> d h t", d=64)

        for j in range(NOWN):
            for which, src in ((0, x_own), (1, x_prev)):
                h_t, B_h = make_h(src[j * 128:(j + 1) * 128, :])
                transpose_to(h_t, B_h, hT2[which][:], B_hT2[which])
            for n in range(4):
                ms = cnt["mm"] % 2
                cnt["mm"] += 1
                for kc in range(KC):
                    k.op(pe, "matmul", mm_ps[ms][:], lhsT=hT2[0][:, kc, :], rhs=Wq[:, kc, n * 512:(n + 1) * 512],
                         start=(kc == 0), stop=(kc == KC - 1), rd=[B_Wq, B_hT2[0]], wr=[B_mm[ms]])
                k.op(dve if n % 2 else act, "tensor_copy" if n % 2 else "copy", out=q_tok[:, n * 512:(n + 1) * 512],
                     in_=mm_ps[ms][:], rd=[B_mm[ms]], wr=[B_qtok])
            for which in range(2):
                ms = cnt["mm"] % 2
                cnt["mm"] += 1
                for kc in range(KC):
                    k.op(pe, "matmul", mm_ps[ms][:], lhsT=hT2[which][:, kc, :], rhs=Wkv[:, kc, :],
                         start=(kc == 0), stop=(kc == KC - 1), rd=[B_Wkv, B_hT2[which]], wr=[B_mm[ms]])
                k.op(dve, "tensor_copy", out=kv_tok[which][:], in_=mm_ps[ms][:], rd=[B_mm[ms]], wr=[B_kvtok[which]])
                kb = 1 - which
                k.op(dve, "tensor_copy", out=va[:, kb, :, 0:64],
                     in_=kv_tok[which][:, 256:512].rearrange("p (h d) -> p h d", h=4), rd=[B_kvtok[which]], wr=[B_va])
            for half, dstT, B_dst in ((0, qaT, B_qaT), (1, qbT, B_qbT)):
                for hh in range(2):
                    s = cnt["hTps"] % len(hT_ps)
                    cnt["hTps"] += 1
                    for i8 in range(8):
                        g_ = hh * 8 + i8
                        c0 = half * 1024 + g_ * 64
                        k.op(pe, "transpose", out=hT_ps[s][0:64, i8 * 128:(i8 + 1) * 128], in_=q_tok[:, c0:c0 + 64],
                             identity=ident_b[:], rd=[B_qtok, B_identb], wr=[B_hTps[s]])
                    k.op(act if hh else dve, "copy" if hh else "tensor_copy", out=dstT[:, hh * 8:(hh + 1) * 8, :],
                         in_=hT_ps[s][0:64, 0:1024].rearrange("p (h t) -> p h t", h=8), rd=[B_hTps[s]], wr=[B_dst])
            k.dma(sp, QT_v[:, :, j * 128:(j + 1) * 128], qbT[:], ds_qb, rd=[B_qbT])
            s = cnt["hTps"] % len(hT_ps)
            cnt["hTps"] += 1
            for which in range(2):
                kb = 1 - which
                for hk in range(4):
                    k.op(pe, "transpose", out=hT_ps[s][0:64, (kb * 4 + hk) * 128:(kb * 4 + hk + 1) * 128],
                         in_=kv_tok[which][:, hk * 64:(hk + 1) * 64], identity=ident_b[:],
                         rd=[B_kvtok[which], B_identb], wr=[B_hTps[s]])
            k.op(dve, "tensor_copy", out=kaT[:].rearrange("p a h t -> p (a h) t"),
                 in_=hT_ps[s][0:64, 0:1024].rearrange("p (h t) -> p h t", h=8), rd=[B_hTps[s]], wr=[B_kaT])
            units = [(hk_, kb_) for hk_ in range(4) for kb_ in range(2)]
            s_bufs = [(s_ps, B_sps), (mm_ps[1], B_mm[1])]
            o_bufs = [(o_ps, B_ops), (mm_ps[0], B_mm[0])]

            def swa_S(u):
                hk, kb = units[u]
                sb_, Bsb = s_bufs[u % 2]
                for i in range(4):
                    k.op(pe, "matmul", sb_[:, i * 128:(i + 1) * 128], lhsT=kaT[:, kb, hk, :],
                         rhs=qaT[:, hk * 4 + i, :], start=True, stop=True, rd=[B_kaT, B_qaT], wr=[Bsb])
                p_ = u % 2
                k.op(act, "activation", out=pT[p_][:], in_=sb_[:], func=AF.Exp, scale=0.125, rd=[Bsb], wr=[B_pT[p_]])
                tab = (0 if j == 0 else 1) if kb == 0 else 2
                a0 = (tab * 4 + hk) * 512
                k.op(dve, "tensor_tensor", out=pT[p_][:], in0=pT[p_][:], in1=swaA_sb[:, a0:a0 + 512], op=ALU.mult,
                     rd=[B_swaA], wr=[B_pT[p_]])

            def swa_PV(u):
                hk, kb = units[u]
                ob, Bob = o_bufs[hk % 2]
                k.op(pe, "matmul", ob[0:65, :], lhsT=va[:, kb, hk, :], rhs=pT[u % 2][:], start=(kb == 0),
                     stop=(kb == 1), rd=[B_va, B_pT[u % 2]], wr=[Bob])
                if kb == 1:
                    k.op(act, "copy", out=oT_sb2[hk % 2][:], in_=ob[0:65, :], rd=[Bob], wr=[B_oTsb2[hk % 2]])

            def swa_tail(hk):
                osb, Bosb = oT_sb2[hk % 2], B_oTsb2[hk % 2]
                for i in range(4):
                    k.op(pe, "transpose", out=tr_ps[:, i * 65:(i + 1) * 65], in_=osb[:, i * 128:(i + 1) * 128],
                         identity=ident_f[0:65, 0:65], rd=[Bosb, B_cst], wr=[B_trps])
                tr3 = tr_ps[:, 0:260].rearrange("p (h c) -> p h c", h=4)
                k.op(dve, "tensor_tensor", out=den[:, 0:4], in0=tr3[:, :, 64], in1=esink[:, hk * 4:(hk + 1) * 4],
                     op=ALU.add, rd=[B_trps, B_esink], wr=[B_den])
                k.op(dve, "reciprocal", out=den[:, 4:8], in_=den[:, 0:4], rd=[B_den], wr=[B_den])
                for i in range(4):
                    g_ = hk * 4 + i
                    k.op(dve, "tensor_scalar", out=o_a[:, j, g_ * 64:(g_ + 1) * 64], in0=tr3[:, i, 0:64],
                         scalar1=den[:, 4 + i:5 + i], scalar2=None, op0=ALU.mult, rd=[B_trps, B_den], wr=[B_oa[j]])

            swa_S(0)
            tail_q = []
            for u in range(8):
                if u + 1 < 8:
                    swa_S(u + 1)
                swa_PV(u)
                if tail_q:
                    swa_tail(tail_q.pop(0))
                if units[u][1] == 1:
                    tail_q.append(units[u][0])
            while tail_q:
                swa_tail(tail_q.pop(0))
        k.barrier()
        stC.close()

        stOB = contextlib.ExitStack()
        es.enter_context(stOB)
        o_b = sb("o_b", [128, NOWN, 1024], BF16, stOB)
        B_ob = [Buf() for _ in range(NOWN)]
        stW = contextlib.ExitStack()
        es.enter_context(stW)
        Wout = sb("Wout", [128, KC, D], BF16, stW)
        B_Wout = Buf()
        w_out_v = w_out.rearrange("(kc p) n -> p kc n", p=128)
        for kc in range(KC):
            for hh in range(2):
                k.dma(pool, Wout[:, kc, hh * 1024:(hh + 1) * 1024], w_out_v[:, kc, hh * 1024:(hh + 1) * 1024], None,
                      wr=[B_Wout])
        stF = contextlib.ExitStack()
        es.enter_context(stF)
        kTa = [sb("kTa%d" % i, [67, S], BF16, stF) for i in range(2)]
        vau = [sb("vau%d" % i, [128, NB, 65], BF16, stF) for i in range(2)]
        qTa = [sb("qTa%d" % i, [67, TOWN], BF16, stF) for i in range(2)]
        B_kTa, B_vau, B_qTa = [Buf(), Buf()], [Buf(), Buf()], [Buf(), Buf()]
        ds_hk = [k.dsem(), k.dsem()]
        ds_hq = [k.dsem(), k.dsem()]
        ds_hv = [k.dsem(), k.dsem()]
        for i in range(2):
            k.op(pool, "memset", kTa[i][64:67, :], 1.0, wr=[B_kTa[i]])
            k.op(pool, "memset", vau[i][:], 1.0, wr=[B_vau[i]])
        fm_sb = sb("fm_sb", [128, 512], BF16, stF)
        B_fm = Buf()
        k.dma(pool, fm_sb[:], fmask, None, wr=[B_fm])
        NPT = 3
        pT = [sb("pTf%d" % i, [128, 1024], BF16, stF) for i in range(NPT)]
        B_pT = [Buf() for _ in range(NPT)]
        NSP = 3
        s_ps = [ps("s_psf%d" % i, [128, 1024], F32, stF) for i in range(NSP)]
        B_sps = [Buf() for _ in range(NSP)]
        o_ps = ps("o_psf", [128, 1024], F32, stF)
        B_ops = Buf()
        tr_ps = [s_ps[0][:, 0:512], s_ps[0][:, 512:1024]]
        B_trps = [B_sps[0], B_sps[0]]
        oT_sb = sb("oT_sbf", [65, 1024], F32, stF)
        B_oTsb = Buf()
        den = sb("denf", [128, 8], F32, stF)
        B_den = Buf()
        VB_v = VB_s.rearrange("(t p) c -> p t c", p=128)
        QAUG_v = QAUG_s.rearrange("(j h) k t -> h k j t", h=16)
        HB = (NOWN + 1) // 2
        halves = [(0, HB), (HB, NOWN)] if NOWN > 1 else [(0, 1)]
        cnt = {"pT": 0, "s": 0, "tr": 0}

        def load_head(h):
            s = h % 2
            k.dma(sp, kTa[s][0:64, :], KT_s[h * 64:(h + 1) * 64, :], ds_hk[s], wr=[B_kTa[s]])
            k.dma(sp, qTa[s][0:64, :], QT_s[h * 64:(h + 1) * 64, :], ds_hq[s], wr=[B_qTa[s]])
            k.dma(sp, qTa[s][64:67, :].rearrange("k (j t) -> k j t", t=128), QAUG_v[h], ds_hq[s], wr=[B_qTa[s]])
            for t0 in range(0, NB, 16):
                t1 = min(NB, t0 + 16)
                k.dma(sp, vau[s][:, t0:t1, 0:64], VB_v[:, t0:t1, h * 64:(h + 1) * 64], ds_hv[s], wr=[B_vau[s]])

        load_head(0)
        for h in range(16):
            hs = h % 2
            if h + 1 < 16:
                load_head(h + 1)
            for (j0, j1) in halves:
                nb = j1 - j0
                def stage1(kt):
                    g = kt // 4
                    u = kt % 4
                    ja = max(g, j0)
                    c_lo = (ja - j0) * 128
                    c_hi = nb * 128
                    ss = cnt["s"] % NSP
                    cnt["s"] += 1
                    for b0 in range(0, 1024, 512):
                        lo, hi = max(c_lo, b0), min(c_hi, b0 + 512)
                        if lo >= hi:
                            continue
                        k.op(pe, "matmul", s_ps[ss][:, lo:hi], lhsT=kTa[hs][:, kt * 128:(kt + 1) * 128],
                             rhs=qTa[hs][:, j0 * 128 + lo:j0 * 128 + hi], start=True, stop=True,
                             rd=[B_kTa[hs], B_qTa[hs]], wr=[B_sps[ss]])
                    p_ = cnt["pT"] % NPT
                    cnt["pT"] += 1
                    k.op(act, "activation", out=pT[p_][:, c_lo:c_hi], in_=s_ps[ss][:, c_lo:c_hi], func=AF.Exp,
                         bias=cumN[:, kt, h:h + 1], scale=0.125, rd=[B_sps[ss], B_cumN], wr=[B_pT[p_]])
                    if g >= j0:
                        k.op(dve, "scalar_tensor_tensor", out=pT[p_][:, c_lo:c_lo + 128], in0=pT[p_][:, c_lo:c_lo + 128],
                             scalar=1e30, in1=fm_sb[:, u * 128:(u + 1) * 128], op0=ALU.min, op1=ALU.mult,
                             rd=[B_fm], wr=[B_pT[p_]])
                    return p_

                def stage2(kt, p_):
                    g = kt // 4
                    u = kt % 4
                    ja = max(g, j0)
                    c_lo = (ja - j0) * 128
                    c_hi = nb * 128
                    started = set()

                    def st_flag(lo_):
                        bank = lo_ // 512
                        if kt == 0 and bank not in started:
                            started.add(bank)
                            return True
                        return False
                    if g >= j0:
                        k.op(pe, "matmul", o_ps[0:65, c_lo:c_lo + 128], lhsT=vau[hs][:, kt, :],
                             rhs=pT[p_][:, c_lo:c_lo + 128], start=st_flag(c_lo), stop=(u == 3), skip_group_check=True,
                             rd=[B_vau[hs], B_pT[p_]], wr=[B_ops])
                        r_lo = c_lo + 128
                    else:
                        r_lo = c_lo
                    for b0 in range(0, 1024, 512):
                        lo, hi = max(r_lo, b0), min(c_hi, b0 + 512)
                        if lo >= hi:
                            continue
                        k.op(pe, "matmul", o_ps[0:65, lo:hi], lhsT=vau[hs][:, kt, :], rhs=pT[p_][:, lo:hi],
                             start=st_flag(lo), stop=False, skip_group_check=True, rd=[B_vau[hs], B_pT[p_]], wr=[B_ops])

                nkt = 4 * j1
                pq = []
                for kt in range(nkt + 2):
                    if kt < nkt:
                        pq.append((kt, stage1(kt)))
                    if kt >= 2:
                        k0, p0 = pq.pop(0)
                        stage2(k0, p0)
                assert not pq
                for b0 in range(0, nb * 128, 512):
                    b1 = min(nb * 128, b0 + 512)
                    k.op(act, "copy", out=oT_sb[:, b0:b1], in_=o_ps[0:65, b0:b1], rd=[B_ops], wr=[B_oTsb])
                for q0 in range(0, nb, 4):
                    q1 = min(nb, q0 + 4)
                    ts_ = cnt["tr"] % 2
                    cnt["tr"] += 1
                    for i in range(q1 - q0):
                        k.op(pe, "transpose", out=tr_ps[ts_][:, i * 65:(i + 1) * 65],
                             in_=oT_sb[:, (q0 + i) * 128:(q0 + i + 1) * 128], identity=ident_f[0:65, 0:65],
                             rd=[B_oTsb, B_cst], wr=[B_trps[ts_]])
                    tr3 = tr_ps[ts_][:, 0:260].rearrange("p (h c) -> p h c", h=4)
                    k.op(dve, "reciprocal", out=den[:, 0:q1 - q0], in_=tr3[:, 0:q1 - q0, 64], rd=[B_trps[ts_]], wr=[B_den])
                    for i in range(q1 - q0):
                        jj = j0 + q0 + i
                        k.op(dve, "tensor_scalar", out=o_b[:, jj, h * 64:(h + 1) * 64], in0=tr3[:, i, 0:64],
                             scalar1=den[:, i:i + 1], scalar2=None, op0=ALU.mult, rd=[B_trps[ts_], B_den], wr=[B_ob[jj]])
        k.barrier()
        stF.close()

        stD = contextlib.ExitStack()
        es.enter_context(stD)
        gout = sb("gout", [128, D], F32, stD)
        GA = sb("GA", [128, D], F32, stD)
        B_gout, B_GA = Buf(), Buf()
        vec_bcast_load(gout[:], B_gout, g_out, k.dsem())
        k.op(dve, "tensor_scalar", out=gout[:], in0=gout[:], scalar1=32.0, scalar2=None, op0=ALU.mult, wr=[B_gout])
        bcast_load(GA, B_GA, 2, k.dsem())
        xts = [sb("xtd%d" % i, [128, D], F32, stD) for i in range(2)]
        B_xt = [Buf(), Buf()]
        ds_xt = [k.dsem(), k.dsem()]
        x1t = [sb("x1t%d" % i, [128, D], F32, stD) for i in range(2)]
        B_x1t = [Buf(), Buf()]
        ds_x1 = [k.dsem(), k.dsem()]
        mixed = [sb("mixed%d" % i, [128, D], BF16, stD) for i in range(2)]
        B_mixed = [Buf(), Buf()]
        mT = [sb("mT%d" % i, [128, KC, 128], BF16, stD) for i in range(2)]
        B_mT = [Buf(), Buf()]
        hT_ps = [ps("hT_psd%d" % i, [128, D], BF16, stD) for i in range(2)]
        B_hTps = [Buf(), Buf()]
        mm_ps = [ps("mm_psd%d" % i, [128, 512], F32, stD) for i in range(4)]
        B_mm = [Buf() for _ in range(4)]
        cnt = {"mm": 0, "hTps": 0}

        def d1_prep(j):
            s_ = j % 2
            k.dma(sp, xts[s_][:], x_own[j * 128:(j + 1) * 128, :], ds_xt[s_], wr=[B_xt[s_]])
            ra, Bra = rms_scale(o_a[:, j, :], B_oa[j], 1024, 1024 * EPS)
            rb, Brb = rms_scale(o_b[:, j, :], B_ob[j], 1024, 1024 * EPS)
            k.op(dve, "scalar_tensor_tensor", out=mixed[s_][:, 0:1024], in0=o_a[:, j, :], scalar=ra, in1=gout[:, 0:1024],
                 op0=ALU.mult, op1=ALU.mult, rd=[B_oa[j], Bra, B_gout], wr=[B_mixed[s_]])
            k.op(dve, "scalar_tensor_tensor", out=mixed[s_][:, 1024:2048], in0=o_b[:, j, :], scalar=rb,
                 in1=gout[:, 1024:2048], op0=ALU.mult, op1=ALU.mult, rd=[B_ob[j], Brb, B_gout], wr=[B_mixed[s_]])

        def d1_tr(j):
            s_ = j % 2
            transpose_to(mixed[s_], B_mixed[s_], mT[s_][:], B_mT[s_])

        d1_prep(0)
        d1_tr(0)
        for j in range(NOWN):
            s = j % 2
            for n in range(4):
                ms = cnt["mm"] % 4
                cnt["mm"] += 1
                for kc in range(KC):
                    k.op(pe, "matmul", mm_ps[ms][:], lhsT=mT[s][:, kc, :], rhs=Wout[:, kc, n * 512:(n + 1) * 512],
                         start=(kc == 0), stop=(kc == KC - 1), rd=[B_Wout, B_mT[s]], wr=[B_mm[ms]])
                sl = slice(n * 512, (n + 1) * 512)
                k.op(dve, "tensor_tensor", out=x1t[s][:, sl], in0=mm_ps[ms][:], in1=GA[:, sl], op=ALU.mult,
                     rd=[B_mm[ms], B_GA], wr=[B_x1t[s]])
                k.op(pool, "tensor_tensor", out=x1t[s][:, sl], in0=x1t[s][:, sl], in1=xts[s][:, sl], op=ALU.add,
                     rd=[B_xt[s]], wr=[B_x1t[s]])
                if j + 1 < NOWN:
                    if n == 0:
                        d1_prep(j + 1)
                    if n == 2:
                        d1_tr(j + 1)
            k.dma(sp, X1_s[j * 128:(j + 1) * 128, :], x1t[s][:], ds_x1[s], rd=[B_x1t[s]])
        k.barrier()
        stD.close()
        stW.close()
        stOB.close()
        stOA.close()

        rt_w = sb("rt_w", [128, NOWN, 2], F32)
        rt_pg = sb("rt_pg", [128, NOWN, 2], I32)
        B_rtw, B_rtpg = Buf(), Buf()
        stR = contextlib.ExitStack()
        es.enter_context(stR)
        G2s = sb("G2s", [128, D], F32, stR)
        SHm = sb("SHm", [128, D], F32, stR)
        tmp2 = sb("tmp2", [128, D], F32, stR)
        B_G2s, B_SHm, B_tmp2 = Buf(), Buf(), Buf()
        bcast_load(G2s, B_G2s, 4, k.dsem())
        bcast_load(SHm, B_SHm, 3, k.dsem())
        vec_bcast_load(tmp2[:], B_tmp2, g_moe, k.dsem())
        k.op(dve, "scalar_tensor_tensor", out=G2s[:], in0=G2s[:], scalar=SQD, in1=tmp2[:], op0=ALU.mult, op1=ALU.mult,
             rd=[B_tmp2], wr=[B_G2s])
        Wr = sb("Wr", [128, KC, 36], F32, stR)
        B_Wr = Buf()
        k.dma(sp, Wr[:], w_r.rearrange("(kc p) n -> p kc n", p=128), k.dsem(), wr=[B_Wr])
        bR = sb("bR", [128, 36], F32, stR)
        B_bR = Buf()
        vec_bcast_load(bR[:], B_bR, b_r, k.dsem(), 36)
        eb_sb = sb("eb_sb", [128, NE], F32, stR)
        B_eb = Buf()
        k.dma(sp, eb_sb[:], ebase, k.dsem(), wr=[B_eb])
        cnt_run = sb("cnt_run", [128, NE], F32, stR)
        B_cnt = Buf()
        k.op(dve, "memset", cnt_run[:], 0.0, wr=[B_cnt])
        zt = sb("zt", [128, D], BF16, stR)
        B_zt = Buf()
        k.op(pool, "memset", zt[:], 0.0, wr=[B_zt])
        ds_z = k.dsem()
        B_Xs = Buf()
        for s0 in range(0, NSLOT, 128):
            k.dma(sp, Xs_s[s0:s0 + 128, :], zt[:], ds_z, rd=[B_zt], wr=[B_Xs])
        x1t = [sb("x1r%d" % i, [128, D], F32, stR) for i in range(2)]
        B_x1t = [Buf(), Buf()]
        ds_x1 = [k.dsem(), k.dsem()]
        h2b = [sb("h2b%d" % i, [128, D], BF16, stR) for i in range(2)]
        B_h2b = [Buf(), Buf()]
        ds_sc = [k.dsem(), k.dsem()]
        h2T = sb("h2T", [128, KC, 128], F32, stR)
        B_h2T = Buf()
        trf_ps = [ps("trf_ps%d" % i, [128, 1024], F32, stR) for i in range(2)]
        B_trf = [Buf(), Buf()]
        lg_ps = ps("lg_ps", [128, 512], F32, stR)
        B_lgps = Buf()
        rk_ps = ps("rk_ps", [128, 512], F32, stR)
        B_rkps = Buf()
        R = sb("R", [128, 512], F32, stR)
        B_R = Buf()
        psc = [sb("psc%d" % i, [128, 2], I32, stR) for i in range(2)]
        B_psc = [Buf(), Buf()]
        BIGI = float(4 * NSLOT)

        def rop(method, **kw):
            return k.op(dve, method, rd=[B_R], wr=[B_R], **kw)

        for j in range(NOWN):
            s = j % 2
            k.dma(sp, x1t[s][:], X1_s[j * 128:(j + 1) * 128, :], ds_x1[s], wr=[B_x1t[s]])
            r2, Br2 = rms_scale(x1t[s][:], B_x1t[s], D, D * EPS)
            k.op(dve, "scalar_tensor_tensor", out=x1t[s][:], in0=x1t[s][:], scalar=r2, in1=G2s[:], op0=ALU.mult,
                 op1=ALU.mult, rd=[Br2, B_G2s], wr=[B_x1t[s]])
            k.op(dve, "tensor_tensor", out=x1t[s][:], in0=x1t[s][:], in1=SHm[:], op=ALU.add, rd=[B_SHm], wr=[B_x1t[s]])
            k.op(act, "copy", out=h2b[s][:], in_=x1t[s][:], rd=[B_x1t[s]], wr=[B_h2b[s]])
            for hh in range(2):
                for i8 in range(8):
                    kc = hh * 8 + i8
                    k.op(pe, "transpose", out=trf_ps[hh][:, i8 * 128:(i8 + 1) * 128], in_=x1t[s][:, kc * 128:(kc + 1) * 128],
                         identity=ident_f, rd=[B_x1t[s], B_cst], wr=[B_trf[hh]])
                k.op(act if hh else dve, "copy" if hh else "tensor_copy", out=h2T[:, hh * 8:(hh + 1) * 8, :],
                     in_=trf_ps[hh][:].rearrange("p (k t) -> p k t", k=8), rd=[B_trf[hh]], wr=[B_h2T])
            for kc in range(KC):
                k.op(pe, "matmul", lg_ps[:, 0:36], lhsT=h2T[:, kc, :], rhs=Wr[:, kc, :], start=(kc == 0),
                     stop=(kc == KC - 1), rd=[B_h2T, B_Wr], wr=[B_lgps])
            LG = R[:, 0:36]; GL = R[:, 0:4]; EL = R[:, 4:36]
            GMAX = R[:, 40:41]; NGMAX = R[:, 41:42]; GSUM = R[:, 42:43]; GVAL = R[:, 43:44]
            GOH = R[:, 44:48]; GPEN = R[:, 48:52]; GEXP = R[:, 52:56]
            EM = R[:, 64:96]; T1 = R[:, 96:97]; T2 = R[:, 97:98]; OH1 = R[:, 100:132]; OH2 = R[:, 132:164]
            E2 = R[:, 164:196]; AA = R[:, 196:228]; RK = R[:, 228:260]; RKB = R[:, 260:292]; TMP = R[:, 292:324]
            DD = R[:, 324:325]; ED = R[:, 325:326]; W1 = R[:, 326:327]; W2 = R[:, 327:328]
            P1 = R[:, 328:329]; P2 = R[:, 329:330]; R1 = R[:, 330:331]; R2 = R[:, 331:332]
            V1 = R[:, 332:333]; V2 = R[:, 333:334]; OF1 = R[:, 334:335]; OF2 = R[:, 335:336]
            PS1 = R[:, 336:337]; PS2 = R[:, 337:338]
            k.op(dve, "tensor_tensor", out=LG, in0=lg_ps[:, 0:36], in1=bR[:], op=ALU.add, rd=[B_lgps, B_bR, B_R], wr=[B_R])
            rop("reduce_max", out=GMAX, in_=GL, axis=AX.X)
            rop("tensor_scalar", out=GOH, in0=GL, scalar1=GMAX, scalar2=None, op0=ALU.is_equal)
            rop("tensor_scalar", out=NGMAX, in0=GMAX, scalar1=-1.0, scalar2=None, op0=ALU.mult)
            k.op(act, "activation", out=GEXP, in_=GL, func=AF.Exp, bias=NGMAX, scale=1.0, rd=[B_R], wr=[B_R])
            rop("reduce_sum", out=GSUM, in_=GEXP, axis=AX.X)
            rop("reciprocal", out=GVAL, in_=GSUM)
            rop("tensor_scalar", out=GPEN, in0=GOH, scalar1=-1.0, scalar2=1e30, op0=ALU.add, op1=ALU.mult)
            for gi in range(4):
                rop("tensor_scalar", out=EM[:, gi * 8:(gi + 1) * 8], in0=EL[:, gi * 8:(gi + 1) * 8],
                    scalar1=GPEN[:, gi:gi + 1], scalar2=None, op0=ALU.add)
            rop("reduce_max", out=T1, in_=EM, axis=AX.X)
            rop("tensor_scalar", out=OH1, in0=EM, scalar1=T1, scalar2=None, op0=ALU.is_equal)
            rop("scalar_tensor_tensor", out=E2, in0=OH1, scalar=-1e30, in1=EM, op0=ALU.mult, op1=ALU.add)
            rop("reduce_max", out=T2, in_=E2, axis=AX.X)
            rop("tensor_scalar", out=OH2, in0=E2, scalar1=T2, scalar2=None, op0=ALU.is_equal)
            rop("tensor_tensor", out=DD, in0=T2, in1=T1, op=ALU.subtract)
            k.op(act, "activation", out=ED, in_=DD, func=AF.Exp, rd=[B_R], wr=[B_R])
            rop("tensor_scalar", out=ED, in0=ED, scalar1=1.0, scalar2=None, op0=ALU.add)
            rop("reciprocal", out=ED, in_=ED)
            rop("tensor_tensor", out=W1, in0=GVAL, in1=ED, op=ALU.mult)
            rop("tensor_tensor", out=W2, in0=GVAL, in1=W1, op=ALU.subtract)
            rop("tensor_tensor", out=AA, in0=OH1, in1=OH2, op=ALU.add)
            k.op(pe, "matmul", rk_ps[:, 0:32], lhsT=U_strict, rhs=AA, start=True, stop=True, rd=[B_R, B_cst], wr=[B_rkps])
            k.op(pe, "matmul", rk_ps[:, 32:64], lhsT=ones_f, rhs=AA, start=True, stop=True, rd=[B_R, B_cst], wr=[B_rkps])
            k.op(dve, "tensor_tensor", out=RK, in0=rk_ps[:, 0:32], in1=cnt_run[:], op=ALU.add, rd=[B_rkps, B_cnt, B_R],
                 wr=[B_R])
            k.op(dve, "tensor_tensor", out=cnt_run[:], in0=rk_ps[:, 32:64], in1=cnt_run[:], op=ALU.add, rd=[B_rkps, B_cnt],
                 wr=[B_cnt])
            k.op(dve, "tensor_tensor", out=RKB, in0=RK, in1=eb_sb[:], op=ALU.add, rd=[B_R, B_eb], wr=[B_R])
            for (OH, P_, R_, V_, OF_, PS_, W_, col) in ((OH1, P1, R1, V1, OF1, PS1, W1, 0), (OH2, P2, R2, V2, OF2, PS2, W2, 1)):
                rop("tensor_tensor", out=TMP, in0=OH, in1=RKB, op=ALU.mult)
                rop("reduce_sum", out=P_, in_=TMP, axis=AX.X)
                rop("tensor_tensor", out=TMP, in0=OH, in1=RK, op=ALU.mult)
                rop("reduce_sum", out=R_, in_=TMP, axis=AX.X)
                rop("tensor_scalar", out=V_, in0=R_, scalar1=float(CAP), scalar2=None, op0=ALU.is_lt)
                rop("tensor_scalar", out=OF_, in0=V_, scalar1=-BIGI, scalar2=BIGI, op0=ALU.mult, op1=ALU.add)
                rop("tensor_tensor", out=PS_, in0=P_, in1=OF_, op=ALU.add)
                k.op(dve, "tensor_copy", out=psc[s][:, col:col + 1], in_=PS_, rd=[B_R], wr=[B_psc[s]])
                rop("tensor_tensor", out=P_, in0=P_, in1=V_, op=ALU.mult)
                k.op(dve, "tensor_copy", out=rt_pg[:, j, col:col + 1], in_=P_, rd=[B_R], wr=[B_rtpg])
                k.op(dve, "tensor_tensor", out=rt_w[:, j, col:col + 1], in0=W_, in1=V_, op=ALU.mult, rd=[B_R], wr=[B_rtw])
            for col in range(2):
                k.idma(Xs_s, bass.IndirectOffsetOnAxis(ap=psc[s][:, col:col + 1], axis=0), h2b[s][:, :], None, ds_sc[s],
                       NSLOT - 1, rd=[B_h2b[s], B_psc[s], B_Xs])
        k.barrier()
        stR.close()

        stE = contextlib.ExitStack()
        es.enter_context(stE)
        wg = [sb("wg%d" % i, [128, KC, DE], BF16, stE) for i in range(2)]
        wu = [sb("wu%d" % i, [128, KC, DE], BF16, stE) for i in range(2)]
        wd = [sb("wd%d" % i, [128, 4, D], BF16, stE) for i in range(2)]
        B_we = [Buf(), Buf()]
        ds_we = [k.dsem(), k.dsem()]
        NXS_E = 4
        xs = [sb("xs%d" % i, [128, D], BF16, stE) for i in range(NXS_E)]
        B_xs = [Buf() for _ in range(NXS_E)]
        ds_xs = [k.dsem() for _ in range(NXS_E)]
        xsT = [sb("xsT%d" % i, [128, KC, 128], BF16, stE) for i in range(2)]
        B_xsT = [Buf(), Buf()]
        sg = [sb("sg%d" % i, [128, DE], BF16, stE) for i in range(2)]
        hid = [sb("hid%d" % i, [128, DE], BF16, stE) for i in range(2)]
        hidT = [sb("hidT%d" % i, [128, 4, 128], BF16, stE) for i in range(2)]
        B_sg, B_hid, B_hidT = [Buf(), Buf()], [Buf(), Buf()], [Buf(), Buf()]
        y_sb = [sb("y_sb%d" % i, [128, D], F32, stE) for i in range(2)]
        B_ysb = [Buf(), Buf()]
        ds_y = [k.dsem(), k.dsem()]
        hT_ps = [ps("hT_pse%d" % i, [128, 1024], BF16, stE) for i in range(1)]
        B_hTps = [Buf()]
        g_ps = [ps("g_ps%d" % i, [128, 512], F32, stE) for i in range(2)]
        u_ps = [ps("u_ps%d" % i, [128, 512], F32, stE) for i in range(2)]
        B_gps, B_ups = [Buf(), Buf()], [Buf(), Buf()]
        ht_ps = ps("ht_ps", [128, 512], BF16, stE)
        B_htps = Buf()
        y_ps = [ps("y_ps%d" % i, [128, 512], F32, stE) for i in range(2)]
        B_yps = [Buf(), Buf()]
        cnt = {"hTps": 0, "y": 0}

        NSTG = 6
        stg = [sb("stg%d" % i, [128, 2048], F32, stE) for i in range(NSTG)]
        B_stg = [Buf() for _ in range(NSTG)]
        ds_stg = [k.dsem() for _ in range(NSTG)]
        cast_rr = [0]
        B_wch = [[Buf() for _ in range(12)] for _ in range(2)]

        def expert_chunks(e):
            s = e % 2
            wgv = w_gate[e].rearrange("(p kc) n -> p kc n", kc=KC)
            wuv = w_up[e].rearrange("(p kc) n -> p kc n", kc=KC)
            wdv = w_down[e].rearrange("(kc p) n -> p kc n", p=128)
            tasks = []
            for q in range(4):
                tasks.append((wgv[:, q * 4:(q + 1) * 4, :], wg[s][:, q * 4:(q + 1) * 4, :], "p (k n) -> p k n", 4))
            for q in range(4):
                tasks.append((wuv[:, q * 4:(q + 1) * 4, :], wu[s][:, q * 4:(q + 1) * 4, :], "p (k n) -> p k n", 4))
            for q in range(4):
                tasks.append((wdv[:, q, :], wd[s][:, q, :], None, 1))
            out_ = []
            for ci, (src, dst, rr, kk) in enumerate(tasks):
                def task(src=src, dst=dst, rr=rr, kk=kk, s=s, ci=ci):
                    i = cast_rr[0] % NSTG
                    c = cast_rr[0]
                    cast_rr[0] += 1
                    sview = stg[i][:].rearrange(rr, k=kk) if rr else stg[i][:]
                    k.dma(sp, sview, src, ds_stg[i], wr=[B_stg[i]])
                    eng = (dve, act)[c % 2]
                    k.op(eng, "copy" if eng is act else "tensor_copy", out=dst, in_=sview, rd=[B_stg[i]], wr=[B_wch[s][ci]])
                out_.append(task)
            return out_

        blocks = [(e, blk) for e in range(NE) for blk in range(NBLK)]
        NBK = len(blocks)

        def xs_load(i):
            e, blk = blocks[i]
            row0 = e * CAP + blk * 128
            sl = i % NXS_E
            k.dma(pool, xs[sl][:], Xs_s[row0:row0 + 128, :], ds_xs[sl], wr=[B_xs[sl]])

        def stA(i):
            sl = i % NXS_E
            tl = i % 2
            for hh in range(2):
                for i8 in range(8):
                    kc = hh * 8 + i8
                    k.op(pe, "transpose", out=hT_ps[0][:, i8 * 128:(i8 + 1) * 128], in_=xs[sl][:, kc:D:KC],
                         identity=ident_b[:], rd=[B_xs[sl], B_identb], wr=[B_hTps[0]])
                k.op(dve if hh else act, "tensor_copy" if hh else "copy", out=xsT[tl][:, hh * 8:(hh + 1) * 8, :],
                     in_=hT_ps[0][:].rearrange("p (k t) -> p k t", k=8), rd=[B_hTps[0]], wr=[B_xsT[tl]])

        def stB(i):
            e, blk = blocks[i]
            es_ = e % 2
            sl = i % 2
            for kc in range(KC):
                k.op(pe, "matmul", g_ps[sl][:], lhsT=xsT[sl][:, kc, :], rhs=wg[es_][:, kc, :], start=(kc == 0),
                     stop=(kc == KC - 1), rd=[B_xsT[sl]] + B_wch[es_], wr=[B_gps[sl]])
            for kc in range(KC):
                k.op(pe, "matmul", u_ps[sl][:], lhsT=xsT[sl][:, kc, :], rhs=wu[es_][:, kc, :], start=(kc == 0),
                     stop=(kc == KC - 1), rd=[B_xsT[sl]] + B_wch[es_], wr=[B_ups[sl]])
            k.op(act, "activation", out=sg[sl][:], in_=g_ps[sl][:], func=AF.Silu, rd=[B_gps[sl]], wr=[B_sg[sl]])
            k.op(dve, "tensor_tensor", out=hid[sl][:], in0=sg[sl][:], in1=u_ps[sl][:], op=ALU.mult,
                 rd=[B_sg[sl], B_ups[sl]], wr=[B_hid[sl]])

        def stC(i):
            e, blk = blocks[i]
            es_ = e % 2
            sl = i % 2
            row0 = e * CAP + blk * 128
            for c in range(4):
                k.op(pe, "transpose", out=ht_ps[:, c * 128:(c + 1) * 128], in_=hid[sl][:, c * 128:(c + 1) * 128],
                     identity=ident_b[:], rd=[B_hid[sl], B_identb], wr=[B_htps])
            k.op(act, "copy", out=hidT[sl][:], in_=ht_ps[:].rearrange("p (k t) -> p k t", k=4), rd=[B_htps],
                 wr=[B_hidT[sl]])

        def stC2(i):
            e, blk = blocks[i]
            es_ = e % 2
            sl = i % 2
            row0 = e * CAP + blk * 128
            for n in range(4):
                yp = n % 2
                for c in range(4):
                    k.op(pe, "matmul", y_ps[yp][:], lhsT=hidT[sl][:, c, :], rhs=wd[es_][:, c, n * 512:(n + 1) * 512],
                         start=(c == 0), stop=(c == 3), rd=[B_hidT[sl]] + B_wch[es_], wr=[B_yps[yp]])
                k.op(act if n % 2 else dve, "copy" if n % 2 else "tensor_copy", out=y_sb[sl][:, n * 512:(n + 1) * 512],
                     in_=y_ps[yp][:], rd=[B_yps[yp]], wr=[B_ysb[sl]])
            k.dma(act, Ys_s[row0:row0 + 128, :], y_sb[sl][:], ds_y[sl], rd=[B_ysb[sl]])

        for e0 in (0, 1):
            for t_ in expert_chunks(e0):
                t_()
        pending = []
        xs_load(0)
        xs_load(1)
        for i in range(NBK + 2):
            if i + 2 < NBK:
                xs_load(i + 2)
            if i >= 2:
                stC(i - 2)
            if i < NBK:
                stA(i)
            if 1 <= i <= NBK:
                stB(i - 1)
            if i >= 2:
                stC2(i - 2)
                e_done, blk_done = blocks[i - 2]
                if blk_done == NBLK - 1 and e_done + 2 < NE:
                    pending += expert_chunks(e_done + 2)
            for _ in range(3):
                if pending:
                    pending.pop(0)()
        assert not pending
        k.barrier()
        stE.close()

        stG = contextlib.ExitStack()
        es.enter_context(stG)
        GM = sb("GM", [128, D], F32, stG)
        FG = sb("FG", [128, D], F32, stG)
        B_GM, B_FG = Buf(), Buf()
        bcast_load(GM, B_GM, 5, k.dsem())
        vec_bcast_load(FG[:], B_FG, g_fin, k.dsem())
        k.op(dve, "tensor_scalar", out=FG[:], in0=FG[:], scalar1=SQD, scalar2=None, op0=ALU.mult, wr=[B_FG])
        NF = 3
        g1 = [sb("g1_%d" % i, [128, D], F32, stG) for i in range(NF)]
        g2 = [sb("g2_%d" % i, [128, D], F32, stG) for i in range(NF)]
        x1t = [sb("x1f%d" % i, [128, D], F32, stG) for i in range(NF)]
        B_g1, B_g2, B_x1t = [Buf() for _ in range(NF)], [Buf() for _ in range(NF)], [Buf() for _ in range(NF)]
        ds_g = [k.dsem() for _ in range(NF)]
        ds_g2 = [k.dsem() for _ in range(NF)]
        ds_x1 = [k.dsem() for _ in range(NF)]
        ds_o = [k.dsem() for _ in range(NF)]

        def f_loads(j):
            s = j % NF
            k.dma(sp, x1t[s][:], X1_s[j * 128:(j + 1) * 128, :], ds_x1[s], wr=[B_x1t[s]])
            k.idma(g1[s][:, :], None, Ys_s, bass.IndirectOffsetOnAxis(ap=rt_pg[:, j, 0:1], axis=0), ds_g[s], NSLOT - 1,
                   rd=[B_rtpg], wr=[B_g1[s]])
            k.idma(g2[s][:, :], None, Ys_s, bass.IndirectOffsetOnAxis(ap=rt_pg[:, j, 1:2], axis=0), ds_g2[s], NSLOT - 1,
                   rd=[B_rtpg], wr=[B_g2[s]])

        f_loads(0)
        if NOWN > 1:
            f_loads(1)
        for j in range(NOWN):
            s = j % NF
            if j + 2 < NOWN:
                f_loads(j + 2)
            k.op(dve, "tensor_scalar", out=g1[s][:], in0=g1[s][:], scalar1=rt_w[:, j, 0:1], scalar2=None, op0=ALU.mult,
                 rd=[B_rtw], wr=[B_g1[s]])
            k.op(dve, "scalar_tensor_tensor", out=g1[s][:], in0=g2[s][:], scalar=rt_w[:, j, 1:2], in1=g1[s][:],
                 op0=ALU.mult, op1=ALU.add, rd=[B_g2[s], B_rtw], wr=[B_g1[s]])
            k.op(pool, "tensor_tensor", out=g1[s][:], in0=g1[s][:], in1=GM[:], op=ALU.mult, rd=[B_GM], wr=[B_g1[s]])
            k.op(dve, "tensor_tensor", out=x1t[s][:], in0=x1t[s][:], in1=g1[s][:], op=ALU.add, rd=[B_g1[s]], wr=[B_x1t[s]])
            rf, Brf = rms_scale(x1t[s][:], B_x1t[s], D, D * EPS)
            k.op(dve, "scalar_tensor_tensor", out=g2[s][:], in0=x1t[s][:], scalar=rf, in1=FG[:], op0=ALU.mult,
                 op1=ALU.mult, rd=[B_x1t[s], Brf, B_FG], wr=[B_g2[s]])
            k.dma(sp, out[j * 128:(j + 1) * 128, :], g2[s][:], ds_o[s], rd=[B_g2[s]], wr=[])
        if dbg:
            k.dma(sp, d_pg, rt_pg[:].rearrange("p j c -> p (j c)"), k.dsem(), rd=[B_rtpg])
            k.dma(sp, d_w, rt_w[:].rearrange("p j c -> p (j c)"), k.dsem(), rd=[B_rtw])
        k.barrier()
        stG.close()
    build.nds = k.nds
    return nc


def _consts(r, CAP):
    p = np.arange(128)
    ident = np.eye(128, dtype=np.float32)
    U_incl = (p[:, None] <= p[None, :]).astype(np.float32)
    U_strict = (p[:, None] < p[None, :]).astype(np.float32)
    ones = np.ones((128, 128), np.float32)
    cst = np.concatenate([ident, U_incl, U_strict, ones], axis=1)
    tri = (p[:, None] <= p[None, :]).astype(np.float32)
    fm = [np.ones((128, 128), np.float32) if u < r else (tri if u == r else np.zeros((128, 128), np.float32)) for u in range(4)]
    fmask = np.concatenate(fm, axis=1)
    slopes = (2.0 ** (-8.0 * np.arange(1, 17) / 16)).astype(np.float64)
    kk, qq = p[:, None].astype(np.float64), p[None, :].astype(np.float64)
    A = np.zeros((128, 3, 4, 4, 128), np.float32)
    for g in range(16):
        prev = np.exp(-slopes[g] * (qq + 128 - kk)) * (kk > qq)
        cur = np.exp(-slopes[g] * (qq - kk)) * (kk <= qq)
        A[:, 0, g // 4, g % 4, :] = prev if r != 0 else 0.0
        A[:, 1, g // 4, g % 4, :] = prev
        A[:, 2, g // 4, g % 4, :] = cur
    rsel = np.zeros((128, 4), np.float32)
    rsel[:, r] = 1.0
    ebase = np.tile((np.arange(NE) * CAP).astype(np.float32)[None, :], (128, 1))
    return dict(cst=cst, fmask=fmask, swaA=A.reshape(128, -1), rsel=rsel, ebase=ebase)


_CACHE = {}


def run(inputs, S, CAP, dbg=False):
    f = lambda a: np.ascontiguousarray(np.asarray(a, dtype=np.float32))
    x = f(inputs["x"])[:, :S]
    B = x.shape[0]
    NB = S // 128
    NOWN = NB // 4
    key = (S, CAP, dbg)
    if key not in _CACHE:
        _CACHE[key] = build(S, CAP, dbg)
    nc = _CACHE[key]
    shared = dict(
        w_ada=f(inputs["w_ada"][0]), b_ada=f(inputs["b_ada"][0]).reshape(1, -1), g_mix=f(inputs["norm_mix_g"][0]).reshape(1, -1),
        w_in=f(inputs["w_in"][0]), b_f=f(inputs["b_forget"][0]).reshape(1, -1), sinks=f(inputs["sinks"][0]).reshape(1, -1),
        g_out=np.concatenate([f(inputs["out_norm_swa_g"][0]), f(inputs["out_norm_fox_g"][0])]).reshape(1, -1),
        w_out=f(inputs["w_out"][0]), g_moe=f(inputs["norm_moe_g"][0]).reshape(1, -1),
        w_r=np.ascontiguousarray(np.concatenate([f(inputs["w_group"][0]), f(inputs["w_expert"][0])], axis=1)),
        b_r=np.concatenate([f(inputs["b_group"][0]), f(inputs["b_expert"][0])]).reshape(1, -1),
        w_gate=f(inputs["w_gate"][0]), w_up=f(inputs["w_up"][0]), w_down=f(inputs["w_down"][0]),
        g_fin=f(inputs["final_g"]).reshape(1, -1),
    )
    in_maps = []
    for core in range(8):
        b, r = core // 4, core % 4
        xb = x[b].reshape(NB, 128, D)
        own = [4 * j + r for j in range(NOWN)]
        x_own = np.ascontiguousarray(xb[own].reshape(-1, D))
        xp = np.zeros((NOWN, 128, D), np.float32)
        for j, t in enumerate(own):
            if t > 0:
                xp[j] = xb[t - 1]
        m = dict(shared)
        m.update(x_seq=np.ascontiguousarray(x[b]), x_own=x_own, x_prev=xp.reshape(-1, D),
                 cT=np.ascontiguousarray(f(inputs["c"])[b].reshape(KC, 128).T))
        m.update(_consts(r, CAP))
        in_maps.append(m)
    res = run_bass_kernel_spmd(nc, in_maps, core_ids=list(range(8)))
    if dbg:
        _CACHE["dbg"] = res
    outp = np.zeros((B, NB, 128, D), np.float32)
    for core in range(8):
        b, r = core // 4, core % 4
        o = np.asarray(res.results[core]["out"]).reshape(NOWN, 128, D)
        for j in range(NOWN):
            outp[b, 4 * j + r] = o[j]
    return outp.reshape(B, S, D)


def kernel(**inputs):
    return run(inputs, 8192, 512)
```

```python
import bisect
import contextlib
import numpy as np
import concourse.bass as bass
import concourse.mybir as mybir
from concourse.bass_utils import run_bass_kernel_spmd

F32 = mybir.dt.float32
BF16 = mybir.dt.bfloat16
I32 = mybir.dt.int32
AF = mybir.ActivationFunctionType
ALU = mybir.AluOpType
AX = mybir.AxisListType

D = 2048
KC = 16
EPS = 1e-6
NE = 32
DE = 512
QA0, KA0, VA0, QB0, KB0, VB0, FB0 = 0, 1024, 1280, 1536, 2560, 3584, 4608


class Eng:
    def __init__(self, nc, e, sem, name, is_pe=False):
        self.nc, self.e, self.sem, self.name, self.is_pe = nc, e, sem, name, is_pe
        self.idx = 0
        self.marks = []
        self.mark_idx = []
        self.count = 0
        self.last = None
        self.waited = {}

    def issue(self, ins, is_dma=False):
        self.idx += 1
        self.last = ins
        self.last_is_dma = is_dma
        return ("E", self, self.idx)

    def mark_now(self):
        if self.marks and self.marks[-1][0] == self.idx:
            return
        self.count += 1
        self.last.then_inc(self.sem, 1)
        self.marks.append((self.idx, self.count))
        self.mark_idx.append(self.idx)

    def resolve(self, idx):
        p = bisect.bisect_left(self.mark_idx, idx)
        if p < len(self.marks):
            return self.marks[p][1]
        assert self.idx >= idx
        if self.last_is_dma:
            if self.name == "act":
                self.issue(self.e.memzero(self.mark_tile))
            else:
                self.issue(self.e.memset(self.mark_tile, 0.0))
        self.mark_now()
        return self.count

    def wait(self, toks):
        for t in toks:
            if t is None:
                continue
            if t[0] == "E":
                eng, idx = t[1], t[2]
                if eng is self and self.is_pe:
                    continue
                val = eng.resolve(idx)
                sem = eng.sem
                key = eng.name
            else:
                _, ds, val = t
                sem = ds.sem
                key = ds.name
            if self.waited.get(key, 0) >= val:
                continue
            self.e.wait_ge(sem, val)
            self.waited[key] = val


class DSem:
    def __init__(self, sem, name):
        self.sem, self.name, self.n = sem, name, 0


class Buf:
    __slots__ = ("w", "r")

    def __init__(self):
        self.w = None
        self.r = {}


class K:
    def __init__(self, nc, es):
        self.nc, self.es = nc, es
        self.dsems = []
        mk = lambda e, n, pe=False: Eng(nc, e, es.enter_context(nc.semaphore("sem_" + n)), n, pe)
        self.pe = mk(nc.tensor, "pe", True)
        self.act = mk(nc.scalar, "act")
        self.dve = mk(nc.vector, "dve")
        self.pool = mk(nc.gpsimd, "pool")
        self.sp = mk(nc.sync, "sp")
        self.engs = [self.pe, self.act, self.dve, self.pool, self.sp]
        self.nds = 0

    def dsem(self, es=None):
        es = es or self.es
        self.nds += 1
        name = "ds%d" % self.nds
        d = DSem(es.enter_context(self.nc.semaphore(name)), name)
        self.dsems.append(d)
        return d

    def _deps(self, eng, rd, wr):
        deps = []
        for b in rd:
            if b.w is not None:
                deps.append(b.w)
        for b in wr:
            if b.w is not None:
                deps.append(b.w)
            for k, t in b.r.items():
                if t[0] == "E" and t[1] is eng:
                    continue
                deps.append(t)
        return deps

    def _upd(self, tok, key, rd, wr):
        for b in rd:
            b.r[key] = tok
        for b in wr:
            b.w = tok
            b.r = {}

    def op(self, eng, method, *a, rd=(), wr=(), **kw):
        eng.wait(self._deps(eng, rd, wr))
        ins = getattr(eng.e, method)(*a, **kw)
        tok = eng.issue(ins)
        if (not eng.is_pe) or kw.get("stop") or method == "transpose":
            eng.mark_now()
        self._upd(tok, eng.name, rd, wr)
        return tok

    def dma(self, q, out, in_, ds, rd=(), wr=(), **kw):
        if ds is None:
            if not hasattr(self, "_ds_by_buf"):
                self._ds_by_buf = {}
            if id(wr[0]) not in self._ds_by_buf:
                self._ds_by_buf[id(wr[0])] = self.dsem()
            ds = self._ds_by_buf[id(wr[0])]
        q.wait(self._deps(q, rd, wr))
        ins = q.e.dma_start(out=out, in_=in_, **kw)
        q.issue(ins, True)
        ds.n += 16
        ins.then_inc(ds.sem, 16)
        tok = ("D", ds, ds.n)
        self._upd(tok, ds.name, rd, wr)
        return tok

    def idma(self, out, out_off, in_, in_off, ds, bound, rd=(), wr=()):
        q = self.pool
        q.wait(self._deps(q, rd, wr))
        if not hasattr(self, "_bound_reg"):
            self._bound_reg = {}
        if bound not in self._bound_reg:
            self._bound_reg[bound] = q.e.to_reg(bound)
        ins = q.e.indirect_dma_start(out=out, out_offset=out_off, in_=in_, in_offset=in_off,
                                     bounds_check=self._bound_reg[bound], oob_is_err=False)
        q.issue(ins, True)
        ds.n += 16
        ins.then_inc(ds.sem, 16)
        tok = ("D", ds, ds.n)
        self._upd(tok, ds.name, rd, wr)
        return tok

    def barrier(self):
        toks = []
        for e in self.engs:
            if e.idx > 0 and e is not self.sp:
                toks.append(("E", e, e.idx))
        for d in self.dsems:
            if d.n > 0:
                toks.append(("D", d, d.n))
        for e in self.engs:
            e.wait([t for t in toks if not (t[0] == "E" and t[1] is e)])


def build(S, CAP, dbg=False):
    NB = S // 128
    NOWN = NB // 4
    NG = NB // 4
    TOWN = NOWN * 128
    NSLOT = NE * CAP
    NBLK = CAP // 128
    SQD = float(np.sqrt(D))

    nc = bass.Bass("TRN2", target_bir_lowering=False)

    def inp(name, shape, dt=F32):
        return nc.dram_tensor(name, shape, dt, kind="ExternalInput").ap()

    def scr(name, shape, dt):
        return nc.dram_tensor(name, shape, dt, kind="Internal").ap()

    x_seq = inp("x_seq", [S, D]); x_own = inp("x_own", [TOWN, D]); x_prev = inp("x_prev", [TOWN, D])
    cT = inp("cT", [128, KC]); w_ada = inp("w_ada", [D, 6 * D]); b_ada = inp("b_ada", [1, 6 * D])
    g_mix = inp("g_mix", [1, D]); w_in = inp("w_in", [D, 4624]); b_f = inp("b_f", [1, 16]); sinks = inp("sinks", [1, 16])
    g_out = inp("g_out", [1, D]); w_out = inp("w_out", [D, D]); g_moe = inp("g_moe", [1, D])
    w_r = inp("w_r", [D, 36]); b_r = inp("b_r", [1, 36])
    w_gate = inp("w_gate", [NE, D, DE]); w_up = inp("w_up", [NE, D, DE]); w_down = inp("w_down", [NE, DE, D])
    g_fin = inp("g_fin", [1, D])
    fmask = inp("fmask", [128, 512]); swaA = inp("swaA", [128, 3 * 4 * 512]); rsel = inp("rsel", [128, 4])
    cst = inp("cst", [128, 4 * 128]); ebase = inp("ebase", [128, NE])
    out = nc.dram_tensor("out", [TOWN, D], F32, kind="ExternalOutput").ap()

    mod_s = scr("mod_s", [1, 6 * D], F32)
    KT_s = scr("KT_s", [16 * 64, S], BF16)
    VB_s = scr("VB_s", [S, 1024], BF16)
    QT_s = scr("QT_s", [16 * 64, TOWN], BF16)
    QAUG_s = scr("QAUG_s", [NOWN * 16, 3, 128], BF16)
    X1_s = scr("X1_s", [TOWN, D], F32)
    if dbg:
        Xs_s = nc.dram_tensor("Xs_s", [NSLOT, D], BF16, kind="ExternalOutput").ap()
        Ys_s = nc.dram_tensor("Ys_s", [NSLOT, D], F32, kind="ExternalOutput").ap()
        d_pg = nc.dram_tensor("d_pg", [128, NOWN * 2], I32, kind="ExternalOutput").ap()
        d_w = nc.dram_tensor("d_w", [128, NOWN * 2], F32, kind="ExternalOutput").ap()
    else:
        Xs_s = scr("Xs_s", [NSLOT, D], BF16)
        Ys_s = scr("Ys_s", [NSLOT, D], F32)

    w_in_v = w_in.rearrange("(kc p) n -> p kc n", p=128)

    with contextlib.ExitStack() as es:
        k = K(nc, es)
        pe, act, dve, pool, sp = k.pe, k.act, k.dve, k.pool, k.sp
        mark_tile = es.enter_context(nc.sbuf_tensor("mark_tile", [128, 8], F32))
        pool.mark_tile = mark_tile[:, 0:4]
        act.mark_tile = mark_tile[:, 4:8]
        for e_ in k.engs:
            e_.last_is_dma = False

        def sb(name, shape, dt, st=None):
            return (st or es).enter_context(nc.sbuf_tensor(name, shape, dt))

        def ps(name, shape, dt, st):
            return st.enter_context(nc.psum_tensor(name, shape, dt))

        cst_f = sb("cst_f", [128, 512], F32)
        ident_b = sb("ident_b", [128, 128], BF16)
        ssq = sb("ssq", [128, NB + 6 * NOWN + 8], F32)
        junk = sb("junk", [128, D], BF16)
        B_cst, B_identb, B_ssq, B_junk = Buf(), Buf(), Buf(), Buf()
        k.dma(sp, cst_f[:], cst, k.dsem(), wr=[B_cst])
        k.op(dve, "tensor_copy", out=ident_b[:], in_=cst_f[:, 0:128], rd=[B_cst], wr=[B_identb])
        k.op(pool, "memset", ssq[:], 0.0, wr=[B_ssq])
        ident_f = cst_f[:, 0:128]
        U_incl = cst_f[:, 128:256]
        U_strict = cst_f[:, 256:384]
        ones_f = cst_f[:, 384:512]
        ssq_ctr = [0]

        def new_ssq():
            i = ssq_ctr[0]
            ssq_ctr[0] += 1
            return ssq[:, i:i + 1]

        rstd_all = sb("rstd_all", [128, NB + 6 * NOWN + 8], F32)

        def rms_scale(src_ap, B_src, width, eps_scaled):
            col = new_ssq()
            i = ssq_ctr[0] - 1
            Bc = Buf()
            Bc.w = B_ssq.w
            k.op(act, "activation", out=junk[:, 0:width], in_=src_ap, func=AF.Square, accum_out=col,
                 rd=[B_src], wr=[Bc, B_junk])
            r = rstd_all[:, i:i + 1]
            Br = Buf()
            k.op(act, "activation", out=r, in_=col, func=AF.Sqrt, bias=float(eps_scaled), scale=1.0, rd=[Bc], wr=[Br])
            k.op(dve, "reciprocal", out=r, in_=r, rd=[Br], wr=[Br])
            return r, Br

        cumN = sb("cumN", [128, NB, 16], F32)
        B_cumN = Buf()
        stA = contextlib.ExitStack()
        es.enter_context(stA)
        stB = stA
        cT_sb = sb("cT_sb", [128, KC], F32, stA)
        sil = sb("sil", [128, KC], BF16, stA)
        B_cT, B_sil = Buf(), Buf()
        k.dma(sp, cT_sb[:], cT, k.dsem(), wr=[B_cT])
        k.op(act, "activation", out=sil[:], in_=cT_sb[:], func=AF.Silu, rd=[B_cT], wr=[B_sil])
        wa = [sb("wa%d" % i, [128, KC, 512], BF16, stA) for i in range(2)]
        B_wa = [Buf(), Buf()]
        ds_wa = [k.dsem(), k.dsem()]
        brow = [sb("brow%d" % i, [1, 512], F32, stA) for i in range(2)]
        B_brow = [Buf(), Buf()]
        ds_br = [k.dsem(), k.dsem()]
        mrow = [sb("mrow%d" % i, [1, 512], F32, stA) for i in range(2)]
        B_mrow = [Buf(), Buf()]
        ds_mr = [k.dsem(), k.dsem()]
        B_mod = [Buf() for _ in range(24)]
        w_ada_v = w_ada.rearrange("(kc p) n -> p kc n", p=128)
        ada_state = {"loaded": 0, "done": 0}

        def ada_load(blk):
            s = blk % 2
            k.dma(pool, wa[s][:], w_ada_v[:, :, blk * 512:(blk + 1) * 512], ds_wa[s], wr=[B_wa[s]])
            k.dma(sp, brow[s][:], b_ada[0:1, blk * 512:(blk + 1) * 512], ds_br[s], wr=[B_brow[s]])

        def ada_block(blk, mod_ps, B_modps):
            s = blk % 2
            for kc in range(KC):
                k.op(pe, "matmul", mod_ps[0:1, :], lhsT=sil[:, kc:kc + 1], rhs=wa[s][:, kc, :], start=(kc == 0),
                     stop=(kc == KC - 1), rd=[B_sil, B_wa[s]], wr=[B_modps])
            if (blk // 4) in (1, 4):
                k.op(dve, "scalar_tensor_tensor", out=mrow[s][:], in0=mod_ps[0:1, :], scalar=1.0, in1=brow[s][:],
                     op0=ALU.add, op1=ALU.add, rd=[B_modps, B_brow[s]], wr=[B_mrow[s]])
            else:
                k.op(dve, "tensor_tensor", out=mrow[s][:], in0=mod_ps[0:1, :], in1=brow[s][:], op=ALU.add,
                     rd=[B_modps, B_brow[s]], wr=[B_mrow[s]])
            k.dma(sp, mod_s[0:1, blk * 512:(blk + 1) * 512], mrow[s][:], ds_mr[s], rd=[B_mrow[s]], wr=[B_mod[blk]])

        def ada_step(mod_ps, B_modps):
            b = ada_state["done"]
            if b >= 24:
                return
            while ada_state["loaded"] < min(24, b + 2):
                ada_load(ada_state["loaded"])
                ada_state["loaded"] += 1
            ada_block(b, mod_ps, B_modps)
            ada_state["done"] += 1

        def bcast_load(dst, B_dst, chunk, ds):
            k.dma(sp, dst[:], mod_s[0:1, chunk * D:(chunk + 1) * D].partition_broadcast(128), ds,
                  rd=[B_mod[chunk * 4 + i] for i in range(4)], wr=[B_dst])

        def vec_bcast_load(dst, B_dst, src, ds, n=D):
            k.dma(sp, dst, src[0:1, 0:n].partition_broadcast(128), ds, wr=[B_dst])

        G1s = sb("G1s", [128, D], F32, stB)
        SHa = sb("SHa", [128, D], F32, stB)
        B_G1s, B_SHa = Buf(), Buf()
        mod_ps = ps("mod_ps", [128, 512], F32, stB)
        B_modps = Buf()
        for _ in range(8):
            ada_step(mod_ps, B_modps)
        B_tmpg = Buf()
        bcast_load(G1s, B_G1s, 1, k.dsem())
        bcast_load(SHa, B_SHa, 0, k.dsem())

        Wkb = sb("Wkb", [128, KC, 1024], BF16, stB)
        Wvb = sb("Wvb", [128, KC, 1024], BF16, stB)
        Wf = sb("Wf", [128, KC, 16], BF16, stB)
        B_Wkb, B_Wvb, B_Wf = Buf(), Buf(), Buf()
        for kc in range(KC):
            k.dma(pool, Wkb[:, kc, :], w_in_v[:, kc, KB0:KB0 + 1024], None, wr=[B_Wkb])
        for kc in range(KC):
            k.dma(pool, Wvb[:, kc, :], w_in_v[:, kc, VB0:VB0 + 1024], None, wr=[B_Wvb])
        k.dma(pool, Wf[:], w_in_v[:, :, FB0:FB0 + 16], None, wr=[B_Wf])
        bF = sb("bF", [128, 16], F32, stB)
        B_bF = Buf()
        vec_bcast_load(bF[:], B_bF, b_f, k.dsem(), 16)

        NXS = 2
        xts = [sb("xt%d" % i, [128, D], F32, stB) for i in range(NXS)]
        B_xt = [Buf() for _ in range(NXS)]
        ds_xt = [k.dsem() for _ in range(NXS)]
        vec_bcast_load(xts[0][:], B_xt[0], g_mix, k.dsem())
        k.op(dve, "scalar_tensor_tensor", out=G1s[:], in0=G1s[:], scalar=SQD, in1=xts[0][:], op0=ALU.mult, op1=ALU.mult,
             rd=[B_xt[0]], wr=[B_G1s])
        hb = [sb("hb%d" % i, [128, D], BF16, stB) for i in range(2)]
        B_hb = [Buf(), Buf()]
        hTg = [sb("hTg%d" % i, [128, KC, 512], BF16, stB) for i in range(2)]
        B_hTg = [[Buf() for _ in range(4)] for _ in range(2)]
        hT_ps = [ps("hT_ps%d" % i, [128, D], BF16, stB) for i in range(1)]
        B_hTps = [Buf()]
        NMM = 3
        mm_ps = [ps("mm_ps%d" % i, [128, 512], F32, stB) for i in range(NMM)]
        B_mm = [Buf() for _ in range(NMM)]
        f_ps2 = [ps("f_ps%d" % i, [128, 512], F32, stB) for i in range(2)]
        B_fps = [Buf(), Buf()]
        kT_sb = [sb("kT_sb%d" % i, [128, 512], BF16, stB) for i in range(2)]
        B_kTsb = [Buf(), Buf()]
        ds_kT = [k.dsem(), k.dsem()]
        v_sb = [sb("v_sb%d" % i, [128, 1024], BF16, stB) for i in range(2)]
        B_vsb = [Buf(), Buf()]
        ds_v = [k.dsem(), k.dsem()]
        zf = sb("zf", [128, NB, 16], F32, stB)
        B_zf = Buf()
        cnt = {"xt": 0, "hb": 0, "mm": 0, "kT": 0, "v": 0, "f": 0, "hTps": 0}

        def make_h(src_rows, Bx_extra_rd=()):
            s = cnt["xt"] % NXS
            cnt["xt"] += 1
            k.dma(sp, xts[s][:], src_rows, ds_xt[s], wr=[B_xt[s]])
            r, Br = rms_scale(xts[s][:], B_xt[s], D, D * EPS)
            k.op(dve, "scalar_tensor_tensor", out=xts[s][:], in0=xts[s][:], scalar=r, in1=G1s[:], op0=ALU.mult,
                 op1=ALU.mult, rd=[Br, B_G1s], wr=[B_xt[s]])
            hs = cnt["hb"] % 2
            cnt["hb"] += 1
            k.op(dve, "tensor_tensor", out=hb[hs][:], in0=xts[s][:], in1=SHa[:], op=ALU.add,
                 rd=[B_xt[s], B_SHa], wr=[B_hb[hs]])
            return hb[hs], B_hb[hs]

        def transpose_to(h_t, B_h, dst_ap3, B_dst, eng=None):
            s = cnt["hTps"] % len(hT_ps)
            cnt["hTps"] += 1
            for kc in range(KC):
                k.op(pe, "transpose", out=hT_ps[s][:, kc * 128:(kc + 1) * 128], in_=h_t[:, kc * 128:(kc + 1) * 128],
                     identity=ident_b[:], rd=[B_h, B_identb], wr=[B_hTps[s]])
            e = eng or act
            if e is act:
                k.op(act, "copy", out=dst_ap3, in_=hT_ps[s][:].rearrange("p (k t) -> p k t", k=KC),
                     rd=[B_hTps[s]], wr=[B_dst])
            else:
                k.op(e, "tensor_copy", out=dst_ap3, in_=hT_ps[s][:].rearrange("p (k t) -> p k t", k=KC),
                     rd=[B_hTps[s]], wr=[B_dst])

        for i in range(4):
            h_t, B_h = make_h(x_seq[i * 128:(i + 1) * 128, :])
            transpose_to(h_t, B_h, hTg[0][:, :, i * 128:(i + 1) * 128], B_hTg[0][i])
        for g in range(NG):
            gs = g % 2
            hq = None
            for c in range(8):
                ms = cnt["mm"] % NMM
                cnt["mm"] += 1
                for kc in range(KC):
                    k.op(pe, "matmul", mm_ps[ms][:], lhsT=Wkb[:, kc, c * 128:(c + 1) * 128], rhs=hTg[gs][:, kc, :],
                         start=(kc == 0), stop=(kc == KC - 1), rd=[B_Wkb] + B_hTg[gs], wr=[B_mm[ms]])
                ks = cnt["kT"] % 2
                cnt["kT"] += 1
                k.op(act if c % 2 else dve, "copy" if c % 2 else "tensor_copy", out=kT_sb[ks][:], in_=mm_ps[ms][:],
                     rd=[B_mm[ms]], wr=[B_kTsb[ks]])
                k.dma(pool, KT_s[c * 128:(c + 1) * 128, g * 512:(g + 1) * 512], kT_sb[ks][:], ds_kT[ks], rd=[B_kTsb[ks]])
                if g + 1 < NG:
                    i_n = c // 2
                    t_n = 4 * (g + 1) + i_n
                    if c % 2 == 0:
                        hq = make_h(x_seq[t_n * 128:(t_n + 1) * 128, :])
                    else:
                        transpose_to(hq[0], hq[1], hTg[1 - gs][:, :, i_n * 128:(i_n + 1) * 128], B_hTg[1 - gs][i_n])
            for i in range(4):
                t = 4 * g + i
                vs = cnt["v"] % 2
                cnt["v"] += 1
                for n in range(2):
                    ms = cnt["mm"] % NMM
                    cnt["mm"] += 1
                    for kc in range(KC):
                        k.op(pe, "matmul", mm_ps[ms][:], lhsT=hTg[gs][:, kc, i * 128:(i + 1) * 128],
                             rhs=Wvb[:, kc, n * 512:(n + 1) * 512], start=(kc == 0), stop=(kc == KC - 1),
                             rd=[B_Wvb, B_hTg[gs][i]], wr=[B_mm[ms]])
                    k.op(act, "copy", out=v_sb[vs][:, n * 512:(n + 1) * 512], in_=mm_ps[ms][:], rd=[B_mm[ms]],
                         wr=[B_vsb[vs]])
                k.dma(pool, VB_s[t * 128:(t + 1) * 128, :], v_sb[vs][:], ds_v[vs], rd=[B_vsb[vs]])
                fs = cnt["f"] % 2
                cnt["f"] += 1
                for kc in range(KC):
                    k.op(pe, "matmul", f_ps2[fs][:, 0:16], lhsT=hTg[gs][:, kc, i * 128:(i + 1) * 128],
                         rhs=Wf[:, kc, :], start=(kc == 0), stop=(kc == KC - 1), rd=[B_Wf, B_hTg[gs][i]],
                         wr=[B_fps[fs]])
                k.op(dve, "tensor_tensor", out=zf[:, t, :], in0=f_ps2[fs][:, 0:16], in1=bF[:], op=ALU.add,
                     rd=[B_fps[fs], B_bF], wr=[B_zf])
            ada_step(mod_ps, B_modps)
        while ada_state["done"] < 24:
            ada_step(mod_ps, B_modps)

        NC16 = NB * 16
        zf2 = zf[:].rearrange("p t h -> p (t h)")
        k.op(act, "activation", out=zf2, in_=zf2, func=AF.Exp, scale=-1.0, rd=[B_zf], wr=[B_zf])
        k.op(act, "activation", out=zf2, in_=zf2, func=AF.Ln, bias=1.0, scale=1.0, rd=[B_zf], wr=[B_zf])
        cumN2 = cumN[:].rearrange("p t h -> p (t h)")
        class _V:
            def __init__(self, t):
                self.t = t
            def __getitem__(self, idx):
                return self.t[:, 0:NB * 16].rearrange("p (t h) -> p t h", h=16)[idx]
        pfx = [_V(xts[i]) for i in range(2)]
        B_pfx = [B_xt[0], B_xt[1]]
        for c0 in range(0, NC16, 512):
            c1 = min(NC16, c0 + 512)
            w = c1 - c0
            k.op(pe, "matmul", mm_ps[0][:, 0:w], lhsT=U_incl, rhs=zf2[:, c0:c1], start=True, stop=True,
                 rd=[B_zf, B_cst], wr=[B_mm[0]])
            k.op(pe, "matmul", mm_ps[1][:, 0:w], lhsT=ones_f, rhs=zf2[:, c0:c1], start=True, stop=True,
                 rd=[B_zf, B_cst], wr=[B_mm[1]])
            k.op(dve, "tensor_copy", out=cumN2[:, c0:c1], in_=mm_ps[0][:, 0:w], rd=[B_mm[0]], wr=[B_cumN])
            k.op(dve, "tensor_copy", out=pfx[0][:].rearrange("p t h -> p (t h)")[:, c0:c1], in_=mm_ps[1][:, 0:w],
                 rd=[B_mm[1]], wr=[B_pfx[0]])
        cur = 0
        sh = 1
        while sh < NB:
            nx = 1 - cur
            k.op(dve, "tensor_copy", out=pfx[nx][:, 0:sh, :], in_=pfx[cur][:, 0:sh, :], rd=[B_pfx[cur]], wr=[B_pfx[nx]])
            k.op(dve, "tensor_tensor", out=pfx[nx][:, sh:NB, :], in0=pfx[cur][:, sh:NB, :], in1=pfx[cur][:, 0:NB - sh, :],
                 op=ALU.add, rd=[B_pfx[cur]], wr=[B_pfx[nx]])
            cur = nx
            sh *= 2
        k.op(dve, "tensor_tensor", out=cumN[:, 1:NB, :], in0=cumN[:, 1:NB, :], in1=pfx[cur][:, 0:NB - 1, :], op=ALU.add,
             rd=[B_pfx[cur]], wr=[B_cumN])
        rsel_sb = sb("rsel_sb", [128, 4], F32, stB)
        B_rsel = Buf()
        k.dma(sp, rsel_sb[:], rsel, k.dsem(), wr=[B_rsel])
        cq = sb("cq", [128, NOWN, 16], F32, stB)
        B_cq = Buf()
        cumN4 = cumN[:].rearrange("p (j u) h -> p j u h", u=4)
        k.op(dve, "tensor_scalar", out=cq[:], in0=cumN4[:, :, 0, :], scalar1=rsel_sb[:, 0:1], scalar2=-8.0, op0=ALU.mult,
             op1=ALU.mult, rd=[B_cumN, B_rsel], wr=[B_cq])
        cq8 = sb("cq8", [128, NOWN, 16], F32, stB)
        for u in range(1, 4):
            k.op(dve, "tensor_scalar", out=cq8[:], in0=cumN4[:, :, u, :], scalar1=rsel_sb[:, u:u + 1], scalar2=-8.0,
                 op0=ALU.mult, op1=ALU.mult, rd=[B_cumN, B_rsel], wr=[B_tmpg])
            k.op(dve, "tensor_tensor", out=cq[:], in0=cq[:], in1=cq8[:], op=ALU.add, rd=[B_tmpg], wr=[B_cq])
        NQ = NOWN * 16
        cqf = cq[:].rearrange("p j h -> p (j h)")
        c3 = [sb("c3_%d" % i, [128, NQ], BF16, stB) for i in range(3)]
        B_c3 = [Buf() for _ in range(3)]
        for i in range(3):
            k.op(dve, "tensor_copy", out=c3[i][:], in_=cqf, rd=[B_cq], wr=[B_c3[i]])
            if i < 2:
                k.op(dve, "tensor_tensor", out=cqf, in0=cqf, in1=c3[i][:], op=ALU.subtract, rd=[B_c3[i]], wr=[B_cq])
        qa_sb = sb("qa_sb", [128, 3, 128], BF16, stB)
        B_qasb = Buf()
        ds_qa = k.dsem()
        for c0 in range(0, NQ, 128):
            w = min(128, NQ - c0)
            for i in range(3):
                k.op(pe, "transpose", out=hT_ps[0][0:w, i * 128:(i + 1) * 128], in_=c3[i][:, c0:c0 + w],
                     identity=ident_b[:], rd=[B_c3[i], B_identb], wr=[B_hTps[0]])
            k.op(dve, "tensor_copy", out=qa_sb[0:w, :, :], in_=hT_ps[0][0:w, 0:384].rearrange("p (k t) -> p k t", k=3),
                 rd=[B_hTps[0]], wr=[B_qasb])
            k.dma(sp, QAUG_s[c0:c0 + w, :, :], qa_sb[0:w, :, :], ds_qa, rd=[B_qasb])
        k.barrier()
        stB.close()

        stOA = contextlib.ExitStack()
        es.enter_context(stOA)
        o_a = sb("o_a", [128, NOWN, 1024], BF16, stOA)
        B_oa = [Buf() for _ in range(NOWN)]
        esink = sb("esink", [128, 16], F32, stOA)
        B_esink = Buf()
        vec_bcast_load(esink[:], B_esink, sinks, k.dsem(), 16)
        k.op(act, "activation", out=esink[:], in_=esink[:], func=AF.Exp, rd=[B_esink], wr=[B_esink])

        stC = contextlib.ExitStack()
        es.enter_context(stC)
        G1s = sb("G1s_c", [128, D], F32, stC)
        SHa = sb("SHa_c", [128, D], F32, stC)
        B_G1s, B_SHa = Buf(), Buf()
        bcast_load(G1s, B_G1s, 1, k.dsem())
        bcast_load(SHa, B_SHa, 0, k.dsem())
        Wq = sb("Wq", [128, KC, 2048], BF16, stC)
        Wkv = sb("Wkv", [128, KC, 512], BF16, stC)
        B_Wq, B_Wkv = Buf(), Buf()
        for kc in range(KC):
            k.dma(pool, Wq[:, kc, 0:1024], w_in_v[:, kc, QA0:QA0 + 1024], None, wr=[B_Wq])
            k.dma(pool, Wq[:, kc, 1024:2048], w_in_v[:, kc, QB0:QB0 + 1024], None, wr=[B_Wq])
        k.dma(pool, Wkv[:], w_in_v[:, :, KA0:KA0 + 512], None, wr=[B_Wkv])
        swaA_sb = sb("swaA_sb", [128, 3 * 4 * 512], BF16, stC)
        B_swaA = Buf()
        for i in range(6):
            k.dma(pool, swaA_sb[:, i * 1024:(i + 1) * 1024], swaA[:, i * 1024:(i + 1) * 1024], None, wr=[B_swaA])
        NXS = 2
        xts = [sb("xtc%d" % i, [128, D], F32, stC) for i in range(NXS)]
        B_xt = [Buf() for _ in range(NXS)]
        vec_bcast_load(xts[0][:], B_xt[0], g_mix, k.dsem())
        k.op(dve, "scalar_tensor_tensor", out=G1s[:], in0=G1s[:], scalar=SQD, in1=xts[0][:], op0=ALU.mult, op1=ALU.mult,
             rd=[B_xt[0]], wr=[B_G1s])
        hb = [sb("hbc%d" % i, [128, D], BF16, stC) for i in range(2)]
        B_hb = [Buf(), Buf()]
        hT2 = [sb("hT2_%d" % i, [128, KC, 128], BF16, stC) for i in range(2)]
        B_hT2 = [Buf(), Buf()]
        hT_ps = [ps("hT_psc%d" % i, [128, D], BF16, stC) for i in range(1)]
        B_hTps = [Buf()]
        mm_ps = [ps("mm_psc%d" % i, [128, 512], F32, stC) for i in range(2)]
        B_mm = [Buf(), Buf()]
        s_ps = ps("s_psc", [128, 512], F32, stC)
        B_sps = Buf()
        o_ps = ps("o_psc", [128, 512], F32, stC)
        B_ops = Buf()
        tr_ps = ps("tr_psc", [128, 512], F32, stC)
        B_trps = Buf()
        q_tok = sb("q_tok", [128, 2048], BF16, stC)
        B_qtok = Buf()
        kv_tok = [sb("kv_tok%d" % i, [128, 512], BF16, stC) for i in range(2)]
        B_kvtok = [Buf(), Buf()]
        qaT = sb("qaT", [64, 16, 128], BF16, stC)
        qbT = sb("qbT", [64, 16, 128], BF16, stC)
        kaT = sb("kaT", [64, 2, 4, 128], BF16, stC)
        va = sb("va", [128, 2, 4, 65], BF16, stC)
        B_qaT, B_qbT, B_kaT, B_va = Buf(), Buf(), Buf(), Buf()
        k.op(pool, "memset", va[:], 1.0, wr=[B_va])
        pT = [sb("pTc%d" % i, [128, 512], BF16, stC) for i in range(2)]
        B_pT = [Buf(), Buf()]
        oT_sb2 = [sb("oT_sbc%d" % i, [65, 512], F32, stC) for i in range(2)]
        B_oTsb2 = [Buf(), Buf()]
        den = sb("denc", [128, 8], F32, stC)
        B_den = Buf()
        ds_qb = k.dsem()
        cnt = {"xt": 0, "hb": 0, "mm": 0, "hTps": 0, "pT": 0}
        QT_v = QT_s.rearrange("(h d) t -> d h t", d=64)

        for j in range(NOWN):
            for which, src in ((0, x_own), (1, x_prev)):
                h_t, B_h = make_h(src[j * 128:(j + 1) * 128, :])
                transpose_to(h_t, B_h, hT2[which][:], B_hT2[which])
            for n in range(4):
                ms = cnt["mm"] % 2
                cnt["mm"] += 1
                for kc in range(KC):
                    k.op(pe, "matmul", mm_ps[ms][:], lhsT=hT2[0][:, kc, :], rhs=Wq[:, kc, n * 512:(n + 1) * 512],
                         start=(kc == 0), stop=(kc == KC - 1), rd=[B_Wq, B_hT2[0]], wr=[B_mm[ms]])
                k.op(dve if n % 2 else act, "tensor_copy" if n % 2 else "copy", out=q_tok[:, n * 512:(n + 1) * 512],
                     in_=mm_ps[ms][:], rd=[B_mm[ms]], wr=[B_qtok])
            for which in range(2):
                ms = cnt["mm"] % 2
                cnt["mm"] += 1
                for kc in range(KC):
                    k.op(pe, "matmul", mm_ps[ms][:], lhsT=hT2[which][:, kc, :], rhs=Wkv[:, kc, :],
                         start=(kc == 0), stop=(kc == KC - 1), rd=[B_Wkv, B_hT2[which]], wr=[B_mm[ms]])
                k.op(dve, "tensor_copy", out=kv_tok[which][:], in_=mm_ps[ms][:], rd=[B_mm[ms]], wr=[B_kvtok[which]])
                kb = 1 - which
                k.op(dve, "tensor_copy", out=va[:, kb, :, 0:64],
                     in_=kv_tok[which][:, 256:512].rearrange("p (h d) -> p h d", h=4), rd=[B_kvtok[which]], wr=[B_va])
            for half, dstT, B_dst in ((0, qaT, B_qaT), (1, qbT, B_qbT)):
                for hh in range(2):
                    s = cnt["hTps"] % len(hT_ps)
                    cnt["hTps"] += 1
                    for i8 in range(8):
                        g_ = hh * 8 + i8
                        c0 = half * 1024 + g_ * 64
                        k.op(pe, "transpose", out=hT_ps[s][0:64, i8 * 128:(i8 + 1) * 128], in_=q_tok[:, c0:c0 + 64],
                             identity=ident_b[:], rd=[B_qtok, B_identb], wr=[B_hTps[s]])
                    k.op(act if hh else dve, "copy" if hh else "tensor_copy", out=dstT[:, hh * 8:(hh + 1) * 8, :],
                         in_=hT_ps[s][0:64, 0:1024].rearrange("p (h t) -> p h t", h=8), rd=[B_hTps[s]], wr=[B_dst])
            k.dma(sp, QT_v[:, :, j * 128:(j + 1) * 128], qbT[:], ds_qb, rd=[B_qbT])
            s = cnt["hTps"] % len(hT_ps)
            cnt["hTps"] += 1
            for which in range(2):
                kb = 1 - which
                for hk in range(4):
                    k.op(pe, "transpose", out=hT_ps[s][0:64, (kb * 4 + hk) * 128:(kb * 4 + hk + 1) * 128],
                         in_=kv_tok[which][:, hk * 64:(hk + 1) * 64], identity=ident_b[:],
                         rd=[B_kvtok[which], B_identb], wr=[B_hTps[s]])
            k.op(dve, "tensor_copy", out=kaT[:].rearrange("p a h t -> p (a h) t"),
                 in_=hT_ps[s][0:64, 0:1024].rearrange("p (h t) -> p h t", h=8), rd=[B_hTps[s]], wr=[B_kaT])
            units = [(hk_, kb_) for hk_ in range(4) for kb_ in range(2)]
            s_bufs = [(s_ps, B_sps), (mm_ps[1], B_mm[1])]
            o_bufs = [(o_ps, B_ops), (mm_ps[0], B_mm[0])]

            def swa_S(u):
                hk, kb = units[u]
                sb_, Bsb = s_bufs[u % 2]
                for i in range(4):
                    k.op(pe, "matmul", sb_[:, i * 128:(i + 1) * 128], lhsT=kaT[:, kb, hk, :],
                         rhs=qaT[:, hk * 4 + i, :], start=True, stop=True, rd=[B_kaT, B_qaT], wr=[Bsb])
                p_ = u % 2
                k.op(act, "activation", out=pT[p_][:], in_=sb_[:], func=AF.Exp, scale=0.125, rd=[Bsb], wr=[B_pT[p_]])
                tab = (0 if j == 0 else 1) if kb == 0 else 2
                a0 = (tab * 4 + hk) * 512
                k.op(dve, "tensor_tensor", out=pT[p_][:], in0=pT[p_][:], in1=swaA_sb[:, a0:a0 + 512], op=ALU.mult,
                     rd=[B_swaA], wr=[B_pT[p_]])

            def swa_PV(u):
                hk, kb = units[u]
                ob, Bob = o_bufs[hk % 2]
                k.op(pe, "matmul", ob[0:65, :], lhsT=va[:, kb, hk, :], rhs=pT[u % 2][:], start=(kb == 0),
                     stop=(kb == 1), rd=[B_va, B_pT[u % 2]], wr=[Bob])
                if kb == 1:
                    k.op(act, "copy", out=oT_sb2[hk % 2][:], in_=ob[0:65, :], rd=[Bob], wr=[B_oTsb2[hk % 2]])

            def swa_tail(hk):
                osb, Bosb = oT_sb2[hk % 2], B_oTsb2[hk % 2]
                for i in range(4):
                    k.op(pe, "transpose", out=tr_ps[:, i * 65:(i + 1) * 65], in_=osb[:, i * 128:(i + 1) * 128],
                         identity=ident_f[0:65, 0:65], rd=[Bosb, B_cst], wr=[B_trps])
                tr3 = tr_ps[:, 0:260].rearrange("p (h c) -> p h c", h=4)
                k.op(dve, "tensor_tensor", out=den[:, 0:4], in0=tr3[:, :, 64], in1=esink[:, hk * 4:(hk + 1) * 4],
                     op=ALU.add, rd=[B_trps, B_esink], wr=[B_den])
                k.op(dve, "reciprocal", out=den[:, 4:8], in_=den[:, 0:4], rd=[B_den], wr=[B_den])
                for i in range(4):
                    g_ = hk * 4 + i
                    k.op(dve, "tensor_scalar", out=o_a[:, j, g_ * 64:(g_ + 1) * 64], in0=tr3[:, i, 0:64],
                         scalar1=den[:, 4 + i:5 + i], scalar2=None, op0=ALU.mult, rd=[B_trps, B_den], wr=[B_oa[j]])

            swa_S(0)
            tail_q = []
            for u in range(8):
                if u + 1 < 8:
                    swa_S(u + 1)
                swa_PV(u)
                if tail_q:
                    swa_tail(tail_q.pop(0))
                if units[u][1] == 1:
                    tail_q.append(units[u][0])
            while tail_q:
                swa_tail(tail_q.pop(0))
        k.barrier()
        stC.close()

        stOB = contextlib.ExitStack()
        es.enter_context(stOB)
        o_b = sb("o_b", [128, NOWN, 1024], BF16, stOB)
        B_ob = [Buf() for _ in range(NOWN)]
        stW = contextlib.ExitStack()
        es.enter_context(stW)
        Wout = sb("Wout", [128, KC, D], BF16, stW)
        B_Wout = Buf()
        w_out_v = w_out.rearrange("(kc p) n -> p kc n", p=128)
        for kc in range(KC):
            for hh in range(2):
                k.dma(pool, Wout[:, kc, hh * 1024:(hh + 1) * 1024], w_out_v[:, kc, hh * 1024:(hh + 1) * 1024], None,
                      wr=[B_Wout])
        stF = contextlib.ExitStack()
        es.enter_context(stF)
        kTa = [sb("kTa%d" % i, [67, S], BF16, stF) for i in range(2)]
        vau = [sb("vau%d" % i, [128, NB, 65], BF16, stF) for i in range(2)]
        qTa = [sb("qTa%d" % i, [67, TOWN], BF16, stF) for i in range(2)]
        B_kTa, B_vau, B_qTa = [Buf(), Buf()], [Buf(), Buf()], [Buf(), Buf()]
        ds_hk = [k.dsem(), k.dsem()]
        ds_hq = [k.dsem(), k.dsem()]
        ds_hv = [k.dsem(), k.dsem()]
        for i in range(2):
            k.op(pool, "memset", kTa[i][64:67, :], 1.0, wr=[B_kTa[i]])
            k.op(pool, "memset", vau[i][:], 1.0, wr=[B_vau[i]])
        fm_sb = sb("fm_sb", [128, 512], BF16, stF)
        B_fm = Buf()
        k.dma(pool, fm_sb[:], fmask, None, wr=[B_fm])
        NPT = 3
        pT = [sb("pTf%d" % i, [128, 1024], BF16, stF) for i in range(NPT)]
        B_pT = [Buf() for _ in range(NPT)]
        NSP = 3
        s_ps = [ps("s_psf%d" % i, [128, 1024], F32, stF) for i in range(NSP)]
        B_sps = [Buf() for _ in range(NSP)]
        o_ps = ps("o_psf", [128, 1024], F32, stF)
        B_ops = Buf()
        tr_ps = [s_ps[0][:, 0:512], s_ps[0][:, 512:1024]]
        B_trps = [B_sps[0], B_sps[0]]
        oT_sb = sb("oT_sbf", [65, 1024], F32, stF)
        B_oTsb = Buf()
        den = sb("denf", [128, 8], F32, stF)
        B_den = Buf()
        VB_v = VB_s.rearrange("(t p) c -> p t c", p=128)
        QAUG_v = QAUG_s.rearrange("(j h) k t -> h k j t", h=16)
        HB = (NOWN + 1) // 2
        halves = [(0, HB), (HB, NOWN)] if NOWN > 1 else [(0, 1)]
        cnt = {"pT": 0, "s": 0, "tr": 0}

        def load_head(h):
            s = h % 2
            k.dma(sp, kTa[s][0:64, :], KT_s[h * 64:(h + 1) * 64, :], ds_hk[s], wr=[B_kTa[s]])
            k.dma(sp, qTa[s][0:64, :], QT_s[h * 64:(h + 1) * 64, :], ds_hq[s], wr=[B_qTa[s]])
            k.dma(sp, qTa[s][64:67, :].rearrange("k (j t) -> k j t", t=128), QAUG_v[h], ds_hq[s], wr=[B_qTa[s]])
            for t0 in range(0, NB, 16):
                t1 = min(NB, t0 + 16)
                k.dma(sp, vau[s][:, t0:t1, 0:64], VB_v[:, t0:t1, h * 64:(h + 1) * 64], ds_hv[s], wr=[B_vau[s]])

        load_head(0)
        for h in range(16):
            hs = h % 2
            if h + 1 < 16:
                load_head(h + 1)
            for (j0, j1) in halves:
                nb = j1 - j0
                def stage1(kt):
                    g = kt // 4
                    u = kt % 4
                    ja = max(g, j0)
                    c_lo = (ja - j0) * 128
                    c_hi = nb * 128
                    ss = cnt["s"] % NSP
                    cnt["s"] += 1
                    for b0 in range(0, 1024, 512):
                        lo, hi = max(c_lo, b0), min(c_hi, b0 + 512)
                        if lo >= hi:
                            continue
                        k.op(pe, "matmul", s_ps[ss][:, lo:hi], lhsT=kTa[hs][:, kt * 128:(kt + 1) * 128],
                             rhs=qTa[hs][:, j0 * 128 + lo:j0 * 128 + hi], start=True, stop=True,
                             rd=[B_kTa[hs], B_qTa[hs]], wr=[B_sps[ss]])
                    p_ = cnt["pT"] % NPT
                    cnt["pT"] += 1
                    k.op(act, "activation", out=pT[p_][:, c_lo:c_hi], in_=s_ps[ss][:, c_lo:c_hi], func=AF.Exp,
                         bias=cumN[:, kt, h:h + 1], scale=0.125, rd=[B_sps[ss], B_cumN], wr=[B_pT[p_]])
                    if g >= j0:
                        k.op(dve, "scalar_tensor_tensor", out=pT[p_][:, c_lo:c_lo + 128], in0=pT[p_][:, c_lo:c_lo + 128],
                             scalar=1e30, in1=fm_sb[:, u * 128:(u + 1) * 128], op0=ALU.min, op1=ALU.mult,
                             rd=[B_fm], wr=[B_pT[p_]])
                    return p_

                def stage2(kt, p_):
                    g = kt // 4
                    u = kt % 4
                    ja = max(g, j0)
                    c_lo = (ja - j0) * 128
                    c_hi = nb * 128
                    started = set()

                    def st_flag(lo_):
                        bank = lo_ // 512
                        if kt == 0 and bank not in started:
                            started.add(bank)
                            return True
                        return False
                    if g >= j0:
                        k.op(pe, "matmul", o_ps[0:65, c_lo:c_lo + 128], lhsT=vau[hs][:, kt, :],
                             rhs=pT[p_][:, c_lo:c_lo + 128], start=st_flag(c_lo), stop=(u == 3), skip_group_check=True,
                             rd=[B_vau[hs], B_pT[p_]], wr=[B_ops])
                        r_lo = c_lo + 128
                    else:
                        r_lo = c_lo
                    for b0 in range(0, 1024, 512):
                        lo, hi = max(r_lo, b0), min(c_hi, b0 + 512)
                        if lo >= hi:
                            continue
                        k.op(pe, "matmul", o_ps[0:65, lo:hi], lhsT=vau[hs][:, kt, :], rhs=pT[p_][:, lo:hi],
                             start=st_flag(lo), stop=False, skip_group_check=True, rd=[B_vau[hs], B_pT[p_]], wr=[B_ops])

                nkt = 4 * j1
                pq = []
                for kt in range(nkt + 2):
                    if kt < nkt:
                        pq.append((kt, stage1(kt)))
                    if kt >= 2:
                        k0, p0 = pq.pop(0)
                        stage2(k0, p0)
                assert not pq
                for b0 in range(0, nb * 128, 512):
                    b1 = min(nb * 128, b0 + 512)
                    k.op(act, "copy", out=oT_sb[:, b0:b1], in_=o_ps[0:65, b0:b1], rd=[B_ops], wr=[B_oTsb])
                for q0 in range(0, nb, 4):
                    q1 = min(nb, q0 + 4)
                    ts_ = cnt["tr"] % 2
                    cnt["tr"] += 1
                    for i in range(q1 - q0):
                        k.op(pe, "transpose", out=tr_ps[ts_][:, i * 65:(i + 1) * 65],
                             in_=oT_sb[:, (q0 + i) * 128:(q0 + i + 1) * 128], identity=ident_f[0:65, 0:65],
                             rd=[B_oTsb, B_cst], wr=[B_trps[ts_]])
                    tr3 = tr_ps[ts_][:, 0:260].rearrange("p (h c) -> p h c", h=4)
                    k.op(dve, "reciprocal", out=den[:, 0:q1 - q0], in_=tr3[:, 0:q1 - q0, 64], rd=[B_trps[ts_]], wr=[B_den])
                    for i in range(q1 - q0):
                        jj = j0 + q0 + i
                        k.op(dve, "tensor_scalar", out=o_b[:, jj, h * 64:(h + 1) * 64], in0=tr3[:, i, 0:64],
                             scalar1=den[:, i:i + 1], scalar2=None, op0=ALU.mult, rd=[B_trps[ts_], B_den], wr=[B_ob[jj]])
        k.barrier()
        stF.close()

        stD = contextlib.ExitStack()
        es.enter_context(stD)
        gout = sb("gout", [128, D], F32, stD)
        GA = sb("GA", [128, D], F32, stD)
        B_gout, B_GA = Buf(), Buf()
        vec_bcast_load(gout[:], B_gout, g_out, k.dsem())
        k.op(dve, "tensor_scalar", out=gout[:], in0=gout[:], scalar1=32.0, scalar2=None, op0=ALU.mult, wr=[B_gout])
        bcast_load(GA, B_GA, 2, k.dsem())
        xts = [sb("xtd%d" % i, [128, D], F32, stD) for i in range(2)]
        B_xt = [Buf(), Buf()]
        ds_xt = [k.dsem(), k.dsem()]
        x1t = [sb("x1t%d" % i, [128, D], F32, stD) for i in range(2)]
        B_x1t = [Buf(), Buf()]
        ds_x1 = [k.dsem(), k.dsem()]
        mixed = [sb("mixed%d" % i, [128, D], BF16, stD) for i in range(2)]
        B_mixed = [Buf(), Buf()]
        mT = [sb("mT%d" % i, [128, KC, 128], BF16, stD) for i in range(2)]
        B_mT = [Buf(), Buf()]
        hT_ps = [ps("hT_psd%d" % i, [128, D], BF16, stD) for i in range(2)]
        B_hTps = [Buf(), Buf()]
        mm_ps = [ps("mm_psd%d" % i, [128, 512], F32, stD) for i in range(4)]
        B_mm = [Buf() for _ in range(4)]
        cnt = {"mm": 0, "hTps": 0}

        def d1_prep(j):
            s_ = j % 2
            k.dma(sp, xts[s_][:], x_own[j * 128:(j + 1) * 128, :], ds_xt[s_], wr=[B_xt[s_]])
            ra, Bra = rms_scale(o_a[:, j, :], B_oa[j], 1024, 1024 * EPS)
            rb, Brb = rms_scale(o_b[:, j, :], B_ob[j], 1024, 1024 * EPS)
            k.op(dve, "scalar_tensor_tensor", out=mixed[s_][:, 0:1024], in0=o_a[:, j, :], scalar=ra, in1=gout[:, 0:1024],
                 op0=ALU.mult, op1=ALU.mult, rd=[B_oa[j], Bra, B_gout], wr=[B_mixed[s_]])
            k.op(dve, "scalar_tensor_tensor", out=mixed[s_][:, 1024:2048], in0=o_b[:, j, :], scalar=rb,
                 in1=gout[:, 1024:2048], op0=ALU.mult, op1=ALU.mult, rd=[B_ob[j], Brb, B_gout], wr=[B_mixed[s_]])

        def d1_tr(j):
            s_ = j % 2
            transpose_to(mixed[s_], B_mixed[s_], mT[s_][:], B_mT[s_])

        d1_prep(0)
        d1_tr(0)
        for j in range(NOWN):
            s = j % 2
            for n in range(4):
                ms = cnt["mm"] % 4
                cnt["mm"] += 1
                for kc in range(KC):
                    k.op(pe, "matmul", mm_ps[ms][:], lhsT=mT[s][:, kc, :], rhs=Wout[:, kc, n * 512:(n + 1) * 512],
                         start=(kc == 0), stop=(kc == KC - 1), rd=[B_Wout, B_mT[s]], wr=[B_mm[ms]])
                sl = slice(n * 512, (n + 1) * 512)
                k.op(dve, "tensor_tensor", out=x1t[s][:, sl], in0=mm_ps[ms][:], in1=GA[:, sl], op=ALU.mult,
                     rd=[B_mm[ms], B_GA], wr=[B_x1t[s]])
                k.op(pool, "tensor_tensor", out=x1t[s][:, sl], in0=x1t[s][:, sl], in1=xts[s][:, sl], op=ALU.add,
                     rd=[B_xt[s]], wr=[B_x1t[s]])
                if j + 1 < NOWN:
                    if n == 0:
                        d1_prep(j + 1)
                    if n == 2:
                        d1_tr(j + 1)
            k.dma(sp, X1_s[j * 128:(j + 1) * 128, :], x1t[s][:], ds_x1[s], rd=[B_x1t[s]])
        k.barrier()
        stD.close()
        stW.close()
        stOB.close()
        stOA.close()

        rt_w = sb("rt_w", [128, NOWN, 2], F32)
        rt_pg = sb("rt_pg", [128, NOWN, 2], I32)
        B_rtw, B_rtpg = Buf(), Buf()
        stR = contextlib.ExitStack()
        es.enter_context(stR)
        G2s = sb("G2s", [128, D], F32, stR)
        SHm = sb("SHm", [128, D], F32, stR)
        tmp2 = sb("tmp2", [128, D], F32, stR)
        B_G2s, B_SHm, B_tmp2 = Buf(), Buf(), Buf()
        bcast_load(G2s, B_G2s, 4, k.dsem())
        bcast_load(SHm, B_SHm, 3, k.dsem())
        vec_bcast_load(tmp2[:], B_tmp2, g_moe, k.dsem())
        k.op(dve, "scalar_tensor_tensor", out=G2s[:], in0=G2s[:], scalar=SQD, in1=tmp2[:], op0=ALU.mult, op1=ALU.mult,
             rd=[B_tmp2], wr=[B_G2s])
        Wr = sb("Wr", [128, KC, 36], F32, stR)
        B_Wr = Buf()
        k.dma(sp, Wr[:], w_r.rearrange("(kc p) n -> p kc n", p=128), k.dsem(), wr=[B_Wr])
        bR = sb("bR", [128, 36], F32, stR)
        B_bR = Buf()
        vec_bcast_load(bR[:], B_bR, b_r, k.dsem(), 36)
        eb_sb = sb("eb_sb", [128, NE], F32, stR)
        B_eb = Buf()
        k.dma(sp, eb_sb[:], ebase, k.dsem(), wr=[B_eb])
        cnt_run = sb("cnt_run", [128, NE], F32, stR)
        B_cnt = Buf()
        k.op(dve, "memset", cnt_run[:], 0.0, wr=[B_cnt])
        zt = sb("zt", [128, D], BF16, stR)
        B_zt = Buf()
        k.op(pool, "memset", zt[:], 0.0, wr=[B_zt])
        ds_z = k.dsem()
        B_Xs = Buf()
        k.dma(sp, Xs_s[0:128, :], zt[:], ds_z, rd=[B_zt], wr=[B_Xs])
        x1t = [sb("x1r%d" % i, [128, D], F32, stR) for i in range(2)]
        B_x1t = [Buf(), Buf()]
        ds_x1 = [k.dsem(), k.dsem()]
        h2b = [sb("h2b%d" % i, [128, D], BF16, stR) for i in range(2)]
        B_h2b = [Buf(), Buf()]
        ds_sc = [k.dsem(), k.dsem()]
        h2T2 = [sb("h2T%d" % i, [128, KC, 128], F32, stR) for i in range(2)]
        B_h2T2 = [Buf(), Buf()]
        trf_ps = [ps("trf_ps%d" % i, [128, 1024], F32, stR) for i in range(2)]
        B_trf = [Buf(), Buf()]
        lg_ps2 = [ps("lg_ps%d" % i, [128, 512], F32, stR) for i in range(2)]
        B_lgps = Buf()
        rk_ps = ps("rk_ps", [128, 512], F32, stR)
        B_rkps = Buf()
        R = sb("R", [128, 512], F32, stR)
        B_R = Buf()
        psc = [sb("psc%d" % i, [128, 2], I32, stR) for i in range(2)]
        B_psc = [Buf(), Buf()]
        BIGI = float(4 * NSLOT)

        def rop(method, **kw):
            return k.op(dve, method, rd=[B_R], wr=[B_R], **kw)

        def d2(j):
            s = j % 2
            h2T, B_h2T = h2T2[s], B_h2T2[s]
            lg = lg_ps2[s][:, 0:36]
            k.dma(sp, x1t[s][:], X1_s[j * 128:(j + 1) * 128, :], ds_x1[s], wr=[B_x1t[s]])
            r2, Br2 = rms_scale(x1t[s][:], B_x1t[s], D, D * EPS)
            k.op(dve, "scalar_tensor_tensor", out=x1t[s][:], in0=x1t[s][:], scalar=r2, in1=G2s[:], op0=ALU.mult,
                 op1=ALU.mult, rd=[Br2, B_G2s], wr=[B_x1t[s]])
            k.op(dve, "tensor_tensor", out=x1t[s][:], in0=x1t[s][:], in1=SHm[:], op=ALU.add, rd=[B_SHm], wr=[B_x1t[s]])
            k.op(act, "copy", out=h2b[s][:], in_=x1t[s][:], rd=[B_x1t[s]], wr=[B_h2b[s]])
            for hh in range(2):
                for i8 in range(8):
                    kc = hh * 8 + i8
                    k.op(pe, "transpose", out=trf_ps[hh][:, i8 * 128:(i8 + 1) * 128], in_=x1t[s][:, kc * 128:(kc + 1) * 128],
                         identity=ident_f, rd=[B_x1t[s], B_cst], wr=[B_trf[hh]])
                k.op(act if hh else dve, "copy" if hh else "tensor_copy", out=h2T[:, hh * 8:(hh + 1) * 8, :],
                     in_=trf_ps[hh][:].rearrange("p (k t) -> p k t", k=8), rd=[B_trf[hh]], wr=[B_h2T])
            for kc in range(KC):
                k.op(pe, "matmul", lg, lhsT=h2T[:, kc, :], rhs=Wr[:, kc, :], start=(kc == 0),
                     stop=(kc == KC - 1), rd=[B_h2T, B_Wr], wr=[B_lgps2[s]])
            yield
            LG = R[:, 0:36]; GL = R[:, 0:4]; EL = R[:, 4:36]
            GMAX = R[:, 40:41]; NGMAX = R[:, 41:42]; GSUM = R[:, 42:43]; GVAL = R[:, 43:44]
            GOH = R[:, 44:48]; GPEN = R[:, 48:52]; GEXP = R[:, 52:56]
            EM = R[:, 64:96]; T1 = R[:, 96:97]; T2 = R[:, 97:98]; OH1 = R[:, 100:132]; OH2 = R[:, 132:164]
            E2 = R[:, 164:196]; AA = R[:, 196:228]; RK = R[:, 228:260]; RKB = R[:, 260:292]; TMP = R[:, 292:324]
            DD = R[:, 324:325]; ED = R[:, 325:326]; W1 = R[:, 326:327]; W2 = R[:, 327:328]
            P1 = R[:, 328:329]; P2 = R[:, 329:330]; R1 = R[:, 330:331]; R2 = R[:, 331:332]
            V1 = R[:, 332:333]; V2 = R[:, 333:334]; OF1 = R[:, 334:335]; OF2 = R[:, 335:336]
            PS1 = R[:, 336:337]; PS2 = R[:, 337:338]
            k.op(dve, "tensor_tensor", out=LG, in0=lg, in1=bR[:], op=ALU.add, rd=[B_lgps2[s], B_bR, B_R], wr=[B_R])
            rop("reduce_max", out=GMAX, in_=GL, axis=AX.X)
            rop("tensor_scalar", out=GOH, in0=GL, scalar1=GMAX, scalar2=None, op0=ALU.is_equal)
            rop("tensor_scalar", out=NGMAX, in0=GMAX, scalar1=-1.0, scalar2=None, op0=ALU.mult)
            k.op(act, "activation", out=GEXP, in_=GL, func=AF.Exp, bias=NGMAX, scale=1.0, rd=[B_R], wr=[B_R])
            rop("reduce_sum", out=GSUM, in_=GEXP, axis=AX.X)
            rop("reciprocal", out=GVAL, in_=GSUM)
            rop("tensor_scalar", out=GPEN, in0=GOH, scalar1=-1.0, scalar2=1e30, op0=ALU.add, op1=ALU.mult)
            for gi in range(4):
                rop("tensor_scalar", out=EM[:, gi * 8:(gi + 1) * 8], in0=EL[:, gi * 8:(gi + 1) * 8],
                    scalar1=GPEN[:, gi:gi + 1], scalar2=None, op0=ALU.add)
            rop("reduce_max", out=T1, in_=EM, axis=AX.X)
            rop("tensor_scalar", out=OH1, in0=EM, scalar1=T1, scalar2=None, op0=ALU.is_equal)
            rop("scalar_tensor_tensor", out=E2, in0=OH1, scalar=-1e30, in1=EM, op0=ALU.mult, op1=ALU.add)
            rop("reduce_max", out=T2, in_=E2, axis=AX.X)
            rop("tensor_scalar", out=OH2, in0=E2, scalar1=T2, scalar2=None, op0=ALU.is_equal)
            rop("tensor_tensor", out=DD, in0=T2, in1=T1, op=ALU.subtract)
            k.op(act, "activation", out=ED, in_=DD, func=AF.Exp, rd=[B_R], wr=[B_R])
            rop("tensor_scalar", out=ED, in0=ED, scalar1=1.0, scalar2=None, op0=ALU.add)
            rop("reciprocal", out=ED, in_=ED)
            rop("tensor_tensor", out=W1, in0=GVAL, in1=ED, op=ALU.mult)
            rop("tensor_tensor", out=W2, in0=GVAL, in1=W1, op=ALU.subtract)
            rop("tensor_tensor", out=AA, in0=OH1, in1=OH2, op=ALU.add)
            k.op(pe, "matmul", rk_ps[:, 0:32], lhsT=U_strict, rhs=AA, start=True, stop=True, rd=[B_R, B_cst], wr=[B_rkps])
            k.op(pe, "matmul", rk_ps[:, 32:64], lhsT=ones_f, rhs=AA, start=True, stop=True, rd=[B_R, B_cst], wr=[B_rkps])
            k.op(dve, "tensor_tensor", out=RK, in0=rk_ps[:, 0:32], in1=cnt_run[:], op=ALU.add, rd=[B_rkps, B_cnt, B_R],
                 wr=[B_R])
            k.op(dve, "tensor_tensor", out=cnt_run[:], in0=rk_ps[:, 32:64], in1=cnt_run[:], op=ALU.add, rd=[B_rkps, B_cnt],
                 wr=[B_cnt])
            k.op(dve, "tensor_tensor", out=RKB, in0=RK, in1=eb_sb[:], op=ALU.add, rd=[B_R, B_eb], wr=[B_R])
            for (OH, P_, R_, V_, OF_, PS_, W_, col) in ((OH1, P1, R1, V1, OF1, PS1, W1, 0), (OH2, P2, R2, V2, OF2, PS2, W2, 1)):
                rop("tensor_tensor", out=TMP, in0=OH, in1=RKB, op=ALU.mult)
                rop("reduce_sum", out=P_, in_=TMP, axis=AX.X)
                rop("tensor_tensor", out=TMP, in0=OH, in1=RK, op=ALU.mult)
                rop("reduce_sum", out=R_, in_=TMP, axis=AX.X)
                rop("tensor_scalar", out=V_, in0=R_, scalar1=float(CAP), scalar2=None, op0=ALU.is_lt)
                rop("tensor_scalar", out=OF_, in0=V_, scalar1=-BIGI, scalar2=BIGI, op0=ALU.mult, op1=ALU.add)
                rop("tensor_tensor", out=PS_, in0=P_, in1=OF_, op=ALU.add)
                k.op(dve, "tensor_copy", out=psc[s][:, col:col + 1], in_=PS_, rd=[B_R], wr=[B_psc[s]])
                rop("tensor_tensor", out=P_, in0=P_, in1=V_, op=ALU.mult)
                k.op(dve, "tensor_copy", out=rt_pg[:, j, col:col + 1], in_=P_, rd=[B_R], wr=[B_rtpg])
                k.op(dve, "tensor_tensor", out=rt_w[:, j, col:col + 1], in0=W_, in1=V_, op=ALU.mult, rd=[B_R], wr=[B_rtw])
            for col in range(2):
                k.idma(Xs_s, bass.IndirectOffsetOnAxis(ap=psc[s][:, col:col + 1], axis=0), h2b[s][:, :], None, ds_sc[s],
                       NSLOT - 1, rd=[B_h2b[s], B_psc[s], B_Xs])

        B_lgps2 = [Buf(), Buf()]
        gens = [d2(j) for j in range(NOWN)]
        next(gens[0])
        for j in range(NOWN):
            if j + 1 < NOWN:
                next(gens[j + 1])
            for _ in gens[j]:
                pass
        k.barrier()
        stR.close()

        stE = contextlib.ExitStack()
        es.enter_context(stE)
        wg = [sb("wg%d" % i, [128, KC, DE], BF16, stE) for i in range(2)]
        wu = [sb("wu%d" % i, [128, KC, DE], BF16, stE) for i in range(2)]
        wd = [sb("wd%d" % i, [128, 4, D], BF16, stE) for i in range(2)]
        B_we = [Buf(), Buf()]
        ds_we = [k.dsem(), k.dsem()]
        NXS_E = 4
        xs = [sb("xs%d" % i, [128, D], BF16, stE) for i in range(NXS_E)]
        B_xs = [Buf() for _ in range(NXS_E)]
        ds_xs = [k.dsem() for _ in range(NXS_E)]
        xsT = [sb("xsT%d" % i, [128, KC, 128], BF16, stE) for i in range(2)]
        B_xsT = [Buf(), Buf()]
        sg = [sb("sg%d" % i, [128, DE], BF16, stE) for i in range(2)]
        hid = [sb("hid%d" % i, [128, DE], BF16, stE) for i in range(2)]
        hidT = [sb("hidT%d" % i, [128, 4, 128], BF16, stE) for i in range(2)]
        B_sg, B_hid, B_hidT = [Buf(), Buf()], [Buf(), Buf()], [Buf(), Buf()]
        y_sb = [sb("y_sb%d" % i, [128, D], F32, stE) for i in range(2)]
        B_ysb = [Buf(), Buf()]
        ds_y = [k.dsem(), k.dsem()]
        hT_ps = [ps("hT_pse%d" % i, [128, 1024], BF16, stE) for i in range(1)]
        B_hTps = [Buf()]
        g_ps = [ps("g_ps%d" % i, [128, 512], F32, stE) for i in range(2)]
        u_ps = [ps("u_ps%d" % i, [128, 512], F32, stE) for i in range(2)]
        B_gps, B_ups = [Buf(), Buf()], [Buf(), Buf()]
        ht_ps = ps("ht_ps", [128, 512], BF16, stE)
        B_htps = Buf()
        y_ps = [ps("y_ps%d" % i, [128, 512], F32, stE) for i in range(2)]
        B_yps = [Buf(), Buf()]
        cnt = {"hTps": 0, "y": 0}

        NSTG = 6
        stg = [sb("stg%d" % i, [128, 2048], F32, stE) for i in range(NSTG)]
        B_stg = [Buf() for _ in range(NSTG)]
        ds_stg = [k.dsem() for _ in range(NSTG)]
        cast_rr = [0]
        B_wch = [[Buf() for _ in range(12)] for _ in range(2)]

        def expert_chunks(e):
            s = e % 2
            wgv = w_gate[e].rearrange("(p kc) n -> p kc n", kc=KC)
            wuv = w_up[e].rearrange("(p kc) n -> p kc n", kc=KC)
            wdv = w_down[e].rearrange("(kc p) n -> p kc n", p=128)
            tasks = []
            for q in range(4):
                tasks.append((wgv[:, q * 4:(q + 1) * 4, :], wg[s][:, q * 4:(q + 1) * 4, :], "p (k n) -> p k n", 4))
            for q in range(4):
                tasks.append((wuv[:, q * 4:(q + 1) * 4, :], wu[s][:, q * 4:(q + 1) * 4, :], "p (k n) -> p k n", 4))
            for q in range(4):
                tasks.append((wdv[:, q, :], wd[s][:, q, :], None, 1))
            out_ = []
            for ci, (src, dst, rr, kk) in enumerate(tasks):
                def task(src=src, dst=dst, rr=rr, kk=kk, s=s, ci=ci):
                    i = cast_rr[0] % NSTG
                    c = cast_rr[0]
                    cast_rr[0] += 1
                    sview = stg[i][:].rearrange(rr, k=kk) if rr else stg[i][:]
                    k.dma(sp, sview, src, ds_stg[i], wr=[B_stg[i]])
                    eng = (dve, act)[c % 2]
                    k.op(eng, "copy" if eng is act else "tensor_copy", out=dst, in_=sview, rd=[B_stg[i]], wr=[B_wch[s][ci]])
                out_.append(task)
            return out_

        blocks = [(e, blk) for e in range(NE) for blk in range(NBLK)]
        NBK = len(blocks)

        def xs_load(i):
            e, blk = blocks[i]
            row0 = e * CAP + blk * 128
            sl = i % NXS_E
            k.dma(pool, xs[sl][:], Xs_s[row0:row0 + 128, :], ds_xs[sl], wr=[B_xs[sl]])

        def stA(i):
            sl = i % NXS_E
            tl = i % 2
            for hh in range(2):
                for i8 in range(8):
                    kc = hh * 8 + i8
                    k.op(pe, "transpose", out=hT_ps[0][:, i8 * 128:(i8 + 1) * 128], in_=xs[sl][:, kc:D:KC],
                         identity=ident_b[:], rd=[B_xs[sl], B_identb], wr=[B_hTps[0]])
                k.op(dve if hh else act, "tensor_copy" if hh else "copy", out=xsT[tl][:, hh * 8:(hh + 1) * 8, :],
                     in_=hT_ps[0][:].rearrange("p (k t) -> p k t", k=8), rd=[B_hTps[0]], wr=[B_xsT[tl]])

        def stB(i):
            e, blk = blocks[i]
            es_ = e % 2
            sl = i % 2
            for kc in range(KC):
                k.op(pe, "matmul", g_ps[sl][:], lhsT=xsT[sl][:, kc, :], rhs=wg[es_][:, kc, :], start=(kc == 0),
                     stop=(kc == KC - 1), rd=[B_xsT[sl]] + B_wch[es_], wr=[B_gps[sl]])
            for kc in range(KC):
                k.op(pe, "matmul", u_ps[sl][:], lhsT=xsT[sl][:, kc, :], rhs=wu[es_][:, kc, :], start=(kc == 0),
                     stop=(kc == KC - 1), rd=[B_xsT[sl]] + B_wch[es_], wr=[B_ups[sl]])
            k.op(act, "activation", out=sg[sl][:], in_=g_ps[sl][:], func=AF.Silu, rd=[B_gps[sl]], wr=[B_sg[sl]])
            k.op(dve, "tensor_tensor", out=hid[sl][:], in0=sg[sl][:], in1=u_ps[sl][:], op=ALU.mult,
                 rd=[B_sg[sl], B_ups[sl]], wr=[B_hid[sl]])

        def stC(i):
            e, blk = blocks[i]
            es_ = e % 2
            sl = i % 2
            row0 = e * CAP + blk * 128
            for c in range(4):
                k.op(pe, "transpose", out=ht_ps[:, c * 128:(c + 1) * 128], in_=hid[sl][:, c * 128:(c + 1) * 128],
                     identity=ident_b[:], rd=[B_hid[sl], B_identb], wr=[B_htps])
            k.op(act, "copy", out=hidT[sl][:], in_=ht_ps[:].rearrange("p (k t) -> p k t", k=4), rd=[B_htps],
                 wr=[B_hidT[sl]])

        def stC2(i):
            e, blk = blocks[i]
            es_ = e % 2
            sl = i % 2
            row0 = e * CAP + blk * 128
            for n in range(4):
                yp = n % 2
                for c in range(4):
                    k.op(pe, "matmul", y_ps[yp][:], lhsT=hidT[sl][:, c, :], rhs=wd[es_][:, c, n * 512:(n + 1) * 512],
                         start=(c == 0), stop=(c == 3), rd=[B_hidT[sl]] + B_wch[es_], wr=[B_yps[yp]])
                k.op(act if n % 2 else dve, "copy" if n % 2 else "tensor_copy", out=y_sb[sl][:, n * 512:(n + 1) * 512],
                     in_=y_ps[yp][:], rd=[B_yps[yp]], wr=[B_ysb[sl]])
            k.dma(act, Ys_s[row0:row0 + 128, :], y_sb[sl][:], ds_y[sl], rd=[B_ysb[sl]])

        for e0 in (0, 1):
            for t_ in expert_chunks(e0):
                t_()
        pending = []
        xs_load(0)
        xs_load(1)
        for i in range(NBK + 2):
            if i + 2 < NBK:
                xs_load(i + 2)
            if i >= 2:
                stC(i - 2)
            if i < NBK:
                stA(i)
            if 1 <= i <= NBK:
                stB(i - 1)
            if i >= 2:
                stC2(i - 2)
                e_done, blk_done = blocks[i - 2]
                if blk_done == NBLK - 1 and e_done + 2 < NE:
                    pending += expert_chunks(e_done + 2)
            for _ in range(3):
                if pending:
                    pending.pop(0)()
        assert not pending
        k.barrier()
        stE.close()

        stG = contextlib.ExitStack()
        es.enter_context(stG)
        GM = sb("GM", [128, D], F32, stG)
        FG = sb("FG", [128, D], F32, stG)
        B_GM, B_FG = Buf(), Buf()
        bcast_load(GM, B_GM, 5, k.dsem())
        vec_bcast_load(FG[:], B_FG, g_fin, k.dsem())
        k.op(dve, "tensor_scalar", out=FG[:], in0=FG[:], scalar1=SQD, scalar2=None, op0=ALU.mult, wr=[B_FG])
        NF = 3
        g1 = [sb("g1_%d" % i, [128, D], F32, stG) for i in range(NF)]
        g2 = [sb("g2_%d" % i, [128, D], F32, stG) for i in range(NF)]
        x1t = [sb("x1f%d" % i, [128, D], F32, stG) for i in range(NF)]
        B_g1, B_g2, B_x1t = [Buf() for _ in range(NF)], [Buf() for _ in range(NF)], [Buf() for _ in range(NF)]
        ds_g = [k.dsem() for _ in range(NF)]
        ds_g2 = [k.dsem() for _ in range(NF)]
        ds_x1 = [k.dsem() for _ in range(NF)]
        ds_o = [k.dsem() for _ in range(NF)]

        def f_loads(j):
            s = j % NF
            k.dma(sp, x1t[s][:], X1_s[j * 128:(j + 1) * 128, :], ds_x1[s], wr=[B_x1t[s]])
            k.idma(g1[s][:, :], None, Ys_s, bass.IndirectOffsetOnAxis(ap=rt_pg[:, j, 0:1], axis=0), ds_g[s], NSLOT - 1,
                   rd=[B_rtpg], wr=[B_g1[s]])
            k.idma(g2[s][:, :], None, Ys_s, bass.IndirectOffsetOnAxis(ap=rt_pg[:, j, 1:2], axis=0), ds_g2[s], NSLOT - 1,
                   rd=[B_rtpg], wr=[B_g2[s]])

        f_loads(0)
        if NOWN > 1:
            f_loads(1)
        for j in range(NOWN):
            s = j % NF
            if j + 2 < NOWN:
                f_loads(j + 2)
            k.op(dve, "tensor_scalar", out=g1[s][:], in0=g1[s][:], scalar1=rt_w[:, j, 0:1], scalar2=None, op0=ALU.mult,
                 rd=[B_rtw], wr=[B_g1[s]])
            k.op(dve, "scalar_tensor_tensor", out=g1[s][:], in0=g2[s][:], scalar=rt_w[:, j, 1:2], in1=g1[s][:],
                 op0=ALU.mult, op1=ALU.add, rd=[B_g2[s], B_rtw], wr=[B_g1[s]])
            k.op(pool, "tensor_tensor", out=g1[s][:], in0=g1[s][:], in1=GM[:], op=ALU.mult, rd=[B_GM], wr=[B_g1[s]])
            k.op(dve, "tensor_tensor", out=x1t[s][:], in0=x1t[s][:], in1=g1[s][:], op=ALU.add, rd=[B_g1[s]], wr=[B_x1t[s]])
            rf, Brf = rms_scale(x1t[s][:], B_x1t[s], D, D * EPS)
            k.op(dve, "scalar_tensor_tensor", out=g2[s][:], in0=x1t[s][:], scalar=rf, in1=FG[:], op0=ALU.mult,
                 op1=ALU.mult, rd=[B_x1t[s], Brf, B_FG], wr=[B_g2[s]])
            k.dma(sp, out[j * 128:(j + 1) * 128, :], g2[s][:], ds_o[s], rd=[B_g2[s]], wr=[])
        if dbg:
            k.dma(sp, d_pg, rt_pg[:].rearrange("p j c -> p (j c)"), k.dsem(), rd=[B_rtpg])
            k.dma(sp, d_w, rt_w[:].rearrange("p j c -> p (j c)"), k.dsem(), rd=[B_rtw])
        k.barrier()
        stG.close()
    build.nds = k.nds
    return nc


def _consts(r, CAP):
    p = np.arange(128)
    ident = np.eye(128, dtype=np.float32)
    U_incl = (p[:, None] <= p[None, :]).astype(np.float32)
    U_strict = (p[:, None] < p[None, :]).astype(np.float32)
    ones = np.ones((128, 128), np.float32)
    cst = np.concatenate([ident, U_incl, U_strict, ones], axis=1)
    tri = (p[:, None] <= p[None, :]).astype(np.float32)
    fm = [np.ones((128, 128), np.float32) if u < r else (tri if u == r else np.zeros((128, 128), np.float32)) for u in range(4)]
    fmask = np.concatenate(fm, axis=1)
    slopes = (2.0 ** (-8.0 * np.arange(1, 17) / 16)).astype(np.float64)
    kk, qq = p[:, None].astype(np.float64), p[None, :].astype(np.float64)
    A = np.zeros((128, 3, 4, 4, 128), np.float32)
    for g in range(16):
        prev = np.exp(-slopes[g] * (qq + 128 - kk)) * (kk > qq)
        cur = np.exp(-slopes[g] * (qq - kk)) * (kk <= qq)
        A[:, 0, g // 4, g % 4, :] = prev if r != 0 else 0.0
        A[:, 1, g // 4, g % 4, :] = prev
        A[:, 2, g // 4, g % 4, :] = cur
    rsel = np.zeros((128, 4), np.float32)
    rsel[:, r] = 1.0
    ebase = np.tile((np.arange(NE) * CAP).astype(np.float32)[None, :], (128, 1))
    return dict(cst=cst, fmask=fmask, swaA=A.reshape(128, -1), rsel=rsel, ebase=ebase)


_CACHE = {}


def run(inputs, S, CAP, dbg=False):
    f = lambda a: np.ascontiguousarray(np.asarray(a, dtype=np.float32))
    x = f(inputs["x"])[:, :S]
    B = x.shape[0]
    NB = S // 128
    NOWN = NB // 4
    key = (S, CAP, dbg)
    if key not in _CACHE:
        _CACHE[key] = build(S, CAP, dbg)
    nc = _CACHE[key]
    shared = dict(
        w_ada=f(inputs["w_ada"][0]), b_ada=f(inputs["b_ada"][0]).reshape(1, -1), g_mix=f(inputs["norm_mix_g"][0]).reshape(1, -1),
        w_in=f(inputs["w_in"][0]), b_f=f(inputs["b_forget"][0]).reshape(1, -1), sinks=f(inputs["sinks"][0]).reshape(1, -1),
        g_out=np.concatenate([f(inputs["out_norm_swa_g"][0]), f(inputs["out_norm_fox_g"][0])]).reshape(1, -1),
        w_out=f(inputs["w_out"][0]), g_moe=f(inputs["norm_moe_g"][0]).reshape(1, -1),
        w_r=np.ascontiguousarray(np.concatenate([f(inputs["w_group"][0]), f(inputs["w_expert"][0])], axis=1)),
        b_r=np.concatenate([f(inputs["b_group"][0]), f(inputs["b_expert"][0])]).reshape(1, -1),
        w_gate=f(inputs["w_gate"][0]), w_up=f(inputs["w_up"][0]), w_down=f(inputs["w_down"][0]),
        g_fin=f(inputs["final_g"]).reshape(1, -1),
    )
    in_maps = []
    for core in range(8):
        b, r = core // 4, core % 4
        xb = x[b].reshape(NB, 128, D)
        own = [4 * j + r for j in range(NOWN)]
        x_own = np.ascontiguousarray(xb[own].reshape(-1, D))
        xp = np.zeros((NOWN, 128, D), np.float32)
        for j, t in enumerate(own):
            if t > 0:
                xp[j] = xb[t - 1]
        m = dict(shared)
        m.update(x_seq=np.ascontiguousarray(x[b]), x_own=x_own, x_prev=xp.reshape(-1, D),
                 cT=np.ascontiguousarray(f(inputs["c"])[b].reshape(KC, 128).T))
        m.update(_consts(r, CAP))
        in_maps.append(m)
    res = run_bass_kernel_spmd(nc, in_maps, core_ids=list(range(8)))
    if dbg:
        _CACHE["dbg"] = res
    outp = np.zeros((B, NB, 128, D), np.float32)
    for core in range(8):
        b, r = core // 4, core % 4
        o = np.asarray(res.results[core]["out"]).reshape(NOWN, 128, D)
        for j in range(NOWN):
            outp[b, 4 * j + r] = o[j]
    return outp.reshape(B, S, D)


def kernel(**inputs):
    return run(inputs, 8192, 512)
```
